# Optimizing a Trainium2 kernel written in Bass

```python
import jax, jax.numpy as jnp
from jax import lax
import numpy as np

D_MODEL = 1024
BATCH = 16
SEQ = 2048
DEPTH = 4

MEM_LEN = 256
EPS = 1e-6
N_NORMS = 8
D_FF = 2816
RW_HEADS = 8
RW_HEAD_DIM = 64
RW_WIDTH = RW_HEADS * RW_HEAD_DIM
RW_DECAY_RANK = 64
RW_AAA_RANK = 64
RW_GATE_RANK = 128
RW_LN_EPS = 64e-5
RW_SIZES = (RW_WIDTH, RW_WIDTH, RW_WIDTH, RW_DECAY_RANK, RW_AAA_RANK, RW_GATE_RANK)
RW_IN = 3 * RW_WIDTH + RW_DECAY_RANK + RW_AAA_RANK + RW_GATE_RANK
RT_HEADS = 4
RT_KEY_DIM = 128
RT_VAL_DIM = 128
RT_WIDTH = RT_HEADS * RT_VAL_DIM
RT_CHUNK = 128
RT_ROPE_BASE = 10000.0
RT_SIZES = (RT_HEADS * RT_KEY_DIM, RT_HEADS * RT_KEY_DIM, RT_WIDTH, RT_WIDTH)
RT_IN = 2 * RT_HEADS * RT_KEY_DIM + 2 * RT_WIDTH
EV_IN = RW_IN + RT_IN
EV_OUT = RW_WIDTH + RT_WIDTH
DSA_HEADS = 8
DSA_HEAD_DIM = 64
DSA_WIDTH = DSA_HEADS * DSA_HEAD_DIM
DSA_Q_RANK = 256
DSA_KV_RANK = 128
IDX_HEADS = 8
IDX_DIM = 64
TOPK_MAX = 256
Q_BLOCK = 128
DSA_SIZES = (DSA_Q_RANK, DSA_KV_RANK, IDX_DIM, IDX_HEADS)
DSA_IN = DSA_Q_RANK + DSA_KV_RANK + IDX_DIM + IDX_HEADS
SC_WIDTH = 512
SC_KERNEL = 3
SC_SIZES = (SC_WIDTH, SC_WIDTH, SC_WIDTH)
SC_IN = 3 * SC_WIDTH
OD_IN = DSA_IN + SC_IN
OD_OUT = DSA_WIDTH + SC_WIDTH
XA_HEADS = 4
XA_HEAD_DIM = 128
XA_WIDTH = XA_HEADS * XA_HEAD_DIM

kernel_name = 'hybrid_rwkv7_retnet_dsa_shortconv_block'

F32 = jnp.float32


def split_cols(t, sizes):
    out, start = [], 0
    for s in sizes:
        out.append(t[..., start:start + s])
        start += s
    return out


def rms_norm(x, g, eps=EPS):
    xf = x.astype(F32)
    y = xf * lax.rsqrt(jnp.mean(xf * xf, axis=-1, keepdims=True) + eps)
    return (y * g.astype(F32)).astype(x.dtype)


def group_norm_heads(x, g, b, eps):
    xf = x.astype(F32)
    mu = jnp.mean(xf, axis=-1, keepdims=True)
    var = jnp.mean(jnp.square(xf - mu), axis=-1, keepdims=True)
    y = ((xf - mu) * lax.rsqrt(var + eps)).reshape(*x.shape[:-2], -1)
    return y * g.astype(F32) + b.astype(F32)


def token_shift(p):
    return jnp.pad(p, ((0, 0), (1, 0), (0, 0)))[:, :-1]


def swiglu(x, w_gate, w_up, w_down):
    return (jax.nn.silu(x @ w_gate) * (x @ w_up)) @ w_down


def rotary(x, positions):
    half = x.shape[-1] // 2
    inv_freq = RT_ROPE_BASE ** (-jnp.arange(half, dtype=F32) / half)
    ang = positions.astype(F32)[:, None] * inv_freq[None, :]
    cos = jnp.cos(ang)[None, :, None, :]
    sin = jnp.sin(ang)[None, :, None, :]
    x1, x2 = x[..., :half].astype(F32), x[..., half:].astype(F32)
    return jnp.concatenate([x1 * cos - x2 * sin, x1 * sin + x2 * cos], axis=-1).astype(x.dtype)


def rwkv7_mix(p, mu, w0, w2, a0, a2, g2, k_k, k_a, r_k, ln_g, ln_b):
    b_, s_, _ = p.shape
    p = p + (token_shift(p) - p) * mu
    r, k, v, xw, xa, xg = split_cols(p, RW_SIZES)
    w = -jax.nn.softplus(-(w0 + jnp.tanh(xw) @ w2)) - 0.5
    decay = jnp.exp(-jnp.exp(w.astype(F32)))
    a = jax.nn.sigmoid(a0 + xa @ a2)
    g = jax.nn.sigmoid(xg) @ g2
    heads = lambda t: t.reshape(b_, s_, RW_HEADS, RW_HEAD_DIM)
    kk = heads(k * k_k).astype(F32)
    kk = kk / jnp.maximum(jnp.sqrt(jnp.sum(kk * kk, axis=-1, keepdims=True)), 1e-12)
    k = k * (1 + (a - 1) * k_a)
    rh, kh, vh, ah = heads(r), heads(k), heads(v), heads(a).astype(F32)
    xs = tuple(jnp.moveaxis(t.astype(F32), 1, 0) for t in (rh, heads(decay), kh, vh, -kk, kk * ah))

    def step(state, inp):
        r_t, w_t, k_t, v_t, a_t, b_t = inp
        sa = jnp.einsum('bhvk,bhk->bhv', state, a_t)
        state = (state * w_t[:, :, None, :] + sa[..., None] * b_t[:, :, None, :]
                 + v_t[..., None] * k_t[:, :, None, :])
        return state, jnp.einsum('bhvk,bhk->bhv', state, r_t)

    state0 = jnp.zeros((b_, RW_HEADS, RW_HEAD_DIM, RW_HEAD_DIM), F32)
    _, y = lax.scan(step, state0, xs)
    y = group_norm_heads(jnp.moveaxis(y, 0, 1), ln_g, ln_b, RW_LN_EPS)
    bonus = jnp.sum(rh * kh * r_k, axis=-1, keepdims=True) * vh
    y = (y + bonus.reshape(b_, s_, RW_WIDTH).astype(F32)) * g.astype(F32)
    return y.astype(p.dtype)


def retention_mix(p, positions, gn_g, gn_b):
    b_, s_, _ = p.shape
    q, k, v, g = split_cols(p, RT_SIZES)
    q = rotary(q.reshape(b_, s_, RT_HEADS, RT_KEY_DIM), positions)
    k = rotary(k.reshape(b_, s_, RT_HEADS, RT_KEY_DIM), positions) * (RT_KEY_DIM ** -0.5)
    v = v.reshape(b_, s_, RT_HEADS, RT_VAL_DIM)
    n_chunks = s_ // RT_CHUNK
    chunk = lambda t: t.reshape(b_, n_chunks, RT_CHUNK, *t.shape[2:]).astype(F32)
    qc, kc, vc = chunk(q), chunk(k), chunk(v)
    log_gamma = jnp.log1p(-jnp.exp2(-5.0 - jnp.arange(RT_HEADS, dtype=F32)))
    idx = jnp.arange(RT_CHUNK, dtype=F32)
    rel = idx[:, None] - idx[None, :]
    decay_mask = jnp.where(rel[None] >= 0,
                           jnp.exp(jnp.maximum(rel, 0.0)[None] * log_gamma[:, None, None]), 0.0)
    scores = jnp.einsum('bnihd,bnjhd->bnhij', qc, kc) * decay_mask
    o_inner = jnp.einsum('bnhij,bnjhe->bnihe', scores, vc)
    zeta = jnp.exp((RT_CHUNK - 1 - idx)[None, :] * log_gamma[:, None])
    kv = jnp.einsum('bnjhd,hj,bnjhe->bnhde', kc, zeta, vc)
    chunk_gamma = jnp.exp(RT_CHUNK * log_gamma)[None, :, None, None]

    def step(state, kv_n):
        return state * chunk_gamma + kv_n, state

    state0 = jnp.zeros((b_, RT_HEADS, RT_KEY_DIM, RT_VAL_DIM), F32)
    _, r_prev = lax.scan(step, state0, jnp.moveaxis(kv, 1, 0))
    r_prev = jnp.moveaxis(r_prev, 0, 1)
    xi = jnp.exp((idx + 1)[:, None] * log_gamma[None, :])
    o_cross = jnp.einsum('bnihd,bnhde->bnihe', qc, r_prev) * xi[:, :, None]
    o = (o_inner + o_cross).reshape(b_, s_, RT_HEADS, RT_VAL_DIM)
    o = group_norm_heads(o, gn_g, gn_b, EPS)
    return (jax.nn.silu(g.astype(F32)) * o).astype(p.dtype)


def dsa_mix(p, q_norm_g, kv_norm_g, w_uq, w_uk, w_uv, w_qi):
    b_, s_, _ = p.shape
    c_q, c_kv, k_idx, w_idx = split_cols(p, DSA_SIZES)
    c_q = rms_norm(c_q, q_norm_g)
    c_kv = rms_norm(c_kv, kv_norm_g)
    q = jnp.einsum('btr,rhd->bthd', c_q, w_uq)
    q_lat = jnp.einsum('bthd,hdc->bthc', q, w_uk)
    q_idx = jnp.einsum('btr,rhd->bthd', c_q, w_qi)
    w_idx = w_idx * ((IDX_HEADS * IDX_DIM) ** -0.5)
    top_k = min(TOPK_MAX, s_ // 4)
    n_blocks = s_ // Q_BLOCK
    key_pos = jnp.arange(s_)

    def to_blocks(t):
        return jnp.moveaxis(t.reshape(b_, n_blocks, Q_BLOCK, *t.shape[2:]), 1, 0)

    def block(args):
        blk, ql, qi, wi = args
        q_pos = blk * Q_BLOCK + jnp.arange(Q_BLOCK)
        logits = jnp.einsum('bthd,bsd->bths', qi, k_idx)
        score = jnp.einsum('bth,bths->bts', wi, jax.nn.relu(logits)).astype(F32)
        causal = key_pos[None, :] <= q_pos[:, None]
        score = jnp.where(causal[None], score, -jnp.inf)
        top_val, top_idx = lax.top_k(score, top_k)
        valid = jnp.isfinite(top_val)
        sel = jax.vmap(lambda c, i: c[i])(c_kv, top_idx)
        s = jnp.einsum('bthc,btkc->bthk', ql, sel).astype(F32) * (DSA_HEAD_DIM ** -0.5)
        s = jnp.where(valid[:, :, None, :], s, -jnp.inf)
        prob = jax.nn.softmax(s, axis=-1).astype(sel.dtype)
        return jnp.einsum('bthk,btkc->bthc', prob, sel)

    o_lat = lax.map(block, (jnp.arange(n_blocks), to_blocks(q_lat), to_blocks(q_idx), to_blocks(w_idx)))
    o_lat = jnp.moveaxis(o_lat, 0, 1).reshape(b_, s_, DSA_HEADS, DSA_KV_RANK)
    o = jnp.einsum('bthc,hcd->bthd', o_lat, w_uv)
    return o.reshape(b_, s_, DSA_WIDTH).astype(p.dtype)


def short_conv_mix(p, conv_w, conv_b):
    h, gate_b, gate_c = split_cols(p, SC_SIZES)
    u = gate_c * h
    y = lax.conv_general_dilated(u, conv_w[:, None, :].astype(u.dtype), window_strides=(1,),
                                 padding=[(SC_KERNEL - 1, 0)],
                                 dimension_numbers=('NWC', 'WIO', 'NWC'),
                                 feature_group_count=SC_WIDTH) + conv_b
    return (gate_b * y).astype(p.dtype)


def memory_xattn(h, mem_n, wq, wk, wv, wo):
    b_, s_, _ = h.shape
    m_ = mem_n.shape[1]
    q = (h @ wq).reshape(b_, s_, XA_HEADS, XA_HEAD_DIM)
    k = (mem_n @ wk).reshape(b_, m_, XA_HEADS, XA_HEAD_DIM)
    v = (mem_n @ wv).reshape(b_, m_, XA_HEADS, XA_HEAD_DIM)
    s = jnp.einsum('bthd,bmhd->bhtm', q, k).astype(F32) * (XA_HEAD_DIM ** -0.5)
    prob = jax.nn.softmax(s, axis=-1).astype(v.dtype)
    o = jnp.einsum('bhtm,bmhd->bthd', prob, v).reshape(b_, s_, XA_WIDTH)
    return o @ wo


def setup_inputs(seed: int = 0) -> dict:
    key = jax.random.key(seed)
    ks = iter(jax.random.split(key, 48))
    n_even = (DEPTH + 1) // 2
    n_odd = DEPTH // 2
    nrm = lambda shape, scale: jax.random.normal(next(ks), shape, F32) * scale
    gain = lambda shape: 1.0 + nrm(shape, 0.02)
    uni = lambda shape, lo, hi: jax.random.uniform(next(ks), shape, F32, lo, hi)
    return {
        'x': nrm((BATCH, SEQ, D_MODEL), 1.0),
        'mem': nrm((BATCH, MEM_LEN, D_MODEL), 1.0),
        'norm_g': gain((DEPTH, N_NORMS, D_MODEL)),
        'mem_norm_g': gain((D_MODEL,)),
        'ffn_w_gate': nrm((DEPTH, 2, D_MODEL, D_FF), D_MODEL ** -0.5),
        'ffn_w_up': nrm((DEPTH, 2, D_MODEL, D_FF), D_MODEL ** -0.5),
        'ffn_w_down': nrm((DEPTH, 2, D_FF, D_MODEL), D_FF ** -0.5),
        'xa_wq': nrm((DEPTH, D_MODEL, XA_WIDTH), D_MODEL ** -0.5),
        'xa_wk': nrm((DEPTH, D_MODEL, XA_WIDTH), D_MODEL ** -0.5),
        'xa_wv': nrm((DEPTH, D_MODEL, XA_WIDTH), D_MODEL ** -0.5),
        'xa_wo': nrm((DEPTH, XA_WIDTH, D_MODEL), XA_WIDTH ** -0.5),
        'ev_w_in': nrm((n_even, D_MODEL, EV_IN), D_MODEL ** -0.5),
        'ev_w_out': nrm((n_even, EV_OUT, D_MODEL), EV_OUT ** -0.5),
        'rw_mu': uni((n_even, RW_IN), 0.0, 1.0),
        'rw_w0': uni((n_even, RW_WIDTH), -6.0, 1.0),
        'rw_w2': nrm((n_even, RW_DECAY_RANK, RW_WIDTH), 0.5 * RW_DECAY_RANK ** -0.5),
        'rw_a0': nrm((n_even, RW_WIDTH), 0.1),
        'rw_a2': nrm((n_even, RW_AAA_RANK, RW_WIDTH), 0.5 * RW_AAA_RANK ** -0.5),
        'rw_g2': nrm((n_even, RW_GATE_RANK, RW_WIDTH), RW_GATE_RANK ** -0.5),
        'rw_k_k': 0.85 + nrm((n_even, RW_WIDTH), 0.05),
        'rw_k_a': 1.0 + nrm((n_even, RW_WIDTH), 0.05),
        'rw_r_k': nrm((n_even, RW_HEADS, RW_HEAD_DIM), 0.1),
        'rw_ln_g': gain((n_even, RW_WIDTH)),
        'rw_ln_b': nrm((n_even, RW_WIDTH), 0.02),
        'rt_gn_g': gain((n_even, RT_WIDTH)),
        'rt_gn_b': nrm((n_even, RT_WIDTH), 0.02),
        'od_w_in': nrm((n_odd, D_MODEL, OD_IN), D_MODEL ** -0.5),
        'od_w_out': nrm((n_odd, OD_OUT, D_MODEL), OD_OUT ** -0.5),
        'dsa_q_norm_g': gain((n_odd, DSA_Q_RANK)),
        'dsa_kv_norm_g': gain((n_odd, DSA_KV_RANK)),
        'dsa_w_uq': nrm((n_odd, DSA_Q_RANK, DSA_HEADS, DSA_HEAD_DIM), DSA_Q_RANK ** -0.5),
        'dsa_w_uk': nrm((n_odd, DSA_HEADS, DSA_HEAD_DIM, DSA_KV_RANK), DSA_HEAD_DIM ** -0.5),
        'dsa_w_uv': nrm((n_odd, DSA_HEADS, DSA_KV_RANK, DSA_HEAD_DIM), DSA_KV_RANK ** -0.5),
        'dsa_w_qi': nrm((n_odd, DSA_Q_RANK, IDX_HEADS, IDX_DIM), DSA_Q_RANK ** -0.5),
        'sc_conv_w': nrm((n_odd, SC_KERNEL, SC_WIDTH), SC_KERNEL ** -0.5),
        'sc_conv_b': nrm((n_odd, SC_WIDTH), 0.02),
    }


def reference(x, mem, norm_g, mem_norm_g, ffn_w_gate, ffn_w_up, ffn_w_down,
              xa_wq, xa_wk, xa_wv, xa_wo, ev_w_in, ev_w_out,
              rw_mu, rw_w0, rw_w2, rw_a0, rw_a2, rw_g2, rw_k_k, rw_k_a, rw_r_k, rw_ln_g, rw_ln_b,
              rt_gn_g, rt_gn_b, od_w_in, od_w_out,
              dsa_q_norm_g, dsa_kv_norm_g, dsa_w_uq, dsa_w_uk, dsa_w_uv, dsa_w_qi,
              sc_conv_w, sc_conv_b):
    mem_n = rms_norm(mem, mem_norm_g)
    positions = jnp.arange(x.shape[1])
    for l in range(DEPTH):
        ng = norm_g[l]
        h = swiglu(rms_norm(x, ng[0]), ffn_w_gate[l, 0], ffn_w_up[l, 0], ffn_w_down[l, 0])
        x = x + 0.5 * rms_norm(h, ng[1])
        h = rms_norm(x, ng[2])
        i = l // 2
        if l % 2 == 0:
            p = h @ ev_w_in[i]
            y_a = rwkv7_mix(p[..., :RW_IN], rw_mu[i], rw_w0[i], rw_w2[i], rw_a0[i], rw_a2[i],
                            rw_g2[i], rw_k_k[i], rw_k_a[i], rw_r_k[i], rw_ln_g[i], rw_ln_b[i])
            y_b = retention_mix(p[..., RW_IN:], positions, rt_gn_g[i], rt_gn_b[i])
            h = jnp.concatenate([y_a, y_b], axis=-1) @ ev_w_out[i]
        else:
            p = h @ od_w_in[i]
            y_c = dsa_mix(p[..., :DSA_IN], dsa_q_norm_g[i], dsa_kv_norm_g[i], dsa_w_uq[i],
                          dsa_w_uk[i], dsa_w_uv[i], dsa_w_qi[i])
            y_d = short_conv_mix(p[..., DSA_IN:], sc_conv_w[i], sc_conv_b[i])
            h = jnp.concatenate([y_c, y_d], axis=-1) @ od_w_out[i]
        x = x + rms_norm(h, ng[3])
        h = memory_xattn(rms_norm(x, ng[4]), mem_n, xa_wq[l], xa_wk[l], xa_wv[l], xa_wo[l])
        x = x + rms_norm(h, ng[5])
        h = swiglu(rms_norm(x, ng[6]), ffn_w_gate[l, 1], ffn_w_up[l, 1], ffn_w_down[l, 1])
        x = x + 0.5 * rms_norm(h, ng[7])
    return x
```

```python
import numpy as np
import concourse.bass as bass
import concourse.mybir as mybir
from concourse.bass_utils import run_bass_kernel_spmd

F32 = mybir.dt.float32
BF16 = mybir.dt.bfloat16
ALU = mybir.AluOpType
AF = mybir.ActivationFunctionType
AX = mybir.AxisListType

D = 1024
SEQ = 2048
NT = SEQ // 128
DEPTH = 4
DFF = 2816
NFC = DFF // 128
MEM = 256
EPS = 1e-6


class Res:
    __slots__ = ("name", "writer", "readers", "parent", "parts", "psum")

    def __init__(self, name, parent=None):
        self.name = name
        self.writer = None
        self.readers = {}
        self.parent = parent
        self.parts = {}
        self.psum = parent.psum if parent is not None else False

    def part(self, key):
        r = self.parts.get(key)
        if r is None:
            r = Res(f"{self.name}/{key}", parent=self)
            self.parts[key] = r
        return r


class T:
    def __init__(self, h, name):
        self.h = h
        self.res = Res(name)

    def __getitem__(self, k):
        return self.h[k]

    def part(self, key):
        return self.res.part(key)


NDSEM = 16
COMPUTE = ("pe", "act", "dve", "pool")


class Sched:
    def __init__(self, nc):
        self.nc = nc
        self.eng = {"pe": nc.tensor, "act": nc.scalar, "dve": nc.vector, "pool": nc.gpsimd, "sp": nc.sync}
        self.sem = {}
        self.cnt = {}
        for e in self.eng:
            self.sem[e] = nc.alloc_semaphore("s_" + e)
            self.cnt[e] = 0
        self.dkeys = []
        for q in ("sp", "pool"):
            for i in range(NDSEM):
                k = ("d", q, i)
                self.sem[k] = nc.alloc_semaphore(f"s_d{q}{i}")
                self.cnt[k] = 0
                self.dkeys.append(k)
        self.known = {e: {} for e in self.eng}
        self.dnext = {"sp": 0, "pool": 0}
        self.pe_pending = None
        self.ninstr = 0

    @staticmethod
    def _rlist(r):
        if isinstance(r, T):
            return r.res
        return r

    def _deps(self, reads, writes, eng=None):
        ev = []
        for r in reads:
            r = self._rlist(r)
            if r.writer:
                ev.append(r.writer)
            if r.parent is not None and r.parent.writer:
                ev.append(r.parent.writer)
            for p in r.parts.values():
                if p.writer:
                    ev.append(p.writer)
            if r.psum:
                chain = [r] + list(r.parts.values()) + ([r.parent] if r.parent is not None else [])
                for c in chain:
                    ev.extend((k, v) for k, v in c.readers.items() if k != eng)
        for w in writes:
            w = self._rlist(w)
            chain = [w] + list(w.parts.values())
            if w.parent is not None:
                chain.append(w.parent)
            for c in chain:
                if c.writer:
                    ev.append(c.writer)
                ev.extend(c.readers.items())
        return ev

    def _wait(self, e, evs):
        kn = self.known[e]
        best = {}
        for k, v in evs:
            if k == e and e in ("pe", "sp"):
                continue
            if kn.get(k, 0) >= v:
                continue
            if best.get(k, 0) < v:
                best[k] = v
        for k, v in best.items():
            self.eng[e].wait_ge(self.sem[k], v)
            kn[k] = v

    def _record(self, ev, reads, writes):
        k, v = ev
        for r in reads:
            r = self._rlist(r)
            if r.readers.get(k, 0) < v:
                r.readers[k] = v
        for w in writes:
            w = self._rlist(w)
            w.writer = ev
            w.readers = {}

    def _flush_pe(self):
        if self.pe_pending is not None:
            self.cnt["pe"] += 1
            self.pe_pending.then_inc(self.sem["pe"], 1)
            self.pe_pending = None

    def op(self, e, fn, reads=(), writes=()):
        if e == "pe":
            self._wait(e, self._deps(reads, writes, e))
            ins = fn(self.eng[e])
            self.pe_pending = ins
            self._record((e, self.cnt[e] + 1), reads, writes)
            self.ninstr += 1
            return ins
        self._flush_pe()
        self._wait(e, self._deps(reads, writes, e))
        ins = fn(self.eng[e])
        self.cnt[e] += 1
        ins.then_inc(self.sem[e], 1)
        self._record((e, self.cnt[e]), reads, writes)
        self.ninstr += 1
        return ins

    def pe(self, fn, reads=(), writes=()):
        return self.op("pe", fn, reads, writes)

    def act(self, fn, reads=(), writes=()):
        return self.op("act", fn, reads, writes)

    def dve(self, fn, reads=(), writes=()):
        return self.op("dve", fn, reads, writes)

    def pool(self, fn, reads=(), writes=()):
        return self.op("pool", fn, reads, writes)

    def dma(self, q, out, in_, reads=(), writes=(), **kw):
        self._flush_pe()
        i = self.dnext[q]
        self.dnext[q] = (i + 1) % NDSEM
        k = ("d", q, i)
        evs = self._deps(reads, writes)
        if self.cnt[k]:
            evs.append((k, self.cnt[k]))
        self._wait(q, evs)
        ins = self.eng[q].dma_start(out=out, in_=in_, **kw)
        self.cnt[k] += 16
        ins.then_inc(self.sem[k], 16)
        self._record((k, self.cnt[k]), reads, writes)
        self.ninstr += 1
        return ins

    def barrier(self):
        self._flush_pe()
        evs = [(e, self.cnt[e]) for e in COMPUTE if self.cnt[e]]
        evs += [(k, self.cnt[k]) for k in self.dkeys if self.cnt[k]]
        for e in list(COMPUTE) + ["sp"]:
            self._wait(e, evs)

    def finish(self):
        self._flush_pe()
        evs = [(e, self.cnt[e]) for e in COMPUTE if self.cnt[e]]
        evs += [(k, self.cnt[k]) for k in self.dkeys if self.cnt[k]]
        self._wait("sp", evs)


class Ctx:
    def __init__(self, nc):
        self.nc = nc
        self.S = Sched(nc)
        self._n = 0

    def sb(self, stack, shape, dt, name=None):
        self._n += 1
        name = f"{name or 'sb'}_{self._n}"
        h = stack.enter_context(self.nc.sbuf_tensor(name, list(shape), dt))
        return T(h, name)

    def ps(self, stack, shape, dt=F32, name=None):
        self._n += 1
        name = f"{name or 'ps'}_{self._n}"
        h = stack.enter_context(self.nc.psum_tensor(name, list(shape), dt))
        t = T(h, name)
        t.res.psum = True
        return t


from contextlib import ExitStack


def load_bcast_row(C, q, dst, src_row, n):
    C.S.dma(q, dst[:, 0:n], src_row.partition_broadcast(128), writes=[dst])


def rms_to_T(C, st, x, g_col, xnT, ntiles, ident, tok0=0):
    S = C.S
    with ExitStack() as es:
        ss = C.sb(es, [128, ntiles], F32, "ss")
        rstd = C.sb(es, [128, ntiles], F32, "rstd")
        junk = C.sb(es, [128, D], BF16, "junk")
        xs = [C.sb(es, [128, D], BF16, f"xs{i}") for i in range(2)]
        tps = [C.ps(es, [128, 8, 128], BF16, f"tp{i}") for i in range(2)]
        S.dve(lambda e: e.memset(ss[:, :], 0.0), writes=[ss])
        for t in range(ntiles):
            S.act(lambda e: e.activation(out=junk[:, :], in_=x[:, tok0 + t, :], func=AF.Square,
                                         accum_out=ss[:, t:t + 1]),
                  reads=[x.part(tok0 + t)], writes=[junk, ss])
        rstd_from_ss(C, ss, rstd, ntiles, 1.0 / D, EPS)
        for t in range(ntiles):
            xb = xs[t % 2]
            tp = tps[t % 2]
            S.act(lambda e: e.activation(out=xb[:, :], in_=x[:, tok0 + t, :], func=AF.Copy,
                                         scale=rstd[:, t:t + 1]),
                  reads=[x.part(tok0 + t), rstd], writes=[xb])
            for kc in range(8):
                S.pe(lambda e: e.transpose(out=tp[:, kc, :], in_=xb[:, kc * 128:(kc + 1) * 128], identity=ident[:, :]),
                     reads=[xb, ident], writes=[tp])
            S.dve(lambda e: e.tensor_tensor(out=xnT[:, :, t * 128:(t + 1) * 128], in0=tp[:, :, :],
                                            in1=g_col.unsqueeze(2).to_broadcast([128, 8, 128]), op=ALU.mult),
                  reads=[tp], writes=[xnT.part(t)])
        S.barrier()


def rstd_from_ss(C, ss, rstd, n, scale, eps):
    S = C.S
    S.dve(lambda e: e.tensor_scalar(out=rstd[:, 0:n], in0=ss[:, 0:n], scalar1=scale, scalar2=eps,
                                    op0=ALU.mult, op1=ALU.add), reads=[ss], writes=[rstd])
    S.act(lambda e: e.activation(out=rstd[:, 0:n], in_=rstd[:, 0:n], func=AF.Sqrt), reads=[rstd], writes=[rstd])
    S.dve(lambda e: e.reciprocal(out=rstd[:, 0:n], in_=rstd[:, 0:n]), reads=[rstd], writes=[rstd])


def post_norm_residual(C, st, x, tile_idx, y_ps, g_bc, coef, scr):
    S = C.S
    ss, rstd, junk, tmp = scr
    S.dve(lambda e: e.memset(ss[:, 0:2], 0.0), writes=[ss])
    for h in range(2):
        S.act(lambda e: e.activation(out=junk[:, 0:512], in_=y_ps[h][:, :], func=AF.Square,
                                     accum_out=ss[:, h:h + 1]), reads=[y_ps[h]], writes=[junk, ss])
    S.dve(lambda e: e.tensor_tensor(out=ss[:, 2:3], in0=ss[:, 0:1], in1=ss[:, 1:2], op=ALU.add), reads=[ss], writes=[ss])
    S.dve(lambda e: e.tensor_scalar(out=rstd[:, 0:1], in0=ss[:, 2:3], scalar1=1.0 / D, scalar2=EPS,
                                    op0=ALU.mult, op1=ALU.add), reads=[ss], writes=[rstd])
    S.act(lambda e: e.activation(out=rstd[:, 0:1], in_=rstd[:, 0:1], func=AF.Sqrt), reads=[rstd], writes=[rstd])
    S.dve(lambda e: e.reciprocal(out=rstd[:, 0:1], in_=rstd[:, 0:1]), reads=[rstd], writes=[rstd])
    if coef != 1.0:
        S.dve(lambda e: e.tensor_scalar(out=rstd[:, 0:1], in0=rstd[:, 0:1], scalar1=float(coef), scalar2=None,
                                        op0=ALU.mult), reads=[rstd], writes=[rstd])
    for h in range(2):
        sl = slice(h * 512, (h + 1) * 512)
        S.dve(lambda e: e.tensor_tensor(out=tmp[:, sl], in0=y_ps[h][:, :], in1=g_bc[:, sl], op=ALU.mult),
              reads=[y_ps[h], g_bc], writes=[tmp])
        S.dve(lambda e: e.scalar_tensor_tensor(out=x[:, tile_idx, sl], in0=tmp[:, sl], scalar=rstd[:, 0:1],
                                               in1=x[:, tile_idx, sl], op0=ALU.mult, op1=ALU.add),
              reads=[tmp, rstd, x.part(tile_idx)], writes=[x.part(tile_idx)])


def ffn_block(C, x, l, j, W, P, ident):
    S = C.S
    nc = C.nc
    n_in = 0 if j == 0 else 6
    n_out = 1 if j == 0 else 7
    wg = W.L("ffn_w_gate", l)[j].rearrange("(kc p) f -> p kc f", p=128)
    wu = W.L("ffn_w_up", l)[j].rearrange("(kc p) f -> p kc f", p=128)
    wd = W.L("ffn_w_down", l)[j].rearrange("(fc p) d -> p fc d", p=128)
    TG = 1024
    with ExitStack() as es:
        g_bc = C.sb(es, [128, D], F32, "g_bc")
        load_bcast_row(C, "sp", g_bc, W.L("norm_g", l)[n_out], D)
        hT = C.sb(es, [128, NFC, TG], BF16, "hT")
        for tg in range(SEQ // TG):
            with ExitStack() as es2:
                xnT = C.sb(es2, [128, 8, TG], BF16, "xnT")
                rms_to_T(C, es2, x, P.cols(("norm_g", l, n_in)), xnT, TG // 128, ident,
                         tok0=tg * (TG // 128))
                wbuf = [(C.sb(es2, [128, 8, 256], BF16, f"wg{i}"), C.sb(es2, [128, 8, 256], BF16, f"wu{i}")) for i in range(2)]
                sg = [C.sb(es2, [128, 512], BF16, f"sg{i}") for i in range(2)]
                gps = [C.ps(es2, [128, 512], F32, f"gps{i}") for i in range(2)]
                ups = [C.ps(es2, [128, 512], F32, f"ups{i}") for i in range(2)]
                it = 0
                for f2 in range(NFC // 2):
                    wgt, wut = wbuf[f2 % 2]
                    S.dma("pool", wgt[:, :, :], wg[:, :, f2 * 256:(f2 + 1) * 256], writes=[wgt])
                    S.dma("pool", wut[:, :, :], wu[:, :, f2 * 256:(f2 + 1) * 256], writes=[wut])
                    for fi in range(2):
                        fc = f2 * 2 + fi
                        for th in range(TG // 512):
                            gp, up, sgt = gps[it % 2], ups[it % 2], sg[it % 2]
                            it += 1
                            for kc in range(8):
                                S.pe(lambda e: e.matmul(out=gp[:, :], lhsT=wgt[:, kc, fi * 128:(fi + 1) * 128],
                                                        rhs=xnT[:, kc, th * 512:(th + 1) * 512],
                                                        start=(kc == 0), stop=(kc == 7)),
                                     reads=[wgt, xnT], writes=[gp])
                            for kc in range(8):
                                S.pe(lambda e: e.matmul(out=up[:, :], lhsT=wut[:, kc, fi * 128:(fi + 1) * 128],
                                                        rhs=xnT[:, kc, th * 512:(th + 1) * 512],
                                                        start=(kc == 0), stop=(kc == 7)),
                                     reads=[wut, xnT], writes=[up])
                            S.act(lambda e: e.activation(out=sgt[:, :], in_=gp[:, :], func=AF.Silu),
                                  reads=[gp], writes=[sgt])
                            S.dve(lambda e: e.tensor_tensor(out=hT[:, fc, th * 512:(th + 1) * 512], in0=up[:, :],
                                                            in1=sgt[:, :], op=ALU.mult),
                                  reads=[up, sgt], writes=[hT.part((fc, th))])
                S.barrier()
            with ExitStack() as es3:
                wdt = C.sb(es3, [128, NFC, D], BF16, "wd")
                for f2 in range(NFC // 2):
                    S.dma("pool", wdt[:, f2 * 2:f2 * 2 + 2, :], wd[:, f2 * 2:f2 * 2 + 2, :], writes=[wdt.part(f2)])
                yps = [[C.ps(es3, [128, 512], F32, f"y{i}{h}") for h in range(2)] for i in range(2)]
                scr = (C.sb(es3, [128, 4], F32, "pss"), C.sb(es3, [128, 2], F32, "prs"),
                       C.sb(es3, [128, 512], BF16, "pjunk"), C.sb(es3, [128, D], F32, "ptmp"))
                for tt in range(TG // 128):
                    yp = yps[tt % 2]
                    for h in range(2):
                        for fc in range(NFC):
                            S.pe(lambda e: e.matmul(out=yp[h][:, :], lhsT=hT[:, fc, tt * 128:(tt + 1) * 128],
                                                    rhs=wdt[:, fc, h * 512:(h + 1) * 512],
                                                    start=(fc == 0), stop=(fc == NFC - 1)),
                                 reads=[hT, wdt.part(fc // 2)], writes=[yp[h]])
                    post_norm_residual(C, es3, x, tg * (TG // 128) + tt, yp, g_bc, 0.5, scr)
                S.barrier()


DBG = {}


class Wts:
    def __init__(self, nc, shapes, loff=0, hoff=0):
        self.nc = nc
        self.shapes = shapes
        self.aps = {}
        self.loff = loff
        self.hoff = hoff

    def __getitem__(self, name):
        if name not in self.aps:
            self.aps[name] = self.nc.dram_tensor(name, list(self.shapes[name]), F32, kind="ExternalInput").ap()
        return self.aps[name]

    def L(self, name, l):
        return self[name][l - self.loff]

    def H(self, name, i):
        return self[name][i - self.hoff]

    def hasL(self, name, l):
        return 0 <= l - self.loff < self.shapes[name][0]

    def hasH(self, name, i):
        return 0 <= i - self.hoff < self.shapes[name][0]


class Params:
    def __init__(self):
        self.rows = {}
        self.t = None

    def cols(self, key, kc0=0, n=8):
        r = self.rows[key]
        return self.t[:, kc0:kc0 + n, r]

    def col(self, key, kc):
        r = self.rows[key]
        return self.t[:, kc, r:r + 1]


def build_params(C, es, W, identf):
    S = C.S
    P = Params()
    rows = []
    for l in range(DEPTH):
        if W.hasL("norm_g", l):
            for n in range(8):
                rows.append((("norm_g", l, n), W.L("norm_g", l)[n], D))
    rows.append((("mem_g",), W["mem_norm_g"], D))
    for i in range(2):
        if "sc_conv_w" in W.shapes and W.hasH("sc_conv_w", i):
            for k in range(3):
                rows.append((("sc_w", i, k), W.H("sc_conv_w", i)[k], 512))
            rows.append((("sc_b", i), W.H("sc_conv_b", i), 512))
            rows.append((("dsa_qg", i), W.H("dsa_q_norm_g", i), 256))
        if "rw_w0" in W.shapes and W.hasH("rw_w0", i):
            for nm in ("rw_w0", "rw_a0", "rw_k_k", "rw_k_a", "rw_ln_g", "rw_ln_b", "rt_gn_g", "rt_gn_b"):
                rows.append(((nm, i), W.H(nm, i), 512))
            rows.append((("rw_r_k", i), W.H("rw_r_k", i).rearrange("h d -> (h d)"), 512))
            rows.append((("rw_mu", i, 0), W.H("rw_mu", i)[0:1024], 1024))
            rows.append((("rw_mu", i, 1), W.H("rw_mu", i)[1024:1792], 768))
    nr = len(rows)
    assert nr <= 128
    PC = C.sb(es, [128, 8, nr], F32, "PC")
    P.t = PC
    with ExitStack() as e1:
        raw = C.sb(e1, [128, D], F32, "praw")
        S.pool(lambda e: e.memset(raw[:, :], 0.0), writes=[raw])
        for r, (key, ap, n) in enumerate(rows):
            P.rows[key] = r
            S.dma("sp", raw[r:r + 1, 0:n], ap.rearrange("(o n) -> o n", o=1), writes=[raw])
        tp = C.ps(e1, [128, 8, 128], F32, "ptp")
        for kc in range(8):
            S.pe(lambda e: e.transpose(out=tp[:, kc, :], in_=raw[:, kc * 128:(kc + 1) * 128], identity=identf[:, :]),
                 reads=[raw, identf], writes=[tp])
        S.dve(lambda e: e.tensor_copy(out=PC[:, :, :], in_=tp[:, :, 0:nr]), reads=[tp], writes=[PC])
        S.barrier()
    return P


def load_w(C, dst, src2d, c0, c1, q="pool"):
    v = src2d.rearrange("(kc p) n -> p kc n", p=128)
    nk = v.shape[1]
    for kc in range(nk):
        C.S.dma(q, dst[:, kc, 0:c1 - c0], v[:, kc, c0:c1], writes=[dst.part(kc)])


def make_consts(C, es):
    S = C.S
    K = {}
    ones_f = C.sb(es, [128, 512], F32, "ones_f")
    S.pool(lambda e: e.memset(ones_f[:, :], 1.0), writes=[ones_f])
    ident = C.sb(es, [128, 128], BF16, "ident")
    identf = C.sb(es, [128, 128], F32, "identf")
    for t in (ident, identf):
        S.pool(lambda e: e.affine_select(out=t[:, :], in_=ones_f[:, 0:128], pattern=[[-1, 128]],
                                         compare_op=ALU.is_equal, fill=0.0, base=0, channel_multiplier=1),
               reads=[ones_f], writes=[t])
    ones_bf = C.sb(es, [128, 128], BF16, "ones_bf")
    S.pool(lambda e: e.memset(ones_bf[:, :], 1.0), writes=[ones_bf])
    selq = C.sb(es, [128, 8, 8], BF16, "selq")
    S.pool(lambda e: e.affine_select(out=selq[:, :, :], in_=ones_f[:, 0:64].rearrange("p (a b) -> p a b", a=8),
                                     pattern=[[1, 8], [-1, 8]], compare_op=ALU.is_equal, fill=0.0, base=0,
                                     channel_multiplier=0), reads=[ones_f], writes=[selq])
    sel8 = C.sb(es, [8, 8, 128], BF16, "sel8")
    with ExitStack() as e0:
        sel8a = C.sb(e0, [8, 8, 128], F32, "sel8a")
        S.pool(lambda e: e.memset(sel8a[:, :, :], 1.0), writes=[sel8a])
        S.pool(lambda e: e.affine_select(out=sel8[:, :, :], in_=sel8a[:, :, :], pattern=[[-1, 8], [0, 128]],
                                         compare_op=ALU.is_equal, fill=0.0, base=0, channel_multiplier=1),
               reads=[sel8a], writes=[sel8])
        S.barrier()
    zer = C.sb(es, [128, 128], F32, "zer")
    S.pool(lambda e: e.memset(zer[:, :], 0.0), writes=[zer])
    cbias = C.sb(es, [128, 128], F32, "cbias")
    S.pool(lambda e: e.affine_select(out=cbias[:, :], in_=zer[:, :], pattern=[[-1, 128]],
                                     compare_op=ALU.is_ge, fill=-1e30, base=0, channel_multiplier=1),
           reads=[zer], writes=[cbias])
    K.update(ones_f=ones_f, ident=ident, identf=identf, ones_bf=ones_bf, selq=selq, sel8=sel8, cbias=cbias, zer=zer)
    return K


def make_negm(C, es, qT, nh, k2m, K, p8):
    S = C.S
    qsq = C.sb(es, [128, nh, 512], BF16, "qsq")
    S.act(lambda e: e.activation(out=qsq[:, :, :], in_=qT[:, 0:nh, :], func=AF.Square), reads=[qT], writes=[qsq])
    for h in range(nh):
        S.pe(lambda e: e.matmul(out=p8[0:8, :], lhsT=K["selq"][:, h, :], rhs=qsq[:, h, :], start=(h == 0), stop=(h == nh - 1)),
             reads=[K["selq"], qsq], writes=[p8])
    nm = C.sb(es, [8, 512], F32, "nm")
    negm8 = C.sb(es, [8, 512], BF16, "negm8")
    S.dve(lambda e: e.tensor_scalar(out=nm[:, :], in0=p8[0:8, :], scalar1=k2m[:, 0:1], scalar2=None, op0=ALU.mult),
          reads=[p8, k2m], writes=[nm])
    S.act(lambda e: e.activation(out=nm[:, :], in_=nm[:, :], func=AF.Sqrt), reads=[nm], writes=[nm])
    S.dve(lambda e: e.tensor_scalar(out=negm8[:, :], in0=nm[:, :], scalar1=-1.0, scalar2=None, op0=ALU.mult),
          reads=[nm], writes=[negm8])
    return negm8


def attn_core(C, K, qT_ap, q_res, ktiles, negm8, h, scale, out_ap, out_res, dv, ps_s, ps_o, ps_r, pts, rinv, mask_eng="pool"):
    S = C.S
    n = len(ktiles)
    for i, (k_ap, v_ap, m_ap, rd) in enumerate(ktiles):
        sp = ps_s[i % 2]
        pt = pts[i % 2]
        S.pe(lambda e: e.matmul(out=sp[:, :], lhsT=k_ap, rhs=qT_ap, start=True, stop=False), reads=rd + [q_res], writes=[sp])
        S.pe(lambda e: e.matmul(out=sp[:, :], lhsT=K["sel8"][:, h, :], rhs=negm8[:, :], start=False, stop=True),
             reads=[K["sel8"], negm8], writes=[sp])
        S.act(lambda e: e.activation(out=pt[:, :], in_=sp[:, :], func=AF.Exp, scale=float(scale)), reads=[sp], writes=[pt])
        if m_ap is not None:
            S.op(mask_eng, lambda e: e.tensor_tensor(out=pt[:, :], in0=pt[:, :], in1=m_ap, op=ALU.mult), reads=rd + [pt], writes=[pt])
        S.pe(lambda e: e.matmul(out=ps_o[0:dv, :], lhsT=v_ap, rhs=pt[:, :], start=(i == 0), stop=(i == n - 1)),
             reads=rd + [pt], writes=[ps_o])
        S.pe(lambda e: e.matmul(out=ps_r[0:dv, :], lhsT=K["ones_bf"][:, 0:dv], rhs=pt[:, :], start=(i == 0), stop=(i == n - 1)),
             reads=[K["ones_bf"], pt], writes=[ps_r])
    S.dve(lambda e: e.reciprocal(out=rinv[0:dv, :], in_=ps_r[0:dv, :]), reads=[ps_r], writes=[rinv])
    S.dve(lambda e: e.tensor_tensor(out=out_ap, in0=ps_o[0:dv, :], in1=rinv[0:dv, :], op=ALU.mult),
          reads=[ps_o, rinv], writes=[out_res])


def out_proj_residual(C, x, chunks, w_T, g_bc, coef, tiles):
    S = C.S
    n = len(chunks)
    with ExitStack() as es:
        yps = [[C.ps(es, [128, 512], F32, f"y{i}{h}") for h in range(2)] for i in range(2)]
        scr = (C.sb(es, [128, 4], F32, "pss"), C.sb(es, [128, 2], F32, "prs"),
               C.sb(es, [128, 512], BF16, "pjunk"), C.sb(es, [128, D], F32, "ptmp"))
        for ti, tt in enumerate(tiles):
            yp = yps[ti % 2]
            for h in range(2):
                for c, (yt, f) in enumerate(chunks):
                    S.pe(lambda e: e.matmul(out=yp[h][:, :], lhsT=f(ti), rhs=w_T[:, c, h * 512:(h + 1) * 512],
                                            start=(c == 0), stop=(c == n - 1)), reads=[yt, w_T], writes=[yp[h]])
            post_norm_residual(C, None, x, tt, yp, g_bc, coef, scr)
        S.barrier()


def xattn_block(C, x, l, W, P, K, memT):
    S = C.S
    ident = K["ident"]
    scale = 128 ** -0.5
    with ExitStack() as es:
        g_bc = C.sb(es, [128, D], F32, "g_bc")
        load_bcast_row(C, "sp", g_bc, W.L("norm_g", l)[5], D)
        wq = C.sb(es, [128, 8, 512], BF16, "wq")
        wk = C.sb(es, [128, 8, 512], BF16, "wk")
        wv = C.sb(es, [128, 8, 512], BF16, "wv")
        wo = C.sb(es, [128, 4, D], BF16, "wo")
        load_w(C, wk, W.L("xa_wk", l), 0, 512)
        load_w(C, wv, W.L("xa_wv", l), 0, 512)
        load_w(C, wq, W.L("xa_wq", l), 0, 512)
        wov = W.L("xa_wo", l).rearrange("(c p) d -> p c d", p=128)
        for c in range(4):
            S.dma("pool", wo[:, c, :], wov[:, c, :], writes=[wo.part(c)])
        kT = C.sb(es, [128, 4, MEM], BF16, "kT")
        vtok = C.sb(es, [128, 2, 512], BF16, "vtok")
        k2m = C.sb(es, [8, 1], F32, "k2m")
        with ExitStack() as e1:
            pk = C.ps(e1, [128, 512], F32, "pk")
            for h in range(4):
                for kc in range(8):
                    S.pe(lambda e: e.matmul(out=pk[:, 0:MEM], lhsT=wk[:, kc, h * 128:(h + 1) * 128], rhs=memT[:, kc, :],
                                            start=(kc == 0), stop=(kc == 7)), reads=[wk, memT], writes=[pk])
                S.act(lambda e: e.copy(out=kT[:, h, :], in_=pk[:, 0:MEM]), reads=[pk], writes=[kT])
            for mt in range(2):
                for kc in range(8):
                    S.pe(lambda e: e.matmul(out=pk[:, :], lhsT=memT[:, kc, mt * 128:(mt + 1) * 128], rhs=wv[:, kc, :],
                                            start=(kc == 0), stop=(kc == 7)), reads=[wv, memT], writes=[pk])
                S.dve(lambda e: e.tensor_copy(out=vtok[:, mt, :], in_=pk[:, :]), reads=[pk], writes=[vtok])
            ksq = C.sb(e1, [128, 4, MEM], BF16, "ksq")
            S.act(lambda e: e.activation(out=ksq[:, :, :], in_=kT[:, :, :], func=AF.Square), reads=[kT], writes=[ksq])
            for h in range(4):
                S.pe(lambda e: e.matmul(out=pk[0:8, 0:MEM], lhsT=K["selq"][:, h, :], rhs=ksq[:, h, :], start=(h == 0), stop=(h == 3)),
                     reads=[K["selq"], ksq], writes=[pk])
            S.dve(lambda e: e.reduce_max(out=k2m[:, 0:1], in_=pk[0:8, 0:MEM], axis=AX.X), reads=[pk], writes=[k2m])
            S.barrier()
        for qg in range(4):
            with ExitStack() as e2:
                oT = C.sb(e2, [128, 4, 512], BF16, "oT")
                with ExitStack() as e3:
                    xnT = C.sb(e3, [128, 8, 512], BF16, "xnT")
                    rms_to_T(C, e3, x, P.cols(("norm_g", l, 4)), xnT, 4, ident, tok0=qg * 4)
                    qT = C.sb(e3, [128, 4, 512], BF16, "qT")
                    pq = [C.ps(e3, [128, 512], F32, f"pq{i}") for i in range(2)]
                    for h in range(4):
                        for kc in range(8):
                            S.pe(lambda e: e.matmul(out=pq[h % 2][:, :], lhsT=wq[:, kc, h * 128:(h + 1) * 128], rhs=xnT[:, kc, :],
                                                    start=(kc == 0), stop=(kc == 7)), reads=[wq, xnT], writes=[pq[h % 2]])
                        S.act(lambda e: e.copy(out=qT[:, h, :], in_=pq[h % 2][:, :]), reads=[pq[h % 2]], writes=[qT.part(h)])
                    negm8 = make_negm(C, e3, qT, 4, k2m, K, pq[0])
                    ps_s = [C.ps(e3, [128, 512], F32, f"ss{i}") for i in range(2)]
                    ps_o = C.ps(e3, [128, 512], F32, "pso")
                    ps_r = C.ps(e3, [128, 512], F32, "psr")
                    pts = [C.sb(e3, [128, 512], BF16, f"pt{i}") for i in range(2)]
                    rinv = C.sb(e3, [128, 512], F32, "rinv")
                    for h in range(4):
                        kt_list = [(kT[:, h, kt * 128:(kt + 1) * 128], vtok[:, kt, h * 128:(h + 1) * 128], None, [kT, vtok])
                                   for kt in range(2)]
                        attn_core(C, K, qT[:, h, :], qT, kt_list, negm8, h, scale, oT[:, h, :], oT.part(h), 128,
                                  ps_s, ps_o, ps_r, pts, rinv)
                    S.barrier()
                out_proj_residual(C, x, [(oT, (lambda ti, c=c: oT[:, c, ti * 128:(ti + 1) * 128])) for c in range(4)],
                                  wo, g_bc, 1.0, [qg * 4 + i for i in range(4)])


def prep_mem(C, es, W, P, K, s, memT):
    S = C.S
    with ExitStack() as e1:
        mt = C.sb(e1, [128, 2, D], F32, "memraw")
        S.dma("sp", mt[:, :, :], W["mem"][s].rearrange("(t p) d -> p t d", p=128), writes=[mt.part(0), mt.part(1)])
        rms_to_T(C, e1, mt, P.cols(("mem_g",)), memT, 2, K["ident"], tok0=0)


def odd_mixer(C, x, l, W, P, K):
    S = C.S
    i = l // 2
    ident = K["ident"]
    w_in = W.H("od_w_in", i)
    NEG_SEL = -3.0e38
    with ExitStack() as es:
        g_bc = C.sb(es, [128, D], F32, "g_bc")
        load_bcast_row(C, "sp", g_bc, W.L("norm_g", l)[3], D)
        ycT = C.sb(es, [128, 4, SEQ], BF16, "ycT")
        ydT = C.sb(es, [128, 4, SEQ], BF16, "ydT")
        cqnT = C.sb(es, [128, 2, SEQ], BF16, "cqnT")
        ckvT = C.sb(es, [128, SEQ], BF16, "ckvT")
        ckvtok = C.sb(es, [128, NT, 128], BF16, "ckvtok")
        kidxT2 = C.sb(es, [128, SEQ], BF16, "kidxT2")
        widx = C.sb(es, [128, NT, 8], F32, "widx")
        absw = C.sb(es, [128, NT, 8], F32, "absw")
        sgnw = C.sb(es, [128, NT, 8], F32, "sgnw")
        carry = C.sb(es, [128, 4, 2], F32, "carry")
        S.pool(lambda e: e.memset(carry[:, :, :], 0.0), writes=[carry])
        gkv_bc = C.sb(es, [128, 128], F32, "gkv_bc")
        load_bcast_row(C, "sp", gkv_bc, W.H("dsa_kv_norm_g", i), 128)
        TG = 1024
        with ExitStack() as e1:
            wsm = C.sb(e1, [128, 8, 456], BF16, "wsm")
            load_w(C, wsm, w_in, 0, 456)
            wscs = [C.sb(e1, [128, 8, 3, 128], BF16, f"wsc{k}") for k in range(2)]
            ss = C.sb(e1, [128, 2], F32, "ss2")
            rs = C.sb(e1, [128, 2], F32, "rs2")
            junk = C.sb(e1, [128, 256], BF16, "junk2")
            cqs = C.sb(e1, [128, 256], BF16, "cqs")
            kid2 = C.sb(e1, [128, 2, 64], BF16, "kid2")
            hs = C.sb(e1, [128, 512], F32, "hs")
            ub = C.sb(e1, [128, 514], F32, "ub")
            yb = C.sb(e1, [128, 512], F32, "yb")
            w_v = w_in.rearrange("(kc p) n -> p kc n", p=128)
            for tg in range(SEQ // TG):
                with ExitStack() as e2:
                    xnT = C.sb(e2, [128, 8, TG], BF16, "xnT")
                    rms_to_T(C, e2, x, P.cols(("norm_g", l, 2)), xnT, TG // 128, ident, tok0=tg * (TG // 128))
                    pp = C.ps(e2, [128, 512], F32, "pp")
                    tp = C.ps(e2, [128, 8, 128], BF16, "tp4")
                    for tt in range(TG // 128 if DBG.get("odd_stop") != 0.25 else 0):
                        Tt = tg * (TG // 128) + tt
                        for kc in range(8):
                            S.pe(lambda e: e.matmul(out=pp[:, 0:456], lhsT=xnT[:, kc, tt * 128:(tt + 1) * 128], rhs=wsm[:, kc, 0:456],
                                                    start=(kc == 0), stop=(kc == 7)), reads=[xnT, wsm], writes=[pp])
                        S.dve(lambda e: e.memset(ss[:, :], 0.0), writes=[ss])
                        S.act(lambda e: e.activation(out=junk[:, 0:256], in_=pp[:, 0:256], func=AF.Square, accum_out=ss[:, 0:1]),
                              reads=[pp], writes=[junk, ss])
                        S.act(lambda e: e.activation(out=junk[:, 0:128], in_=pp[:, 256:384], func=AF.Square, accum_out=ss[:, 1:2]),
                              reads=[pp], writes=[junk, ss])
                        S.dve(lambda e: e.tensor_scalar(out=rs[:, 0:1], in0=ss[:, 0:1], scalar1=1.0 / 256, scalar2=EPS,
                                                        op0=ALU.mult, op1=ALU.add), reads=[ss], writes=[rs])
                        S.dve(lambda e: e.tensor_scalar(out=rs[:, 1:2], in0=ss[:, 1:2], scalar1=1.0 / 128, scalar2=EPS,
                                                        op0=ALU.mult, op1=ALU.add), reads=[ss], writes=[rs])
                        S.act(lambda e: e.activation(out=rs[:, :], in_=rs[:, :], func=AF.Sqrt), reads=[rs], writes=[rs])
                        S.dve(lambda e: e.reciprocal(out=rs[:, :], in_=rs[:, :]), reads=[rs], writes=[rs])
                        S.act(lambda e: e.activation(out=cqs[:, :], in_=pp[:, 0:256], func=AF.Copy, scale=rs[:, 0:1]),
                              reads=[pp, rs], writes=[cqs])
                        S.dve(lambda e: e.scalar_tensor_tensor(out=ckvtok[:, Tt, :], in0=pp[:, 256:384], scalar=rs[:, 1:2],
                                                               in1=gkv_bc[:, :], op0=ALU.mult, op1=ALU.mult),
                              reads=[pp, rs, gkv_bc], writes=[ckvtok.part(Tt)])
                        S.dve(lambda e: e.tensor_copy(out=kid2[:, :, :], in_=pp[:, 384:448].unsqueeze(1).to_broadcast([128, 2, 64])),
                              reads=[pp], writes=[kid2])
                        S.dve(lambda e: e.tensor_copy(out=widx[:, Tt, :], in_=pp[:, 448:456]), reads=[pp], writes=[widx.part(Tt)])
                        for c in range(2):
                            S.pe(lambda e: e.transpose(out=tp[:, c, :], in_=cqs[:, c * 128:(c + 1) * 128], identity=ident[:, :]),
                                 reads=[cqs, ident], writes=[tp])
                        S.pe(lambda e: e.transpose(out=tp[:, 2, :], in_=ckvtok[:, Tt, :], identity=ident[:, :]),
                             reads=[ckvtok.part(Tt), ident], writes=[tp])
                        S.pe(lambda e: e.transpose(out=tp[:, 3, :], in_=kid2[:, :, :].rearrange("p a b -> p (a b)"), identity=ident[:, :]),
                             reads=[kid2, ident], writes=[tp])
                        tsl = slice(Tt * 128, (Tt + 1) * 128)
                        S.dve(lambda e: e.tensor_tensor(out=cqnT[:, :, tsl], in0=tp[:, 0:2, :],
                                                        in1=P.cols(("dsa_qg", i), 0, 2).unsqueeze(2).to_broadcast([128, 2, 128]),
                                                        op=ALU.mult), reads=[tp, P.t], writes=[cqnT.part(Tt)])
                        S.act(lambda e: e.copy(out=ckvT[:, tsl], in_=tp[:, 2, :]), reads=[tp], writes=[ckvT.part(Tt)])
                        S.act(lambda e: e.copy(out=kidxT2[:, tsl], in_=tp[:, 3, :]), reads=[tp], writes=[kidxT2.part(Tt)])
                    pcs = [C.ps(e2, [128, 512], F32, f"pc{k}") for k in range(3)]
                    for fc in range(4 if DBG.get("odd_stop") != 0.5 else 0):
                        wsc = wscs[fc % 2]
                        for kc in range(8):
                            S.dma("pool", wsc[:, kc, :, :],
                                  w_v[:, kc, 456:1992].rearrange("p (j c) -> p j c", j=3)[:, :, fc * 128:(fc + 1) * 128],
                                  writes=[wsc.part(kc)])
                        for th in range(TG // 512):
                            tok0 = tg * TG + th * 512
                            for j3 in range(3):
                                for kc in range(8):
                                    S.pe(lambda e: e.matmul(out=pcs[j3][:, :], lhsT=wsc[:, kc, j3, :], rhs=xnT[:, kc, th * 512:(th + 1) * 512],
                                                            start=(kc == 0), stop=(kc == 7)), reads=[wsc, xnT], writes=[pcs[j3]])
                            S.act(lambda e: e.copy(out=hs[:, :], in_=pcs[0][:, :]), reads=[pcs[0]], writes=[hs])
                            S.pool(lambda e: e.tensor_copy(out=ub[:, 0:2], in_=carry[:, fc, :]), reads=[carry.part(fc)], writes=[ub])
                            S.dve(lambda e: e.tensor_tensor(out=ub[:, 2:514], in0=pcs[2][:, :], in1=hs[:, :], op=ALU.mult),
                                  reads=[pcs[2], hs], writes=[ub])
                            S.pool(lambda e: e.tensor_copy(out=carry[:, fc, :], in_=ub[:, 512:514]), reads=[ub], writes=[carry.part(fc)])
                            S.pool(lambda e: e.tensor_scalar(out=yb[:, :], in0=ub[:, 2:514], scalar1=P.col(("sc_w", i, 2), fc),
                                                             scalar2=P.col(("sc_b", i), fc), op0=ALU.mult, op1=ALU.add),
                                   reads=[ub, P.t], writes=[yb])
                            S.dve(lambda e: e.scalar_tensor_tensor(out=yb[:, :], in0=ub[:, 1:513], scalar=P.col(("sc_w", i, 1), fc),
                                                                   in1=yb[:, :], op0=ALU.mult, op1=ALU.add), reads=[ub, yb, P.t], writes=[yb])
                            S.dve(lambda e: e.scalar_tensor_tensor(out=yb[:, :], in0=ub[:, 0:512], scalar=P.col(("sc_w", i, 0), fc),
                                                                   in1=yb[:, :], op0=ALU.mult, op1=ALU.add), reads=[ub, yb, P.t], writes=[yb])
                            S.dve(lambda e: e.tensor_tensor(out=ydT[:, fc, tok0:tok0 + 512], in0=pcs[1][:, :], in1=yb[:, :], op=ALU.mult),
                                  reads=[pcs[1], yb], writes=[ydT.part((fc, tok0))])
                    S.barrier()
            S.act(lambda e: e.activation(out=absw[:, :, :], in_=widx[:, :, :], func=AF.Abs), reads=[widx], writes=[absw])
            S.act(lambda e: e.activation(out=sgnw[:, :, :], in_=widx[:, :, :], func=AF.Sign), reads=[widx], writes=[sgnw])
            S.barrier()
        if DBG.get("odd_stop") in (1, 0.5, 0.25):
            return
        with ExitStack() as e1:
            wqi = C.sb(e1, [128, 2, 512], BF16, "wqi")
            wuq = C.sb(e1, [128, 2, 512], BF16, "wuq")
            wuk = C.sb(e1, [128, 4, 128], BF16, "wuk")
            wuv = C.sb(e1, [128, 8, 64], BF16, "wuv")
            load_w(C, wqi, W.H("dsa_w_qi", i).rearrange("r h d -> r (h d)"), 0, 512)
            load_w(C, wuq, W.H("dsa_w_uq", i).rearrange("r h d -> r (h d)"), 0, 512)
            S.dma("pool", wuk[:, :, :], W.H("dsa_w_uk", i).rearrange("(hp e) d c -> (e d) hp c", e=2), writes=[wuk])
            S.dma("pool", wuv[:, :, :], W.H("dsa_w_uv", i).rearrange("h c d -> c h d"), writes=[wuv])
            k2m = C.sb(e1, [8, 1], F32, "k2m")
            with ExitStack() as e2:
                ksq = C.sb(e2, [128, SEQ], BF16, "ksq")
                k2b = C.sb(e2, [8, 4], F32, "k2b")
                pk = C.ps(e2, [128, 512], F32, "pk")
                S.act(lambda e: e.activation(out=ksq[:, :], in_=ckvT[:, :], func=AF.Square), reads=[ckvT], writes=[ksq])
                for b in range(4):
                    S.pe(lambda e: e.matmul(out=pk[0:8, :], lhsT=K["ones_bf"][:, 0:8], rhs=ksq[:, b * 512:(b + 1) * 512], start=True, stop=True),
                         reads=[K["ones_bf"], ksq], writes=[pk])
                    S.dve(lambda e: e.reduce_max(out=k2b[:, b:b + 1], in_=pk[0:8, :], axis=AX.X), reads=[pk], writes=[k2b])
                S.dve(lambda e: e.reduce_max(out=k2m[:, 0:1], in_=k2b[:, :], axis=AX.X), reads=[k2b], writes=[k2m])
                S.barrier()
            for qg in range(4):
                tsl = slice(qg * 512, (qg + 1) * 512)
                nkt = 4 * qg + 4
                with ExitStack() as e2:
                    qidxT = C.sb(e2, [128, 4, 512], BF16, "qidxT")
                    qhT = C.sb(e2, [128, 4, 512], BF16, "qhT")
                    qlatT = C.sb(e2, [128, 8, 512], BF16, "qlatT")
                    maskT = C.sb(e2, [128, nkt, 512], BF16, "maskT")
                    S.pool(lambda e: e.memset(maskT[:, :, :], 0.0), writes=[maskT])
                    with ExitStack() as e3:
                        pq = [C.ps(e3, [128, 512], F32, f"pq{k}") for k in range(2)]
                        n_ = 0
                        for (wt, dst) in ((wqi, qidxT), (wuq, qhT)):
                            for hp in range(4):
                                p_ = pq[n_ % 2]
                                n_ += 1
                                for rc in range(2):
                                    S.pe(lambda e: e.matmul(out=p_[:, :], lhsT=wt[:, rc, hp * 128:(hp + 1) * 128], rhs=cqnT[:, rc, tsl],
                                                            start=(rc == 0), stop=(rc == 1)), reads=[wt, cqnT], writes=[p_])
                                S.act(lambda e: e.copy(out=dst[:, hp, :], in_=p_[:, :]), reads=[p_], writes=[dst.part(hp)])
                        for h in range(8):
                            hp, e_ = h // 2, h % 2
                            p_ = pq[h % 2]
                            S.pe(lambda e: e.matmul(out=p_[:, :], lhsT=wuk[e_ * 64:(e_ + 1) * 64, hp, :], rhs=qhT[e_ * 64:(e_ + 1) * 64, hp, :],
                                                    start=True, stop=True), reads=[wuk, qhT], writes=[p_])
                            S.dve(lambda e: e.tensor_copy(out=qlatT[:, h, :], in_=p_[:, :]), reads=[p_], writes=[qlatT.part(h)])
                        S.barrier()
                    if DBG.get("odd_stop") == 2:
                        continue
                    with ExitStack() as e3:
                        lps = [C.ps(e3, [128, 512], F32, f"lp{k}") for k in range(2)]
                        tpm = C.ps(e3, [128, 8, 128], BF16, "tpm")
                        sc = C.sb(e3, [128, SEQ], F32, "sc")
                        mk = C.sb(e3, [128, SEQ], BF16, "mk")
                        rb = [C.sb(e3, [128, 512], F32, f"rb{k}") for k in range(2)]
                        m8 = C.sb(e3, [128, 8], F32, "m8")
                        n_ = 0
                        for ql in range(4):
                            qt = 4 * qg + ql
                            nk = (qt + 1) * 128
                            for kb in range((nk + 511) // 512):
                                n = min(512, nk - kb * 512)
                                ksl = slice(kb * 512, kb * 512 + n)
                                for h in range(8):
                                    hp, e_ = h // 2, h % 2
                                    lp = lps[n_ % 2]
                                    r_ = rb[n_ % 2]
                                    n_ += 1
                                    S.pe(lambda e: e.matmul(out=lp[:, 0:n], lhsT=qidxT[e_ * 64:(e_ + 1) * 64, hp, ql * 128:(ql + 1) * 128],
                                                            rhs=kidxT2[e_ * 64:(e_ + 1) * 64, ksl], start=True, stop=True),
                                         reads=[qidxT, kidxT2], writes=[lp])
                                    S.act(lambda e: e.activation(out=r_[:, 0:n], in_=lp[:, 0:n], func=AF.Relu, scale=absw[:, qt, h:h + 1]),
                                          reads=[lp, absw], writes=[r_])
                                    eng = "pool" if h % 2 else "dve"
                                    if h == 0:
                                        S.op(eng, lambda e: e.tensor_scalar(out=sc[:, ksl], in0=r_[:, 0:n], scalar1=sgnw[:, qt, 0:1],
                                                                            scalar2=None, op0=ALU.mult), reads=[r_, sgnw], writes=[sc])
                                    elif eng == "dve":
                                        S.dve(lambda e: e.scalar_tensor_tensor(out=sc[:, ksl], in0=r_[:, 0:n], scalar=sgnw[:, qt, h:h + 1],
                                                                               in1=sc[:, ksl], op0=ALU.mult, op1=ALU.add),
                                              reads=[r_, sgnw, sc], writes=[sc])
                                    else:
                                        S.pool(lambda e: e.tensor_scalar(out=r_[:, 0:n], in0=r_[:, 0:n], scalar1=sgnw[:, qt, h:h + 1],
                                                                         scalar2=None, op0=ALU.mult), reads=[r_, sgnw], writes=[r_])
                                        S.pool(lambda e: e.tensor_tensor(out=sc[:, ksl], in0=sc[:, ksl], in1=r_[:, 0:n], op=ALU.add),
                                               reads=[r_, sc], writes=[sc])
                            dsl = slice(qt * 128, (qt + 1) * 128)
                            S.pool(lambda e: e.tensor_tensor(out=sc[:, dsl], in0=sc[:, dsl], in1=K["cbias"][:, :], op=ALU.add),
                                   reads=[sc, K["cbias"]], writes=[sc])
                            if qt >= 2:
                                for r in range(32):
                                    S.dve(lambda e: e.max(out=m8[:, :], in_=sc[:, 0:nk]), reads=[sc], writes=[m8])
                                    S.dve(lambda e: e.match_replace(out=sc[:, 0:nk], in_to_replace=m8[:, :], in_values=sc[:, 0:nk],
                                                                    imm_value=NEG_SEL), reads=[sc, m8], writes=[sc])
                                S.dve(lambda e: e.tensor_single_scalar(out=mk[:, 0:nk], in_=sc[:, 0:nk], scalar=-1e35, op=ALU.is_lt),
                                      reads=[sc], writes=[mk])
                            else:
                                S.dve(lambda e: e.tensor_single_scalar(out=mk[:, 0:nk], in_=sc[:, 0:nk], scalar=-1e29, op=ALU.is_gt),
                                      reads=[sc], writes=[mk])
                            for k0 in range(0, qt + 1, 8):
                                cnt = min(8, qt + 1 - k0)
                                for kk in range(cnt):
                                    kt = k0 + kk
                                    S.pe(lambda e: e.transpose(out=tpm[:, kk, :], in_=mk[:, kt * 128:(kt + 1) * 128], identity=ident[:, :]),
                                         reads=[mk, ident], writes=[tpm])
                                S.act(lambda e: e.copy(out=maskT[:, k0:k0 + cnt, ql * 128:(ql + 1) * 128], in_=tpm[:, 0:cnt, :]),
                                      reads=[tpm], writes=[maskT])
                        S.barrier()
                    if DBG.get("odd_stop") == 3:
                        continue
                    with ExitStack() as e3:
                        p8 = C.ps(e3, [128, 512], F32, "p8")
                        negm8 = make_negm(C, e3, qlatT, 8, k2m, K, p8)
                        ps_s = [C.ps(e3, [128, 512], F32, f"ss{k}") for k in range(2)]
                        ps_o = C.ps(e3, [128, 512], F32, "pso")
                        ps_r = C.ps(e3, [128, 512], F32, "psr")
                        po = C.ps(e3, [128, 512], F32, "po")
                        pts = [C.sb(e3, [128, 512], BF16, f"pt{k}") for k in range(2)]
                        rinv = C.sb(e3, [128, 512], F32, "rinv")
                        olat = [C.sb(e3, [128, 512], BF16, f"olat{k}") for k in range(2)]
                        for h in range(8):
                            hp, e_ = h // 2, h % 2
                            kt_list = [(ckvT[:, kt * 128:(kt + 1) * 128], ckvtok[:, kt, :], maskT[:, kt, :], [ckvT, ckvtok, maskT])
                                       for kt in range(nkt)]
                            ol = olat[h % 2]
                            attn_core(C, K, qlatT[:, h, :], qlatT, kt_list, negm8, h, 64 ** -0.5, ol[:, :], ol, 128,
                                      ps_s, ps_o, ps_r, pts, rinv, mask_eng=("pool" if h % 2 else "dve"))
                            S.pe(lambda e: e.matmul(out=po[e_ * 64:(e_ + 1) * 64, :], lhsT=wuv[:, h, :], rhs=ol[:, :], start=True, stop=True),
                                 reads=[wuv, ol], writes=[po])
                            if e_ == 1:
                                S.act(lambda e: e.copy(out=ycT[:, hp, tsl], in_=po[:, :]), reads=[po], writes=[ycT.part((hp, qg))])
                        S.barrier()
        with ExitStack() as e1:
            wo = C.sb(e1, [128, 8, D], BF16, "wo")
            load_w(C, wo, W.H("od_w_out", i), 0, D)
            for g4 in range(4):
                chunks = [(ycT, (lambda ti, c=c, g4=g4: ycT[:, c, (g4 * 4 + ti) * 128:(g4 * 4 + ti + 1) * 128])) for c in range(4)]
                chunks += [(ydT, (lambda ti, c=c, g4=g4: ydT[:, c, (g4 * 4 + ti) * 128:(g4 * 4 + ti + 1) * 128])) for c in range(4)]
                out_proj_residual(C, x, chunks, wo, g_bc, 1.0, [g4 * 4 + t_ for t_ in range(4)])


RW_LN_EPS = 64e-5
TWO_PI = 6.283185307179586


def make_even_consts(C, es, K):
    S = C.S
    ones_f = K["ones_f"]
    E = {}
    o4 = ones_f[:, 0:512].rearrange("p (a b) -> p a b", a=4)
    for nm, pat, cm, op in (("m_su", [[0, 4], [1, 128]], -1, ALU.is_gt), ("m_ui", [[0, 4], [1, 128]], -1, ALU.is_ge),
                            ("m_sl", [[0, 4], [-1, 128]], 1, ALU.is_gt)):
        t = C.sb(es, [128, 4, 128], F32, nm)
        S.pool(lambda e: e.affine_select(out=t[:, :, :], in_=o4, pattern=pat, compare_op=op, fill=0.0, base=0,
                                         channel_multiplier=cm), reads=[ones_f], writes=[t])
        E[nm] = t
    lvm = C.sb(es, [128, 7, 128], BF16, "lvm")
    lvmT = C.sb(es, [128, 7, 128], BF16, "lvmT")
    with ExitStack() as e0:
        I32 = mybir.dt.int32
        pi = C.sb(e0, [128, 128], I32, "lv_pi")
        fi = C.sb(e0, [128, 128], I32, "lv_fi")
        S.pool(lambda e: e.iota(pi[:, :], pattern=[[0, 128]], base=0, channel_multiplier=1), writes=[pi])
        S.pool(lambda e: e.iota(fi[:, :], pattern=[[1, 128]], base=0, channel_multiplier=0), writes=[fi])
        ta = C.sb(e0, [128, 128], I32, "lv_ta")
        tb = C.sb(e0, [128, 128], I32, "lv_tb")
        eq = C.sb(e0, [128, 128], F32, "lv_eq")
        bp = C.sb(e0, [128, 128], F32, "lv_bp")
        bq = C.sb(e0, [128, 128], F32, "lv_bq")
        nbp = C.sb(e0, [128, 128], F32, "lv_nbp")
        nbq = C.sb(e0, [128, 128], F32, "lv_nbq")
        for s in range(7):
            S.dve(lambda e: e.tensor_scalar(out=ta[:, :], in0=pi[:, :], scalar1=s + 1, scalar2=None, op0=ALU.arith_shift_right), reads=[pi], writes=[ta])
            S.dve(lambda e: e.tensor_scalar(out=tb[:, :], in0=fi[:, :], scalar1=s + 1, scalar2=None, op0=ALU.arith_shift_right), reads=[fi], writes=[tb])
            S.dve(lambda e: e.tensor_tensor(out=eq[:, :], in0=ta[:, :], in1=tb[:, :], op=ALU.is_equal), reads=[ta, tb], writes=[eq])
            S.dve(lambda e: e.tensor_scalar(out=ta[:, :], in0=pi[:, :], scalar1=s, scalar2=1, op0=ALU.arith_shift_right, op1=ALU.bitwise_and), reads=[pi], writes=[ta])
            S.dve(lambda e: e.tensor_scalar(out=tb[:, :], in0=fi[:, :], scalar1=s, scalar2=1, op0=ALU.arith_shift_right, op1=ALU.bitwise_and), reads=[fi], writes=[tb])
            S.dve(lambda e: e.tensor_copy(out=bp[:, :], in_=ta[:, :]), reads=[ta], writes=[bp])
            S.dve(lambda e: e.tensor_copy(out=bq[:, :], in_=tb[:, :]), reads=[tb], writes=[bq])
            S.dve(lambda e: e.tensor_scalar(out=nbp[:, :], in0=bp[:, :], scalar1=-1.0, scalar2=1.0, op0=ALU.mult, op1=ALU.add), reads=[bp], writes=[nbp])
            S.dve(lambda e: e.tensor_scalar(out=nbq[:, :], in0=bq[:, :], scalar1=-1.0, scalar2=1.0, op0=ALU.mult, op1=ALU.add), reads=[bq], writes=[nbq])
            S.dve(lambda e: e.tensor_tensor(out=nbp[:, :], in0=nbp[:, :], in1=bq[:, :], op=ALU.mult), reads=[nbp, bq], writes=[nbp])
            S.dve(lambda e: e.tensor_tensor(out=lvm[:, s, :], in0=nbp[:, :], in1=eq[:, :], op=ALU.mult), reads=[nbp, eq], writes=[lvm])
            S.dve(lambda e: e.tensor_tensor(out=nbq[:, :], in0=nbq[:, :], in1=bp[:, :], op=ALU.mult), reads=[nbq, bp], writes=[nbq])
            S.dve(lambda e: e.tensor_tensor(out=lvmT[:, s, :], in0=nbq[:, :], in1=eq[:, :], op=ALU.mult), reads=[nbq, eq], writes=[lvmT])
        S.barrier()
    E.update(lvm=lvm, lvmT=lvmT)
    id8 = C.sb(es, [128, 8, 128], BF16, "id8")
    with ExitStack() as e0:
        ones8 = C.sb(e0, [128, 8, 128], F32, "ones8")
        S.pool(lambda e: e.memset(ones8[:, :, :], 1.0), writes=[ones8])
        S.pool(lambda e: e.affine_select(out=id8[:, :, :], in_=ones8[:, :, :], pattern=[[0, 8], [-1, 128]], compare_op=ALU.is_equal, fill=0.0,
                                         base=0, channel_multiplier=1), reads=[ones8], writes=[id8])
        S.barrier()
    E["id8"] = id8
    bo = C.sb(es, [128, 128], BF16, "blockones")
    bof = C.sb(es, [128, 128], BF16, "blockones_f")
    S.pool(lambda e: e.memset(bo[:, :], 0.0), writes=[bo])
    S.pool(lambda e: e.memset(bof[:, :], 0.0), writes=[bof])
    for b in range(2):
        S.pool(lambda e: e.memset(bo[b * 64:(b + 1) * 64, b * 64:(b + 1) * 64], 1.0), writes=[bo])
        S.pool(lambda e: e.memset(bof[b * 64:(b + 1) * 64, b * 64:(b + 1) * 64], 1.0 / 64), writes=[bof])
    on128 = C.sb(es, [128, 128], BF16, "on128")
    S.pool(lambda e: e.memset(on128[:, :], 1.0 / 128), writes=[on128])
    E.update(bo=bo, bof=bof, on128=on128)
    seg = C.sb(es, [128, 4, 128], F32, "seg")
    S.pool(lambda e: e.memset(seg[:, :, :], 1.0), writes=[seg])
    S.pool(lambda e: e.memset(seg[:, :, 0:1], 0.0), writes=[seg])
    E["seg"] = seg
    cosT = C.sb(es, [128, SEQ], BF16, "cosT")
    sinT = C.sb(es, [128, SEQ], BF16, "sinT")
    with ExitStack() as e1:
        jc_i = C.sb(e1, [128, 1], mybir.dt.int32, "jc_i")
        for b in range(2):
            S.pool(lambda e: e.iota(jc_i[b * 64:(b + 1) * 64, :], pattern=[[0, 1]], base=0, channel_multiplier=1), writes=[jc_i])
        jc = C.sb(e1, [128, 1], F32, "jc")
        S.dve(lambda e: e.tensor_copy(out=jc[:, :], in_=jc_i[:, :]), reads=[jc_i], writes=[jc])
        invf = C.sb(e1, [128, 1], F32, "invf")
        S.act(lambda e: e.activation(out=invf[:, :], in_=jc[:, :], func=AF.Exp, scale=-float(np.log(10000.0)) / 64.0),
              reads=[jc], writes=[invf])
        tp_i = C.sb(e1, [128, SEQ], mybir.dt.int32, "tp_i")
        S.pool(lambda e: e.iota(tp_i[:, :], pattern=[[1, SEQ]], base=0, channel_multiplier=0), writes=[tp_i])
        ang = C.sb(e1, [128, SEQ], F32, "ang")
        S.dve(lambda e: e.tensor_copy(out=ang[:, :], in_=tp_i[:, :]), reads=[tp_i], writes=[ang])
        S.dve(lambda e: e.tensor_scalar(out=ang[:, :], in0=ang[:, :], scalar1=invf[:, 0:1], scalar2=None, op0=ALU.mult),
              reads=[ang, invf], writes=[ang])
        sgn = C.sb(e1, [128, 1], F32, "sgn")
        S.pool(lambda e: e.memset(sgn[0:64, :], -1.0), writes=[sgn])
        S.pool(lambda e: e.memset(sgn[64:128, :], 1.0), writes=[sgn])
        red = C.sb(e1, [128, SEQ], F32, "red")
        qi = C.sb(e1, [128, SEQ], mybir.dt.int32, "qi")
        qf = C.sb(e1, [128, SEQ], F32, "qf")
        for (dst, shift) in ((sinT, 0.0), (cosT, float(np.pi / 2))):
            S.dve(lambda e: e.tensor_scalar(out=qf[:, :], in0=ang[:, :], scalar1=shift, scalar2=1.0 / TWO_PI, op0=ALU.add, op1=ALU.mult),
                  reads=[ang], writes=[qf])
            S.dve(lambda e: e.tensor_copy(out=qi[:, :], in_=qf[:, :]), reads=[qf], writes=[qi])
            S.dve(lambda e: e.tensor_copy(out=qf[:, :], in_=qi[:, :]), reads=[qi], writes=[qf])
            S.dve(lambda e: e.scalar_tensor_tensor(out=red[:, :], in0=qf[:, :], scalar=-TWO_PI, in1=ang[:, :], op0=ALU.mult, op1=ALU.add),
                  reads=[qf, ang], writes=[red])
            if shift:
                S.dve(lambda e: e.tensor_scalar(out=red[:, :], in0=red[:, :], scalar1=shift, scalar2=None, op0=ALU.add), reads=[red], writes=[red])
            S.dve(lambda e: e.tensor_scalar(out=qf[:, :], in0=red[:, :], scalar1=float(np.pi), scalar2=-TWO_PI, op0=ALU.is_gt, op1=ALU.mult),
                  reads=[red], writes=[qf])
            S.dve(lambda e: e.tensor_tensor(out=red[:, :], in0=red[:, :], in1=qf[:, :], op=ALU.add), reads=[red, qf], writes=[red])
            S.dve(lambda e: e.tensor_scalar(out=qf[:, :], in0=red[:, :], scalar1=-float(np.pi), scalar2=TWO_PI, op0=ALU.is_lt, op1=ALU.mult),
                  reads=[red], writes=[qf])
            S.dve(lambda e: e.tensor_tensor(out=red[:, :], in0=red[:, :], in1=qf[:, :], op=ALU.add), reads=[red, qf], writes=[red])
            S.dve(lambda e: e.tensor_scalar(out=red[:, :], in0=red[:, :], scalar1=3.14159, scalar2=-3.14159, op0=ALU.min, op1=ALU.max),
                  reads=[red], writes=[red])
            if shift:
                S.act(lambda e: e.activation(out=dst[:, :], in_=red[:, :], func=AF.Sin), reads=[red], writes=[dst])
            else:
                S.act(lambda e: e.activation(out=red[:, :], in_=red[:, :], func=AF.Sin), reads=[red], writes=[red])
                S.dve(lambda e: e.tensor_scalar(out=dst[:, :], in0=red[:, :], scalar1=sgn[:, 0:1], scalar2=None, op0=ALU.mult),
                      reads=[red, sgn], writes=[dst])
        S.barrier()
    E.update(cosT=cosT, sinT=sinT)
    lg = [float(np.log1p(-2.0 ** (-5.0 - h))) for h in range(4)]
    E["lg"] = lg
    scale = 128 ** -0.5
    dmT = C.sb(es, [128, 4, 128], F32, "dmT")
    xiT = C.sb(es, [128, 4, 128], BF16, "xiT")
    zcol = C.sb(es, [128, 4], F32, "zcol")
    with ExitStack() as e1:
        d_i = C.sb(e1, [128, 128], mybir.dt.int32, "d_i")
        d_f = C.sb(e1, [128, 128], F32, "d_f")
        S.pool(lambda e: e.iota(d_i[:, :], pattern=[[1, 128]], base=0, channel_multiplier=-1), writes=[d_i])
        S.dve(lambda e: e.tensor_copy(out=d_f[:, :], in_=d_i[:, :]), reads=[d_i], writes=[d_f])
        S.dve(lambda e: e.tensor_scalar(out=d_f[:, :], in0=d_f[:, :], scalar1=0.0, scalar2=None, op0=ALU.max), reads=[d_f], writes=[d_f])
        i_i = C.sb(e1, [128, 128], mybir.dt.int32, "i_i")
        i_f = C.sb(e1, [128, 128], F32, "i_f")
        S.pool(lambda e: e.iota(i_i[:, :], pattern=[[1, 128]], base=1, channel_multiplier=0), writes=[i_i])
        S.dve(lambda e: e.tensor_copy(out=i_f[:, :], in_=i_i[:, :]), reads=[i_i], writes=[i_f])
        p_i = C.sb(e1, [128, 1], mybir.dt.int32, "p_i")
        p_f = C.sb(e1, [128, 1], F32, "p_f")
        S.pool(lambda e: e.iota(p_i[:, :], pattern=[[0, 1]], base=127, channel_multiplier=-1), writes=[p_i])
        S.dve(lambda e: e.tensor_copy(out=p_f[:, :], in_=p_i[:, :]), reads=[p_i], writes=[p_f])
        tmp = C.sb(e1, [128, 128], F32, "tmpd")
        for h in range(4):
            S.act(lambda e: e.activation(out=tmp[:, :], in_=d_f[:, :], func=AF.Exp, scale=lg[h]), reads=[d_f], writes=[tmp])
            S.dve(lambda e: e.scalar_tensor_tensor(out=dmT[:, h, :], in0=tmp[:, :], scalar=scale, in1=E["m_ui"][:, 0, :],
                                                   op0=ALU.mult, op1=ALU.mult), reads=[tmp, E["m_ui"]], writes=[dmT])
            S.act(lambda e: e.activation(out=xiT[:, h, :], in_=i_f[:, :], func=AF.Exp, scale=lg[h]), reads=[i_f], writes=[xiT])
            S.act(lambda e: e.activation(out=zcol[:, h:h + 1], in_=p_f[:, :], func=AF.Exp, scale=lg[h]), reads=[p_f], writes=[zcol])
        S.dve(lambda e: e.tensor_scalar(out=zcol[:, :], in0=zcol[:, :], scalar1=scale, scalar2=None, op0=ALU.mult), reads=[zcol], writes=[zcol])
        S.barrier()
    E.update(dmT=dmT, xiT=xiT, zcol=zcol)
    return E


def group_norm_T(C, es, y, nch, onesmat, eps, gcol_fn, bcol_fn, post_fn, pm, pq):
    S = C.S
    sq = C.sb(es, [128, 512], BF16, "gn_sq")
    yb16 = C.sb(es, [128, 512], BF16, "gn_yb")
    m2 = C.sb(es, [128, 512], F32, "gn_m2")
    rs = C.sb(es, [128, 512], F32, "gn_rs")
    dd = C.sb(es, [128, 512], F32, "gn_dd")
    for c in range(nch):
        S.dve(lambda e: e.tensor_copy(out=yb16[:, :], in_=y[:, c, :]), reads=[y], writes=[yb16])
        S.pe(lambda e: e.matmul(out=pm[:, :], lhsT=onesmat[:, :], rhs=yb16[:, :], start=True, stop=True), reads=[onesmat, yb16], writes=[pm])
        S.act(lambda e: e.activation(out=sq[:, :], in_=y[:, c, :], func=AF.Square), reads=[y], writes=[sq])
        S.pe(lambda e: e.matmul(out=pq[:, :], lhsT=onesmat[:, :], rhs=sq[:, :], start=True, stop=True), reads=[onesmat, sq], writes=[pq])
        S.act(lambda e: e.activation(out=m2[:, :], in_=pm[:, :], func=AF.Square), reads=[pm], writes=[m2])
        S.dve(lambda e: e.tensor_tensor(out=rs[:, :], in0=pq[:, :], in1=m2[:, :], op=ALU.subtract), reads=[pq, m2], writes=[rs])
        S.dve(lambda e: e.tensor_scalar(out=rs[:, :], in0=rs[:, :], scalar1=0.0, scalar2=float(eps), op0=ALU.max, op1=ALU.add),
              reads=[rs], writes=[rs])
        S.act(lambda e: e.activation(out=rs[:, :], in_=rs[:, :], func=AF.Sqrt), reads=[rs], writes=[rs])
        S.dve(lambda e: e.reciprocal(out=rs[:, :], in_=rs[:, :]), reads=[rs], writes=[rs])
        S.dve(lambda e: e.tensor_tensor(out=dd[:, :], in0=y[:, c, :], in1=pm[:, :], op=ALU.subtract), reads=[y, pm], writes=[dd])
        S.dve(lambda e: e.tensor_tensor(out=dd[:, :], in0=dd[:, :], in1=rs[:, :], op=ALU.mult), reads=[dd, rs], writes=[dd])
        S.pool(lambda e: e.tensor_scalar(out=dd[:, :], in0=dd[:, :], scalar1=gcol_fn(c), scalar2=bcol_fn(c), op0=ALU.mult, op1=ALU.add),
               reads=[dd], writes=[dd])
        post_fn(c, dd)


def even_mixer(C, x, l, W, P, K):
    S = C.S
    i = l // 2
    ident = K["ident"]
    w_in = W.H("ev_w_in", i)
    w_v = w_in.rearrange("(kc p) n -> p kc n", p=128)
    if DBG.get("even_stop") == 0:
        return
    with ExitStack() as es:
        E = make_even_consts(C, es, K)
        lg = E["lg"]
        gamC = [float(np.exp(128.0 * lg[h])) for h in range(4)]
        yaT = C.sb(es, [128, 4, SEQ], BF16, "yaT")
        with ExitStack() as er:
            wa2 = C.sb(er, [128, 512], BF16, "wa2")
            g2 = C.sb(er, [128, 512], BF16, "g2")
            S.dma("pool", wa2[0:64, :], W.H("rw_w2", i), writes=[wa2])
            S.dma("pool", wa2[64:128, :], W.H("rw_a2", i), writes=[wa2])
            S.dma("pool", g2[:, :], W.H("rw_g2", i), writes=[g2])
            omk = C.sb(er, [128, 4], F32, "omk")
            S.dve(lambda e: e.tensor_scalar(out=omk[:, :], in0=P.cols(("rw_k_a", i), 0, 4), scalar1=-1.0, scalar2=1.0, op0=ALU.mult, op1=ALU.add),
                  reads=[P.t], writes=[omk])
            St = C.sb(er, [128, 4, 64], F32, "St")
            Sb = C.sb(er, [128, 4, 2, 64], BF16, "Sbd")
            S.pool(lambda e: e.memset(St[:, :, :], 0.0), writes=[St])
            S.pool(lambda e: e.memset(Sb[:, :, :, :], 0.0), writes=[Sb])
            pcar = C.sb(er, [128, 14], F32, "pcar")
            S.pool(lambda e: e.memset(pcar[:, :], 0.0), writes=[pcar])
            wch = [C.sb(er, [128, 8, 128], BF16, f"wch{k}") for k in range(2)]
            nw = [0]

            def mu_col(c):
                return P.col(("rw_mu", i, 0), c) if c < 8 else P.col(("rw_mu", i, 1), c - 8)

            for blk in range(4):
                t0 = blk * 512
                with ExitStack() as e1:
                    xnT = C.sb(e1, [128, 8, 512], BF16, "xnT")
                    rms_to_T(C, e1, x, P.cols(("norm_g", l, 2)), xnT, 4, ident, tok0=blk * 4)
                    with ExitStack() as e2:
                        At = C.sb(e2, [128, 4, 512], BF16, "At")
                        Bt = C.sb(e2, [128, 4, 512], BF16, "Bt")
                        Kt = C.sb(e2, [128, 4, 512], BF16, "Kt")
                        Rq = C.sb(e2, [128, 4, 512], BF16, "Rq")
                        vT = C.sb(e2, [128, 4, 512], BF16, "vT")
                        bon = C.sb(e2, [128, 4, 512], BF16, "bon")
                        gT = C.sb(e2, [128, 4, 512], BF16, "gT")
                        gC = C.sb(e2, [128, 4, 4], F32, "gC")
                        yraw = C.sb(e2, [128, 4, 512], F32, "yraw")
                        with ExitStack() as e3:
                            pps = [C.ps(e3, [128, 512], F32, f"pp{k}") for k in range(3)]
                            pa = C.ps(e3, [128, 512], F32, "pa")
                            pb = C.ps(e3, [128, 512], F32, "pb")
                            pT = C.sb(e3, [128, 513], F32, "pT")
                            mx = [C.sb(e3, [128, 512], F32, f"mx{k}") for k in range(3)]
                            dtmp = C.sb(e3, [128, 512], F32, "dtmp")
                            xwa = C.sb(e3, [128, 512], BF16, "xwa")
                            sxg = C.sb(e3, [128, 512], BF16, "sxg")
                            kkb = C.sb(e3, [128, 512], BF16, "kkb")
                            bA = C.sb(e3, [128, 512], F32, "bA")
                            bB = C.sb(e3, [128, 512], F32, "bB")
                            eL = C.sb(e3, [128, 512], F32, "eL")
                            eLm = C.sb(e3, [128, 512], F32, "eLm")
                            asig = C.sb(e3, [128, 512], F32, "asig")
                            kk = C.sb(e3, [128, 512], F32, "kk")
                            t2 = C.sb(e3, [128, 512], F32, "t2")

                            def proj_mix(c, k_):
                                pp, m_ = pps[k_], mx[k_]
                                wt = wch[nw[0] % 2]
                                nw[0] += 1
                                S.dma("pool", wt[:, :, :], w_v[:, :, c * 128:(c + 1) * 128], writes=[wt])
                                for kc in range(8):
                                    S.pe(lambda e: e.matmul(out=pp[:, :], lhsT=wt[:, kc, :], rhs=xnT[:, kc, :],
                                                            start=(kc == 0), stop=(kc == 7)), reads=[wt, xnT], writes=[pp])
                                S.act(lambda e: e.copy(out=pT[:, 1:513], in_=pp[:, :]), reads=[pp], writes=[pT])
                                S.pool(lambda e: e.tensor_copy(out=pT[:, 0:1], in_=pcar[:, c:c + 1]), reads=[pcar.part(c)], writes=[pT])
                                S.pool(lambda e: e.tensor_copy(out=pcar[:, c:c + 1], in_=pT[:, 512:513]), reads=[pT], writes=[pcar.part(c)])
                                S.dve(lambda e: e.tensor_tensor(out=dtmp[:, :], in0=pT[:, 0:512], in1=pT[:, 1:513], op=ALU.subtract),
                                      reads=[pT], writes=[dtmp])
                                S.dve(lambda e: e.scalar_tensor_tensor(out=m_[:, :], in0=dtmp[:, :], scalar=mu_col(c), in1=pT[:, 1:513],
                                                                       op0=ALU.mult, op1=ALU.add), reads=[dtmp, pT, P.t], writes=[m_])
                                return m_

                            m_ = proj_mix(12, 0)
                            S.act(lambda e: e.activation(out=xwa[0:64, :], in_=m_[0:64, :], func=AF.Tanh), reads=[m_], writes=[xwa])
                            S.act(lambda e: e.copy(out=xwa[64:128, :], in_=m_[64:128, :]), reads=[m_], writes=[xwa])
                            m_ = proj_mix(13, 1)
                            S.act(lambda e: e.activation(out=sxg[:, :], in_=m_[:, :], func=AF.Sigmoid), reads=[m_], writes=[sxg])
                            for hp in range(4):
                                rr = proj_mix(hp, 0)
                                kx = proj_mix(4 + hp, 1)
                                vv = proj_mix(8 + hp, 2)
                                S.pe(lambda e: e.matmul(out=pa[:, :], lhsT=wa2[0:64, hp * 128:(hp + 1) * 128], rhs=xwa[0:64, :], start=True, stop=True),
                                     reads=[wa2, xwa], writes=[pa])
                                S.act(lambda e: e.activation(out=bA[:, :], in_=pa[:, :], func=AF.Sigmoid, bias=P.col(("rw_w0", i), hp)),
                                      reads=[pa, P.t], writes=[bA])
                                S.dve(lambda e: e.tensor_scalar(out=bA[:, :], in0=bA[:, :], scalar1=-float(np.exp(-0.5)), scalar2=None, op0=ALU.mult),
                                      reads=[bA], writes=[bA])
                                S.dve(lambda e: e.tensor_tensor_scan(out=bB[:, :], data0=E["seg"][:, :, :].rearrange("p a b -> p (a b)"),
                                                                     data1=bA[:, :], initial=0.0, op0=ALU.mult, op1=ALU.add),
                                      reads=[bA, E["seg"]], writes=[bB])
                                S.act(lambda e: e.activation(out=eL[:, :], in_=bB[:, :], func=AF.Exp), reads=[bB], writes=[eL])
                                S.act(lambda e: e.activation(out=eLm[:, :], in_=bB[:, :], func=AF.Exp, scale=-1.0), reads=[bB], writes=[eLm])
                                S.dve(lambda e: e.tensor_tensor(out=bA[:, :], in0=bB[:, :], in1=bA[:, :], op=ALU.subtract), reads=[bB, bA], writes=[bA])
                                S.act(lambda e: e.activation(out=bA[:, :], in_=bA[:, :], func=AF.Exp), reads=[bA], writes=[bA])
                                S.pool(lambda e: e.tensor_copy(out=gC[:, :, hp], in_=eL[:, :].rearrange("p (a b) -> p a b", a=4)[:, :, 127]),
                                       reads=[eL], writes=[gC])
                                S.pe(lambda e: e.matmul(out=pb[:, :], lhsT=wa2[64:128, hp * 128:(hp + 1) * 128], rhs=xwa[64:128, :], start=True, stop=True),
                                     reads=[wa2, xwa], writes=[pb])
                                S.act(lambda e: e.activation(out=asig[:, :], in_=pb[:, :], func=AF.Sigmoid, bias=P.col(("rw_a0", i), hp)),
                                      reads=[pb, P.t], writes=[asig])
                                S.pool(lambda e: e.tensor_scalar(out=kk[:, :], in0=kx[:, :], scalar1=P.col(("rw_k_k", i), hp), scalar2=None, op0=ALU.mult),
                                       reads=[kx, P.t], writes=[kk])
                                S.act(lambda e: e.activation(out=kkb[:, :], in_=kk[:, :], func=AF.Square), reads=[kk], writes=[kkb])
                                S.pe(lambda e: e.matmul(out=pa[:, :], lhsT=E["bo"][:, :], rhs=kkb[:, :], start=True, stop=True),
                                     reads=[E["bo"], kkb], writes=[pa])
                                S.dve(lambda e: e.tensor_scalar(out=bB[:, :], in0=pa[:, :], scalar1=1e-24, scalar2=None, op0=ALU.max), reads=[pa], writes=[bB])
                                S.act(lambda e: e.activation(out=bB[:, :], in_=bB[:, :], func=AF.Sqrt), reads=[bB], writes=[bB])
                                S.dve(lambda e: e.reciprocal(out=bB[:, :], in_=bB[:, :]), reads=[bB], writes=[bB])
                                S.dve(lambda e: e.tensor_tensor(out=kk[:, :], in0=kk[:, :], in1=bB[:, :], op=ALU.mult), reads=[kk, bB], writes=[kk])
                                S.dve(lambda e: e.scalar_tensor_tensor(out=At[:, hp, :], in0=kk[:, :], scalar=-1.0, in1=bA[:, :], op0=ALU.mult, op1=ALU.mult),
                                      reads=[kk, bA], writes=[At.part(hp)])
                                S.pool(lambda e: e.tensor_tensor(out=bB[:, :], in0=kk[:, :], in1=asig[:, :], op=ALU.mult), reads=[kk, asig], writes=[bB])
                                S.pool(lambda e: e.tensor_tensor(out=Bt[:, hp, :], in0=bB[:, :], in1=eLm[:, :], op=ALU.mult), reads=[bB, eLm], writes=[Bt.part(hp)])
                                S.dve(lambda e: e.tensor_scalar(out=t2[:, :], in0=asig[:, :], scalar1=P.col(("rw_k_a", i), hp), scalar2=omk[:, hp:hp + 1],
                                                                op0=ALU.mult, op1=ALU.add), reads=[asig, P.t, omk], writes=[t2])
                                S.dve(lambda e: e.tensor_tensor(out=t2[:, :], in0=t2[:, :], in1=kx[:, :], op=ALU.mult), reads=[t2, kx], writes=[t2])
                                S.pool(lambda e: e.tensor_tensor(out=Kt[:, hp, :], in0=t2[:, :], in1=eLm[:, :], op=ALU.mult), reads=[t2, eLm], writes=[Kt.part(hp)])
                                S.dve(lambda e: e.tensor_tensor(out=Rq[:, hp, :], in0=rr[:, :], in1=eL[:, :], op=ALU.mult), reads=[rr, eL], writes=[Rq.part(hp)])
                                S.act(lambda e: e.copy(out=vT[:, hp, :], in_=vv[:, :]), reads=[vv], writes=[vT.part(hp)])
                                S.dve(lambda e: e.scalar_tensor_tensor(out=kkb[:, :], in0=rr[:, :], scalar=P.col(("rw_r_k", i), hp), in1=t2[:, :],
                                                                       op0=ALU.mult, op1=ALU.mult), reads=[rr, t2, P.t], writes=[kkb])
                                S.pe(lambda e: e.matmul(out=pb[:, :], lhsT=E["bo"][:, :], rhs=kkb[:, :], start=True, stop=True),
                                     reads=[E["bo"], kkb], writes=[pb])
                                S.dve(lambda e: e.tensor_tensor(out=bon[:, hp, :], in0=pb[:, :], in1=vv[:, :], op=ALU.mult), reads=[pb, vv], writes=[bon.part(hp)])
                                S.pe(lambda e: e.matmul(out=pa[:, :], lhsT=g2[:, hp * 128:(hp + 1) * 128], rhs=sxg[:, :], start=True, stop=True),
                                     reads=[g2, sxg], writes=[pa])
                                S.act(lambda e: e.copy(out=gT[:, hp, :], in_=pa[:, :]), reads=[pa], writes=[gT.part(hp)])
                            S.barrier()
                        if DBG.get("even_stop") == 1:
                            continue
                        with ExitStack() as e3:
                            def bank(nm, dt=F32):
                                return C.ps(e3, [128, 512] if dt == F32 else [128, 8, 128], dt, nm)
                            pA = [bank(f"pA{k}") for k in range(3)]
                            pX = [bank(f"pX{k}") for k in range(2)]
                            ptr = bank("ptr", BF16)
                            pS = bank("pS")
                            BtT = C.sb(e3, [128, 512], BF16, "BtT")
                            KtT = C.sb(e3, [128, 512], BF16, "KtT")
                            Vtk = C.sb(e3, [128, 512], BF16, "Vtk")
                            Mak = C.sb(e3, [128, 8, 128], BF16, "Mak")
                            Nbr = C.sb(e3, [128, 8, 128], BF16, "Nbr")
                            Nkr = C.sb(e3, [128, 8, 128], BF16, "Nkr")
                            Pm = [C.sb(e3, [128, 8, 128], BF16, "Mm")]
                            PTm = [C.sb(e3, [128, 8, 128], BF16, "MTm")]
                            Xm = [C.sb(e3, [128, 8, 128], BF16, f"Xm{k}") for k in range(2)]
                            XTm = [C.sb(e3, [128, 8, 128], BF16, f"XTm{k}") for k in range(2)]
                            Ts = C.sb(e3, [128, 8, 128], BF16, "Ts")
                            TsT = C.sb(e3, [128, 8, 128], BF16, "TsT")
                            Y1s = C.sb(e3, [128, 8, 128], BF16, "Y1s")
                            Z1s = C.sb(e3, [128, 8, 128], BF16, "Z1s")
                            Gt = C.sb(e3, [128, 512], BF16, "Gt")
                            Ut = C.sb(e3, [128, 512], BF16, "Ut")
                            stmp = C.sb(e3, [128, 4, 64], F32, "stmp")

                            def hv(t_, h, csl):
                                hp_, e_ = h // 2, h % 2
                                return t_[e_ * 64:(e_ + 1) * 64, hp_, csl]

                            for c in range(4):
                                csl = slice(c * 128, (c + 1) * 128)
                                for (src, dst) in ((Bt, BtT), (Kt, KtT), (vT, Vtk)):
                                    for hp in range(4):
                                        S.pe(lambda e: e.transpose(out=ptr[:, hp, :], in_=src[:, hp, csl], identity=ident[:, :]),
                                             reads=[src, ident], writes=[ptr])
                                    S.act(lambda e: e.copy(out=dst[:, :], in_=ptr[:, 0:4, :].rearrange("p a b -> p (a b)")), reads=[ptr], writes=[dst])
                                if DBG.get('even_stop') == 1.2:
                                    continue
                                prods = ((Bt, At, Pm[0], "m_su"), (At, Bt, PTm[0], "m_sl"), (Kt, At, Mak, "m_su"),
                                         (Bt, Rq, Nbr, "m_ui"), (Kt, Rq, Nkr, "m_ui"))
                                nb = 0
                                for (lt, rt_, dst, mk) in prods:
                                    for e_ in range(2):
                                        pbk = pA[nb % 3]
                                        nb += 1
                                        for hh in range(4):
                                            h = 2 * hh + e_
                                            S.pe(lambda e: e.matmul(out=pbk[:, hh * 128:(hh + 1) * 128], lhsT=hv(lt, h, csl), rhs=hv(rt_, h, csl),
                                                                    start=True, stop=True), reads=[lt, rt_], writes=[pbk])
                                        S.dve(lambda e: e.tensor_tensor(out=dst[:, :, :].rearrange("p (a two) b -> p a two b", two=2)[:, :, e_, :],
                                                                        in0=pbk[:, :].rearrange("p (a b) -> p a b", a=4), in1=E[mk][:, :, :], op=ALU.mult),
                                              reads=[pbk, E[mk]], writes=[dst.part(("e", e_))])
                                if DBG.get('even_stop') == 1.4:
                                    continue
                                Mm, MTm = Pm[0], PTm[0]

                                def lvl(src, msk, s, dst, eng):
                                    S.op(eng, lambda e: e.tensor_tensor(out=dst[:, :, :], in0=src[:, :, :],
                                                                        in1=E[msk][:, s, :].unsqueeze(1).to_broadcast([128, 8, 128]), op=ALU.mult),
                                         reads=[src, E[msk]], writes=[dst])

                                lvl(Mm, "lvm", 0, Ts, "pool")
                                lvl(MTm, "lvmT", 0, TsT, "pool")
                                S.pool(lambda e: e.tensor_tensor(out=Xm[0][:, :, :], in0=Ts[:, :, :], in1=E["id8"][:, :, :], op=ALU.add),
                                       reads=[Ts, E["id8"]], writes=[Xm[0]])
                                S.pool(lambda e: e.tensor_tensor(out=XTm[0][:, :, :], in0=TsT[:, :, :], in1=E["id8"][:, :, :], op=ALU.add),
                                       reads=[TsT, E["id8"]], writes=[XTm[0]])
                                cur = 0
                                for s in range(1, 7):
                                    nxt = 1 - cur
                                    last = (s == 6)
                                    lvl(Mm, "lvm", s, Ts, "pool")
                                    lvl(MTm, "lvmT", s, TsT, "dve")
                                    for half in range(2):
                                        hs_ = range(half * 4, half * 4 + 4)
                                        pbk = pA[nb % 3]
                                        nb += 1
                                        for hh, h in enumerate(hs_):
                                            S.pe(lambda e: e.matmul(out=pbk[:, hh * 128:(hh + 1) * 128], lhsT=TsT[:, h, :], rhs=Xm[cur][:, h, :],
                                                                    start=True, stop=True), reads=[TsT, Xm[cur]], writes=[pbk])
                                        S.act(lambda e: e.copy(out=Y1s[:, half * 4:half * 4 + 4, :], in_=pbk[:, :].rearrange("p (a b) -> p a b", a=4)),
                                              reads=[pbk], writes=[Y1s.part(half)])
                                        if not last:
                                            pbk = pA[nb % 3]
                                            nb += 1
                                            for hh, h in enumerate(hs_):
                                                S.pe(lambda e: e.matmul(out=pbk[:, hh * 128:(hh + 1) * 128], lhsT=Ts[:, h, :], rhs=XTm[cur][:, h, :],
                                                                        start=True, stop=True), reads=[Ts, XTm[cur]], writes=[pbk])
                                            S.dve(lambda e: e.tensor_copy(out=Z1s[:, half * 4:half * 4 + 4, :], in_=pbk[:, :].rearrange("p (a b) -> p a b", a=4)),
                                                  reads=[pbk], writes=[Z1s.part(half)])
                                    for half in range(2):
                                        hs_ = range(half * 4, half * 4 + 4)
                                        px = pX[half]
                                        for hh, h in enumerate(hs_):
                                            S.pe(lambda e: e.matmul(out=px[:, hh * 128:(hh + 1) * 128], lhsT=ident[:, :], rhs=Xm[cur][:, h, :],
                                                                    start=True, stop=False), reads=[ident, Xm[cur]], writes=[px])
                                            S.pe(lambda e: e.matmul(out=px[:, hh * 128:(hh + 1) * 128], lhsT=XTm[cur][:, h, :], rhs=Y1s[:, h, :],
                                                                    start=False, stop=True), reads=[XTm[cur], Y1s], writes=[px])
                                        S.act(lambda e: e.copy(out=Xm[nxt][:, half * 4:half * 4 + 4, :], in_=px[:, :].rearrange("p (a b) -> p a b", a=4)),
                                              reads=[px], writes=[Xm[nxt].part(half)])
                                        if not last:
                                            pbk = pA[nb % 3]
                                            nb += 1
                                            for hh, h in enumerate(hs_):
                                                S.pe(lambda e: e.matmul(out=pbk[:, hh * 128:(hh + 1) * 128], lhsT=ident[:, :], rhs=XTm[cur][:, h, :],
                                                                        start=True, stop=False), reads=[ident, XTm[cur]], writes=[pbk])
                                                S.pe(lambda e: e.matmul(out=pbk[:, hh * 128:(hh + 1) * 128], lhsT=Xm[cur][:, h, :], rhs=Z1s[:, h, :],
                                                                        start=False, stop=True), reads=[Xm[cur], Z1s], writes=[pbk])
                                            S.dve(lambda e: e.tensor_copy(out=XTm[nxt][:, half * 4:half * 4 + 4, :], in_=pbk[:, :].rearrange("p (a b) -> p a b", a=4)),
                                                  reads=[pbk], writes=[XTm[nxt].part(half)])
                                    cur = nxt
                                if DBG.get('even_stop') == 1.6:
                                    continue
                                Xf = Xm[cur]
                                pg = pA[nb % 3]
                                nb += 1
                                for h in range(8):
                                    hp, e_ = h // 2, h % 2
                                    S.pe(lambda e: e.matmul(out=pg[:, h * 64:(h + 1) * 64], lhsT=At[:, hp, csl], rhs=Sb[:, hp, e_, :],
                                                            start=True, stop=False), reads=[At, Sb], writes=[pg])
                                    S.pe(lambda e: e.matmul(out=pg[:, h * 64:(h + 1) * 64], lhsT=Mak[:, h, :], rhs=Vtk[:, h * 64:(h + 1) * 64],
                                                            start=False, stop=True), reads=[Mak, Vtk], writes=[pg])
                                S.act(lambda e: e.copy(out=Gt[:, :], in_=pg[:, :]), reads=[pg], writes=[Gt])
                                if DBG.get('even_stop') == 1.65:
                                    continue
                                pu = pA[nb % 3]
                                nb += 1
                                for h in range(8):
                                    S.pe(lambda e: e.matmul(out=pu[:, h * 64:(h + 1) * 64], lhsT=Xf[:, h, :], rhs=Gt[:, h * 64:(h + 1) * 64],
                                                            start=True, stop=True), reads=[Xf, Gt], writes=[pu])
                                S.dve(lambda e: e.tensor_copy(out=Ut[:, :], in_=pu[:, :]), reads=[pu], writes=[Ut])
                                if DBG.get('even_stop') == 1.7:
                                    continue
                                py = pA[nb % 3]
                                nb += 1
                                for h in range(8):
                                    hp, e_ = h // 2, h % 2
                                    o_ = py[e_ * 64:(e_ + 1) * 64, hp * 128:(hp + 1) * 128]
                                    S.pe(lambda e: e.matmul(out=o_, lhsT=Sb[:, hp, e_, :], rhs=Rq[:, hp, csl], start=True, stop=False),
                                         reads=[Sb, Rq], writes=[py])
                                    S.pe(lambda e: e.matmul(out=o_, lhsT=Ut[:, h * 64:(h + 1) * 64], rhs=Nbr[:, h, :], start=False, stop=False),
                                         reads=[Ut, Nbr], writes=[py])
                                    S.pe(lambda e: e.matmul(out=o_, lhsT=Vtk[:, h * 64:(h + 1) * 64], rhs=Nkr[:, h, :], start=False, stop=True),
                                         reads=[Vtk, Nkr], writes=[py])
                                S.act(lambda e: e.copy(out=yraw[:, :, csl], in_=py[:, :].rearrange("p (a b) -> p a b", a=4)), reads=[py], writes=[yraw.part(c)])
                                if DBG.get('even_stop') == 1.75:
                                    continue
                                for hp in range(4):
                                    o_ = pS[:, hp * 128:(hp + 1) * 128]
                                    S.pe(lambda e: e.matmul(out=o_, lhsT=BtT[:, hp * 128:(hp + 1) * 128], rhs=Ut[:, hp * 128:(hp + 1) * 128], start=True, stop=False),
                                         reads=[BtT, Ut], writes=[pS])
                                    S.pe(lambda e: e.matmul(out=o_, lhsT=KtT[:, hp * 128:(hp + 1) * 128], rhs=Vtk[:, hp * 128:(hp + 1) * 128], start=False, stop=True),
                                         reads=[KtT, Vtk], writes=[pS])
                                for e_ in range(2):
                                    S.dve(lambda e: e.tensor_tensor(out=stmp[e_ * 64:(e_ + 1) * 64, :, :],
                                                                    in0=pS[e_ * 64:(e_ + 1) * 64, :].rearrange("p (a b c) -> p a b c", a=4, b=2)[:, :, e_, :],
                                                                    in1=St[e_ * 64:(e_ + 1) * 64, :, :], op=ALU.add),
                                          reads=[pS, St], writes=[stmp])
                                S.dve(lambda e: e.tensor_tensor(out=St[:, :, :], in0=stmp[:, :, :], in1=gC[:, c, :].unsqueeze(2).to_broadcast([128, 4, 64]),
                                                                op=ALU.mult), reads=[stmp, gC], writes=[St])
                                for e_ in range(2):
                                    S.act(lambda e: e.copy(out=Sb[e_ * 64:(e_ + 1) * 64, :, e_, :], in_=St[e_ * 64:(e_ + 1) * 64, :, :]), reads=[St], writes=[Sb])
                            S.barrier()
                        if DBG.get('even_stop') == 1.8:
                            continue
                        with ExitStack() as e3:
                            pm = C.ps(e3, [128, 512], F32, "gn_pm")
                            pq = C.ps(e3, [128, 512], F32, "gn_pq")

                            def post_a(c, dd):
                                S.pool(lambda e: e.tensor_tensor(out=dd[:, :], in0=dd[:, :], in1=bon[:, c, :], op=ALU.add), reads=[dd, bon], writes=[dd])
                                S.dve(lambda e: e.tensor_tensor(out=yaT[:, c, t0:t0 + 512], in0=dd[:, :], in1=gT[:, c, :], op=ALU.mult), reads=[dd, gT], writes=[yaT.part((c, blk))])

                            group_norm_T(C, e3, yraw, 4, E["bof"], RW_LN_EPS, lambda c: P.col(("rw_ln_g", i), c), lambda c: P.col(("rw_ln_b", i), c),
                                         post_a, pm, pq)
                            S.barrier()
        if DBG.get("even_stop") in (1, 1.2, 1.4, 1.6, 1.65, 1.7, 1.75, 1.8, 2):
            return
        ybT = C.sb(es, [128, 4, SEQ], BF16, "ybT")
        with ExitStack() as er:
            Rt = C.sb(er, [128, 4, 128], F32, "Rt")
            Rb = C.sb(er, [128, 4, 128], BF16, "Rb")
            for t_ in (Rt, Rb):
                S.pool(lambda e: e.memset(t_[:, :, :], 0.0), writes=[t_])
            wvr = C.sb(er, [128, 8, 512], BF16, "wvr")
            load_w(C, wvr, w_in, 1792 + 1024, 1792 + 1536)
            wch = [C.sb(er, [128, 8, 128], BF16, f"wchr{k}") for k in range(2)]
            nw = [0]

            def wchunk(c0):
                wt = wch[nw[0] % 2]
                nw[0] += 1
                S.dma("pool", wt[:, :, :], w_v[:, :, 1792 + c0:1792 + c0 + 128], writes=[wt])
                return wt

            for blk in range(4):
                t0 = blk * 512
                with ExitStack() as e1:
                    xnT = C.sb(e1, [128, 8, 512], BF16, "xnT")
                    rms_to_T(C, e1, x, P.cols(("norm_g", l, 2)), xnT, 4, ident, tok0=blk * 4)
                    with ExitStack() as e2:
                        qr = C.sb(e2, [128, 4, 512], BF16, "qr")
                        qx = C.sb(e2, [128, 4, 512], BF16, "qx")
                        kr = C.sb(e2, [128, 4, 512], BF16, "kr")
                        sg = C.sb(e2, [128, 4, 512], BF16, "sg")
                        vtk = C.sb(e2, [128, 4, 512], BF16, "vtk")
                        oraw = C.sb(e2, [128, 4, 512], F32, "oraw")
                        cs = E["cosT"][:, t0:t0 + 512]
                        sn = E["sinT"][:, t0:t0 + 512]
                        with ExitStack() as e3:
                            pq_ = [C.ps(e3, [128, 512], F32, f"rq{k}") for k in range(2)]
                            ps_ = [C.ps(e3, [128, 512], F32, f"rs{k}") for k in range(2)]
                            t1 = C.sb(e3, [128, 512], F32, "rt1")
                            t2 = C.sb(e3, [128, 512], F32, "rt2")
                            n_ = 0
                            for (c0, dst) in ((0, qr), (512, kr)):
                                for h in range(4):
                                    pa_, pb_ = pq_[n_ % 2], ps_[n_ % 2]
                                    n_ += 1
                                    cb = c0 + h * 128
                                    wrt = wchunk(cb)
                                    for kc in range(8):
                                        S.pe(lambda e: e.matmul(out=pa_[:, :], lhsT=wrt[:, kc, :], rhs=xnT[:, kc, :], start=(kc == 0), stop=(kc == 7)),
                                             reads=[wrt, xnT], writes=[pa_])
                                    for half in range(2):
                                        for kc in range(8):
                                            S.pe(lambda e: e.matmul(out=pb_[half * 64:(half + 1) * 64, :], lhsT=wrt[:, kc, (1 - half) * 64:(1 - half) * 64 + 64],
                                                                    rhs=xnT[:, kc, :], start=(kc == 0), stop=(kc == 7)), reads=[wrt, xnT], writes=[pb_])
                                    S.dve(lambda e: e.tensor_tensor(out=t1[:, :], in0=pa_[:, :], in1=cs, op=ALU.mult), reads=[pa_, E["cosT"]], writes=[t1])
                                    S.dve(lambda e: e.tensor_tensor(out=t2[:, :], in0=pb_[:, :], in1=sn, op=ALU.mult), reads=[pb_, E["sinT"]], writes=[t2])
                                    S.pool(lambda e: e.tensor_tensor(out=dst[:, h, :], in0=t1[:, :], in1=t2[:, :], op=ALU.add), reads=[t1, t2], writes=[dst.part(h)])
                                    if c0 == 0:
                                        S.pool(lambda e: e.tensor_tensor(out=qx[:, h, :].rearrange("p (a b) -> p a b", a=4),
                                                                         in0=qr[:, h, :].rearrange("p (a b) -> p a b", a=4),
                                                                         in1=E["xiT"][:, h, :].unsqueeze(1).to_broadcast([128, 4, 128]), op=ALU.mult),
                                               reads=[qr.part(h), E["xiT"]], writes=[qx.part(h)])
                            for h in range(4):
                                pa_ = pq_[h % 2]
                                wrt = wchunk(1536 + h * 128)
                                for kc in range(8):
                                    S.pe(lambda e: e.matmul(out=pa_[:, :], lhsT=wrt[:, kc, :], rhs=xnT[:, kc, :],
                                                            start=(kc == 0), stop=(kc == 7)), reads=[wrt, xnT], writes=[pa_])
                                S.act(lambda e: e.activation(out=sg[:, h, :], in_=pa_[:, :], func=AF.Silu), reads=[pa_], writes=[sg.part(h)])
                            for c in range(4):
                                pa_ = ps_[c % 2]
                                for kc in range(8):
                                    S.pe(lambda e: e.matmul(out=pa_[:, :], lhsT=xnT[:, kc, c * 128:(c + 1) * 128], rhs=wvr[:, kc, :],
                                                            start=(kc == 0), stop=(kc == 7)), reads=[wvr, xnT], writes=[pa_])
                                S.act(lambda e: e.copy(out=vtk[:, c, :], in_=pa_[:, :]), reads=[pa_], writes=[vtk.part(c)])
                            S.barrier()
                        with ExitStack() as e3:
                            psc = C.ps(e3, [128, 512], F32, "psc")
                            po_ = C.ps(e3, [128, 512], F32, "po_r")
                            pkv = C.ps(e3, [128, 512], F32, "pkv")
                            ptr = C.ps(e3, [128, 8, 128], BF16, "ptr_r")
                            scT = C.sb(e3, [128, 4, 128], BF16, "scT")
                            ktk = C.sb(e3, [128, 4, 128], BF16, "ktk")
                            for c in range(4):
                                csl = slice(c * 128, (c + 1) * 128)
                                for h in range(4):
                                    S.pe(lambda e: e.matmul(out=psc[:, h * 128:(h + 1) * 128], lhsT=kr[:, h, csl], rhs=qr[:, h, csl], start=True, stop=True),
                                         reads=[kr, qr], writes=[psc])
                                    S.pe(lambda e: e.transpose(out=ptr[:, h, :], in_=kr[:, h, csl], identity=ident[:, :]), reads=[kr, ident], writes=[ptr])
                                S.dve(lambda e: e.tensor_tensor(out=scT[:, :, :], in0=psc[:, :].rearrange("p (a b) -> p a b", a=4), in1=E["dmT"][:, :, :], op=ALU.mult),
                                      reads=[psc, E["dmT"]], writes=[scT])
                                S.dve(lambda e: e.tensor_tensor(out=ktk[:, :, :], in0=ptr[:, 0:4, :], in1=E["zcol"][:, :].unsqueeze(2).to_broadcast([128, 4, 128]),
                                                                op=ALU.mult), reads=[ptr, E["zcol"]], writes=[ktk])
                                for h in range(4):
                                    o_ = po_[:, h * 128:(h + 1) * 128]
                                    S.pe(lambda e: e.matmul(out=o_, lhsT=vtk[:, c, h * 128:(h + 1) * 128], rhs=scT[:, h, :], start=True, stop=False),
                                         reads=[vtk, scT], writes=[po_])
                                    S.pe(lambda e: e.matmul(out=o_, lhsT=Rb[:, h, :], rhs=qx[:, h, csl], start=False, stop=True), reads=[Rb, qx], writes=[po_])
                                S.act(lambda e: e.copy(out=oraw[:, :, csl], in_=po_[:, :].rearrange("p (a b) -> p a b", a=4)), reads=[po_], writes=[oraw.part(c)])
                                for h in range(4):
                                    S.pe(lambda e: e.matmul(out=pkv[:, h * 128:(h + 1) * 128], lhsT=ktk[:, h, :], rhs=vtk[:, c, h * 128:(h + 1) * 128],
                                                            start=True, stop=True), reads=[ktk, vtk], writes=[pkv])
                                for h in range(4):
                                    S.dve(lambda e: e.scalar_tensor_tensor(out=Rt[:, h, :], in0=Rt[:, h, :], scalar=gamC[h], in1=pkv[:, h * 128:(h + 1) * 128],
                                                                           op0=ALU.mult, op1=ALU.add), reads=[Rt, pkv], writes=[Rt])
                                S.act(lambda e: e.copy(out=Rb[:, :, :], in_=Rt[:, :, :]), reads=[Rt], writes=[Rb])
                            S.barrier()
                        with ExitStack() as e3:
                            pm = C.ps(e3, [128, 512], F32, "gn_pm")
                            pq = C.ps(e3, [128, 512], F32, "gn_pq")

                            def post_b(c, dd):
                                S.dve(lambda e: e.tensor_tensor(out=ybT[:, c, t0:t0 + 512], in0=dd[:, :], in1=sg[:, c, :], op=ALU.mult), reads=[dd, sg], writes=[ybT.part((c, blk))])

                            group_norm_T(C, e3, oraw, 4, E["on128"], EPS, lambda c: P.col(("rt_gn_g", i), c), lambda c: P.col(("rt_gn_b", i), c),
                                         post_b, pm, pq)
                            S.barrier()
        with ExitStack() as eo:
            g_bc = C.sb(eo, [128, D], F32, "g_bc")
            load_bcast_row(C, "sp", g_bc, W.L("norm_g", l)[3], D)
            wo = C.sb(eo, [128, 8, D], BF16, "wo_ev")
            load_w(C, wo, W.H("ev_w_out", i), 0, D)
            for g4 in range(4):
                chunks = [(yaT, (lambda ti, c=c, g4=g4: yaT[:, c, (g4 * 4 + ti) * 128:(g4 * 4 + ti + 1) * 128])) for c in range(4)]
                chunks += [(ybT, (lambda ti, c=c, g4=g4: ybT[:, c, (g4 * 4 + ti) * 128:(g4 * 4 + ti + 1) * 128])) for c in range(4)]
                out_proj_residual(C, x, chunks, wo, g_bc, 1.0, [g4 * 4 + t_ for t_ in range(4)])


def build_program(shapes, nseq=2, plan=None, loff=0, hoff=0):
    nc = bass.Bass("TRN2", target_bir_lowering=False)
    W = Wts(nc, shapes, loff, hoff)
    out = nc.dram_tensor("out", [nseq, SEQ, D], F32, kind="ExternalOutput").ap()
    C = Ctx(nc)
    S = C.S
    if plan is None:
        plan = [(l, ph) for l in range(DEPTH) for ph in ("ffn1", "mix", "xa", "ffn2")]
    need_mem = any(ph == "xa" for _, ph in plan)
    with ExitStack() as es:
        K = make_consts(C, es)
        P = build_params(C, es, W, K["identf"])
        x = C.sb(es, [128, NT, D], F32, "xres")
        memT = C.sb(es, [128, 8, MEM], BF16, "memT") if need_mem else None
        for s in range(nseq):
            for t4 in range(NT // 4):
                S.dma("sp", x[:, t4 * 4:(t4 + 1) * 4, :],
                      W["x"][s, t4 * 512:(t4 + 1) * 512, :].rearrange("(t p) d -> p t d", p=128),
                      writes=[x.part(t4 * 4 + i) for i in range(4)])
            if need_mem:
                prep_mem(C, es, W, P, K, s, memT)
            for (l, ph) in plan:
                if ph == "ffn1":
                    ffn_block(C, x, l, 0, W, P, K["ident"])
                elif ph == "ffn2":
                    ffn_block(C, x, l, 1, W, P, K["ident"])
                elif ph == "xa":
                    xattn_block(C, x, l, W, P, K, memT)
                elif ph == "mix":
                    if l % 2 == 0:
                        even_mixer(C, x, l, W, P, K)
                    else:
                        odd_mixer(C, x, l, W, P, K)
            for t4 in range(NT // 4):
                S.dma("sp", out[s, t4 * 512:(t4 + 1) * 512, :].rearrange("(t p) d -> p t d", p=128),
                      x[:, t4 * 4:(t4 + 1) * 4, :], reads=[x.part(t4 * 4 + i) for i in range(4)])
            S.barrier()
        S.finish()
    return nc, W


def kernel(**inputs):
    n = 8
    arrs = {k: np.ascontiguousarray(np.asarray(v), dtype=np.float32) for k, v in inputs.items()}
    per = arrs["x"].shape[0] // n
    shapes = {}
    for k, a in arrs.items():
        shapes[k] = ((per,) + a.shape[1:]) if k in ("x", "mem") else a.shape
    nc, W = build_program(shapes, nseq=per)
    in_maps = []
    for c in range(n):
        m = {}
        for k in W.aps:
            a = arrs[k]
            m[k] = a[c * per:(c + 1) * per] if k in ("x", "mem") else a
        in_maps.append(m)
    res = run_bass_kernel_spmd(nc, in_maps, core_ids=list(range(n)))
    return np.concatenate([r["out"] for r in res.results], axis=0).astype(np.float32)
```

```python
import numpy as np
import concourse.bass as bass
import concourse.mybir as mybir
from concourse.bass_utils import run_bass_kernel_spmd

F32 = mybir.dt.float32
BF16 = mybir.dt.bfloat16
ALU = mybir.AluOpType
AF = mybir.ActivationFunctionType
AX = mybir.AxisListType

D = 1024
SEQ = 2048
NT = SEQ // 128
DEPTH = 4
DFF = 2816
NFC = DFF // 128
MEM = 256
EPS = 1e-6


class Res:
    __slots__ = ("name", "writer", "readers", "parent", "parts", "psum")

    def __init__(self, name, parent=None):
        self.name = name
        self.writer = None
        self.readers = {}
        self.parent = parent
        self.parts = {}
        self.psum = parent.psum if parent is not None else False

    def part(self, key):
        r = self.parts.get(key)
        if r is None:
            r = Res(f"{self.name}/{key}", parent=self)
            self.parts[key] = r
        return r


class T:
    def __init__(self, h, name):
        self.h = h
        self.res = Res(name)

    def __getitem__(self, k):
        return self.h[k]

    def part(self, key):
        return self.res.part(key)


NDSEM = 16
COMPUTE = ("pe", "act", "dve", "pool")


class Sched:
    def __init__(self, nc):
        self.nc = nc
        self.eng = {"pe": nc.tensor, "act": nc.scalar, "dve": nc.vector, "pool": nc.gpsimd, "sp": nc.sync}
        self.sem = {}
        self.cnt = {}
        for e in self.eng:
            self.sem[e] = nc.alloc_semaphore("s_" + e)
            self.cnt[e] = 0
        self.dkeys = []
        for q in ("sp", "pool"):
            for i in range(NDSEM):
                k = ("d", q, i)
                self.sem[k] = nc.alloc_semaphore(f"s_d{q}{i}")
                self.cnt[k] = 0
                self.dkeys.append(k)
        self.known = {e: {} for e in self.eng}
        self.dnext = {"sp": 0, "pool": 0}
        self.pe_pending = None
        self.ninstr = 0

    @staticmethod
    def _rlist(r):
        if isinstance(r, T):
            return r.res
        return r

    def _deps(self, reads, writes, eng=None):
        ev = []
        for r in reads:
            r = self._rlist(r)
            if r.writer:
                ev.append(r.writer)
            if r.parent is not None and r.parent.writer:
                ev.append(r.parent.writer)
            for p in r.parts.values():
                if p.writer:
                    ev.append(p.writer)
            if r.psum:
                chain = [r] + list(r.parts.values()) + ([r.parent] if r.parent is not None else [])
                for c in chain:
                    ev.extend((k, v) for k, v in c.readers.items() if k != eng)
        for w in writes:
            w = self._rlist(w)
            chain = [w] + list(w.parts.values())
            if w.parent is not None:
                chain.append(w.parent)
            for c in chain:
                if c.writer:
                    ev.append(c.writer)
                ev.extend(c.readers.items())
        return ev

    def _wait(self, e, evs):
        kn = self.known[e]
        best = {}
        for k, v in evs:
            if k == e and e in ("pe", "sp"):
                continue
            if kn.get(k, 0) >= v:
                continue
            if best.get(k, 0) < v:
                best[k] = v
        for k, v in best.items():
            self.eng[e].wait_ge(self.sem[k], v)
            kn[k] = v

    def _record(self, ev, reads, writes):
        k, v = ev
        for r in reads:
            r = self._rlist(r)
            if r.readers.get(k, 0) < v:
                r.readers[k] = v
        for w in writes:
            w = self._rlist(w)
            w.writer = ev
            w.readers = {}

    def _flush_pe(self):
        if self.pe_pending is not None:
            self.cnt["pe"] += 1
            self.pe_pending.then_inc(self.sem["pe"], 1)
            self.pe_pending = None

    def op(self, e, fn, reads=(), writes=()):
        if e == "pe":
            self._wait(e, self._deps(reads, writes, e))
            ins = fn(self.eng[e])
            self.pe_pending = ins
            self._record((e, self.cnt[e] + 1), reads, writes)
            self.ninstr += 1
            return ins
        self._flush_pe()
        self._wait(e, self._deps(reads, writes, e))
        ins = fn(self.eng[e])
        self.cnt[e] += 1
        ins.then_inc(self.sem[e], 1)
        self._record((e, self.cnt[e]), reads, writes)
        self.ninstr += 1
        return ins

    def pe(self, fn, reads=(), writes=()):
        return self.op("pe", fn, reads, writes)

    def act(self, fn, reads=(), writes=()):
        return self.op("act", fn, reads, writes)

    def dve(self, fn, reads=(), writes=()):
        return self.op("dve", fn, reads, writes)

    def pool(self, fn, reads=(), writes=()):
        return self.op("pool", fn, reads, writes)

    def dma(self, q, out, in_, reads=(), writes=(), **kw):
        self._flush_pe()
        i = self.dnext[q]
        self.dnext[q] = (i + 1) % NDSEM
        k = ("d", q, i)
        evs = self._deps(reads, writes)
        if self.cnt[k]:
            evs.append((k, self.cnt[k]))
        self._wait(q, evs)
        ins = self.eng[q].dma_start(out=out, in_=in_, **kw)
        self.cnt[k] += 16
        ins.then_inc(self.sem[k], 16)
        self._record((k, self.cnt[k]), reads, writes)
        self.ninstr += 1
        return ins

    def barrier(self):
        self._flush_pe()
        evs = [(e, self.cnt[e]) for e in COMPUTE if self.cnt[e]]
        evs += [(k, self.cnt[k]) for k in self.dkeys if self.cnt[k]]
        for e in list(COMPUTE) + ["sp"]:
            self._wait(e, evs)

    def finish(self):
        self._flush_pe()
        evs = [(e, self.cnt[e]) for e in COMPUTE if self.cnt[e]]
        evs += [(k, self.cnt[k]) for k in self.dkeys if self.cnt[k]]
        self._wait("sp", evs)


class Ctx:
    def __init__(self, nc):
        self.nc = nc
        self.S = Sched(nc)
        self._n = 0

    def sb(self, stack, shape, dt, name=None):
        self._n += 1
        name = f"{name or 'sb'}_{self._n}"
        h = stack.enter_context(self.nc.sbuf_tensor(name, list(shape), dt))
        return T(h, name)

    def ps(self, stack, shape, dt=F32, name=None):
        self._n += 1
        name = f"{name or 'ps'}_{self._n}"
        h = stack.enter_context(self.nc.psum_tensor(name, list(shape), dt))
        t = T(h, name)
        t.res.psum = True
        return t


from contextlib import ExitStack


def load_bcast_row(C, q, dst, src_row, n):
    C.S.dma(q, dst[:, 0:n], src_row.partition_broadcast(128), writes=[dst])


def rms_to_T(C, st, x, g_col, xnT, ntiles, ident, tok0=0):
    S = C.S
    with ExitStack() as es:
        ss = C.sb(es, [128, ntiles], F32, "ss")
        rstd = C.sb(es, [128, ntiles], F32, "rstd")
        junk = C.sb(es, [128, D], BF16, "junk")
        xs = [C.sb(es, [128, D], BF16, f"xs{i}") for i in range(2)]
        tps = [C.ps(es, [128, 8, 128], BF16, f"tp{i}") for i in range(2)]
        S.dve(lambda e: e.memset(ss[:, :], 0.0), writes=[ss])
        for t in range(ntiles):
            S.act(lambda e: e.activation(out=junk[:, :], in_=x[:, tok0 + t, :], func=AF.Square,
                                         accum_out=ss[:, t:t + 1]),
                  reads=[x.part(tok0 + t)], writes=[junk, ss])
        rstd_from_ss(C, ss, rstd, ntiles, 1.0 / D, EPS)
        for t in range(ntiles):
            xb = xs[t % 2]
            tp = tps[t % 2]
            S.act(lambda e: e.activation(out=xb[:, :], in_=x[:, tok0 + t, :], func=AF.Copy,
                                         scale=rstd[:, t:t + 1]),
                  reads=[x.part(tok0 + t), rstd], writes=[xb])
            for kc in range(8):
                S.pe(lambda e: e.transpose(out=tp[:, kc, :], in_=xb[:, kc * 128:(kc + 1) * 128], identity=ident[:, :]),
                     reads=[xb, ident], writes=[tp])
            S.dve(lambda e: e.tensor_tensor(out=xnT[:, :, t * 128:(t + 1) * 128], in0=tp[:, :, :],
                                            in1=g_col.unsqueeze(2).to_broadcast([128, 8, 128]), op=ALU.mult),
                  reads=[tp], writes=[xnT.part(t)])
        S.barrier()


def rstd_from_ss(C, ss, rstd, n, scale, eps):
    S = C.S
    S.dve(lambda e: e.tensor_scalar(out=rstd[:, 0:n], in0=ss[:, 0:n], scalar1=scale, scalar2=eps,
                                    op0=ALU.mult, op1=ALU.add), reads=[ss], writes=[rstd])
    S.act(lambda e: e.activation(out=rstd[:, 0:n], in_=rstd[:, 0:n], func=AF.Sqrt), reads=[rstd], writes=[rstd])
    S.dve(lambda e: e.reciprocal(out=rstd[:, 0:n], in_=rstd[:, 0:n]), reads=[rstd], writes=[rstd])


def post_norm_residual(C, st, x, tile_idx, y_ps, g_bc, coef, scr):
    S = C.S
    ss, rstd, junk, tmp = scr
    S.dve(lambda e: e.memset(ss[:, 0:2], 0.0), writes=[ss])
    for h in range(2):
        S.act(lambda e: e.activation(out=junk[:, 0:512], in_=y_ps[h][:, :], func=AF.Square,
                                     accum_out=ss[:, h:h + 1]), reads=[y_ps[h]], writes=[junk, ss])
    S.dve(lambda e: e.tensor_tensor(out=ss[:, 2:3], in0=ss[:, 0:1], in1=ss[:, 1:2], op=ALU.add), reads=[ss], writes=[ss])
    S.dve(lambda e: e.tensor_scalar(out=rstd[:, 0:1], in0=ss[:, 2:3], scalar1=1.0 / D, scalar2=EPS,
                                    op0=ALU.mult, op1=ALU.add), reads=[ss], writes=[rstd])
    S.act(lambda e: e.activation(out=rstd[:, 0:1], in_=rstd[:, 0:1], func=AF.Sqrt), reads=[rstd], writes=[rstd])
    S.dve(lambda e: e.reciprocal(out=rstd[:, 0:1], in_=rstd[:, 0:1]), reads=[rstd], writes=[rstd])
    if coef != 1.0:
        S.dve(lambda e: e.tensor_scalar(out=rstd[:, 0:1], in0=rstd[:, 0:1], scalar1=float(coef), scalar2=None,
                                        op0=ALU.mult), reads=[rstd], writes=[rstd])
    for h in range(2):
        sl = slice(h * 512, (h + 1) * 512)
        S.dve(lambda e: e.tensor_tensor(out=tmp[:, sl], in0=y_ps[h][:, :], in1=g_bc[:, sl], op=ALU.mult),
              reads=[y_ps[h], g_bc], writes=[tmp])
        S.dve(lambda e: e.scalar_tensor_tensor(out=x[:, tile_idx, sl], in0=tmp[:, sl], scalar=rstd[:, 0:1],
                                               in1=x[:, tile_idx, sl], op0=ALU.mult, op1=ALU.add),
              reads=[tmp, rstd, x.part(tile_idx)], writes=[x.part(tile_idx)])


def ffn_block(C, x, l, j, W, P, ident):
    S = C.S
    nc = C.nc
    n_in = 0 if j == 0 else 6
    n_out = 1 if j == 0 else 7
    wg = W.L("ffn_w_gate", l)[j].rearrange("(kc p) f -> p kc f", p=128)
    wu = W.L("ffn_w_up", l)[j].rearrange("(kc p) f -> p kc f", p=128)
    wd = W.L("ffn_w_down", l)[j].rearrange("(fc p) d -> p fc d", p=128)
    TG = 1024
    with ExitStack() as es:
        g_bc = C.sb(es, [128, D], F32, "g_bc")
        load_bcast_row(C, "sp", g_bc, W.L("norm_g", l)[n_out], D)
        hT = C.sb(es, [128, NFC, TG], BF16, "hT")
        for tg in range(SEQ // TG):
            with ExitStack() as es2:
                xnT = C.sb(es2, [128, 8, TG], BF16, "xnT")
                rms_to_T(C, es2, x, P.cols(("norm_g", l, n_in)), xnT, TG // 128, ident,
                         tok0=tg * (TG // 128))
                wbuf = [(C.sb(es2, [128, 8, 256], BF16, f"wg{i}"), C.sb(es2, [128, 8, 256], BF16, f"wu{i}")) for i in range(2)]
                sg = [C.sb(es2, [128, 512], BF16, f"sg{i}") for i in range(2)]
                gps = [C.ps(es2, [128, 512], F32, f"gps{i}") for i in range(2)]
                ups = [C.ps(es2, [128, 512], F32, f"ups{i}") for i in range(2)]
                it = 0
                for f2 in range(NFC // 2):
                    wgt, wut = wbuf[f2 % 2]
                    S.dma("pool", wgt[:, :, :], wg[:, :, f2 * 256:(f2 + 1) * 256], writes=[wgt])
                    S.dma("pool", wut[:, :, :], wu[:, :, f2 * 256:(f2 + 1) * 256], writes=[wut])
                    for fi in range(2):
                        fc = f2 * 2 + fi
                        for th in range(TG // 512):
                            gp, up, sgt = gps[it % 2], ups[it % 2], sg[it % 2]
                            it += 1
                            for kc in range(8):
                                S.pe(lambda e: e.matmul(out=gp[:, :], lhsT=wgt[:, kc, fi * 128:(fi + 1) * 128],
                                                        rhs=xnT[:, kc, th * 512:(th + 1) * 512],
                                                        start=(kc == 0), stop=(kc == 7)),
                                     reads=[wgt, xnT], writes=[gp])
                            for kc in range(8):
                                S.pe(lambda e: e.matmul(out=up[:, :], lhsT=wut[:, kc, fi * 128:(fi + 1) * 128],
                                                        rhs=xnT[:, kc, th * 512:(th + 1) * 512],
                                                        start=(kc == 0), stop=(kc == 7)),
                                     reads=[wut, xnT], writes=[up])
                            S.act(lambda e: e.activation(out=sgt[:, :], in_=gp[:, :], func=AF.Silu),
                                  reads=[gp], writes=[sgt])
                            S.dve(lambda e: e.tensor_tensor(out=hT[:, fc, th * 512:(th + 1) * 512], in0=up[:, :],
                                                            in1=sgt[:, :], op=ALU.mult),
                                  reads=[up, sgt], writes=[hT.part((fc, th))])
                S.barrier()
            with ExitStack() as es3:
                wdt = C.sb(es3, [128, NFC, D], BF16, "wd")
                for f2 in range(NFC // 2):
                    S.dma("pool", wdt[:, f2 * 2:f2 * 2 + 2, :], wd[:, f2 * 2:f2 * 2 + 2, :], writes=[wdt.part(f2)])
                yps = [[C.ps(es3, [128, 512], F32, f"y{i}{h}") for h in range(2)] for i in range(2)]
                scr = (C.sb(es3, [128, 4], F32, "pss"), C.sb(es3, [128, 2], F32, "prs"),
                       C.sb(es3, [128, 512], BF16, "pjunk"), C.sb(es3, [128, D], F32, "ptmp"))
                for tt in range(TG // 128):
                    yp = yps[tt % 2]
                    for h in range(2):
                        for fc in range(NFC):
                            S.pe(lambda e: e.matmul(out=yp[h][:, :], lhsT=hT[:, fc, tt * 128:(tt + 1) * 128],
                                                    rhs=wdt[:, fc, h * 512:(h + 1) * 512],
                                                    start=(fc == 0), stop=(fc == NFC - 1)),
                                 reads=[hT, wdt.part(fc // 2)], writes=[yp[h]])
                    post_norm_residual(C, es3, x, tg * (TG // 128) + tt, yp, g_bc, 0.5, scr)
                S.barrier()


DBG = {}


class Wts:
    def __init__(self, nc, shapes, loff=0, hoff=0):
        self.nc = nc
        self.shapes = shapes
        self.aps = {}
        self.loff = loff
        self.hoff = hoff

    def __getitem__(self, name):
        if name not in self.aps:
            self.aps[name] = self.nc.dram_tensor(name, list(self.shapes[name]), F32, kind="ExternalInput").ap()
        return self.aps[name]

    def L(self, name, l):
        return self[name][l - self.loff]

    def H(self, name, i):
        return self[name][i - self.hoff]

    def hasL(self, name, l):
        return 0 <= l - self.loff < self.shapes[name][0]

    def hasH(self, name, i):
        return 0 <= i - self.hoff < self.shapes[name][0]


class Params:
    def __init__(self):
        self.rows = {}
        self.t = None

    def cols(self, key, kc0=0, n=8):
        r = self.rows[key]
        return self.t[:, kc0:kc0 + n, r]

    def col(self, key, kc):
        r = self.rows[key]
        return self.t[:, kc, r:r + 1]


def build_params(C, es, W, identf):
    S = C.S
    P = Params()
    rows = []
    for l in range(DEPTH):
        if W.hasL("norm_g", l):
            for n in range(8):
                rows.append((("norm_g", l, n), W.L("norm_g", l)[n], D))
    rows.append((("mem_g",), W["mem_norm_g"], D))
    for i in range(2):
        if "sc_conv_w" in W.shapes and W.hasH("sc_conv_w", i):
            for k in range(3):
                rows.append((("sc_w", i, k), W.H("sc_conv_w", i)[k], 512))
            rows.append((("sc_b", i), W.H("sc_conv_b", i), 512))
            rows.append((("dsa_qg", i), W.H("dsa_q_norm_g", i), 256))
        if "rw_w0" in W.shapes and W.hasH("rw_w0", i):
            for nm in ("rw_w0", "rw_a0", "rw_k_k", "rw_k_a", "rw_ln_g", "rw_ln_b", "rt_gn_g", "rt_gn_b"):
                rows.append(((nm, i), W.H(nm, i), 512))
            rows.append((("rw_r_k", i), W.H("rw_r_k", i).rearrange("h d -> (h d)"), 512))
            rows.append((("rw_mu", i, 0), W.H("rw_mu", i)[0:1024], 1024))
            rows.append((("rw_mu", i, 1), W.H("rw_mu", i)[1024:1792], 768))
    nr = len(rows)
    assert nr <= 128
    PC = C.sb(es, [128, 8, nr], F32, "PC")
    P.t = PC
    with ExitStack() as e1:
        raw = C.sb(e1, [128, D], F32, "praw")
        S.pool(lambda e: e.memset(raw[:, :], 0.0), writes=[raw])
        for r, (key, ap, n) in enumerate(rows):
            P.rows[key] = r
            S.dma("sp", raw[r:r + 1, 0:n], ap.rearrange("(o n) -> o n", o=1), writes=[raw])
        tp = C.ps(e1, [128, 8, 128], F32, "ptp")
        for kc in range(8):
            S.pe(lambda e: e.transpose(out=tp[:, kc, :], in_=raw[:, kc * 128:(kc + 1) * 128], identity=identf[:, :]),
                 reads=[raw, identf], writes=[tp])
        S.dve(lambda e: e.tensor_copy(out=PC[:, :, :], in_=tp[:, :, 0:nr]), reads=[tp], writes=[PC])
        S.barrier()
    return P


def load_w(C, dst, src2d, c0, c1, q="pool"):
    v = src2d.rearrange("(kc p) n -> p kc n", p=128)
    nk = v.shape[1]
    for kc in range(nk):
        C.S.dma(q, dst[:, kc, 0:c1 - c0], v[:, kc, c0:c1], writes=[dst.part(kc)])


def make_consts(C, es):
    S = C.S
    K = {}
    ones_f = C.sb(es, [128, 512], F32, "ones_f")
    S.pool(lambda e: e.memset(ones_f[:, :], 1.0), writes=[ones_f])
    ident = C.sb(es, [128, 128], BF16, "ident")
    identf = C.sb(es, [128, 128], F32, "identf")
    for t in (ident, identf):
        S.pool(lambda e: e.affine_select(out=t[:, :], in_=ones_f[:, 0:128], pattern=[[-1, 128]],
                                         compare_op=ALU.is_equal, fill=0.0, base=0, channel_multiplier=1),
               reads=[ones_f], writes=[t])
    ones_bf = C.sb(es, [128, 128], BF16, "ones_bf")
    S.pool(lambda e: e.memset(ones_bf[:, :], 1.0), writes=[ones_bf])
    selq = C.sb(es, [128, 8, 8], BF16, "selq")
    S.pool(lambda e: e.affine_select(out=selq[:, :, :], in_=ones_f[:, 0:64].rearrange("p (a b) -> p a b", a=8),
                                     pattern=[[1, 8], [-1, 8]], compare_op=ALU.is_equal, fill=0.0, base=0,
                                     channel_multiplier=0), reads=[ones_f], writes=[selq])
    sel8 = C.sb(es, [8, 8, 128], BF16, "sel8")
    with ExitStack() as e0:
        sel8a = C.sb(e0, [8, 8, 128], F32, "sel8a")
        S.pool(lambda e: e.memset(sel8a[:, :, :], 1.0), writes=[sel8a])
        S.pool(lambda e: e.affine_select(out=sel8[:, :, :], in_=sel8a[:, :, :], pattern=[[-1, 8], [0, 128]],
                                         compare_op=ALU.is_equal, fill=0.0, base=0, channel_multiplier=1),
               reads=[sel8a], writes=[sel8])
        S.barrier()
    zer = C.sb(es, [128, 128], F32, "zer")
    S.pool(lambda e: e.memset(zer[:, :], 0.0), writes=[zer])
    cbias = C.sb(es, [128, 128], F32, "cbias")
    S.pool(lambda e: e.affine_select(out=cbias[:, :], in_=zer[:, :], pattern=[[-1, 128]],
                                     compare_op=ALU.is_ge, fill=-1e30, base=0, channel_multiplier=1),
           reads=[zer], writes=[cbias])
    K.update(ones_f=ones_f, ident=ident, identf=identf, ones_bf=ones_bf, selq=selq, sel8=sel8, cbias=cbias, zer=zer)
    return K


def make_negm(C, es, qT, nh, k2m, K, p8):
    S = C.S
    qsq = C.sb(es, [128, nh, 512], BF16, "qsq")
    S.act(lambda e: e.activation(out=qsq[:, :, :], in_=qT[:, 0:nh, :], func=AF.Square), reads=[qT], writes=[qsq])
    for h in range(nh):
        S.pe(lambda e: e.matmul(out=p8[0:8, :], lhsT=K["selq"][:, h, :], rhs=qsq[:, h, :], start=(h == 0), stop=(h == nh - 1)),
             reads=[K["selq"], qsq], writes=[p8])
    nm = C.sb(es, [8, 512], F32, "nm")
    negm8 = C.sb(es, [8, 512], BF16, "negm8")
    S.dve(lambda e: e.tensor_scalar(out=nm[:, :], in0=p8[0:8, :], scalar1=k2m[:, 0:1], scalar2=None, op0=ALU.mult),
          reads=[p8, k2m], writes=[nm])
    S.act(lambda e: e.activation(out=nm[:, :], in_=nm[:, :], func=AF.Sqrt), reads=[nm], writes=[nm])
    S.dve(lambda e: e.tensor_scalar(out=negm8[:, :], in0=nm[:, :], scalar1=-1.0, scalar2=None, op0=ALU.mult),
          reads=[nm], writes=[negm8])
    return negm8


def attn_core(C, K, qT_ap, q_res, ktiles, negm8, h, scale, out_ap, out_res, dv, ps_s, ps_o, ps_r, pts, rinv, mask_eng="pool"):
    S = C.S
    n = len(ktiles)
    for i, (k_ap, v_ap, m_ap, rd) in enumerate(ktiles):
        sp = ps_s[i % 2]
        pt = pts[i % 2]
        S.pe(lambda e: e.matmul(out=sp[:, :], lhsT=k_ap, rhs=qT_ap, start=True, stop=False), reads=rd + [q_res], writes=[sp])
        S.pe(lambda e: e.matmul(out=sp[:, :], lhsT=K["sel8"][:, h, :], rhs=negm8[:, :], start=False, stop=True),
             reads=[K["sel8"], negm8], writes=[sp])
        S.act(lambda e: e.activation(out=pt[:, :], in_=sp[:, :], func=AF.Exp, scale=float(scale)), reads=[sp], writes=[pt])
        if m_ap is not None:
            S.op(mask_eng, lambda e: e.tensor_tensor(out=pt[:, :], in0=pt[:, :], in1=m_ap, op=ALU.mult), reads=rd + [pt], writes=[pt])
        S.pe(lambda e: e.matmul(out=ps_o[0:dv, :], lhsT=v_ap, rhs=pt[:, :], start=(i == 0), stop=(i == n - 1)),
             reads=rd + [pt], writes=[ps_o])
        S.pe(lambda e: e.matmul(out=ps_r[0:dv, :], lhsT=K["ones_bf"][:, 0:dv], rhs=pt[:, :], start=(i == 0), stop=(i == n - 1)),
             reads=[K["ones_bf"], pt], writes=[ps_r])
    S.dve(lambda e: e.reciprocal(out=rinv[0:dv, :], in_=ps_r[0:dv, :]), reads=[ps_r], writes=[rinv])
    S.dve(lambda e: e.tensor_tensor(out=out_ap, in0=ps_o[0:dv, :], in1=rinv[0:dv, :], op=ALU.mult),
          reads=[ps_o, rinv], writes=[out_res])


def out_proj_residual(C, x, chunks, w_T, g_bc, coef, tiles):
    S = C.S
    n = len(chunks)
    with ExitStack() as es:
        yps = [[C.ps(es, [128, 512], F32, f"y{i}{h}") for h in range(2)] for i in range(2)]
        scr = (C.sb(es, [128, 4], F32, "pss"), C.sb(es, [128, 2], F32, "prs"),
               C.sb(es, [128, 512], BF16, "pjunk"), C.sb(es, [128, D], F32, "ptmp"))
        for ti, tt in enumerate(tiles):
            yp = yps[ti % 2]
            for h in range(2):
                for c, (yt, f) in enumerate(chunks):
                    S.pe(lambda e: e.matmul(out=yp[h][:, :], lhsT=f(ti), rhs=w_T[:, c, h * 512:(h + 1) * 512],
                                            start=(c == 0), stop=(c == n - 1)), reads=[yt, w_T], writes=[yp[h]])
            post_norm_residual(C, None, x, tt, yp, g_bc, coef, scr)
        S.barrier()


def xattn_block(C, x, l, W, P, K, memT):
    S = C.S
    ident = K["ident"]
    scale = 128 ** -0.5
    with ExitStack() as es:
        g_bc = C.sb(es, [128, D], F32, "g_bc")
        load_bcast_row(C, "sp", g_bc, W.L("norm_g", l)[5], D)
        wq = C.sb(es, [128, 8, 512], BF16, "wq")
        wk = C.sb(es, [128, 8, 512], BF16, "wk")
        wv = C.sb(es, [128, 8, 512], BF16, "wv")
        wo = C.sb(es, [128, 4, D], BF16, "wo")
        load_w(C, wk, W.L("xa_wk", l), 0, 512)
        load_w(C, wv, W.L("xa_wv", l), 0, 512)
        load_w(C, wq, W.L("xa_wq", l), 0, 512)
        wov = W.L("xa_wo", l).rearrange("(c p) d -> p c d", p=128)
        for c in range(4):
            S.dma("pool", wo[:, c, :], wov[:, c, :], writes=[wo.part(c)])
        kT = C.sb(es, [128, 4, MEM], BF16, "kT")
        vtok = C.sb(es, [128, 2, 512], BF16, "vtok")
        k2m = C.sb(es, [8, 1], F32, "k2m")
        with ExitStack() as e1:
            pk = C.ps(e1, [128, 512], F32, "pk")
            for h in range(4):
                for kc in range(8):
                    S.pe(lambda e: e.matmul(out=pk[:, 0:MEM], lhsT=wk[:, kc, h * 128:(h + 1) * 128], rhs=memT[:, kc, :],
                                            start=(kc == 0), stop=(kc == 7)), reads=[wk, memT], writes=[pk])
                S.act(lambda e: e.copy(out=kT[:, h, :], in_=pk[:, 0:MEM]), reads=[pk], writes=[kT])
            for mt in range(2):
                for kc in range(8):
                    S.pe(lambda e: e.matmul(out=pk[:, :], lhsT=memT[:, kc, mt * 128:(mt + 1) * 128], rhs=wv[:, kc, :],
                                            start=(kc == 0), stop=(kc == 7)), reads=[wv, memT], writes=[pk])
                S.dve(lambda e: e.tensor_copy(out=vtok[:, mt, :], in_=pk[:, :]), reads=[pk], writes=[vtok])
            ksq = C.sb(e1, [128, 4, MEM], BF16, "ksq")
            S.act(lambda e: e.activation(out=ksq[:, :, :], in_=kT[:, :, :], func=AF.Square), reads=[kT], writes=[ksq])
            for h in range(4):
                S.pe(lambda e: e.matmul(out=pk[0:8, 0:MEM], lhsT=K["selq"][:, h, :], rhs=ksq[:, h, :], start=(h == 0), stop=(h == 3)),
                     reads=[K["selq"], ksq], writes=[pk])
            S.dve(lambda e: e.reduce_max(out=k2m[:, 0:1], in_=pk[0:8, 0:MEM], axis=AX.X), reads=[pk], writes=[k2m])
            S.barrier()
        for qg in range(4):
            with ExitStack() as e2:
                oT = C.sb(e2, [128, 4, 512], BF16, "oT")
                with ExitStack() as e3:
                    xnT = C.sb(e3, [128, 8, 512], BF16, "xnT")
                    rms_to_T(C, e3, x, P.cols(("norm_g", l, 4)), xnT, 4, ident, tok0=qg * 4)
                    qT = C.sb(e3, [128, 4, 512], BF16, "qT")
                    pq = [C.ps(e3, [128, 512], F32, f"pq{i}") for i in range(2)]
                    for h in range(4):
                        for kc in range(8):
                            S.pe(lambda e: e.matmul(out=pq[h % 2][:, :], lhsT=wq[:, kc, h * 128:(h + 1) * 128], rhs=xnT[:, kc, :],
                                                    start=(kc == 0), stop=(kc == 7)), reads=[wq, xnT], writes=[pq[h % 2]])
                        S.act(lambda e: e.copy(out=qT[:, h, :], in_=pq[h % 2][:, :]), reads=[pq[h % 2]], writes=[qT.part(h)])
                    negm8 = make_negm(C, e3, qT, 4, k2m, K, pq[0])
                    ps_s = [C.ps(e3, [128, 512], F32, f"ss{i}") for i in range(2)]
                    ps_o = C.ps(e3, [128, 512], F32, "pso")
                    ps_r = C.ps(e3, [128, 512], F32, "psr")
                    pts = [C.sb(e3, [128, 512], BF16, f"pt{i}") for i in range(2)]
                    rinv = C.sb(e3, [128, 512], F32, "rinv")
                    for h in range(4):
                        kt_list = [(kT[:, h, kt * 128:(kt + 1) * 128], vtok[:, kt, h * 128:(h + 1) * 128], None, [kT, vtok])
                                   for kt in range(2)]
                        attn_core(C, K, qT[:, h, :], qT, kt_list, negm8, h, scale, oT[:, h, :], oT.part(h), 128,
                                  ps_s, ps_o, ps_r, pts, rinv)
                    S.barrier()
                out_proj_residual(C, x, [(oT, (lambda ti, c=c: oT[:, c, ti * 128:(ti + 1) * 128])) for c in range(4)],
                                  wo, g_bc, 1.0, [qg * 4 + i for i in range(4)])


def prep_mem(C, es, W, P, K, s, memT):
    S = C.S
    with ExitStack() as e1:
        mt = C.sb(e1, [128, 2, D], F32, "memraw")
        S.dma("sp", mt[:, :, :], W["mem"][s].rearrange("(t p) d -> p t d", p=128), writes=[mt.part(0), mt.part(1)])
        rms_to_T(C, e1, mt, P.cols(("mem_g",)), memT, 2, K["ident"], tok0=0)


def odd_mixer(C, x, l, W, P, K):
    S = C.S
    i = l // 2
    ident = K["ident"]
    w_in = W.H("od_w_in", i)
    NEG_SEL = -3.0e38
    with ExitStack() as es:
        g_bc = C.sb(es, [128, D], F32, "g_bc")
        load_bcast_row(C, "sp", g_bc, W.L("norm_g", l)[3], D)
        ycT = C.sb(es, [128, 4, SEQ], BF16, "ycT")
        ydT = C.sb(es, [128, 4, SEQ], BF16, "ydT")
        cqnT = C.sb(es, [128, 2, SEQ], BF16, "cqnT")
        ckvT = C.sb(es, [128, SEQ], BF16, "ckvT")
        ckvtok = C.sb(es, [128, NT, 128], BF16, "ckvtok")
        kidxT2 = C.sb(es, [128, SEQ], BF16, "kidxT2")
        widx = C.sb(es, [128, NT, 8], F32, "widx")
        absw = C.sb(es, [128, NT, 8], F32, "absw")
        sgnw = C.sb(es, [128, NT, 8], F32, "sgnw")
        carry = C.sb(es, [128, 4, 2], F32, "carry")
        S.pool(lambda e: e.memset(carry[:, :, :], 0.0), writes=[carry])
        gkv_bc = C.sb(es, [128, 128], F32, "gkv_bc")
        load_bcast_row(C, "sp", gkv_bc, W.H("dsa_kv_norm_g", i), 128)
        TG = 1024
        with ExitStack() as e1:
            wsm = C.sb(e1, [128, 8, 456], BF16, "wsm")
            load_w(C, wsm, w_in, 0, 456)
            wscs = [C.sb(e1, [128, 8, 3, 128], BF16, f"wsc{k}") for k in range(2)]
            ss = C.sb(e1, [128, 2], F32, "ss2")
            rs = C.sb(e1, [128, 2], F32, "rs2")
            junk = C.sb(e1, [128, 256], BF16, "junk2")
            cqs = C.sb(e1, [128, 256], BF16, "cqs")
            kid2 = C.sb(e1, [128, 2, 64], BF16, "kid2")
            hs = C.sb(e1, [128, 512], F32, "hs")
            ub = C.sb(e1, [128, 514], F32, "ub")
            yb = C.sb(e1, [128, 512], F32, "yb")
            w_v = w_in.rearrange("(kc p) n -> p kc n", p=128)
            for tg in range(SEQ // TG):
                with ExitStack() as e2:
                    xnT = C.sb(e2, [128, 8, TG], BF16, "xnT")
                    rms_to_T(C, e2, x, P.cols(("norm_g", l, 2)), xnT, TG // 128, ident, tok0=tg * (TG // 128))
                    pp = C.ps(e2, [128, 512], F32, "pp")
                    tp = C.ps(e2, [128, 8, 128], BF16, "tp4")
                    for tt in range(TG // 128 if DBG.get("odd_stop") != 0.25 else 0):
                        Tt = tg * (TG // 128) + tt
                        for kc in range(8):
                            S.pe(lambda e: e.matmul(out=pp[:, 0:456], lhsT=xnT[:, kc, tt * 128:(tt + 1) * 128], rhs=wsm[:, kc, 0:456],
                                                    start=(kc == 0), stop=(kc == 7)), reads=[xnT, wsm], writes=[pp])
                        S.dve(lambda e: e.memset(ss[:, :], 0.0), writes=[ss])
                        S.act(lambda e: e.activation(out=junk[:, 0:256], in_=pp[:, 0:256], func=AF.Square, accum_out=ss[:, 0:1]),
                              reads=[pp], writes=[junk, ss])
                        S.act(lambda e: e.activation(out=junk[:, 0:128], in_=pp[:, 256:384], func=AF.Square, accum_out=ss[:, 1:2]),
                              reads=[pp], writes=[junk, ss])
                        S.dve(lambda e: e.tensor_scalar(out=rs[:, 0:1], in0=ss[:, 0:1], scalar1=1.0 / 256, scalar2=EPS,
                                                        op0=ALU.mult, op1=ALU.add), reads=[ss], writes=[rs])
                        S.dve(lambda e: e.tensor_scalar(out=rs[:, 1:2], in0=ss[:, 1:2], scalar1=1.0 / 128, scalar2=EPS,
                                                        op0=ALU.mult, op1=ALU.add), reads=[ss], writes=[rs])
                        S.act(lambda e: e.activation(out=rs[:, :], in_=rs[:, :], func=AF.Sqrt), reads=[rs], writes=[rs])
                        S.dve(lambda e: e.reciprocal(out=rs[:, :], in_=rs[:, :]), reads=[rs], writes=[rs])
                        S.act(lambda e: e.activation(out=cqs[:, :], in_=pp[:, 0:256], func=AF.Copy, scale=rs[:, 0:1]),
                              reads=[pp, rs], writes=[cqs])
                        S.dve(lambda e: e.scalar_tensor_tensor(out=ckvtok[:, Tt, :], in0=pp[:, 256:384], scalar=rs[:, 1:2],
                                                               in1=gkv_bc[:, :], op0=ALU.mult, op1=ALU.mult),
                              reads=[pp, rs, gkv_bc], writes=[ckvtok.part(Tt)])
                        S.dve(lambda e: e.tensor_copy(out=kid2[:, :, :], in_=pp[:, 384:448].unsqueeze(1).to_broadcast([128, 2, 64])),
                              reads=[pp], writes=[kid2])
                        S.dve(lambda e: e.tensor_copy(out=widx[:, Tt, :], in_=pp[:, 448:456]), reads=[pp], writes=[widx.part(Tt)])
                        for c in range(2):
                            S.pe(lambda e: e.transpose(out=tp[:, c, :], in_=cqs[:, c * 128:(c + 1) * 128], identity=ident[:, :]),
                                 reads=[cqs, ident], writes=[tp])
                        S.pe(lambda e: e.transpose(out=tp[:, 2, :], in_=ckvtok[:, Tt, :], identity=ident[:, :]),
                             reads=[ckvtok.part(Tt), ident], writes=[tp])
                        S.pe(lambda e: e.transpose(out=tp[:, 3, :], in_=kid2[:, :, :].rearrange("p a b -> p (a b)"), identity=ident[:, :]),
                             reads=[kid2, ident], writes=[tp])
                        tsl = slice(Tt * 128, (Tt + 1) * 128)
                        S.dve(lambda e: e.tensor_tensor(out=cqnT[:, :, tsl], in0=tp[:, 0:2, :],
                                                        in1=P.cols(("dsa_qg", i), 0, 2).unsqueeze(2).to_broadcast([128, 2, 128]),
                                                        op=ALU.mult), reads=[tp, P.t], writes=[cqnT.part(Tt)])
                        S.act(lambda e: e.copy(out=ckvT[:, tsl], in_=tp[:, 2, :]), reads=[tp], writes=[ckvT.part(Tt)])
                        S.act(lambda e: e.copy(out=kidxT2[:, tsl], in_=tp[:, 3, :]), reads=[tp], writes=[kidxT2.part(Tt)])
                    pcs = [C.ps(e2, [128, 512], F32, f"pc{k}") for k in range(3)]
                    for fc in range(4 if DBG.get("odd_stop") != 0.5 else 0):
                        wsc = wscs[fc % 2]
                        for kc in range(8):
                            S.dma("pool", wsc[:, kc, :, :],
                                  w_v[:, kc, 456:1992].rearrange("p (j c) -> p j c", j=3)[:, :, fc * 128:(fc + 1) * 128],
                                  writes=[wsc.part(kc)])
                        for th in range(TG // 512):
                            tok0 = tg * TG + th * 512
                            for j3 in range(3):
                                for kc in range(8):
                                    S.pe(lambda e: e.matmul(out=pcs[j3][:, :], lhsT=wsc[:, kc, j3, :], rhs=xnT[:, kc, th * 512:(th + 1) * 512],
                                                            start=(kc == 0), stop=(kc == 7)), reads=[wsc, xnT], writes=[pcs[j3]])
                            S.act(lambda e: e.copy(out=hs[:, :], in_=pcs[0][:, :]), reads=[pcs[0]], writes=[hs])
                            S.pool(lambda e: e.tensor_copy(out=ub[:, 0:2], in_=carry[:, fc, :]), reads=[carry.part(fc)], writes=[ub])
                            S.dve(lambda e: e.tensor_tensor(out=ub[:, 2:514], in0=pcs[2][:, :], in1=hs[:, :], op=ALU.mult),
                                  reads=[pcs[2], hs], writes=[ub])
                            S.pool(lambda e: e.tensor_copy(out=carry[:, fc, :], in_=ub[:, 512:514]), reads=[ub], writes=[carry.part(fc)])
                            S.pool(lambda e: e.tensor_scalar(out=yb[:, :], in0=ub[:, 2:514], scalar1=P.col(("sc_w", i, 2), fc),
                                                             scalar2=P.col(("sc_b", i), fc), op0=ALU.mult, op1=ALU.add),
                                   reads=[ub, P.t], writes=[yb])
                            S.dve(lambda e: e.scalar_tensor_tensor(out=yb[:, :], in0=ub[:, 1:513], scalar=P.col(("sc_w", i, 1), fc),
                                                                   in1=yb[:, :], op0=ALU.mult, op1=ALU.add), reads=[ub, yb, P.t], writes=[yb])
                            S.dve(lambda e: e.scalar_tensor_tensor(out=yb[:, :], in0=ub[:, 0:512], scalar=P.col(("sc_w", i, 0), fc),
                                                                   in1=yb[:, :], op0=ALU.mult, op1=ALU.add), reads=[ub, yb, P.t], writes=[yb])
                            S.dve(lambda e: e.tensor_tensor(out=ydT[:, fc, tok0:tok0 + 512], in0=pcs[1][:, :], in1=yb[:, :], op=ALU.mult),
                                  reads=[pcs[1], yb], writes=[ydT.part((fc, tok0))])
                    S.barrier()
            S.act(lambda e: e.activation(out=absw[:, :, :], in_=widx[:, :, :], func=AF.Abs), reads=[widx], writes=[absw])
            S.act(lambda e: e.activation(out=sgnw[:, :, :], in_=widx[:, :, :], func=AF.Sign), reads=[widx], writes=[sgnw])
            S.barrier()
        if DBG.get("odd_stop") in (1, 0.5, 0.25):
            return
        with ExitStack() as e1:
            wqi = C.sb(e1, [128, 2, 512], BF16, "wqi")
            wuq = C.sb(e1, [128, 2, 512], BF16, "wuq")
            wuk = C.sb(e1, [128, 4, 128], BF16, "wuk")
            wuv = C.sb(e1, [128, 8, 64], BF16, "wuv")
            load_w(C, wqi, W.H("dsa_w_qi", i).rearrange("r h d -> r (h d)"), 0, 512)
            load_w(C, wuq, W.H("dsa_w_uq", i).rearrange("r h d -> r (h d)"), 0, 512)
            S.dma("pool", wuk[:, :, :], W.H("dsa_w_uk", i).rearrange("(hp e) d c -> (e d) hp c", e=2), writes=[wuk])
            S.dma("pool", wuv[:, :, :], W.H("dsa_w_uv", i).rearrange("h c d -> c h d"), writes=[wuv])
            k2m = C.sb(e1, [8, 1], F32, "k2m")
            with ExitStack() as e2:
                ksq = C.sb(e2, [128, SEQ], BF16, "ksq")
                k2b = C.sb(e2, [8, 4], F32, "k2b")
                pk = C.ps(e2, [128, 512], F32, "pk")
                S.act(lambda e: e.activation(out=ksq[:, :], in_=ckvT[:, :], func=AF.Square), reads=[ckvT], writes=[ksq])
                for b in range(4):
                    S.pe(lambda e: e.matmul(out=pk[0:8, :], lhsT=K["ones_bf"][:, 0:8], rhs=ksq[:, b * 512:(b + 1) * 512], start=True, stop=True),
                         reads=[K["ones_bf"], ksq], writes=[pk])
                    S.dve(lambda e: e.reduce_max(out=k2b[:, b:b + 1], in_=pk[0:8, :], axis=AX.X), reads=[pk], writes=[k2b])
                S.dve(lambda e: e.reduce_max(out=k2m[:, 0:1], in_=k2b[:, :], axis=AX.X), reads=[k2b], writes=[k2m])
                S.barrier()
            for qg in range(4):
                tsl = slice(qg * 512, (qg + 1) * 512)
                nkt = 4 * qg + 4
                with ExitStack() as e2:
                    qidxT = C.sb(e2, [128, 4, 512], BF16, "qidxT")
                    qhT = C.sb(e2, [128, 4, 512], BF16, "qhT")
                    qlatT = C.sb(e2, [128, 8, 512], BF16, "qlatT")
                    maskT = C.sb(e2, [128, nkt, 512], BF16, "maskT")
                    S.pool(lambda e: e.memset(maskT[:, :, :], 0.0), writes=[maskT])
                    with ExitStack() as e3:
                        pq = [C.ps(e3, [128, 512], F32, f"pq{k}") for k in range(2)]
                        n_ = 0
                        for (wt, dst) in ((wqi, qidxT), (wuq, qhT)):
                            for hp in range(4):
                                p_ = pq[n_ % 2]
                                n_ += 1
                                for rc in range(2):
                                    S.pe(lambda e: e.matmul(out=p_[:, :], lhsT=wt[:, rc, hp * 128:(hp + 1) * 128], rhs=cqnT[:, rc, tsl],
                                                            start=(rc == 0), stop=(rc == 1)), reads=[wt, cqnT], writes=[p_])
                                S.act(lambda e: e.copy(out=dst[:, hp, :], in_=p_[:, :]), reads=[p_], writes=[dst.part(hp)])
                        for h in range(8):
                            hp, e_ = h // 2, h % 2
                            p_ = pq[h % 2]
                            S.pe(lambda e: e.matmul(out=p_[:, :], lhsT=wuk[e_ * 64:(e_ + 1) * 64, hp, :], rhs=qhT[e_ * 64:(e_ + 1) * 64, hp, :],
                                                    start=True, stop=True), reads=[wuk, qhT], writes=[p_])
                            S.dve(lambda e: e.tensor_copy(out=qlatT[:, h, :], in_=p_[:, :]), reads=[p_], writes=[qlatT.part(h)])
                        S.barrier()
                    if DBG.get("odd_stop") == 2:
                        continue
                    with ExitStack() as e3:
                        lps = [C.ps(e3, [128, 512], F32, f"lp{k}") for k in range(2)]
                        tpm = C.ps(e3, [128, 8, 128], BF16, "tpm")
                        sc = C.sb(e3, [128, SEQ], F32, "sc")
                        mk = C.sb(e3, [128, SEQ], BF16, "mk")
                        rb = [C.sb(e3, [128, 512], F32, f"rb{k}") for k in range(2)]
                        st4 = C.sb(e3, [128, 4], F32, "st4")
                        uu = C.sb(e3, [128, 1], F32, "uu")
                        cntt = C.sb(e3, [128, 1], F32, "cntt")
                        dd_ = C.sb(e3, [128, 1], F32, "dd_")
                        n_ = 0
                        for ql in range(4):
                            qt = 4 * qg + ql
                            nk = (qt + 1) * 128
                            for kb in range((nk + 511) // 512):
                                n = min(512, nk - kb * 512)
                                ksl = slice(kb * 512, kb * 512 + n)
                                for h in range(8):
                                    hp, e_ = h // 2, h % 2
                                    lp = lps[n_ % 2]
                                    r_ = rb[n_ % 2]
                                    n_ += 1
                                    S.pe(lambda e: e.matmul(out=lp[:, 0:n], lhsT=qidxT[e_ * 64:(e_ + 1) * 64, hp, ql * 128:(ql + 1) * 128],
                                                            rhs=kidxT2[e_ * 64:(e_ + 1) * 64, ksl], start=True, stop=True),
                                         reads=[qidxT, kidxT2], writes=[lp])
                                    S.act(lambda e: e.activation(out=r_[:, 0:n], in_=lp[:, 0:n], func=AF.Relu, scale=absw[:, qt, h:h + 1]),
                                          reads=[lp, absw], writes=[r_])
                                    eng = "dve"
                                    if h == 0:
                                        S.op(eng, lambda e: e.tensor_scalar(out=sc[:, ksl], in0=r_[:, 0:n], scalar1=sgnw[:, qt, 0:1],
                                                                            scalar2=None, op0=ALU.mult), reads=[r_, sgnw], writes=[sc])
                                    elif eng == "dve":
                                        S.dve(lambda e: e.scalar_tensor_tensor(out=sc[:, ksl], in0=r_[:, 0:n], scalar=sgnw[:, qt, h:h + 1],
                                                                               in1=sc[:, ksl], op0=ALU.mult, op1=ALU.add),
                                              reads=[r_, sgnw, sc], writes=[sc])
                                    else:
                                        S.pool(lambda e: e.tensor_scalar(out=r_[:, 0:n], in0=r_[:, 0:n], scalar1=sgnw[:, qt, h:h + 1],
                                                                         scalar2=None, op0=ALU.mult), reads=[r_, sgnw], writes=[r_])
                                        S.pool(lambda e: e.tensor_tensor(out=sc[:, ksl], in0=sc[:, ksl], in1=r_[:, 0:n], op=ALU.add),
                                               reads=[r_, sc], writes=[sc])
                            dsl = slice(qt * 128, (qt + 1) * 128)
                            if qt >= 2:
                                S.dve(lambda e: e.tensor_reduce(out=st4[:, 0:1], in_=sc[:, 0:nk], axis=AX.X, op=ALU.max), reads=[sc], writes=[st4])
                                S.dve(lambda e: e.tensor_reduce(out=st4[:, 1:2], in_=sc[:, 0:nk], axis=AX.X, op=ALU.min), reads=[sc], writes=[st4])
                                S.dve(lambda e: e.tensor_tensor(out=st4[:, 2:3], in0=st4[:, 0:1], in1=st4[:, 1:2], op=ALU.subtract), reads=[st4], writes=[st4])
                                S.dve(lambda e: e.tensor_scalar(out=st4[:, 2:3], in0=st4[:, 2:3], scalar1=1e-30, scalar2=None, op0=ALU.max), reads=[st4], writes=[st4])
                                S.dve(lambda e: e.reciprocal(out=st4[:, 3:4], in_=st4[:, 2:3]), reads=[st4], writes=[st4])
                                S.dve(lambda e: e.tensor_scalar(out=sc[:, 0:nk], in0=sc[:, 0:nk], scalar1=st4[:, 1:2], scalar2=st4[:, 3:4],
                                                                op0=ALU.subtract, op1=ALU.mult), reads=[sc, st4], writes=[sc])
                                S.pool(lambda e: e.tensor_tensor(out=sc[:, dsl], in0=sc[:, dsl], in1=K["cbias"][:, :], op=ALU.add),
                                       reads=[sc, K["cbias"]], writes=[sc])
                                S.dve(lambda e: e.memset(uu[:, :], 0.5), writes=[uu])
                                for it in range(16):
                                    S.dve(lambda e: e.tensor_scalar(out=mk[:, 0:nk], in0=sc[:, 0:nk], scalar1=uu[:, 0:1], scalar2=0.0,
                                                                    op0=ALU.is_ge, op1=ALU.add, accum_out=cntt[:, 0:1]),
                                          reads=[sc, uu], writes=[mk, cntt])
                                    S.dve(lambda e: e.tensor_scalar(out=dd_[:, :], in0=cntt[:, :], scalar1=255.5, scalar2=float(2.0 ** -(it + 1)),
                                                                    op0=ALU.is_ge, op1=ALU.mult), reads=[cntt], writes=[dd_])
                                    S.dve(lambda e: e.scalar_tensor_tensor(out=uu[:, :], in0=dd_[:, :], scalar=-float(2.0 ** -(it + 2)), in1=uu[:, :],
                                                                           op0=ALU.add, op1=ALU.add), reads=[dd_, uu], writes=[uu])
                                S.dve(lambda e: e.tensor_scalar(out=mk[:, 0:nk], in0=sc[:, 0:nk], scalar1=uu[:, 0:1], scalar2=None, op0=ALU.is_ge),
                                      reads=[sc, uu], writes=[mk])
                            else:
                                S.pool(lambda e: e.tensor_tensor(out=sc[:, dsl], in0=sc[:, dsl], in1=K["cbias"][:, :], op=ALU.add),
                                       reads=[sc, K["cbias"]], writes=[sc])
                                S.dve(lambda e: e.tensor_single_scalar(out=mk[:, 0:nk], in_=sc[:, 0:nk], scalar=-1e29, op=ALU.is_gt),
                                      reads=[sc], writes=[mk])
                            for k0 in range(0, qt + 1, 8):
                                cnt = min(8, qt + 1 - k0)
                                for kk in range(cnt):
                                    kt = k0 + kk
                                    S.pe(lambda e: e.transpose(out=tpm[:, kk, :], in_=mk[:, kt * 128:(kt + 1) * 128], identity=ident[:, :]),
                                         reads=[mk, ident], writes=[tpm])
                                S.act(lambda e: e.copy(out=maskT[:, k0:k0 + cnt, ql * 128:(ql + 1) * 128], in_=tpm[:, 0:cnt, :]),
                                      reads=[tpm], writes=[maskT])
                        S.barrier()
                    if DBG.get("odd_stop") == 3:
                        continue
                    with ExitStack() as e3:
                        p8 = C.ps(e3, [128, 512], F32, "p8")
                        negm8 = make_negm(C, e3, qlatT, 8, k2m, K, p8)
                        ps_s = [C.ps(e3, [128, 512], F32, f"ss{k}") for k in range(2)]
                        ps_o = C.ps(e3, [128, 512], F32, "pso")
                        ps_r = C.ps(e3, [128, 512], F32, "psr")
                        po = C.ps(e3, [128, 512], F32, "po")
                        pts = [C.sb(e3, [128, 512], BF16, f"pt{k}") for k in range(2)]
                        rinv = C.sb(e3, [128, 512], F32, "rinv")
                        olat = [C.sb(e3, [128, 512], BF16, f"olat{k}") for k in range(2)]
                        for h in range(8):
                            hp, e_ = h // 2, h % 2
                            kt_list = [(ckvT[:, kt * 128:(kt + 1) * 128], ckvtok[:, kt, :], maskT[:, kt, :], [ckvT, ckvtok, maskT])
                                       for kt in range(nkt)]
                            ol = olat[h % 2]
                            attn_core(C, K, qlatT[:, h, :], qlatT, kt_list, negm8, h, 64 ** -0.5, ol[:, :], ol, 128,
                                      ps_s, ps_o, ps_r, pts, rinv, mask_eng=("pool" if h % 2 else "dve"))
                            S.pe(lambda e: e.matmul(out=po[e_ * 64:(e_ + 1) * 64, :], lhsT=wuv[:, h, :], rhs=ol[:, :], start=True, stop=True),
                                 reads=[wuv, ol], writes=[po])
                            if e_ == 1:
                                S.act(lambda e: e.copy(out=ycT[:, hp, tsl], in_=po[:, :]), reads=[po], writes=[ycT.part((hp, qg))])
                        S.barrier()
        with ExitStack() as e1:
            wo = C.sb(e1, [128, 8, D], BF16, "wo")
            load_w(C, wo, W.H("od_w_out", i), 0, D)
            for g4 in range(4):
                chunks = [(ycT, (lambda ti, c=c, g4=g4: ycT[:, c, (g4 * 4 + ti) * 128:(g4 * 4 + ti + 1) * 128])) for c in range(4)]
                chunks += [(ydT, (lambda ti, c=c, g4=g4: ydT[:, c, (g4 * 4 + ti) * 128:(g4 * 4 + ti + 1) * 128])) for c in range(4)]
                out_proj_residual(C, x, chunks, wo, g_bc, 1.0, [g4 * 4 + t_ for t_ in range(4)])


RW_LN_EPS = 64e-5
TWO_PI = 6.283185307179586


def make_even_consts(C, es, K):
    S = C.S
    ones_f = K["ones_f"]
    E = {}
    o4 = ones_f[:, 0:512].rearrange("p (a b) -> p a b", a=4)
    for nm, pat, cm, op in (("m_su", [[0, 4], [1, 128]], -1, ALU.is_gt), ("m_ui", [[0, 4], [1, 128]], -1, ALU.is_ge),
                            ("m_sl", [[0, 4], [-1, 128]], 1, ALU.is_gt)):
        t = C.sb(es, [128, 4, 128], F32, nm)
        S.pool(lambda e: e.affine_select(out=t[:, :, :], in_=o4, pattern=pat, compare_op=op, fill=0.0, base=0,
                                         channel_multiplier=cm), reads=[ones_f], writes=[t])
        E[nm] = t
    lvm = C.sb(es, [128, 7, 128], BF16, "lvm")
    lvmT = C.sb(es, [128, 7, 128], BF16, "lvmT")
    with ExitStack() as e0:
        I32 = mybir.dt.int32
        pi = C.sb(e0, [128, 128], I32, "lv_pi")
        fi = C.sb(e0, [128, 128], I32, "lv_fi")
        S.pool(lambda e: e.iota(pi[:, :], pattern=[[0, 128]], base=0, channel_multiplier=1), writes=[pi])
        S.pool(lambda e: e.iota(fi[:, :], pattern=[[1, 128]], base=0, channel_multiplier=0), writes=[fi])
        ta = C.sb(e0, [128, 128], I32, "lv_ta")
        tb = C.sb(e0, [128, 128], I32, "lv_tb")
        eq = C.sb(e0, [128, 128], F32, "lv_eq")
        bp = C.sb(e0, [128, 128], F32, "lv_bp")
        bq = C.sb(e0, [128, 128], F32, "lv_bq")
        nbp = C.sb(e0, [128, 128], F32, "lv_nbp")
        nbq = C.sb(e0, [128, 128], F32, "lv_nbq")
        for s in range(7):
            S.dve(lambda e: e.tensor_scalar(out=ta[:, :], in0=pi[:, :], scalar1=s + 1, scalar2=None, op0=ALU.arith_shift_right), reads=[pi], writes=[ta])
            S.dve(lambda e: e.tensor_scalar(out=tb[:, :], in0=fi[:, :], scalar1=s + 1, scalar2=None, op0=ALU.arith_shift_right), reads=[fi], writes=[tb])
            S.dve(lambda e: e.tensor_tensor(out=eq[:, :], in0=ta[:, :], in1=tb[:, :], op=ALU.is_equal), reads=[ta, tb], writes=[eq])
            S.dve(lambda e: e.tensor_scalar(out=ta[:, :], in0=pi[:, :], scalar1=s, scalar2=1, op0=ALU.arith_shift_right, op1=ALU.bitwise_and), reads=[pi], writes=[ta])
            S.dve(lambda e: e.tensor_scalar(out=tb[:, :], in0=fi[:, :], scalar1=s, scalar2=1, op0=ALU.arith_shift_right, op1=ALU.bitwise_and), reads=[fi], writes=[tb])
            S.dve(lambda e: e.tensor_copy(out=bp[:, :], in_=ta[:, :]), reads=[ta], writes=[bp])
            S.dve(lambda e: e.tensor_copy(out=bq[:, :], in_=tb[:, :]), reads=[tb], writes=[bq])
            S.dve(lambda e: e.tensor_scalar(out=nbp[:, :], in0=bp[:, :], scalar1=-1.0, scalar2=1.0, op0=ALU.mult, op1=ALU.add), reads=[bp], writes=[nbp])
            S.dve(lambda e: e.tensor_scalar(out=nbq[:, :], in0=bq[:, :], scalar1=-1.0, scalar2=1.0, op0=ALU.mult, op1=ALU.add), reads=[bq], writes=[nbq])
            S.dve(lambda e: e.tensor_tensor(out=nbp[:, :], in0=nbp[:, :], in1=bq[:, :], op=ALU.mult), reads=[nbp, bq], writes=[nbp])
            S.dve(lambda e: e.tensor_tensor(out=lvm[:, s, :], in0=nbp[:, :], in1=eq[:, :], op=ALU.mult), reads=[nbp, eq], writes=[lvm])
            S.dve(lambda e: e.tensor_tensor(out=nbq[:, :], in0=nbq[:, :], in1=bp[:, :], op=ALU.mult), reads=[nbq, bp], writes=[nbq])
            S.dve(lambda e: e.tensor_tensor(out=lvmT[:, s, :], in0=nbq[:, :], in1=eq[:, :], op=ALU.mult), reads=[nbq, eq], writes=[lvmT])
        S.barrier()
    E.update(lvm=lvm, lvmT=lvmT)
    id8 = C.sb(es, [128, 8, 128], BF16, "id8")
    with ExitStack() as e0:
        ones8 = C.sb(e0, [128, 8, 128], F32, "ones8")
        S.pool(lambda e: e.memset(ones8[:, :, :], 1.0), writes=[ones8])
        S.pool(lambda e: e.affine_select(out=id8[:, :, :], in_=ones8[:, :, :], pattern=[[0, 8], [-1, 128]], compare_op=ALU.is_equal, fill=0.0,
                                         base=0, channel_multiplier=1), reads=[ones8], writes=[id8])
        S.barrier()
    E["id8"] = id8
    bo = C.sb(es, [128, 128], BF16, "blockones")
    bof = C.sb(es, [128, 128], BF16, "blockones_f")
    S.pool(lambda e: e.memset(bo[:, :], 0.0), writes=[bo])
    S.pool(lambda e: e.memset(bof[:, :], 0.0), writes=[bof])
    for b in range(2):
        S.pool(lambda e: e.memset(bo[b * 64:(b + 1) * 64, b * 64:(b + 1) * 64], 1.0), writes=[bo])
        S.pool(lambda e: e.memset(bof[b * 64:(b + 1) * 64, b * 64:(b + 1) * 64], 1.0 / 64), writes=[bof])
    on128 = C.sb(es, [128, 128], BF16, "on128")
    S.pool(lambda e: e.memset(on128[:, :], 1.0 / 128), writes=[on128])
    E.update(bo=bo, bof=bof, on128=on128)
    seg = C.sb(es, [128, 4, 128], F32, "seg")
    S.pool(lambda e: e.memset(seg[:, :, :], 1.0), writes=[seg])
    S.pool(lambda e: e.memset(seg[:, :, 0:1], 0.0), writes=[seg])
    E["seg"] = seg
    cosT = C.sb(es, [128, SEQ], BF16, "cosT")
    sinT = C.sb(es, [128, SEQ], BF16, "sinT")
    with ExitStack() as e1:
        jc_i = C.sb(e1, [128, 1], mybir.dt.int32, "jc_i")
        for b in range(2):
            S.pool(lambda e: e.iota(jc_i[b * 64:(b + 1) * 64, :], pattern=[[0, 1]], base=0, channel_multiplier=1), writes=[jc_i])
        jc = C.sb(e1, [128, 1], F32, "jc")
        S.dve(lambda e: e.tensor_copy(out=jc[:, :], in_=jc_i[:, :]), reads=[jc_i], writes=[jc])
        invf = C.sb(e1, [128, 1], F32, "invf")
        S.act(lambda e: e.activation(out=invf[:, :], in_=jc[:, :], func=AF.Exp, scale=-float(np.log(10000.0)) / 64.0),
              reads=[jc], writes=[invf])
        tp_i = C.sb(e1, [128, SEQ], mybir.dt.int32, "tp_i")
        S.pool(lambda e: e.iota(tp_i[:, :], pattern=[[1, SEQ]], base=0, channel_multiplier=0), writes=[tp_i])
        ang = C.sb(e1, [128, SEQ], F32, "ang")
        S.dve(lambda e: e.tensor_copy(out=ang[:, :], in_=tp_i[:, :]), reads=[tp_i], writes=[ang])
        S.dve(lambda e: e.tensor_scalar(out=ang[:, :], in0=ang[:, :], scalar1=invf[:, 0:1], scalar2=None, op0=ALU.mult),
              reads=[ang, invf], writes=[ang])
        sgn = C.sb(e1, [128, 1], F32, "sgn")
        S.pool(lambda e: e.memset(sgn[0:64, :], -1.0), writes=[sgn])
        S.pool(lambda e: e.memset(sgn[64:128, :], 1.0), writes=[sgn])
        red = C.sb(e1, [128, SEQ], F32, "red")
        qi = C.sb(e1, [128, SEQ], mybir.dt.int32, "qi")
        qf = C.sb(e1, [128, SEQ], F32, "qf")
        for (dst, shift) in ((sinT, 0.0), (cosT, float(np.pi / 2))):
            S.dve(lambda e: e.tensor_scalar(out=qf[:, :], in0=ang[:, :], scalar1=shift, scalar2=1.0 / TWO_PI, op0=ALU.add, op1=ALU.mult),
                  reads=[ang], writes=[qf])
            S.dve(lambda e: e.tensor_copy(out=qi[:, :], in_=qf[:, :]), reads=[qf], writes=[qi])
            S.dve(lambda e: e.tensor_copy(out=qf[:, :], in_=qi[:, :]), reads=[qi], writes=[qf])
            S.dve(lambda e: e.scalar_tensor_tensor(out=red[:, :], in0=qf[:, :], scalar=-TWO_PI, in1=ang[:, :], op0=ALU.mult, op1=ALU.add),
                  reads=[qf, ang], writes=[red])
            if shift:
                S.dve(lambda e: e.tensor_scalar(out=red[:, :], in0=red[:, :], scalar1=shift, scalar2=None, op0=ALU.add), reads=[red], writes=[red])
            S.dve(lambda e: e.tensor_scalar(out=qf[:, :], in0=red[:, :], scalar1=float(np.pi), scalar2=-TWO_PI, op0=ALU.is_gt, op1=ALU.mult),
                  reads=[red], writes=[qf])
            S.dve(lambda e: e.tensor_tensor(out=red[:, :], in0=red[:, :], in1=qf[:, :], op=ALU.add), reads=[red, qf], writes=[red])
            S.dve(lambda e: e.tensor_scalar(out=qf[:, :], in0=red[:, :], scalar1=-float(np.pi), scalar2=TWO_PI, op0=ALU.is_lt, op1=ALU.mult),
                  reads=[red], writes=[qf])
            S.dve(lambda e: e.tensor_tensor(out=red[:, :], in0=red[:, :], in1=qf[:, :], op=ALU.add), reads=[red, qf], writes=[red])
            S.dve(lambda e: e.tensor_scalar(out=red[:, :], in0=red[:, :], scalar1=3.14159, scalar2=-3.14159, op0=ALU.min, op1=ALU.max),
                  reads=[red], writes=[red])
            if shift:
                S.act(lambda e: e.activation(out=dst[:, :], in_=red[:, :], func=AF.Sin), reads=[red], writes=[dst])
            else:
                S.act(lambda e: e.activation(out=red[:, :], in_=red[:, :], func=AF.Sin), reads=[red], writes=[red])
                S.dve(lambda e: e.tensor_scalar(out=dst[:, :], in0=red[:, :], scalar1=sgn[:, 0:1], scalar2=None, op0=ALU.mult),
                      reads=[red, sgn], writes=[dst])
        S.barrier()
    E.update(cosT=cosT, sinT=sinT)
    lg = [float(np.log1p(-2.0 ** (-5.0 - h))) for h in range(4)]
    E["lg"] = lg
    scale = 128 ** -0.5
    dmT = C.sb(es, [128, 4, 128], F32, "dmT")
    xiT = C.sb(es, [128, 4, 128], BF16, "xiT")
    zcol = C.sb(es, [128, 4], F32, "zcol")
    with ExitStack() as e1:
        d_i = C.sb(e1, [128, 128], mybir.dt.int32, "d_i")
        d_f = C.sb(e1, [128, 128], F32, "d_f")
        S.pool(lambda e: e.iota(d_i[:, :], pattern=[[1, 128]], base=0, channel_multiplier=-1), writes=[d_i])
        S.dve(lambda e: e.tensor_copy(out=d_f[:, :], in_=d_i[:, :]), reads=[d_i], writes=[d_f])
        S.dve(lambda e: e.tensor_scalar(out=d_f[:, :], in0=d_f[:, :], scalar1=0.0, scalar2=None, op0=ALU.max), reads=[d_f], writes=[d_f])
        i_i = C.sb(e1, [128, 128], mybir.dt.int32, "i_i")
        i_f = C.sb(e1, [128, 128], F32, "i_f")
        S.pool(lambda e: e.iota(i_i[:, :], pattern=[[1, 128]], base=1, channel_multiplier=0), writes=[i_i])
        S.dve(lambda e: e.tensor_copy(out=i_f[:, :], in_=i_i[:, :]), reads=[i_i], writes=[i_f])
        p_i = C.sb(e1, [128, 1], mybir.dt.int32, "p_i")
        p_f = C.sb(e1, [128, 1], F32, "p_f")
        S.pool(lambda e: e.iota(p_i[:, :], pattern=[[0, 1]], base=127, channel_multiplier=-1), writes=[p_i])
        S.dve(lambda e: e.tensor_copy(out=p_f[:, :], in_=p_i[:, :]), reads=[p_i], writes=[p_f])
        tmp = C.sb(e1, [128, 128], F32, "tmpd")
        for h in range(4):
            S.act(lambda e: e.activation(out=tmp[:, :], in_=d_f[:, :], func=AF.Exp, scale=lg[h]), reads=[d_f], writes=[tmp])
            S.dve(lambda e: e.scalar_tensor_tensor(out=dmT[:, h, :], in0=tmp[:, :], scalar=scale, in1=E["m_ui"][:, 0, :],
                                                   op0=ALU.mult, op1=ALU.mult), reads=[tmp, E["m_ui"]], writes=[dmT])
            S.act(lambda e: e.activation(out=xiT[:, h, :], in_=i_f[:, :], func=AF.Exp, scale=lg[h]), reads=[i_f], writes=[xiT])
            S.act(lambda e: e.activation(out=zcol[:, h:h + 1], in_=p_f[:, :], func=AF.Exp, scale=lg[h]), reads=[p_f], writes=[zcol])
        S.dve(lambda e: e.tensor_scalar(out=zcol[:, :], in0=zcol[:, :], scalar1=scale, scalar2=None, op0=ALU.mult), reads=[zcol], writes=[zcol])
        S.barrier()
    E.update(dmT=dmT, xiT=xiT, zcol=zcol)
    return E


def group_norm_T(C, es, y, nch, onesmat, eps, gcol_fn, bcol_fn, post_fn, pm, pq):
    S = C.S
    sq = C.sb(es, [128, 512], BF16, "gn_sq")
    yb16 = C.sb(es, [128, 512], BF16, "gn_yb")
    m2 = C.sb(es, [128, 512], F32, "gn_m2")
    rs = C.sb(es, [128, 512], F32, "gn_rs")
    dd = C.sb(es, [128, 512], F32, "gn_dd")
    for c in range(nch):
        S.dve(lambda e: e.tensor_copy(out=yb16[:, :], in_=y[:, c, :]), reads=[y], writes=[yb16])
        S.pe(lambda e: e.matmul(out=pm[:, :], lhsT=onesmat[:, :], rhs=yb16[:, :], start=True, stop=True), reads=[onesmat, yb16], writes=[pm])
        S.act(lambda e: e.activation(out=sq[:, :], in_=y[:, c, :], func=AF.Square), reads=[y], writes=[sq])
        S.pe(lambda e: e.matmul(out=pq[:, :], lhsT=onesmat[:, :], rhs=sq[:, :], start=True, stop=True), reads=[onesmat, sq], writes=[pq])
        S.act(lambda e: e.activation(out=m2[:, :], in_=pm[:, :], func=AF.Square), reads=[pm], writes=[m2])
        S.dve(lambda e: e.tensor_tensor(out=rs[:, :], in0=pq[:, :], in1=m2[:, :], op=ALU.subtract), reads=[pq, m2], writes=[rs])
        S.dve(lambda e: e.tensor_scalar(out=rs[:, :], in0=rs[:, :], scalar1=0.0, scalar2=float(eps), op0=ALU.max, op1=ALU.add),
              reads=[rs], writes=[rs])
        S.act(lambda e: e.activation(out=rs[:, :], in_=rs[:, :], func=AF.Sqrt), reads=[rs], writes=[rs])
        S.dve(lambda e: e.reciprocal(out=rs[:, :], in_=rs[:, :]), reads=[rs], writes=[rs])
        S.dve(lambda e: e.tensor_tensor(out=dd[:, :], in0=y[:, c, :], in1=pm[:, :], op=ALU.subtract), reads=[y, pm], writes=[dd])
        S.dve(lambda e: e.tensor_tensor(out=dd[:, :], in0=dd[:, :], in1=rs[:, :], op=ALU.mult), reads=[dd, rs], writes=[dd])
        S.pool(lambda e: e.tensor_scalar(out=dd[:, :], in0=dd[:, :], scalar1=gcol_fn(c), scalar2=bcol_fn(c), op0=ALU.mult, op1=ALU.add),
               reads=[dd], writes=[dd])
        post_fn(c, dd)


def even_mixer(C, x, l, W, P, K):
    S = C.S
    i = l // 2
    ident = K["ident"]
    w_in = W.H("ev_w_in", i)
    w_v = w_in.rearrange("(kc p) n -> p kc n", p=128)
    if DBG.get("even_stop") == 0:
        return
    with ExitStack() as es:
        E = make_even_consts(C, es, K)
        lg = E["lg"]
        gamC = [float(np.exp(128.0 * lg[h])) for h in range(4)]
        yaT = C.sb(es, [128, 4, SEQ], BF16, "yaT")
        with ExitStack() as er:
            wa2 = C.sb(er, [128, 512], BF16, "wa2")
            g2 = C.sb(er, [128, 512], BF16, "g2")
            S.dma("pool", wa2[0:64, :], W.H("rw_w2", i), writes=[wa2])
            S.dma("pool", wa2[64:128, :], W.H("rw_a2", i), writes=[wa2])
            S.dma("pool", g2[:, :], W.H("rw_g2", i), writes=[g2])
            omk = C.sb(er, [128, 4], F32, "omk")
            S.dve(lambda e: e.tensor_scalar(out=omk[:, :], in0=P.cols(("rw_k_a", i), 0, 4), scalar1=-1.0, scalar2=1.0, op0=ALU.mult, op1=ALU.add),
                  reads=[P.t], writes=[omk])
            St = C.sb(er, [128, 4, 64], F32, "St")
            Sb = C.sb(er, [128, 4, 2, 64], BF16, "Sbd")
            S.pool(lambda e: e.memset(St[:, :, :], 0.0), writes=[St])
            S.pool(lambda e: e.memset(Sb[:, :, :, :], 0.0), writes=[Sb])
            pcar = C.sb(er, [128, 14], F32, "pcar")
            S.pool(lambda e: e.memset(pcar[:, :], 0.0), writes=[pcar])
            wch = [C.sb(er, [128, 8, 128], BF16, f"wch{k}") for k in range(2)]
            nw = [0]

            def mu_col(c):
                return P.col(("rw_mu", i, 0), c) if c < 8 else P.col(("rw_mu", i, 1), c - 8)

            for blk in range(4):
                t0 = blk * 512
                with ExitStack() as e1:
                    xnT = C.sb(e1, [128, 8, 512], BF16, "xnT")
                    rms_to_T(C, e1, x, P.cols(("norm_g", l, 2)), xnT, 4, ident, tok0=blk * 4)
                    with ExitStack() as e2:
                        At = C.sb(e2, [128, 4, 512], BF16, "At")
                        Bt = C.sb(e2, [128, 4, 512], BF16, "Bt")
                        Kt = C.sb(e2, [128, 4, 512], BF16, "Kt")
                        Rq = C.sb(e2, [128, 4, 512], BF16, "Rq")
                        vT = C.sb(e2, [128, 4, 512], BF16, "vT")
                        bon = C.sb(e2, [128, 4, 512], BF16, "bon")
                        gT = C.sb(e2, [128, 4, 512], BF16, "gT")
                        gC = C.sb(e2, [128, 4, 4], F32, "gC")
                        yraw = C.sb(e2, [128, 4, 512], F32, "yraw")
                        with ExitStack() as e3:
                            pps = [C.ps(e3, [128, 512], F32, f"pp{k}") for k in range(3)]
                            pa = C.ps(e3, [128, 512], F32, "pa")
                            pb = C.ps(e3, [128, 512], F32, "pb")
                            pT = C.sb(e3, [128, 513], F32, "pT")
                            mx = [C.sb(e3, [128, 512], F32, f"mx{k}") for k in range(3)]
                            dtmp = C.sb(e3, [128, 512], F32, "dtmp")
                            xwa = C.sb(e3, [128, 512], BF16, "xwa")
                            sxg = C.sb(e3, [128, 512], BF16, "sxg")
                            kkb = C.sb(e3, [128, 512], BF16, "kkb")
                            bA = C.sb(e3, [128, 512], F32, "bA")
                            bB = C.sb(e3, [128, 512], F32, "bB")
                            eL = C.sb(e3, [128, 512], F32, "eL")
                            eLm = C.sb(e3, [128, 512], F32, "eLm")
                            asig = C.sb(e3, [128, 512], F32, "asig")
                            kk = C.sb(e3, [128, 512], F32, "kk")
                            t2 = C.sb(e3, [128, 512], F32, "t2")

                            def proj_mix(c, k_):
                                pp, m_ = pps[k_], mx[k_]
                                wt = wch[nw[0] % 2]
                                nw[0] += 1
                                S.dma("pool", wt[:, :, :], w_v[:, :, c * 128:(c + 1) * 128], writes=[wt])
                                for kc in range(8):
                                    S.pe(lambda e: e.matmul(out=pp[:, :], lhsT=wt[:, kc, :], rhs=xnT[:, kc, :],
                                                            start=(kc == 0), stop=(kc == 7)), reads=[wt, xnT], writes=[pp])
                                S.act(lambda e: e.copy(out=pT[:, 1:513], in_=pp[:, :]), reads=[pp], writes=[pT])
                                S.pool(lambda e: e.tensor_copy(out=pT[:, 0:1], in_=pcar[:, c:c + 1]), reads=[pcar.part(c)], writes=[pT])
                                S.pool(lambda e: e.tensor_copy(out=pcar[:, c:c + 1], in_=pT[:, 512:513]), reads=[pT], writes=[pcar.part(c)])
                                S.dve(lambda e: e.tensor_tensor(out=dtmp[:, :], in0=pT[:, 0:512], in1=pT[:, 1:513], op=ALU.subtract),
                                      reads=[pT], writes=[dtmp])
                                S.dve(lambda e: e.scalar_tensor_tensor(out=m_[:, :], in0=dtmp[:, :], scalar=mu_col(c), in1=pT[:, 1:513],
                                                                       op0=ALU.mult, op1=ALU.add), reads=[dtmp, pT, P.t], writes=[m_])
                                return m_

                            m_ = proj_mix(12, 0)
                            S.act(lambda e: e.activation(out=xwa[0:64, :], in_=m_[0:64, :], func=AF.Tanh), reads=[m_], writes=[xwa])
                            S.act(lambda e: e.copy(out=xwa[64:128, :], in_=m_[64:128, :]), reads=[m_], writes=[xwa])
                            m_ = proj_mix(13, 1)
                            S.act(lambda e: e.activation(out=sxg[:, :], in_=m_[:, :], func=AF.Sigmoid), reads=[m_], writes=[sxg])
                            for hp in range(4):
                                rr = proj_mix(hp, 0)
                                kx = proj_mix(4 + hp, 1)
                                vv = proj_mix(8 + hp, 2)
                                S.pe(lambda e: e.matmul(out=pa[:, :], lhsT=wa2[0:64, hp * 128:(hp + 1) * 128], rhs=xwa[0:64, :], start=True, stop=True),
                                     reads=[wa2, xwa], writes=[pa])
                                S.act(lambda e: e.activation(out=bA[:, :], in_=pa[:, :], func=AF.Sigmoid, bias=P.col(("rw_w0", i), hp)),
                                      reads=[pa, P.t], writes=[bA])
                                S.dve(lambda e: e.tensor_scalar(out=bA[:, :], in0=bA[:, :], scalar1=-float(np.exp(-0.5)), scalar2=None, op0=ALU.mult),
                                      reads=[bA], writes=[bA])
                                S.dve(lambda e: e.tensor_tensor_scan(out=bB[:, :], data0=E["seg"][:, :, :].rearrange("p a b -> p (a b)"),
                                                                     data1=bA[:, :], initial=0.0, op0=ALU.mult, op1=ALU.add),
                                      reads=[bA, E["seg"]], writes=[bB])
                                S.act(lambda e: e.activation(out=eL[:, :], in_=bB[:, :], func=AF.Exp), reads=[bB], writes=[eL])
                                S.act(lambda e: e.activation(out=eLm[:, :], in_=bB[:, :], func=AF.Exp, scale=-1.0), reads=[bB], writes=[eLm])
                                S.dve(lambda e: e.tensor_tensor(out=bA[:, :], in0=bB[:, :], in1=bA[:, :], op=ALU.subtract), reads=[bB, bA], writes=[bA])
                                S.act(lambda e: e.activation(out=bA[:, :], in_=bA[:, :], func=AF.Exp), reads=[bA], writes=[bA])
                                S.pool(lambda e: e.tensor_copy(out=gC[:, :, hp], in_=eL[:, :].rearrange("p (a b) -> p a b", a=4)[:, :, 127]),
                                       reads=[eL], writes=[gC])
                                S.pe(lambda e: e.matmul(out=pb[:, :], lhsT=wa2[64:128, hp * 128:(hp + 1) * 128], rhs=xwa[64:128, :], start=True, stop=True),
                                     reads=[wa2, xwa], writes=[pb])
                                S.act(lambda e: e.activation(out=asig[:, :], in_=pb[:, :], func=AF.Sigmoid, bias=P.col(("rw_a0", i), hp)),
                                      reads=[pb, P.t], writes=[asig])
                                S.pool(lambda e: e.tensor_scalar(out=kk[:, :], in0=kx[:, :], scalar1=P.col(("rw_k_k", i), hp), scalar2=None, op0=ALU.mult),
                                       reads=[kx, P.t], writes=[kk])
                                S.act(lambda e: e.activation(out=kkb[:, :], in_=kk[:, :], func=AF.Square), reads=[kk], writes=[kkb])
                                S.pe(lambda e: e.matmul(out=pa[:, :], lhsT=E["bo"][:, :], rhs=kkb[:, :], start=True, stop=True),
                                     reads=[E["bo"], kkb], writes=[pa])
                                S.dve(lambda e: e.tensor_scalar(out=bB[:, :], in0=pa[:, :], scalar1=1e-24, scalar2=None, op0=ALU.max), reads=[pa], writes=[bB])
                                S.act(lambda e: e.activation(out=bB[:, :], in_=bB[:, :], func=AF.Sqrt), reads=[bB], writes=[bB])
                                S.dve(lambda e: e.reciprocal(out=bB[:, :], in_=bB[:, :]), reads=[bB], writes=[bB])
                                S.dve(lambda e: e.tensor_tensor(out=kk[:, :], in0=kk[:, :], in1=bB[:, :], op=ALU.mult), reads=[kk, bB], writes=[kk])
                                S.dve(lambda e: e.scalar_tensor_tensor(out=At[:, hp, :], in0=kk[:, :], scalar=-1.0, in1=bA[:, :], op0=ALU.mult, op1=ALU.mult),
                                      reads=[kk, bA], writes=[At.part(hp)])
                                S.pool(lambda e: e.tensor_tensor(out=bB[:, :], in0=kk[:, :], in1=asig[:, :], op=ALU.mult), reads=[kk, asig], writes=[bB])
                                S.pool(lambda e: e.tensor_tensor(out=Bt[:, hp, :], in0=bB[:, :], in1=eLm[:, :], op=ALU.mult), reads=[bB, eLm], writes=[Bt.part(hp)])
                                S.dve(lambda e: e.tensor_scalar(out=t2[:, :], in0=asig[:, :], scalar1=P.col(("rw_k_a", i), hp), scalar2=omk[:, hp:hp + 1],
                                                                op0=ALU.mult, op1=ALU.add), reads=[asig, P.t, omk], writes=[t2])
                                S.dve(lambda e: e.tensor_tensor(out=t2[:, :], in0=t2[:, :], in1=kx[:, :], op=ALU.mult), reads=[t2, kx], writes=[t2])
                                S.pool(lambda e: e.tensor_tensor(out=Kt[:, hp, :], in0=t2[:, :], in1=eLm[:, :], op=ALU.mult), reads=[t2, eLm], writes=[Kt.part(hp)])
                                S.dve(lambda e: e.tensor_tensor(out=Rq[:, hp, :], in0=rr[:, :], in1=eL[:, :], op=ALU.mult), reads=[rr, eL], writes=[Rq.part(hp)])
                                S.act(lambda e: e.copy(out=vT[:, hp, :], in_=vv[:, :]), reads=[vv], writes=[vT.part(hp)])
                                S.dve(lambda e: e.scalar_tensor_tensor(out=kkb[:, :], in0=rr[:, :], scalar=P.col(("rw_r_k", i), hp), in1=t2[:, :],
                                                                       op0=ALU.mult, op1=ALU.mult), reads=[rr, t2, P.t], writes=[kkb])
                                S.pe(lambda e: e.matmul(out=pb[:, :], lhsT=E["bo"][:, :], rhs=kkb[:, :], start=True, stop=True),
                                     reads=[E["bo"], kkb], writes=[pb])
                                S.dve(lambda e: e.tensor_tensor(out=bon[:, hp, :], in0=pb[:, :], in1=vv[:, :], op=ALU.mult), reads=[pb, vv], writes=[bon.part(hp)])
                                S.pe(lambda e: e.matmul(out=pa[:, :], lhsT=g2[:, hp * 128:(hp + 1) * 128], rhs=sxg[:, :], start=True, stop=True),
                                     reads=[g2, sxg], writes=[pa])
                                S.act(lambda e: e.copy(out=gT[:, hp, :], in_=pa[:, :]), reads=[pa], writes=[gT.part(hp)])
                            S.barrier()
                        if DBG.get("even_stop") == 1:
                            continue
                        with ExitStack() as e3:
                            def bank(nm, dt=F32):
                                return C.ps(e3, [128, 512] if dt == F32 else [128, 8, 128], dt, nm)
                            pA = [bank(f"pA{k}") for k in range(3)]
                            pX = [bank(f"pX{k}") for k in range(2)]
                            ptr = bank("ptr", BF16)
                            pS = bank("pS")
                            BtT = C.sb(e3, [128, 512], BF16, "BtT")
                            KtT = C.sb(e3, [128, 512], BF16, "KtT")
                            Vtk = C.sb(e3, [128, 512], BF16, "Vtk")
                            Mak = C.sb(e3, [128, 8, 128], BF16, "Mak")
                            Nbr = C.sb(e3, [128, 8, 128], BF16, "Nbr")
                            Nkr = C.sb(e3, [128, 8, 128], BF16, "Nkr")
                            Pm = [C.sb(e3, [128, 8, 128], BF16, "Mm")]
                            PTm = [C.sb(e3, [128, 8, 128], BF16, "MTm")]
                            Xm = [C.sb(e3, [128, 8, 128], BF16, f"Xm{k}") for k in range(2)]
                            XTm = [C.sb(e3, [128, 8, 128], BF16, f"XTm{k}") for k in range(2)]
                            Ts = C.sb(e3, [128, 8, 128], BF16, "Ts")
                            TsT = C.sb(e3, [128, 8, 128], BF16, "TsT")
                            Y1s = C.sb(e3, [128, 8, 128], BF16, "Y1s")
                            Z1s = C.sb(e3, [128, 8, 128], BF16, "Z1s")
                            Gt = C.sb(e3, [128, 512], BF16, "Gt")
                            Ut = C.sb(e3, [128, 512], BF16, "Ut")
                            stmp = C.sb(e3, [128, 4, 64], F32, "stmp")

                            def hv(t_, h, csl):
                                hp_, e_ = h // 2, h % 2
                                return t_[e_ * 64:(e_ + 1) * 64, hp_, csl]

                            for c in range(4):
                                csl = slice(c * 128, (c + 1) * 128)
                                for (src, dst) in ((Bt, BtT), (Kt, KtT), (vT, Vtk)):
                                    for hp in range(4):
                                        S.pe(lambda e: e.transpose(out=ptr[:, hp, :], in_=src[:, hp, csl], identity=ident[:, :]),
                                             reads=[src, ident], writes=[ptr])
                                    S.act(lambda e: e.copy(out=dst[:, :], in_=ptr[:, 0:4, :].rearrange("p a b -> p (a b)")), reads=[ptr], writes=[dst])
                                if DBG.get('even_stop') == 1.2:
                                    continue
                                prods = ((Bt, At, Pm[0], "m_su"), (At, Bt, PTm[0], "m_sl"), (Kt, At, Mak, "m_su"),
                                         (Bt, Rq, Nbr, "m_ui"), (Kt, Rq, Nkr, "m_ui"))
                                nb = 0
                                for (lt, rt_, dst, mk) in prods:
                                    for e_ in range(2):
                                        pbk = pA[nb % 3]
                                        nb += 1
                                        for hh in range(4):
                                            h = 2 * hh + e_
                                            S.pe(lambda e: e.matmul(out=pbk[:, hh * 128:(hh + 1) * 128], lhsT=hv(lt, h, csl), rhs=hv(rt_, h, csl),
                                                                    start=True, stop=True), reads=[lt, rt_], writes=[pbk])
                                        S.dve(lambda e: e.tensor_tensor(out=dst[:, :, :].rearrange("p (a two) b -> p a two b", two=2)[:, :, e_, :],
                                                                        in0=pbk[:, :].rearrange("p (a b) -> p a b", a=4), in1=E[mk][:, :, :], op=ALU.mult),
                                              reads=[pbk, E[mk]], writes=[dst.part(("e", e_))])
                                if DBG.get('even_stop') == 1.4:
                                    continue
                                Mm, MTm = Pm[0], PTm[0]

                                def lvl(src, msk, s, dst, eng):
                                    S.op(eng, lambda e: e.tensor_tensor(out=dst[:, :, :], in0=src[:, :, :],
                                                                        in1=E[msk][:, s, :].unsqueeze(1).to_broadcast([128, 8, 128]), op=ALU.mult),
                                         reads=[src, E[msk]], writes=[dst])

                                lvl(Mm, "lvm", 0, Ts, "pool")
                                lvl(MTm, "lvmT", 0, TsT, "pool")
                                S.pool(lambda e: e.tensor_tensor(out=Xm[0][:, :, :], in0=Ts[:, :, :], in1=E["id8"][:, :, :], op=ALU.add),
                                       reads=[Ts, E["id8"]], writes=[Xm[0]])
                                S.pool(lambda e: e.tensor_tensor(out=XTm[0][:, :, :], in0=TsT[:, :, :], in1=E["id8"][:, :, :], op=ALU.add),
                                       reads=[TsT, E["id8"]], writes=[XTm[0]])
                                cur = 0
                                for s in range(1, 7):
                                    nxt = 1 - cur
                                    last = (s == 6)
                                    lvl(Mm, "lvm", s, Ts, "pool")
                                    lvl(MTm, "lvmT", s, TsT, "dve")
                                    for half in range(2):
                                        hs_ = range(half * 4, half * 4 + 4)
                                        pbk = pA[nb % 3]
                                        nb += 1
                                        for hh, h in enumerate(hs_):
                                            S.pe(lambda e: e.matmul(out=pbk[:, hh * 128:(hh + 1) * 128], lhsT=TsT[:, h, :], rhs=Xm[cur][:, h, :],
                                                                    start=True, stop=True), reads=[TsT, Xm[cur]], writes=[pbk])
                                        S.act(lambda e: e.copy(out=Y1s[:, half * 4:half * 4 + 4, :], in_=pbk[:, :].rearrange("p (a b) -> p a b", a=4)),
                                              reads=[pbk], writes=[Y1s.part(half)])
                                        if not last:
                                            pbk = pA[nb % 3]
                                            nb += 1
                                            for hh, h in enumerate(hs_):
                                                S.pe(lambda e: e.matmul(out=pbk[:, hh * 128:(hh + 1) * 128], lhsT=Ts[:, h, :], rhs=XTm[cur][:, h, :],
                                                                        start=True, stop=True), reads=[Ts, XTm[cur]], writes=[pbk])
                                            S.dve(lambda e: e.tensor_copy(out=Z1s[:, half * 4:half * 4 + 4, :], in_=pbk[:, :].rearrange("p (a b) -> p a b", a=4)),
                                                  reads=[pbk], writes=[Z1s.part(half)])
                                    for half in range(2):
                                        hs_ = range(half * 4, half * 4 + 4)
                                        px = pX[half]
                                        for hh, h in enumerate(hs_):
                                            S.pe(lambda e: e.matmul(out=px[:, hh * 128:(hh + 1) * 128], lhsT=ident[:, :], rhs=Xm[cur][:, h, :],
                                                                    start=True, stop=False), reads=[ident, Xm[cur]], writes=[px])
                                            S.pe(lambda e: e.matmul(out=px[:, hh * 128:(hh + 1) * 128], lhsT=XTm[cur][:, h, :], rhs=Y1s[:, h, :],
                                                                    start=False, stop=True), reads=[XTm[cur], Y1s], writes=[px])
                                        S.act(lambda e: e.copy(out=Xm[nxt][:, half * 4:half * 4 + 4, :], in_=px[:, :].rearrange("p (a b) -> p a b", a=4)),
                                              reads=[px], writes=[Xm[nxt].part(half)])
                                        if not last:
                                            pbk = pA[nb % 3]
                                            nb += 1
                                            for hh, h in enumerate(hs_):
                                                S.pe(lambda e: e.matmul(out=pbk[:, hh * 128:(hh + 1) * 128], lhsT=ident[:, :], rhs=XTm[cur][:, h, :],
                                                                        start=True, stop=False), reads=[ident, XTm[cur]], writes=[pbk])
                                                S.pe(lambda e: e.matmul(out=pbk[:, hh * 128:(hh + 1) * 128], lhsT=Xm[cur][:, h, :], rhs=Z1s[:, h, :],
                                                                        start=False, stop=True), reads=[Xm[cur], Z1s], writes=[pbk])
                                            S.dve(lambda e: e.tensor_copy(out=XTm[nxt][:, half * 4:half * 4 + 4, :], in_=pbk[:, :].rearrange("p (a b) -> p a b", a=4)),
                                                  reads=[pbk], writes=[XTm[nxt].part(half)])
                                    cur = nxt
                                if DBG.get('even_stop') == 1.6:
                                    continue
                                Xf = Xm[cur]
                                pg = pA[nb % 3]
                                nb += 1
                                for h in range(8):
                                    hp, e_ = h // 2, h % 2
                                    S.pe(lambda e: e.matmul(out=pg[:, h * 64:(h + 1) * 64], lhsT=At[:, hp, csl], rhs=Sb[:, hp, e_, :],
                                                            start=True, stop=False), reads=[At, Sb], writes=[pg])
                                    S.pe(lambda e: e.matmul(out=pg[:, h * 64:(h + 1) * 64], lhsT=Mak[:, h, :], rhs=Vtk[:, h * 64:(h + 1) * 64],
                                                            start=False, stop=True), reads=[Mak, Vtk], writes=[pg])
                                S.act(lambda e: e.copy(out=Gt[:, :], in_=pg[:, :]), reads=[pg], writes=[Gt])
                                if DBG.get('even_stop') == 1.65:
                                    continue
                                pu = pA[nb % 3]
                                nb += 1
                                for h in range(8):
                                    S.pe(lambda e: e.matmul(out=pu[:, h * 64:(h + 1) * 64], lhsT=Xf[:, h, :], rhs=Gt[:, h * 64:(h + 1) * 64],
                                                            start=True, stop=True), reads=[Xf, Gt], writes=[pu])
                                S.dve(lambda e: e.tensor_copy(out=Ut[:, :], in_=pu[:, :]), reads=[pu], writes=[Ut])
                                if DBG.get('even_stop') == 1.7:
                                    continue
                                py = pA[nb % 3]
                                nb += 1
                                for h in range(8):
                                    hp, e_ = h // 2, h % 2
                                    o_ = py[e_ * 64:(e_ + 1) * 64, hp * 128:(hp + 1) * 128]
                                    S.pe(lambda e: e.matmul(out=o_, lhsT=Sb[:, hp, e_, :], rhs=Rq[:, hp, csl], start=True, stop=False),
                                         reads=[Sb, Rq], writes=[py])
                                    S.pe(lambda e: e.matmul(out=o_, lhsT=Ut[:, h * 64:(h + 1) * 64], rhs=Nbr[:, h, :], start=False, stop=False),
                                         reads=[Ut, Nbr], writes=[py])
                                    S.pe(lambda e: e.matmul(out=o_, lhsT=Vtk[:, h * 64:(h + 1) * 64], rhs=Nkr[:, h, :], start=False, stop=True),
                                         reads=[Vtk, Nkr], writes=[py])
                                S.act(lambda e: e.copy(out=yraw[:, :, csl], in_=py[:, :].rearrange("p (a b) -> p a b", a=4)), reads=[py], writes=[yraw.part(c)])
                                if DBG.get('even_stop') == 1.75:
                                    continue
                                for hp in range(4):
                                    o_ = pS[:, hp * 128:(hp + 1) * 128]
                                    S.pe(lambda e: e.matmul(out=o_, lhsT=BtT[:, hp * 128:(hp + 1) * 128], rhs=Ut[:, hp * 128:(hp + 1) * 128], start=True, stop=False),
                                         reads=[BtT, Ut], writes=[pS])
                                    S.pe(lambda e: e.matmul(out=o_, lhsT=KtT[:, hp * 128:(hp + 1) * 128], rhs=Vtk[:, hp * 128:(hp + 1) * 128], start=False, stop=True),
                                         reads=[KtT, Vtk], writes=[pS])
                                for e_ in range(2):
                                    S.dve(lambda e: e.tensor_tensor(out=stmp[e_ * 64:(e_ + 1) * 64, :, :],
                                                                    in0=pS[e_ * 64:(e_ + 1) * 64, :].rearrange("p (a b c) -> p a b c", a=4, b=2)[:, :, e_, :],
                                                                    in1=St[e_ * 64:(e_ + 1) * 64, :, :], op=ALU.add),
                                          reads=[pS, St], writes=[stmp])
                                S.dve(lambda e: e.tensor_tensor(out=St[:, :, :], in0=stmp[:, :, :], in1=gC[:, c, :].unsqueeze(2).to_broadcast([128, 4, 64]),
                                                                op=ALU.mult), reads=[stmp, gC], writes=[St])
                                for e_ in range(2):
                                    S.act(lambda e: e.copy(out=Sb[e_ * 64:(e_ + 1) * 64, :, e_, :], in_=St[e_ * 64:(e_ + 1) * 64, :, :]), reads=[St], writes=[Sb])
                            S.barrier()
                        if DBG.get('even_stop') == 1.8:
                            continue
                        with ExitStack() as e3:
                            pm = C.ps(e3, [128, 512], F32, "gn_pm")
                            pq = C.ps(e3, [128, 512], F32, "gn_pq")

                            def post_a(c, dd):
                                S.pool(lambda e: e.tensor_tensor(out=dd[:, :], in0=dd[:, :], in1=bon[:, c, :], op=ALU.add), reads=[dd, bon], writes=[dd])
                                S.dve(lambda e: e.tensor_tensor(out=yaT[:, c, t0:t0 + 512], in0=dd[:, :], in1=gT[:, c, :], op=ALU.mult), reads=[dd, gT], writes=[yaT.part((c, blk))])

                            group_norm_T(C, e3, yraw, 4, E["bof"], RW_LN_EPS, lambda c: P.col(("rw_ln_g", i), c), lambda c: P.col(("rw_ln_b", i), c),
                                         post_a, pm, pq)
                            S.barrier()
        if DBG.get("even_stop") in (1, 1.2, 1.4, 1.6, 1.65, 1.7, 1.75, 1.8, 2):
            return
        ybT = C.sb(es, [128, 4, SEQ], BF16, "ybT")
        with ExitStack() as er:
            Rt = C.sb(er, [128, 4, 128], F32, "Rt")
            Rb = C.sb(er, [128, 4, 128], BF16, "Rb")
            for t_ in (Rt, Rb):
                S.pool(lambda e: e.memset(t_[:, :, :], 0.0), writes=[t_])
            wvr = C.sb(er, [128, 8, 512], BF16, "wvr")
            load_w(C, wvr, w_in, 1792 + 1024, 1792 + 1536)
            wch = [C.sb(er, [128, 8, 128], BF16, f"wchr{k}") for k in range(2)]
            nw = [0]

            def wchunk(c0):
                wt = wch[nw[0] % 2]
                nw[0] += 1
                S.dma("pool", wt[:, :, :], w_v[:, :, 1792 + c0:1792 + c0 + 128], writes=[wt])
                return wt

            for blk in range(4):
                t0 = blk * 512
                with ExitStack() as e1:
                    xnT = C.sb(e1, [128, 8, 512], BF16, "xnT")
                    rms_to_T(C, e1, x, P.cols(("norm_g", l, 2)), xnT, 4, ident, tok0=blk * 4)
                    with ExitStack() as e2:
                        qr = C.sb(e2, [128, 4, 512], BF16, "qr")
                        qx = C.sb(e2, [128, 4, 512], BF16, "qx")
                        kr = C.sb(e2, [128, 4, 512], BF16, "kr")
                        sg = C.sb(e2, [128, 4, 512], BF16, "sg")
                        vtk = C.sb(e2, [128, 4, 512], BF16, "vtk")
                        oraw = C.sb(e2, [128, 4, 512], F32, "oraw")
                        cs = E["cosT"][:, t0:t0 + 512]
                        sn = E["sinT"][:, t0:t0 + 512]
                        with ExitStack() as e3:
                            pq_ = [C.ps(e3, [128, 512], F32, f"rq{k}") for k in range(2)]
                            ps_ = [C.ps(e3, [128, 512], F32, f"rs{k}") for k in range(2)]
                            t1 = C.sb(e3, [128, 512], F32, "rt1")
                            t2 = C.sb(e3, [128, 512], F32, "rt2")
                            n_ = 0
                            for (c0, dst) in ((0, qr), (512, kr)):
                                for h in range(4):
                                    pa_, pb_ = pq_[n_ % 2], ps_[n_ % 2]
                                    n_ += 1
                                    cb = c0 + h * 128
                                    wrt = wchunk(cb)
                                    for kc in range(8):
                                        S.pe(lambda e: e.matmul(out=pa_[:, :], lhsT=wrt[:, kc, :], rhs=xnT[:, kc, :], start=(kc == 0), stop=(kc == 7)),
                                             reads=[wrt, xnT], writes=[pa_])
                                    for half in range(2):
                                        for kc in range(8):
                                            S.pe(lambda e: e.matmul(out=pb_[half * 64:(half + 1) * 64, :], lhsT=wrt[:, kc, (1 - half) * 64:(1 - half) * 64 + 64],
                                                                    rhs=xnT[:, kc, :], start=(kc == 0), stop=(kc == 7)), reads=[wrt, xnT], writes=[pb_])
                                    S.dve(lambda e: e.tensor_tensor(out=t1[:, :], in0=pa_[:, :], in1=cs, op=ALU.mult), reads=[pa_, E["cosT"]], writes=[t1])
                                    S.dve(lambda e: e.tensor_tensor(out=t2[:, :], in0=pb_[:, :], in1=sn, op=ALU.mult), reads=[pb_, E["sinT"]], writes=[t2])
                                    S.pool(lambda e: e.tensor_tensor(out=dst[:, h, :], in0=t1[:, :], in1=t2[:, :], op=ALU.add), reads=[t1, t2], writes=[dst.part(h)])
                                    if c0 == 0:
                                        S.pool(lambda e: e.tensor_tensor(out=qx[:, h, :].rearrange("p (a b) -> p a b", a=4),
                                                                         in0=qr[:, h, :].rearrange("p (a b) -> p a b", a=4),
                                                                         in1=E["xiT"][:, h, :].unsqueeze(1).to_broadcast([128, 4, 128]), op=ALU.mult),
                                               reads=[qr.part(h), E["xiT"]], writes=[qx.part(h)])
                            for h in range(4):
                                pa_ = pq_[h % 2]
                                wrt = wchunk(1536 + h * 128)
                                for kc in range(8):
                                    S.pe(lambda e: e.matmul(out=pa_[:, :], lhsT=wrt[:, kc, :], rhs=xnT[:, kc, :],
                                                            start=(kc == 0), stop=(kc == 7)), reads=[wrt, xnT], writes=[pa_])
                                S.act(lambda e: e.activation(out=sg[:, h, :], in_=pa_[:, :], func=AF.Silu), reads=[pa_], writes=[sg.part(h)])
                            for c in range(4):
                                pa_ = ps_[c % 2]
                                for kc in range(8):
                                    S.pe(lambda e: e.matmul(out=pa_[:, :], lhsT=xnT[:, kc, c * 128:(c + 1) * 128], rhs=wvr[:, kc, :],
                                                            start=(kc == 0), stop=(kc == 7)), reads=[wvr, xnT], writes=[pa_])
                                S.act(lambda e: e.copy(out=vtk[:, c, :], in_=pa_[:, :]), reads=[pa_], writes=[vtk.part(c)])
                            S.barrier()
                        with ExitStack() as e3:
                            psc = C.ps(e3, [128, 512], F32, "psc")
                            po_ = C.ps(e3, [128, 512], F32, "po_r")
                            pkv = C.ps(e3, [128, 512], F32, "pkv")
                            ptr = C.ps(e3, [128, 8, 128], BF16, "ptr_r")
                            scT = C.sb(e3, [128, 4, 128], BF16, "scT")
                            ktk = C.sb(e3, [128, 4, 128], BF16, "ktk")
                            for c in range(4):
                                csl = slice(c * 128, (c + 1) * 128)
                                for h in range(4):
                                    S.pe(lambda e: e.matmul(out=psc[:, h * 128:(h + 1) * 128], lhsT=kr[:, h, csl], rhs=qr[:, h, csl], start=True, stop=True),
                                         reads=[kr, qr], writes=[psc])
                                    S.pe(lambda e: e.transpose(out=ptr[:, h, :], in_=kr[:, h, csl], identity=ident[:, :]), reads=[kr, ident], writes=[ptr])
                                S.dve(lambda e: e.tensor_tensor(out=scT[:, :, :], in0=psc[:, :].rearrange("p (a b) -> p a b", a=4), in1=E["dmT"][:, :, :], op=ALU.mult),
                                      reads=[psc, E["dmT"]], writes=[scT])
                                S.dve(lambda e: e.tensor_tensor(out=ktk[:, :, :], in0=ptr[:, 0:4, :], in1=E["zcol"][:, :].unsqueeze(2).to_broadcast([128, 4, 128]),
                                                                op=ALU.mult), reads=[ptr, E["zcol"]], writes=[ktk])
                                for h in range(4):
                                    o_ = po_[:, h * 128:(h + 1) * 128]
                                    S.pe(lambda e: e.matmul(out=o_, lhsT=vtk[:, c, h * 128:(h + 1) * 128], rhs=scT[:, h, :], start=True, stop=False),
                                         reads=[vtk, scT], writes=[po_])
                                    S.pe(lambda e: e.matmul(out=o_, lhsT=Rb[:, h, :], rhs=qx[:, h, csl], start=False, stop=True), reads=[Rb, qx], writes=[po_])
                                S.act(lambda e: e.copy(out=oraw[:, :, csl], in_=po_[:, :].rearrange("p (a b) -> p a b", a=4)), reads=[po_], writes=[oraw.part(c)])
                                for h in range(4):
                                    S.pe(lambda e: e.matmul(out=pkv[:, h * 128:(h + 1) * 128], lhsT=ktk[:, h, :], rhs=vtk[:, c, h * 128:(h + 1) * 128],
                                                            start=True, stop=True), reads=[ktk, vtk], writes=[pkv])
                                for h in range(4):
                                    S.dve(lambda e: e.scalar_tensor_tensor(out=Rt[:, h, :], in0=Rt[:, h, :], scalar=gamC[h], in1=pkv[:, h * 128:(h + 1) * 128],
                                                                           op0=ALU.mult, op1=ALU.add), reads=[Rt, pkv], writes=[Rt])
                                S.act(lambda e: e.copy(out=Rb[:, :, :], in_=Rt[:, :, :]), reads=[Rt], writes=[Rb])
                            S.barrier()
                        with ExitStack() as e3:
                            pm = C.ps(e3, [128, 512], F32, "gn_pm")
                            pq = C.ps(e3, [128, 512], F32, "gn_pq")

                            def post_b(c, dd):
                                S.dve(lambda e: e.tensor_tensor(out=ybT[:, c, t0:t0 + 512], in0=dd[:, :], in1=sg[:, c, :], op=ALU.mult), reads=[dd, sg], writes=[ybT.part((c, blk))])

                            group_norm_T(C, e3, oraw, 4, E["on128"], EPS, lambda c: P.col(("rt_gn_g", i), c), lambda c: P.col(("rt_gn_b", i), c),
                                         post_b, pm, pq)
                            S.barrier()
        with ExitStack() as eo:
            g_bc = C.sb(eo, [128, D], F32, "g_bc")
            load_bcast_row(C, "sp", g_bc, W.L("norm_g", l)[3], D)
            wo = C.sb(eo, [128, 8, D], BF16, "wo_ev")
            load_w(C, wo, W.H("ev_w_out", i), 0, D)
            for g4 in range(4):
                chunks = [(yaT, (lambda ti, c=c, g4=g4: yaT[:, c, (g4 * 4 + ti) * 128:(g4 * 4 + ti + 1) * 128])) for c in range(4)]
                chunks += [(ybT, (lambda ti, c=c, g4=g4: ybT[:, c, (g4 * 4 + ti) * 128:(g4 * 4 + ti + 1) * 128])) for c in range(4)]
                out_proj_residual(C, x, chunks, wo, g_bc, 1.0, [g4 * 4 + t_ for t_ in range(4)])


def build_program(shapes, nseq=2, plan=None, loff=0, hoff=0):
    nc = bass.Bass("TRN2", target_bir_lowering=False)
    W = Wts(nc, shapes, loff, hoff)
    out = nc.dram_tensor("out", [nseq, SEQ, D], F32, kind="ExternalOutput").ap()
    C = Ctx(nc)
    S = C.S
    if plan is None:
        plan = [(l, ph) for l in range(DEPTH) for ph in ("ffn1", "mix", "xa", "ffn2")]
    need_mem = any(ph == "xa" for _, ph in plan)
    with ExitStack() as es:
        K = make_consts(C, es)
        P = build_params(C, es, W, K["identf"])
        x = C.sb(es, [128, NT, D], F32, "xres")
        memT = C.sb(es, [128, 8, MEM], BF16, "memT") if need_mem else None
        for s in range(nseq):
            for t4 in range(NT // 4):
                S.dma("sp", x[:, t4 * 4:(t4 + 1) * 4, :],
                      W["x"][s, t4 * 512:(t4 + 1) * 512, :].rearrange("(t p) d -> p t d", p=128),
                      writes=[x.part(t4 * 4 + i) for i in range(4)])
            if need_mem:
                prep_mem(C, es, W, P, K, s, memT)
            for (l, ph) in plan:
                if ph == "ffn1":
                    ffn_block(C, x, l, 0, W, P, K["ident"])
                elif ph == "ffn2":
                    ffn_block(C, x, l, 1, W, P, K["ident"])
                elif ph == "xa":
                    xattn_block(C, x, l, W, P, K, memT)
                elif ph == "mix":
                    if l % 2 == 0:
                        even_mixer(C, x, l, W, P, K)
                    else:
                        odd_mixer(C, x, l, W, P, K)
            for t4 in range(NT // 4):
                S.dma("sp", out[s, t4 * 512:(t4 + 1) * 512, :].rearrange("(t p) d -> p t d", p=128),
                      x[:, t4 * 4:(t4 + 1) * 4, :], reads=[x.part(t4 * 4 + i) for i in range(4)])
            S.barrier()
        S.finish()
    return nc, W


def kernel(**inputs):
    n = 8
    arrs = {k: np.ascontiguousarray(np.asarray(v), dtype=np.float32) for k, v in inputs.items()}
    per = arrs["x"].shape[0] // n
    shapes = {}
    for k, a in arrs.items():
        shapes[k] = ((per,) + a.shape[1:]) if k in ("x", "mem") else a.shape
    nc, W = build_program(shapes, nseq=per)
    in_maps = []
    for c in range(n):
        m = {}
        for k in W.aps:
            a = arrs[k]
            m[k] = a[c * per:(c + 1) * per] if k in ("x", "mem") else a
        in_maps.append(m)
    res = run_bass_kernel_spmd(nc, in_maps, core_ids=list(range(n)))
    return np.concatenate([r["out"] for r in res.results], axis=0).astype(np.float32)
```

```python
import numpy as np
import concourse.bass as bass
import concourse.mybir as mybir
from concourse.bass_utils import run_bass_kernel_spmd

F32 = mybir.dt.float32
BF16 = mybir.dt.bfloat16
ALU = mybir.AluOpType
AF = mybir.ActivationFunctionType
AX = mybir.AxisListType

D = 1024
SEQ = 2048
NT = SEQ // 128
DEPTH = 4
DFF = 2816
NFC = DFF // 128
MEM = 256
EPS = 1e-6


class Res:
    __slots__ = ("name", "writer", "readers", "parent", "parts", "psum")

    def __init__(self, name, parent=None):
        self.name = name
        self.writer = None
        self.readers = {}
        self.parent = parent
        self.parts = {}
        self.psum = parent.psum if parent is not None else False

    def part(self, key):
        r = self.parts.get(key)
        if r is None:
            r = Res(f"{self.name}/{key}", parent=self)
            self.parts[key] = r
        return r


class T:
    def __init__(self, h, name):
        self.h = h
        self.res = Res(name)

    def __getitem__(self, k):
        return self.h[k]

    def part(self, key):
        return self.res.part(key)


NDSEM = 16
COMPUTE = ("pe", "act", "dve", "pool")


class Sched:
    def __init__(self, nc):
        self.nc = nc
        self.eng = {"pe": nc.tensor, "act": nc.scalar, "dve": nc.vector, "pool": nc.gpsimd, "sp": nc.sync}
        self.sem = {}
        self.cnt = {}
        for e in self.eng:
            self.sem[e] = nc.alloc_semaphore("s_" + e)
            self.cnt[e] = 0
        self.dkeys = []
        for q in ("sp", "pool"):
            for i in range(NDSEM):
                k = ("d", q, i)
                self.sem[k] = nc.alloc_semaphore(f"s_d{q}{i}")
                self.cnt[k] = 0
                self.dkeys.append(k)
        self.known = {e: {} for e in self.eng}
        self.dnext = {"sp": 0, "pool": 0}
        self.pe_pending = None
        self.ninstr = 0

    @staticmethod
    def _rlist(r):
        if isinstance(r, T):
            return r.res
        return r

    def _deps(self, reads, writes, eng=None):
        ev = []
        for r in reads:
            r = self._rlist(r)
            if r.writer:
                ev.append(r.writer)
            if r.parent is not None and r.parent.writer:
                ev.append(r.parent.writer)
            for p in r.parts.values():
                if p.writer:
                    ev.append(p.writer)
            if r.psum:
                chain = [r] + list(r.parts.values()) + ([r.parent] if r.parent is not None else [])
                for c in chain:
                    ev.extend((k, v) for k, v in c.readers.items() if k != eng)
        for w in writes:
            w = self._rlist(w)
            chain = [w] + list(w.parts.values())
            if w.parent is not None:
                chain.append(w.parent)
            for c in chain:
                if c.writer:
                    ev.append(c.writer)
                ev.extend(c.readers.items())
        return ev

    def _wait(self, e, evs):
        kn = self.known[e]
        best = {}
        for k, v in evs:
            if k == e and e in ("pe", "sp"):
                continue
            if kn.get(k, 0) >= v:
                continue
            if best.get(k, 0) < v:
                best[k] = v
        for k, v in best.items():
            self.eng[e].wait_ge(self.sem[k], v)
            kn[k] = v

    def _record(self, ev, reads, writes):
        k, v = ev
        for r in reads:
            r = self._rlist(r)
            if r.readers.get(k, 0) < v:
                r.readers[k] = v
        for w in writes:
            w = self._rlist(w)
            w.writer = ev
            w.readers = {}

    def _flush_pe(self):
        if self.pe_pending is not None:
            self.cnt["pe"] += 1
            self.pe_pending.then_inc(self.sem["pe"], 1)
            self.pe_pending = None

    def op(self, e, fn, reads=(), writes=()):
        if e == "pe":
            self._wait(e, self._deps(reads, writes, e))
            ins = fn(self.eng[e])
            self.pe_pending = ins
            self._record((e, self.cnt[e] + 1), reads, writes)
            self.ninstr += 1
            return ins
        self._flush_pe()
        self._wait(e, self._deps(reads, writes, e))
        ins = fn(self.eng[e])
        self.cnt[e] += 1
        ins.then_inc(self.sem[e], 1)
        self._record((e, self.cnt[e]), reads, writes)
        self.ninstr += 1
        return ins

    def pe(self, fn, reads=(), writes=()):
        return self.op("pe", fn, reads, writes)

    def act(self, fn, reads=(), writes=()):
        return self.op("act", fn, reads, writes)

    def dve(self, fn, reads=(), writes=()):
        return self.op("dve", fn, reads, writes)

    def pool(self, fn, reads=(), writes=()):
        return self.op("pool", fn, reads, writes)

    def dma(self, q, out, in_, reads=(), writes=(), **kw):
        self._flush_pe()
        i = self.dnext[q]
        self.dnext[q] = (i + 1) % NDSEM
        k = ("d", q, i)
        evs = self._deps(reads, writes)
        if self.cnt[k]:
            evs.append((k, self.cnt[k]))
        self._wait(q, evs)
        ins = self.eng[q].dma_start(out=out, in_=in_, **kw)
        self.cnt[k] += 16
        ins.then_inc(self.sem[k], 16)
        self._record((k, self.cnt[k]), reads, writes)
        self.ninstr += 1
        return ins

    def barrier(self):
        self._flush_pe()
        evs = [(e, self.cnt[e]) for e in COMPUTE if self.cnt[e]]
        evs += [(k, self.cnt[k]) for k in self.dkeys if self.cnt[k]]
        for e in list(COMPUTE) + ["sp"]:
            self._wait(e, evs)

    def finish(self):
        self._flush_pe()
        evs = [(e, self.cnt[e]) for e in COMPUTE if self.cnt[e]]
        evs += [(k, self.cnt[k]) for k in self.dkeys if self.cnt[k]]
        self._wait("sp", evs)


class Ctx:
    def __init__(self, nc):
        self.nc = nc
        self.S = Sched(nc)
        self._n = 0

    def sb(self, stack, shape, dt, name=None):
        self._n += 1
        name = f"{name or 'sb'}_{self._n}"
        h = stack.enter_context(self.nc.sbuf_tensor(name, list(shape), dt))
        return T(h, name)

    def ps(self, stack, shape, dt=F32, name=None):
        self._n += 1
        name = f"{name or 'ps'}_{self._n}"
        h = stack.enter_context(self.nc.psum_tensor(name, list(shape), dt))
        t = T(h, name)
        t.res.psum = True
        return t


from contextlib import ExitStack


def load_bcast_row(C, q, dst, src_row, n):
    C.S.dma(q, dst[:, 0:n], src_row.partition_broadcast(128), writes=[dst])


def rms_to_T(C, st, x, g_col, xnT, ntiles, ident, tok0=0):
    S = C.S
    with ExitStack() as es:
        ss = C.sb(es, [128, ntiles], F32, "ss")
        rstd = C.sb(es, [128, ntiles], F32, "rstd")
        junk = C.sb(es, [128, D], BF16, "junk")
        xs = [C.sb(es, [128, D], BF16, f"xs{i}") for i in range(2)]
        tps = [C.ps(es, [128, 8, 128], BF16, f"tp{i}") for i in range(2)]
        S.dve(lambda e: e.memset(ss[:, :], 0.0), writes=[ss])
        for t in range(ntiles):
            S.act(lambda e: e.activation(out=junk[:, :], in_=x[:, tok0 + t, :], func=AF.Square,
                                         accum_out=ss[:, t:t + 1]),
                  reads=[x.part(tok0 + t)], writes=[junk, ss])
        rstd_from_ss(C, ss, rstd, ntiles, 1.0 / D, EPS)
        for t in range(ntiles):
            xb = xs[t % 2]
            tp = tps[t % 2]
            S.act(lambda e: e.activation(out=xb[:, :], in_=x[:, tok0 + t, :], func=AF.Copy,
                                         scale=rstd[:, t:t + 1]),
                  reads=[x.part(tok0 + t), rstd], writes=[xb])
            for kc in range(8):
                S.pe(lambda e: e.transpose(out=tp[:, kc, :], in_=xb[:, kc * 128:(kc + 1) * 128], identity=ident[:, :]),
                     reads=[xb, ident], writes=[tp])
            S.dve(lambda e: e.tensor_tensor(out=xnT[:, :, t * 128:(t + 1) * 128], in0=tp[:, :, :],
                                            in1=g_col.unsqueeze(2).to_broadcast([128, 8, 128]), op=ALU.mult),
                  reads=[tp], writes=[xnT.part(t)])
        S.barrier()


def rstd_from_ss(C, ss, rstd, n, scale, eps):
    S = C.S
    S.dve(lambda e: e.tensor_scalar(out=rstd[:, 0:n], in0=ss[:, 0:n], scalar1=scale, scalar2=eps,
                                    op0=ALU.mult, op1=ALU.add), reads=[ss], writes=[rstd])
    S.act(lambda e: e.activation(out=rstd[:, 0:n], in_=rstd[:, 0:n], func=AF.Sqrt), reads=[rstd], writes=[rstd])
    S.dve(lambda e: e.reciprocal(out=rstd[:, 0:n], in_=rstd[:, 0:n]), reads=[rstd], writes=[rstd])


def post_norm_residual(C, st, x, tile_idx, y_ps, g_bc, coef, scr):
    S = C.S
    ss, rstd, junk, tmp = scr
    S.dve(lambda e: e.memset(ss[:, 0:2], 0.0), writes=[ss])
    for h in range(2):
        S.act(lambda e: e.activation(out=junk[:, 0:512], in_=y_ps[h][:, :], func=AF.Square,
                                     accum_out=ss[:, h:h + 1]), reads=[y_ps[h]], writes=[junk, ss])
    S.dve(lambda e: e.tensor_tensor(out=ss[:, 2:3], in0=ss[:, 0:1], in1=ss[:, 1:2], op=ALU.add), reads=[ss], writes=[ss])
    S.dve(lambda e: e.tensor_scalar(out=rstd[:, 0:1], in0=ss[:, 2:3], scalar1=1.0 / D, scalar2=EPS,
                                    op0=ALU.mult, op1=ALU.add), reads=[ss], writes=[rstd])
    S.act(lambda e: e.activation(out=rstd[:, 0:1], in_=rstd[:, 0:1], func=AF.Sqrt), reads=[rstd], writes=[rstd])
    S.dve(lambda e: e.reciprocal(out=rstd[:, 0:1], in_=rstd[:, 0:1]), reads=[rstd], writes=[rstd])
    if coef != 1.0:
        S.dve(lambda e: e.tensor_scalar(out=rstd[:, 0:1], in0=rstd[:, 0:1], scalar1=float(coef), scalar2=None,
                                        op0=ALU.mult), reads=[rstd], writes=[rstd])
    for h in range(2):
        sl = slice(h * 512, (h + 1) * 512)
        S.dve(lambda e: e.tensor_tensor(out=tmp[:, sl], in0=y_ps[h][:, :], in1=g_bc[:, sl], op=ALU.mult),
              reads=[y_ps[h], g_bc], writes=[tmp])
        S.dve(lambda e: e.scalar_tensor_tensor(out=x[:, tile_idx, sl], in0=tmp[:, sl], scalar=rstd[:, 0:1],
                                               in1=x[:, tile_idx, sl], op0=ALU.mult, op1=ALU.add),
              reads=[tmp, rstd, x.part(tile_idx)], writes=[x.part(tile_idx)])


def ffn_block(C, x, l, j, W, P, ident):
    S = C.S
    nc = C.nc
    n_in = 0 if j == 0 else 6
    n_out = 1 if j == 0 else 7
    wg = W.L("ffn_w_gate", l)[j].rearrange("(kc p) f -> p kc f", p=128)
    wu = W.L("ffn_w_up", l)[j].rearrange("(kc p) f -> p kc f", p=128)
    wd = W.L("ffn_w_down", l)[j].rearrange("(fc p) d -> p fc d", p=128)
    TG = 1024
    with ExitStack() as es:
        g_bc = C.sb(es, [128, D], F32, "g_bc")
        load_bcast_row(C, "sp", g_bc, W.L("norm_g", l)[n_out], D)
        hT = C.sb(es, [128, NFC, TG], BF16, "hT")
        for tg in range(SEQ // TG):
            with ExitStack() as es2:
                xnT = C.sb(es2, [128, 8, TG], BF16, "xnT")
                rms_to_T(C, es2, x, P.cols(("norm_g", l, n_in)), xnT, TG // 128, ident,
                         tok0=tg * (TG // 128))
                wbuf = [(C.sb(es2, [128, 8, 256], BF16, f"wg{i}"), C.sb(es2, [128, 8, 256], BF16, f"wu{i}")) for i in range(2)]
                sg = [C.sb(es2, [128, 512], BF16, f"sg{i}") for i in range(2)]
                gps = [C.ps(es2, [128, 512], F32, f"gps{i}") for i in range(2)]
                ups = [C.ps(es2, [128, 512], F32, f"ups{i}") for i in range(2)]
                it = 0
                for f2 in range(NFC // 2):
                    wgt, wut = wbuf[f2 % 2]
                    S.dma("pool", wgt[:, :, :], wg[:, :, f2 * 256:(f2 + 1) * 256], writes=[wgt])
                    S.dma("pool", wut[:, :, :], wu[:, :, f2 * 256:(f2 + 1) * 256], writes=[wut])
                    for fi in range(2):
                        fc = f2 * 2 + fi
                        for th in range(TG // 512):
                            gp, up, sgt = gps[it % 2], ups[it % 2], sg[it % 2]
                            it += 1
                            for kc in range(8):
                                S.pe(lambda e: e.matmul(out=gp[:, :], lhsT=wgt[:, kc, fi * 128:(fi + 1) * 128],
                                                        rhs=xnT[:, kc, th * 512:(th + 1) * 512],
                                                        start=(kc == 0), stop=(kc == 7)),
                                     reads=[wgt, xnT], writes=[gp])
                            for kc in range(8):
                                S.pe(lambda e: e.matmul(out=up[:, :], lhsT=wut[:, kc, fi * 128:(fi + 1) * 128],
                                                        rhs=xnT[:, kc, th * 512:(th + 1) * 512],
                                                        start=(kc == 0), stop=(kc == 7)),
                                     reads=[wut, xnT], writes=[up])
                            S.act(lambda e: e.activation(out=sgt[:, :], in_=gp[:, :], func=AF.Silu),
                                  reads=[gp], writes=[sgt])
                            S.dve(lambda e: e.tensor_tensor(out=hT[:, fc, th * 512:(th + 1) * 512], in0=up[:, :],
                                                            in1=sgt[:, :], op=ALU.mult),
                                  reads=[up, sgt], writes=[hT.part((fc, th))])
                S.barrier()
            with ExitStack() as es3:
                wdt = C.sb(es3, [128, NFC, D], BF16, "wd")
                for f2 in range(NFC // 2):
                    S.dma("pool", wdt[:, f2 * 2:f2 * 2 + 2, :], wd[:, f2 * 2:f2 * 2 + 2, :], writes=[wdt.part(f2)])
                yps = [[C.ps(es3, [128, 512], F32, f"y{i}{h}") for h in range(2)] for i in range(2)]
                scr = (C.sb(es3, [128, 4], F32, "pss"), C.sb(es3, [128, 2], F32, "prs"),
                       C.sb(es3, [128, 512], BF16, "pjunk"), C.sb(es3, [128, D], F32, "ptmp"))
                for tt in range(TG // 128):
                    yp = yps[tt % 2]
                    for h in range(2):
                        for fc in range(NFC):
                            S.pe(lambda e: e.matmul(out=yp[h][:, :], lhsT=hT[:, fc, tt * 128:(tt + 1) * 128],
                                                    rhs=wdt[:, fc, h * 512:(h + 1) * 512],
                                                    start=(fc == 0), stop=(fc == NFC - 1)),
                                 reads=[hT, wdt.part(fc // 2)], writes=[yp[h]])
                    post_norm_residual(C, es3, x, tg * (TG // 128) + tt, yp, g_bc, 0.5, scr)
                S.barrier()


DBG = {}


class Wts:
    def __init__(self, nc, shapes, loff=0, hoff=0):
        self.nc = nc
        self.shapes = shapes
        self.aps = {}
        self.loff = loff
        self.hoff = hoff

    def __getitem__(self, name):
        if name not in self.aps:
            self.aps[name] = self.nc.dram_tensor(name, list(self.shapes[name]), F32, kind="ExternalInput").ap()
        return self.aps[name]

    def L(self, name, l):
        return self[name][l - self.loff]

    def H(self, name, i):
        return self[name][i - self.hoff]

    def hasL(self, name, l):
        return 0 <= l - self.loff < self.shapes[name][0]

    def hasH(self, name, i):
        return 0 <= i - self.hoff < self.shapes[name][0]


class Params:
    def __init__(self):
        self.rows = {}
        self.t = None

    def cols(self, key, kc0=0, n=8):
        r = self.rows[key]
        return self.t[:, kc0:kc0 + n, r]

    def col(self, key, kc):
        r = self.rows[key]
        return self.t[:, kc, r:r + 1]


def build_params(C, es, W, identf):
    S = C.S
    P = Params()
    rows = []
    for l in range(DEPTH):
        if W.hasL("norm_g", l):
            for n in range(8):
                rows.append((("norm_g", l, n), W.L("norm_g", l)[n], D))
    rows.append((("mem_g",), W["mem_norm_g"], D))
    for i in range(2):
        if "sc_conv_w" in W.shapes and W.hasH("sc_conv_w", i):
            for k in range(3):
                rows.append((("sc_w", i, k), W.H("sc_conv_w", i)[k], 512))
            rows.append((("sc_b", i), W.H("sc_conv_b", i), 512))
            rows.append((("dsa_qg", i), W.H("dsa_q_norm_g", i), 256))
        if "rw_w0" in W.shapes and W.hasH("rw_w0", i):
            for nm in ("rw_w0", "rw_a0", "rw_k_k", "rw_k_a", "rw_ln_g", "rw_ln_b", "rt_gn_g", "rt_gn_b"):
                rows.append(((nm, i), W.H(nm, i), 512))
            rows.append((("rw_r_k", i), W.H("rw_r_k", i).rearrange("h d -> (h d)"), 512))
            rows.append((("rw_mu", i, 0), W.H("rw_mu", i)[0:1024], 1024))
            rows.append((("rw_mu", i, 1), W.H("rw_mu", i)[1024:1792], 768))
    nr = len(rows)
    assert nr <= 128
    PC = C.sb(es, [128, 8, nr], F32, "PC")
    P.t = PC
    with ExitStack() as e1:
        raw = C.sb(e1, [128, D], F32, "praw")
        S.pool(lambda e: e.memset(raw[:, :], 0.0), writes=[raw])
        for r, (key, ap, n) in enumerate(rows):
            P.rows[key] = r
            S.dma("sp", raw[r:r + 1, 0:n], ap.rearrange("(o n) -> o n", o=1), writes=[raw])
        tp = C.ps(e1, [128, 8, 128], F32, "ptp")
        for kc in range(8):
            S.pe(lambda e: e.transpose(out=tp[:, kc, :], in_=raw[:, kc * 128:(kc + 1) * 128], identity=identf[:, :]),
                 reads=[raw, identf], writes=[tp])
        S.dve(lambda e: e.tensor_copy(out=PC[:, :, :], in_=tp[:, :, 0:nr]), reads=[tp], writes=[PC])
        S.barrier()
    return P


def load_w(C, dst, src2d, c0, c1, q="pool"):
    v = src2d.rearrange("(kc p) n -> p kc n", p=128)
    nk = v.shape[1]
    for kc in range(nk):
        C.S.dma(q, dst[:, kc, 0:c1 - c0], v[:, kc, c0:c1], writes=[dst.part(kc)])


def make_consts(C, es):
    S = C.S
    K = {}
    ones_f = C.sb(es, [128, 512], F32, "ones_f")
    S.pool(lambda e: e.memset(ones_f[:, :], 1.0), writes=[ones_f])
    ident = C.sb(es, [128, 128], BF16, "ident")
    identf = C.sb(es, [128, 128], F32, "identf")
    for t in (ident, identf):
        S.pool(lambda e: e.affine_select(out=t[:, :], in_=ones_f[:, 0:128], pattern=[[-1, 128]],
                                         compare_op=ALU.is_equal, fill=0.0, base=0, channel_multiplier=1),
               reads=[ones_f], writes=[t])
    ones_bf = C.sb(es, [128, 128], BF16, "ones_bf")
    S.pool(lambda e: e.memset(ones_bf[:, :], 1.0), writes=[ones_bf])
    selq = C.sb(es, [128, 8, 8], BF16, "selq")
    S.pool(lambda e: e.affine_select(out=selq[:, :, :], in_=ones_f[:, 0:64].rearrange("p (a b) -> p a b", a=8),
                                     pattern=[[1, 8], [-1, 8]], compare_op=ALU.is_equal, fill=0.0, base=0,
                                     channel_multiplier=0), reads=[ones_f], writes=[selq])
    sel8 = C.sb(es, [8, 8, 128], BF16, "sel8")
    with ExitStack() as e0:
        sel8a = C.sb(e0, [8, 8, 128], F32, "sel8a")
        S.pool(lambda e: e.memset(sel8a[:, :, :], 1.0), writes=[sel8a])
        S.pool(lambda e: e.affine_select(out=sel8[:, :, :], in_=sel8a[:, :, :], pattern=[[-1, 8], [0, 128]],
                                         compare_op=ALU.is_equal, fill=0.0, base=0, channel_multiplier=1),
               reads=[sel8a], writes=[sel8])
        S.barrier()
    zer = C.sb(es, [128, 128], F32, "zer")
    S.pool(lambda e: e.memset(zer[:, :], 0.0), writes=[zer])
    cbias = C.sb(es, [128, 128], F32, "cbias")
    S.pool(lambda e: e.affine_select(out=cbias[:, :], in_=zer[:, :], pattern=[[-1, 128]],
                                     compare_op=ALU.is_ge, fill=-1e30, base=0, channel_multiplier=1),
           reads=[zer], writes=[cbias])
    K.update(ones_f=ones_f, ident=ident, identf=identf, ones_bf=ones_bf, selq=selq, sel8=sel8, cbias=cbias, zer=zer)
    return K


def make_negm(C, es, qT, nh, k2m, K, p8):
    S = C.S
    qsq = C.sb(es, [128, nh, 512], BF16, "qsq")
    S.act(lambda e: e.activation(out=qsq[:, :, :], in_=qT[:, 0:nh, :], func=AF.Square), reads=[qT], writes=[qsq])
    for h in range(nh):
        S.pe(lambda e: e.matmul(out=p8[0:8, :], lhsT=K["selq"][:, h, :], rhs=qsq[:, h, :], start=(h == 0), stop=(h == nh - 1)),
             reads=[K["selq"], qsq], writes=[p8])
    nm = C.sb(es, [8, 512], F32, "nm")
    negm8 = C.sb(es, [8, 512], BF16, "negm8")
    S.dve(lambda e: e.tensor_scalar(out=nm[:, :], in0=p8[0:8, :], scalar1=k2m[:, 0:1], scalar2=None, op0=ALU.mult),
          reads=[p8, k2m], writes=[nm])
    S.act(lambda e: e.activation(out=nm[:, :], in_=nm[:, :], func=AF.Sqrt), reads=[nm], writes=[nm])
    S.dve(lambda e: e.tensor_scalar(out=negm8[:, :], in0=nm[:, :], scalar1=-1.0, scalar2=None, op0=ALU.mult),
          reads=[nm], writes=[negm8])
    return negm8


def attn_core(C, K, qT_ap, q_res, ktiles, negm8, h, scale, out_ap, out_res, dv, ps_s, ps_o, ps_r, pts, rinv, mask_eng="pool"):
    S = C.S
    n = len(ktiles)
    for i, (k_ap, v_ap, m_ap, rd) in enumerate(ktiles):
        sp = ps_s[i % 2]
        pt = pts[i % 2]
        S.pe(lambda e: e.matmul(out=sp[:, :], lhsT=k_ap, rhs=qT_ap, start=True, stop=False), reads=rd + [q_res], writes=[sp])
        S.pe(lambda e: e.matmul(out=sp[:, :], lhsT=K["sel8"][:, h, :], rhs=negm8[:, :], start=False, stop=(m_ap is None)),
             reads=[K["sel8"], negm8], writes=[sp])
        if m_ap is not None:
            S.pe(lambda e: e.matmul(out=sp[:, :], lhsT=K["ident"][:, :], rhs=m_ap, start=False, stop=True),
                 reads=rd + [K["ident"]], writes=[sp])
        S.act(lambda e: e.activation(out=pt[:, :], in_=sp[:, :], func=AF.Exp, scale=float(scale)), reads=[sp], writes=[pt])
        S.pe(lambda e: e.matmul(out=ps_o[0:dv, :], lhsT=v_ap, rhs=pt[:, :], start=(i == 0), stop=(i == n - 1)),
             reads=rd + [pt], writes=[ps_o])
        S.pe(lambda e: e.matmul(out=ps_r[0:dv, :], lhsT=K["ones_bf"][:, 0:dv], rhs=pt[:, :], start=(i == 0), stop=(i == n - 1)),
             reads=[K["ones_bf"], pt], writes=[ps_r])
    S.dve(lambda e: e.reciprocal(out=rinv[0:dv, :], in_=ps_r[0:dv, :]), reads=[ps_r], writes=[rinv])
    S.dve(lambda e: e.tensor_tensor(out=out_ap, in0=ps_o[0:dv, :], in1=rinv[0:dv, :], op=ALU.mult),
          reads=[ps_o, rinv], writes=[out_res])


def out_proj_residual(C, x, chunks, w_T, g_bc, coef, tiles):
    S = C.S
    n = len(chunks)
    with ExitStack() as es:
        yps = [[C.ps(es, [128, 512], F32, f"y{i}{h}") for h in range(2)] for i in range(2)]
        scr = (C.sb(es, [128, 4], F32, "pss"), C.sb(es, [128, 2], F32, "prs"),
               C.sb(es, [128, 512], BF16, "pjunk"), C.sb(es, [128, D], F32, "ptmp"))
        for ti, tt in enumerate(tiles):
            yp = yps[ti % 2]
            for h in range(2):
                for c, (yt, f) in enumerate(chunks):
                    S.pe(lambda e: e.matmul(out=yp[h][:, :], lhsT=f(ti), rhs=w_T[:, c, h * 512:(h + 1) * 512],
                                            start=(c == 0), stop=(c == n - 1)), reads=[yt, w_T], writes=[yp[h]])
            post_norm_residual(C, None, x, tt, yp, g_bc, coef, scr)
        S.barrier()


def xattn_block(C, x, l, W, P, K, memT):
    S = C.S
    ident = K["ident"]
    scale = 128 ** -0.5
    with ExitStack() as es:
        g_bc = C.sb(es, [128, D], F32, "g_bc")
        load_bcast_row(C, "sp", g_bc, W.L("norm_g", l)[5], D)
        wq = C.sb(es, [128, 8, 512], BF16, "wq")
        wk = C.sb(es, [128, 8, 512], BF16, "wk")
        wv = C.sb(es, [128, 8, 512], BF16, "wv")
        wo = C.sb(es, [128, 4, D], BF16, "wo")
        load_w(C, wk, W.L("xa_wk", l), 0, 512)
        load_w(C, wv, W.L("xa_wv", l), 0, 512)
        load_w(C, wq, W.L("xa_wq", l), 0, 512)
        wov = W.L("xa_wo", l).rearrange("(c p) d -> p c d", p=128)
        for c in range(4):
            S.dma("pool", wo[:, c, :], wov[:, c, :], writes=[wo.part(c)])
        kT = C.sb(es, [128, 4, MEM], BF16, "kT")
        vtok = C.sb(es, [128, 2, 512], BF16, "vtok")
        k2m = C.sb(es, [8, 1], F32, "k2m")
        with ExitStack() as e1:
            pk = C.ps(e1, [128, 512], F32, "pk")
            for h in range(4):
                for kc in range(8):
                    S.pe(lambda e: e.matmul(out=pk[:, 0:MEM], lhsT=wk[:, kc, h * 128:(h + 1) * 128], rhs=memT[:, kc, :],
                                            start=(kc == 0), stop=(kc == 7)), reads=[wk, memT], writes=[pk])
                S.act(lambda e: e.copy(out=kT[:, h, :], in_=pk[:, 0:MEM]), reads=[pk], writes=[kT])
            for mt in range(2):
                for kc in range(8):
                    S.pe(lambda e: e.matmul(out=pk[:, :], lhsT=memT[:, kc, mt * 128:(mt + 1) * 128], rhs=wv[:, kc, :],
                                            start=(kc == 0), stop=(kc == 7)), reads=[wv, memT], writes=[pk])
                S.dve(lambda e: e.tensor_copy(out=vtok[:, mt, :], in_=pk[:, :]), reads=[pk], writes=[vtok])
            ksq = C.sb(e1, [128, 4, MEM], BF16, "ksq")
            S.act(lambda e: e.activation(out=ksq[:, :, :], in_=kT[:, :, :], func=AF.Square), reads=[kT], writes=[ksq])
            for h in range(4):
                S.pe(lambda e: e.matmul(out=pk[0:8, 0:MEM], lhsT=K["selq"][:, h, :], rhs=ksq[:, h, :], start=(h == 0), stop=(h == 3)),
                     reads=[K["selq"], ksq], writes=[pk])
            S.dve(lambda e: e.reduce_max(out=k2m[:, 0:1], in_=pk[0:8, 0:MEM], axis=AX.X), reads=[pk], writes=[k2m])
            S.barrier()
        for qg in range(4):
            with ExitStack() as e2:
                oT = C.sb(e2, [128, 4, 512], BF16, "oT")
                with ExitStack() as e3:
                    xnT = C.sb(e3, [128, 8, 512], BF16, "xnT")
                    rms_to_T(C, e3, x, P.cols(("norm_g", l, 4)), xnT, 4, ident, tok0=qg * 4)
                    qT = C.sb(e3, [128, 4, 512], BF16, "qT")
                    pq = [C.ps(e3, [128, 512], F32, f"pq{i}") for i in range(2)]
                    for h in range(4):
                        for kc in range(8):
                            S.pe(lambda e: e.matmul(out=pq[h % 2][:, :], lhsT=wq[:, kc, h * 128:(h + 1) * 128], rhs=xnT[:, kc, :],
                                                    start=(kc == 0), stop=(kc == 7)), reads=[wq, xnT], writes=[pq[h % 2]])
                        S.act(lambda e: e.copy(out=qT[:, h, :], in_=pq[h % 2][:, :]), reads=[pq[h % 2]], writes=[qT.part(h)])
                    negm8 = make_negm(C, e3, qT, 4, k2m, K, pq[0])
                    ps_s = [C.ps(e3, [128, 512], F32, f"ss{i}") for i in range(2)]
                    ps_o = C.ps(e3, [128, 512], F32, "pso")
                    ps_r = C.ps(e3, [128, 512], F32, "psr")
                    pts = [C.sb(e3, [128, 512], BF16, f"pt{i}") for i in range(2)]
                    rinv = C.sb(e3, [128, 512], F32, "rinv")
                    for h in range(4):
                        kt_list = [(kT[:, h, kt * 128:(kt + 1) * 128], vtok[:, kt, h * 128:(h + 1) * 128], None, [kT, vtok])
                                   for kt in range(2)]
                        attn_core(C, K, qT[:, h, :], qT, kt_list, negm8, h, scale, oT[:, h, :], oT.part(h), 128,
                                  ps_s, ps_o, ps_r, pts, rinv)
                    S.barrier()
                out_proj_residual(C, x, [(oT, (lambda ti, c=c: oT[:, c, ti * 128:(ti + 1) * 128])) for c in range(4)],
                                  wo, g_bc, 1.0, [qg * 4 + i for i in range(4)])


def prep_mem(C, es, W, P, K, s, memT):
    S = C.S
    with ExitStack() as e1:
        mt = C.sb(e1, [128, 2, D], F32, "memraw")
        S.dma("sp", mt[:, :, :], W["mem"][s].rearrange("(t p) d -> p t d", p=128), writes=[mt.part(0), mt.part(1)])
        rms_to_T(C, e1, mt, P.cols(("mem_g",)), memT, 2, K["ident"], tok0=0)


def odd_mixer(C, x, l, W, P, K):
    S = C.S
    i = l // 2
    ident = K["ident"]
    w_in = W.H("od_w_in", i)
    NEG_SEL = -3.0e38
    with ExitStack() as es:
        g_bc = C.sb(es, [128, D], F32, "g_bc")
        load_bcast_row(C, "sp", g_bc, W.L("norm_g", l)[3], D)
        ycT = C.sb(es, [128, 4, SEQ], BF16, "ycT")
        ydT = C.sb(es, [128, 4, SEQ], BF16, "ydT")
        cqnT = C.sb(es, [128, 2, SEQ], BF16, "cqnT")
        ckvT = C.sb(es, [128, SEQ], BF16, "ckvT")
        ckvtok = C.sb(es, [128, NT, 128], BF16, "ckvtok")
        kidxT2 = C.sb(es, [128, SEQ], BF16, "kidxT2")
        widx = C.sb(es, [128, NT, 8], F32, "widx")
        absw = C.sb(es, [128, NT, 8], F32, "absw")
        sgnw = C.sb(es, [128, NT, 8], F32, "sgnw")
        carry = C.sb(es, [128, 4, 2], F32, "carry")
        S.pool(lambda e: e.memset(carry[:, :, :], 0.0), writes=[carry])
        gkv_bc = C.sb(es, [128, 128], F32, "gkv_bc")
        load_bcast_row(C, "sp", gkv_bc, W.H("dsa_kv_norm_g", i), 128)
        TG = 1024
        with ExitStack() as e1:
            wsm = C.sb(e1, [128, 8, 456], BF16, "wsm")
            load_w(C, wsm, w_in, 0, 456)
            wscs = [C.sb(e1, [128, 8, 3, 128], BF16, f"wsc{k}") for k in range(2)]
            ss = C.sb(e1, [128, 2], F32, "ss2")
            rs = C.sb(e1, [128, 2], F32, "rs2")
            junk = C.sb(e1, [128, 256], BF16, "junk2")
            cqs = C.sb(e1, [128, 256], BF16, "cqs")
            kid2 = C.sb(e1, [128, 2, 64], BF16, "kid2")
            hs = C.sb(e1, [128, 512], F32, "hs")
            ub = C.sb(e1, [128, 514], F32, "ub")
            yb = C.sb(e1, [128, 512], F32, "yb")
            w_v = w_in.rearrange("(kc p) n -> p kc n", p=128)
            for tg in range(SEQ // TG):
                with ExitStack() as e2:
                    xnT = C.sb(e2, [128, 8, TG], BF16, "xnT")
                    rms_to_T(C, e2, x, P.cols(("norm_g", l, 2)), xnT, TG // 128, ident, tok0=tg * (TG // 128))
                    pp = C.ps(e2, [128, 512], F32, "pp")
                    tp = C.ps(e2, [128, 8, 128], BF16, "tp4")
                    for tt in range(TG // 128 if DBG.get("odd_stop") != 0.25 else 0):
                        Tt = tg * (TG // 128) + tt
                        for kc in range(8):
                            S.pe(lambda e: e.matmul(out=pp[:, 0:456], lhsT=xnT[:, kc, tt * 128:(tt + 1) * 128], rhs=wsm[:, kc, 0:456],
                                                    start=(kc == 0), stop=(kc == 7)), reads=[xnT, wsm], writes=[pp])
                        S.dve(lambda e: e.memset(ss[:, :], 0.0), writes=[ss])
                        S.act(lambda e: e.activation(out=junk[:, 0:256], in_=pp[:, 0:256], func=AF.Square, accum_out=ss[:, 0:1]),
                              reads=[pp], writes=[junk, ss])
                        S.act(lambda e: e.activation(out=junk[:, 0:128], in_=pp[:, 256:384], func=AF.Square, accum_out=ss[:, 1:2]),
                              reads=[pp], writes=[junk, ss])
                        S.dve(lambda e: e.tensor_scalar(out=rs[:, 0:1], in0=ss[:, 0:1], scalar1=1.0 / 256, scalar2=EPS,
                                                        op0=ALU.mult, op1=ALU.add), reads=[ss], writes=[rs])
                        S.dve(lambda e: e.tensor_scalar(out=rs[:, 1:2], in0=ss[:, 1:2], scalar1=1.0 / 128, scalar2=EPS,
                                                        op0=ALU.mult, op1=ALU.add), reads=[ss], writes=[rs])
                        S.act(lambda e: e.activation(out=rs[:, :], in_=rs[:, :], func=AF.Sqrt), reads=[rs], writes=[rs])
                        S.dve(lambda e: e.reciprocal(out=rs[:, :], in_=rs[:, :]), reads=[rs], writes=[rs])
                        S.act(lambda e: e.activation(out=cqs[:, :], in_=pp[:, 0:256], func=AF.Copy, scale=rs[:, 0:1]),
                              reads=[pp, rs], writes=[cqs])
                        S.dve(lambda e: e.scalar_tensor_tensor(out=ckvtok[:, Tt, :], in0=pp[:, 256:384], scalar=rs[:, 1:2],
                                                               in1=gkv_bc[:, :], op0=ALU.mult, op1=ALU.mult),
                              reads=[pp, rs, gkv_bc], writes=[ckvtok.part(Tt)])
                        S.dve(lambda e: e.tensor_copy(out=kid2[:, :, :], in_=pp[:, 384:448].unsqueeze(1).to_broadcast([128, 2, 64])),
                              reads=[pp], writes=[kid2])
                        S.dve(lambda e: e.tensor_copy(out=widx[:, Tt, :], in_=pp[:, 448:456]), reads=[pp], writes=[widx.part(Tt)])
                        for c in range(2):
                            S.pe(lambda e: e.transpose(out=tp[:, c, :], in_=cqs[:, c * 128:(c + 1) * 128], identity=ident[:, :]),
                                 reads=[cqs, ident], writes=[tp])
                        S.pe(lambda e: e.transpose(out=tp[:, 2, :], in_=ckvtok[:, Tt, :], identity=ident[:, :]),
                             reads=[ckvtok.part(Tt), ident], writes=[tp])
                        S.pe(lambda e: e.transpose(out=tp[:, 3, :], in_=kid2[:, :, :].rearrange("p a b -> p (a b)"), identity=ident[:, :]),
                             reads=[kid2, ident], writes=[tp])
                        tsl = slice(Tt * 128, (Tt + 1) * 128)
                        S.dve(lambda e: e.tensor_tensor(out=cqnT[:, :, tsl], in0=tp[:, 0:2, :],
                                                        in1=P.cols(("dsa_qg", i), 0, 2).unsqueeze(2).to_broadcast([128, 2, 128]),
                                                        op=ALU.mult), reads=[tp, P.t], writes=[cqnT.part(Tt)])
                        S.act(lambda e: e.copy(out=ckvT[:, tsl], in_=tp[:, 2, :]), reads=[tp], writes=[ckvT.part(Tt)])
                        S.act(lambda e: e.copy(out=kidxT2[:, tsl], in_=tp[:, 3, :]), reads=[tp], writes=[kidxT2.part(Tt)])
                    pcs = [C.ps(e2, [128, 512], F32, f"pc{k}") for k in range(3)]
                    for fc in range(4 if DBG.get("odd_stop") != 0.5 else 0):
                        wsc = wscs[fc % 2]
                        for kc in range(8):
                            S.dma("pool", wsc[:, kc, :, :],
                                  w_v[:, kc, 456:1992].rearrange("p (j c) -> p j c", j=3)[:, :, fc * 128:(fc + 1) * 128],
                                  writes=[wsc.part(kc)])
                        for th in range(TG // 512):
                            tok0 = tg * TG + th * 512
                            for j3 in range(3):
                                for kc in range(8):
                                    S.pe(lambda e: e.matmul(out=pcs[j3][:, :], lhsT=wsc[:, kc, j3, :], rhs=xnT[:, kc, th * 512:(th + 1) * 512],
                                                            start=(kc == 0), stop=(kc == 7)), reads=[wsc, xnT], writes=[pcs[j3]])
                            S.act(lambda e: e.copy(out=hs[:, :], in_=pcs[0][:, :]), reads=[pcs[0]], writes=[hs])
                            S.pool(lambda e: e.tensor_copy(out=ub[:, 0:2], in_=carry[:, fc, :]), reads=[carry.part(fc)], writes=[ub])
                            S.dve(lambda e: e.tensor_tensor(out=ub[:, 2:514], in0=pcs[2][:, :], in1=hs[:, :], op=ALU.mult),
                                  reads=[pcs[2], hs], writes=[ub])
                            S.pool(lambda e: e.tensor_copy(out=carry[:, fc, :], in_=ub[:, 512:514]), reads=[ub], writes=[carry.part(fc)])
                            S.pool(lambda e: e.tensor_scalar(out=yb[:, :], in0=ub[:, 2:514], scalar1=P.col(("sc_w", i, 2), fc),
                                                             scalar2=P.col(("sc_b", i), fc), op0=ALU.mult, op1=ALU.add),
                                   reads=[ub, P.t], writes=[yb])
                            S.dve(lambda e: e.scalar_tensor_tensor(out=yb[:, :], in0=ub[:, 1:513], scalar=P.col(("sc_w", i, 1), fc),
                                                                   in1=yb[:, :], op0=ALU.mult, op1=ALU.add), reads=[ub, yb, P.t], writes=[yb])
                            S.dve(lambda e: e.scalar_tensor_tensor(out=yb[:, :], in0=ub[:, 0:512], scalar=P.col(("sc_w", i, 0), fc),
                                                                   in1=yb[:, :], op0=ALU.mult, op1=ALU.add), reads=[ub, yb, P.t], writes=[yb])
                            S.dve(lambda e: e.tensor_tensor(out=ydT[:, fc, tok0:tok0 + 512], in0=pcs[1][:, :], in1=yb[:, :], op=ALU.mult),
                                  reads=[pcs[1], yb], writes=[ydT.part((fc, tok0))])
                    S.barrier()
            S.act(lambda e: e.activation(out=absw[:, :, :], in_=widx[:, :, :], func=AF.Abs), reads=[widx], writes=[absw])
            S.act(lambda e: e.activation(out=sgnw[:, :, :], in_=widx[:, :, :], func=AF.Sign), reads=[widx], writes=[sgnw])
            S.barrier()
        if DBG.get("odd_stop") in (1, 0.5, 0.25):
            return
        with ExitStack() as e1:
            wqi = C.sb(e1, [128, 2, 512], BF16, "wqi")
            wuq = C.sb(e1, [128, 2, 512], BF16, "wuq")
            wuk = C.sb(e1, [128, 4, 128], BF16, "wuk")
            wuv = C.sb(e1, [128, 8, 64], BF16, "wuv")
            load_w(C, wqi, W.H("dsa_w_qi", i).rearrange("r h d -> r (h d)"), 0, 512)
            load_w(C, wuq, W.H("dsa_w_uq", i).rearrange("r h d -> r (h d)"), 0, 512)
            S.dma("pool", wuk[:, :, :], W.H("dsa_w_uk", i).rearrange("(hp e) d c -> (e d) hp c", e=2), writes=[wuk])
            S.dma("pool", wuv[:, :, :], W.H("dsa_w_uv", i).rearrange("h c d -> c h d"), writes=[wuv])
            k2m = C.sb(e1, [8, 1], F32, "k2m")
            with ExitStack() as e2:
                ksq = C.sb(e2, [128, SEQ], BF16, "ksq")
                k2b = C.sb(e2, [8, 4], F32, "k2b")
                pk = C.ps(e2, [128, 512], F32, "pk")
                S.act(lambda e: e.activation(out=ksq[:, :], in_=ckvT[:, :], func=AF.Square), reads=[ckvT], writes=[ksq])
                for b in range(4):
                    S.pe(lambda e: e.matmul(out=pk[0:8, :], lhsT=K["ones_bf"][:, 0:8], rhs=ksq[:, b * 512:(b + 1) * 512], start=True, stop=True),
                         reads=[K["ones_bf"], ksq], writes=[pk])
                    S.dve(lambda e: e.reduce_max(out=k2b[:, b:b + 1], in_=pk[0:8, :], axis=AX.X), reads=[pk], writes=[k2b])
                S.dve(lambda e: e.reduce_max(out=k2m[:, 0:1], in_=k2b[:, :], axis=AX.X), reads=[k2b], writes=[k2m])
                S.barrier()
            for qg in range(4):
                tsl = slice(qg * 512, (qg + 1) * 512)
                nkt = 4 * qg + 4
                with ExitStack() as e2:
                    qidxT = C.sb(e2, [128, 4, 512], BF16, "qidxT")
                    qhT = C.sb(e2, [128, 4, 512], BF16, "qhT")
                    qlatT = C.sb(e2, [128, 8, 512], BF16, "qlatT")
                    maskT = C.sb(e2, [128, nkt, 512], BF16, "maskT")
                    S.pool(lambda e: e.memset(maskT[:, :, :], -30000.0), writes=[maskT])
                    with ExitStack() as e3:
                        pq = [C.ps(e3, [128, 512], F32, f"pq{k}") for k in range(2)]
                        n_ = 0
                        for (wt, dst) in ((wqi, qidxT), (wuq, qhT)):
                            for hp in range(4):
                                p_ = pq[n_ % 2]
                                n_ += 1
                                for rc in range(2):
                                    S.pe(lambda e: e.matmul(out=p_[:, :], lhsT=wt[:, rc, hp * 128:(hp + 1) * 128], rhs=cqnT[:, rc, tsl],
                                                            start=(rc == 0), stop=(rc == 1)), reads=[wt, cqnT], writes=[p_])
                                S.act(lambda e: e.copy(out=dst[:, hp, :], in_=p_[:, :]), reads=[p_], writes=[dst.part(hp)])
                        for h in range(8):
                            hp, e_ = h // 2, h % 2
                            p_ = pq[h % 2]
                            S.pe(lambda e: e.matmul(out=p_[:, :], lhsT=wuk[e_ * 64:(e_ + 1) * 64, hp, :], rhs=qhT[e_ * 64:(e_ + 1) * 64, hp, :],
                                                    start=True, stop=True), reads=[wuk, qhT], writes=[p_])
                            S.dve(lambda e: e.tensor_copy(out=qlatT[:, h, :], in_=p_[:, :]), reads=[p_], writes=[qlatT.part(h)])
                        S.barrier()
                    if DBG.get("odd_stop") == 2:
                        continue
                    with ExitStack() as e3:
                        lps = [C.ps(e3, [128, 512], F32, f"lp{k}") for k in range(2)]
                        tpm = C.ps(e3, [128, 8, 128], BF16, "tpm")
                        sc = C.sb(e3, [128, SEQ], F32, "sc")
                        mk = C.sb(e3, [128, SEQ], BF16, "mk")
                        rb = [C.sb(e3, [128, 512], F32, f"rb{k}") for k in range(2)]
                        st4 = C.sb(e3, [128, 4], F32, "st4")
                        uu = C.sb(e3, [128, 1], F32, "uu")
                        cntt = C.sb(e3, [128, 1], F32, "cntt")
                        dd_ = C.sb(e3, [128, 1], F32, "dd_")
                        n_ = 0
                        for ql in range(4):
                            qt = 4 * qg + ql
                            nk = (qt + 1) * 128
                            for kb in range((nk + 511) // 512):
                                n = min(512, nk - kb * 512)
                                ksl = slice(kb * 512, kb * 512 + n)
                                for h in range(8):
                                    hp, e_ = h // 2, h % 2
                                    lp = lps[n_ % 2]
                                    r_ = rb[n_ % 2]
                                    n_ += 1
                                    S.pe(lambda e: e.matmul(out=lp[:, 0:n], lhsT=qidxT[e_ * 64:(e_ + 1) * 64, hp, ql * 128:(ql + 1) * 128],
                                                            rhs=kidxT2[e_ * 64:(e_ + 1) * 64, ksl], start=True, stop=True),
                                         reads=[qidxT, kidxT2], writes=[lp])
                                    S.act(lambda e: e.activation(out=r_[:, 0:n], in_=lp[:, 0:n], func=AF.Relu, scale=absw[:, qt, h:h + 1]),
                                          reads=[lp, absw], writes=[r_])
                                    eng = "dve"
                                    if h == 0:
                                        S.op(eng, lambda e: e.tensor_scalar(out=sc[:, ksl], in0=r_[:, 0:n], scalar1=sgnw[:, qt, 0:1],
                                                                            scalar2=None, op0=ALU.mult), reads=[r_, sgnw], writes=[sc])
                                    elif eng == "dve":
                                        S.dve(lambda e: e.scalar_tensor_tensor(out=sc[:, ksl], in0=r_[:, 0:n], scalar=sgnw[:, qt, h:h + 1],
                                                                               in1=sc[:, ksl], op0=ALU.mult, op1=ALU.add),
                                              reads=[r_, sgnw, sc], writes=[sc])
                                    else:
                                        S.pool(lambda e: e.tensor_scalar(out=r_[:, 0:n], in0=r_[:, 0:n], scalar1=sgnw[:, qt, h:h + 1],
                                                                         scalar2=None, op0=ALU.mult), reads=[r_, sgnw], writes=[r_])
                                        S.pool(lambda e: e.tensor_tensor(out=sc[:, ksl], in0=sc[:, ksl], in1=r_[:, 0:n], op=ALU.add),
                                               reads=[r_, sc], writes=[sc])
                            dsl = slice(qt * 128, (qt + 1) * 128)
                            if qt >= 2:
                                S.dve(lambda e: e.tensor_reduce(out=st4[:, 0:1], in_=sc[:, 0:nk], axis=AX.X, op=ALU.max), reads=[sc], writes=[st4])
                                S.dve(lambda e: e.tensor_reduce(out=st4[:, 1:2], in_=sc[:, 0:nk], axis=AX.X, op=ALU.min), reads=[sc], writes=[st4])
                                S.dve(lambda e: e.tensor_tensor(out=st4[:, 2:3], in0=st4[:, 0:1], in1=st4[:, 1:2], op=ALU.subtract), reads=[st4], writes=[st4])
                                S.dve(lambda e: e.tensor_scalar(out=st4[:, 2:3], in0=st4[:, 2:3], scalar1=1e-30, scalar2=None, op0=ALU.max), reads=[st4], writes=[st4])
                                S.dve(lambda e: e.reciprocal(out=st4[:, 3:4], in_=st4[:, 2:3]), reads=[st4], writes=[st4])
                                S.dve(lambda e: e.tensor_scalar(out=sc[:, 0:nk], in0=sc[:, 0:nk], scalar1=st4[:, 1:2], scalar2=st4[:, 3:4],
                                                                op0=ALU.subtract, op1=ALU.mult), reads=[sc, st4], writes=[sc])
                                S.pool(lambda e: e.tensor_tensor(out=sc[:, dsl], in0=sc[:, dsl], in1=K["cbias"][:, :], op=ALU.add),
                                       reads=[sc, K["cbias"]], writes=[sc])
                                S.dve(lambda e: e.memset(uu[:, :], 0.5), writes=[uu])
                                for it in range(16):
                                    S.dve(lambda e: e.tensor_scalar(out=mk[:, 0:nk], in0=sc[:, 0:nk], scalar1=uu[:, 0:1], scalar2=0.0,
                                                                    op0=ALU.is_ge, op1=ALU.add, accum_out=cntt[:, 0:1]),
                                          reads=[sc, uu], writes=[mk, cntt])
                                    S.dve(lambda e: e.tensor_scalar(out=dd_[:, :], in0=cntt[:, :], scalar1=255.5, scalar2=float(2.0 ** -(it + 1)),
                                                                    op0=ALU.is_ge, op1=ALU.mult), reads=[cntt], writes=[dd_])
                                    S.dve(lambda e: e.scalar_tensor_tensor(out=uu[:, :], in0=dd_[:, :], scalar=-float(2.0 ** -(it + 2)), in1=uu[:, :],
                                                                           op0=ALU.add, op1=ALU.add), reads=[dd_, uu], writes=[uu])
                                S.dve(lambda e: e.tensor_scalar(out=mk[:, 0:nk], in0=sc[:, 0:nk], scalar1=uu[:, 0:1], scalar2=None, op0=ALU.is_ge),
                                      reads=[sc, uu], writes=[mk])
                            else:
                                S.pool(lambda e: e.tensor_tensor(out=sc[:, dsl], in0=sc[:, dsl], in1=K["cbias"][:, :], op=ALU.add),
                                       reads=[sc, K["cbias"]], writes=[sc])
                                S.dve(lambda e: e.tensor_single_scalar(out=mk[:, 0:nk], in_=sc[:, 0:nk], scalar=-1e29, op=ALU.is_gt),
                                      reads=[sc], writes=[mk])
                            for k0 in range(0, qt + 1, 8):
                                cnt = min(8, qt + 1 - k0)
                                for kk in range(cnt):
                                    kt = k0 + kk
                                    S.pe(lambda e: e.transpose(out=tpm[:, kk, :], in_=mk[:, kt * 128:(kt + 1) * 128], identity=ident[:, :]),
                                         reads=[mk, ident], writes=[tpm])
                                S.dve(lambda e: e.tensor_scalar(out=maskT[:, k0:k0 + cnt, ql * 128:(ql + 1) * 128], in0=tpm[:, 0:cnt, :],
                                                                scalar1=30000.0, scalar2=-30000.0, op0=ALU.mult, op1=ALU.add),
                                      reads=[tpm], writes=[maskT])
                        S.barrier()
                    if DBG.get("odd_stop") == 3:
                        continue
                    with ExitStack() as e3:
                        p8 = C.ps(e3, [128, 512], F32, "p8")
                        negm8 = make_negm(C, e3, qlatT, 8, k2m, K, p8)
                        ps_s = [C.ps(e3, [128, 512], F32, f"ss{k}") for k in range(2)]
                        ps_o = C.ps(e3, [128, 512], F32, "pso")
                        ps_r = C.ps(e3, [128, 512], F32, "psr")
                        po = C.ps(e3, [128, 512], F32, "po")
                        pts = [C.sb(e3, [128, 512], BF16, f"pt{k}") for k in range(2)]
                        rinv = C.sb(e3, [128, 512], F32, "rinv")
                        olat = [C.sb(e3, [128, 512], BF16, f"olat{k}") for k in range(2)]
                        for h in range(8):
                            hp, e_ = h // 2, h % 2
                            kt_list = [(ckvT[:, kt * 128:(kt + 1) * 128], ckvtok[:, kt, :], maskT[:, kt, :], [ckvT, ckvtok, maskT])
                                       for kt in range(nkt)]
                            ol = olat[h % 2]
                            attn_core(C, K, qlatT[:, h, :], qlatT, kt_list, negm8, h, 64 ** -0.5, ol[:, :], ol, 128,
                                      ps_s, ps_o, ps_r, pts, rinv, mask_eng=("pool" if h % 2 else "dve"))
                            S.pe(lambda e: e.matmul(out=po[e_ * 64:(e_ + 1) * 64, :], lhsT=wuv[:, h, :], rhs=ol[:, :], start=True, stop=True),
                                 reads=[wuv, ol], writes=[po])
                            if e_ == 1:
                                S.act(lambda e: e.copy(out=ycT[:, hp, tsl], in_=po[:, :]), reads=[po], writes=[ycT.part((hp, qg))])
                        S.barrier()
        with ExitStack() as e1:
            wo = C.sb(e1, [128, 8, D], BF16, "wo")
            load_w(C, wo, W.H("od_w_out", i), 0, D)
            for g4 in range(4):
                chunks = [(ycT, (lambda ti, c=c, g4=g4: ycT[:, c, (g4 * 4 + ti) * 128:(g4 * 4 + ti + 1) * 128])) for c in range(4)]
                chunks += [(ydT, (lambda ti, c=c, g4=g4: ydT[:, c, (g4 * 4 + ti) * 128:(g4 * 4 + ti + 1) * 128])) for c in range(4)]
                out_proj_residual(C, x, chunks, wo, g_bc, 1.0, [g4 * 4 + t_ for t_ in range(4)])


RW_LN_EPS = 64e-5
TWO_PI = 6.283185307179586


def make_even_consts(C, es, K):
    S = C.S
    ones_f = K["ones_f"]
    E = {}
    o4 = ones_f[:, 0:512].rearrange("p (a b) -> p a b", a=4)
    for nm, pat, cm, op in (("m_su", [[0, 4], [1, 128]], -1, ALU.is_gt), ("m_ui", [[0, 4], [1, 128]], -1, ALU.is_ge),
                            ("m_sl", [[0, 4], [-1, 128]], 1, ALU.is_gt)):
        t = C.sb(es, [128, 4, 128], F32, nm)
        S.pool(lambda e: e.affine_select(out=t[:, :, :], in_=o4, pattern=pat, compare_op=op, fill=0.0, base=0,
                                         channel_multiplier=cm), reads=[ones_f], writes=[t])
        E[nm] = t
    lvm = C.sb(es, [128, 7, 128], BF16, "lvm")
    lvmT = C.sb(es, [128, 7, 128], BF16, "lvmT")
    with ExitStack() as e0:
        I32 = mybir.dt.int32
        pi = C.sb(e0, [128, 128], I32, "lv_pi")
        fi = C.sb(e0, [128, 128], I32, "lv_fi")
        S.pool(lambda e: e.iota(pi[:, :], pattern=[[0, 128]], base=0, channel_multiplier=1), writes=[pi])
        S.pool(lambda e: e.iota(fi[:, :], pattern=[[1, 128]], base=0, channel_multiplier=0), writes=[fi])
        ta = C.sb(e0, [128, 128], I32, "lv_ta")
        tb = C.sb(e0, [128, 128], I32, "lv_tb")
        eq = C.sb(e0, [128, 128], F32, "lv_eq")
        bp = C.sb(e0, [128, 128], F32, "lv_bp")
        bq = C.sb(e0, [128, 128], F32, "lv_bq")
        nbp = C.sb(e0, [128, 128], F32, "lv_nbp")
        nbq = C.sb(e0, [128, 128], F32, "lv_nbq")
        for s in range(7):
            S.dve(lambda e: e.tensor_scalar(out=ta[:, :], in0=pi[:, :], scalar1=s + 1, scalar2=None, op0=ALU.arith_shift_right), reads=[pi], writes=[ta])
            S.dve(lambda e: e.tensor_scalar(out=tb[:, :], in0=fi[:, :], scalar1=s + 1, scalar2=None, op0=ALU.arith_shift_right), reads=[fi], writes=[tb])
            S.dve(lambda e: e.tensor_tensor(out=eq[:, :], in0=ta[:, :], in1=tb[:, :], op=ALU.is_equal), reads=[ta, tb], writes=[eq])
            S.dve(lambda e: e.tensor_scalar(out=ta[:, :], in0=pi[:, :], scalar1=s, scalar2=1, op0=ALU.arith_shift_right, op1=ALU.bitwise_and), reads=[pi], writes=[ta])
            S.dve(lambda e: e.tensor_scalar(out=tb[:, :], in0=fi[:, :], scalar1=s, scalar2=1, op0=ALU.arith_shift_right, op1=ALU.bitwise_and), reads=[fi], writes=[tb])
            S.dve(lambda e: e.tensor_copy(out=bp[:, :], in_=ta[:, :]), reads=[ta], writes=[bp])
            S.dve(lambda e: e.tensor_copy(out=bq[:, :], in_=tb[:, :]), reads=[tb], writes=[bq])
            S.dve(lambda e: e.tensor_scalar(out=nbp[:, :], in0=bp[:, :], scalar1=-1.0, scalar2=1.0, op0=ALU.mult, op1=ALU.add), reads=[bp], writes=[nbp])
            S.dve(lambda e: e.tensor_scalar(out=nbq[:, :], in0=bq[:, :], scalar1=-1.0, scalar2=1.0, op0=ALU.mult, op1=ALU.add), reads=[bq], writes=[nbq])
            S.dve(lambda e: e.tensor_tensor(out=nbp[:, :], in0=nbp[:, :], in1=bq[:, :], op=ALU.mult), reads=[nbp, bq], writes=[nbp])
            S.dve(lambda e: e.tensor_tensor(out=lvm[:, s, :], in0=nbp[:, :], in1=eq[:, :], op=ALU.mult), reads=[nbp, eq], writes=[lvm])
            S.dve(lambda e: e.tensor_tensor(out=nbq[:, :], in0=nbq[:, :], in1=bp[:, :], op=ALU.mult), reads=[nbq, bp], writes=[nbq])
            S.dve(lambda e: e.tensor_tensor(out=lvmT[:, s, :], in0=nbq[:, :], in1=eq[:, :], op=ALU.mult), reads=[nbq, eq], writes=[lvmT])
        S.barrier()
    E.update(lvm=lvm, lvmT=lvmT)
    id8 = C.sb(es, [128, 8, 128], BF16, "id8")
    with ExitStack() as e0:
        ones8 = C.sb(e0, [128, 8, 128], F32, "ones8")
        S.pool(lambda e: e.memset(ones8[:, :, :], 1.0), writes=[ones8])
        S.pool(lambda e: e.affine_select(out=id8[:, :, :], in_=ones8[:, :, :], pattern=[[0, 8], [-1, 128]], compare_op=ALU.is_equal, fill=0.0,
                                         base=0, channel_multiplier=1), reads=[ones8], writes=[id8])
        S.barrier()
    E["id8"] = id8
    bo = C.sb(es, [128, 128], BF16, "blockones")
    bof = C.sb(es, [128, 128], BF16, "blockones_f")
    S.pool(lambda e: e.memset(bo[:, :], 0.0), writes=[bo])
    S.pool(lambda e: e.memset(bof[:, :], 0.0), writes=[bof])
    for b in range(2):
        S.pool(lambda e: e.memset(bo[b * 64:(b + 1) * 64, b * 64:(b + 1) * 64], 1.0), writes=[bo])
        S.pool(lambda e: e.memset(bof[b * 64:(b + 1) * 64, b * 64:(b + 1) * 64], 1.0 / 64), writes=[bof])
    on128 = C.sb(es, [128, 128], BF16, "on128")
    S.pool(lambda e: e.memset(on128[:, :], 1.0 / 128), writes=[on128])
    E.update(bo=bo, bof=bof, on128=on128)
    seg = C.sb(es, [128, 4, 128], F32, "seg")
    S.pool(lambda e: e.memset(seg[:, :, :], 1.0), writes=[seg])
    S.pool(lambda e: e.memset(seg[:, :, 0:1], 0.0), writes=[seg])
    E["seg"] = seg
    cosT = C.sb(es, [128, SEQ], BF16, "cosT")
    sinT = C.sb(es, [128, SEQ], BF16, "sinT")
    with ExitStack() as e1:
        jc_i = C.sb(e1, [128, 1], mybir.dt.int32, "jc_i")
        for b in range(2):
            S.pool(lambda e: e.iota(jc_i[b * 64:(b + 1) * 64, :], pattern=[[0, 1]], base=0, channel_multiplier=1), writes=[jc_i])
        jc = C.sb(e1, [128, 1], F32, "jc")
        S.dve(lambda e: e.tensor_copy(out=jc[:, :], in_=jc_i[:, :]), reads=[jc_i], writes=[jc])
        invf = C.sb(e1, [128, 1], F32, "invf")
        S.act(lambda e: e.activation(out=invf[:, :], in_=jc[:, :], func=AF.Exp, scale=-float(np.log(10000.0)) / 64.0),
              reads=[jc], writes=[invf])
        tp_i = C.sb(e1, [128, SEQ], mybir.dt.int32, "tp_i")
        S.pool(lambda e: e.iota(tp_i[:, :], pattern=[[1, SEQ]], base=0, channel_multiplier=0), writes=[tp_i])
        ang = C.sb(e1, [128, SEQ], F32, "ang")
        S.dve(lambda e: e.tensor_copy(out=ang[:, :], in_=tp_i[:, :]), reads=[tp_i], writes=[ang])
        S.dve(lambda e: e.tensor_scalar(out=ang[:, :], in0=ang[:, :], scalar1=invf[:, 0:1], scalar2=None, op0=ALU.mult),
              reads=[ang, invf], writes=[ang])
        sgn = C.sb(e1, [128, 1], F32, "sgn")
        S.pool(lambda e: e.memset(sgn[0:64, :], -1.0), writes=[sgn])
        S.pool(lambda e: e.memset(sgn[64:128, :], 1.0), writes=[sgn])
        red = C.sb(e1, [128, SEQ], F32, "red")
        qi = C.sb(e1, [128, SEQ], mybir.dt.int32, "qi")
        qf = C.sb(e1, [128, SEQ], F32, "qf")
        for (dst, shift) in ((sinT, 0.0), (cosT, float(np.pi / 2))):
            S.dve(lambda e: e.tensor_scalar(out=qf[:, :], in0=ang[:, :], scalar1=shift, scalar2=1.0 / TWO_PI, op0=ALU.add, op1=ALU.mult),
                  reads=[ang], writes=[qf])
            S.dve(lambda e: e.tensor_copy(out=qi[:, :], in_=qf[:, :]), reads=[qf], writes=[qi])
            S.dve(lambda e: e.tensor_copy(out=qf[:, :], in_=qi[:, :]), reads=[qi], writes=[qf])
            S.dve(lambda e: e.scalar_tensor_tensor(out=red[:, :], in0=qf[:, :], scalar=-TWO_PI, in1=ang[:, :], op0=ALU.mult, op1=ALU.add),
                  reads=[qf, ang], writes=[red])
            if shift:
                S.dve(lambda e: e.tensor_scalar(out=red[:, :], in0=red[:, :], scalar1=shift, scalar2=None, op0=ALU.add), reads=[red], writes=[red])
            S.dve(lambda e: e.tensor_scalar(out=qf[:, :], in0=red[:, :], scalar1=float(np.pi), scalar2=-TWO_PI, op0=ALU.is_gt, op1=ALU.mult),
                  reads=[red], writes=[qf])
            S.dve(lambda e: e.tensor_tensor(out=red[:, :], in0=red[:, :], in1=qf[:, :], op=ALU.add), reads=[red, qf], writes=[red])
            S.dve(lambda e: e.tensor_scalar(out=qf[:, :], in0=red[:, :], scalar1=-float(np.pi), scalar2=TWO_PI, op0=ALU.is_lt, op1=ALU.mult),
                  reads=[red], writes=[qf])
            S.dve(lambda e: e.tensor_tensor(out=red[:, :], in0=red[:, :], in1=qf[:, :], op=ALU.add), reads=[red, qf], writes=[red])
            S.dve(lambda e: e.tensor_scalar(out=red[:, :], in0=red[:, :], scalar1=3.14159, scalar2=-3.14159, op0=ALU.min, op1=ALU.max),
                  reads=[red], writes=[red])
            if shift:
                S.act(lambda e: e.activation(out=dst[:, :], in_=red[:, :], func=AF.Sin), reads=[red], writes=[dst])
            else:
                S.act(lambda e: e.activation(out=red[:, :], in_=red[:, :], func=AF.Sin), reads=[red], writes=[red])
                S.dve(lambda e: e.tensor_scalar(out=dst[:, :], in0=red[:, :], scalar1=sgn[:, 0:1], scalar2=None, op0=ALU.mult),
                      reads=[red, sgn], writes=[dst])
        S.barrier()
    E.update(cosT=cosT, sinT=sinT)
    lg = [float(np.log1p(-2.0 ** (-5.0 - h))) for h in range(4)]
    E["lg"] = lg
    scale = 128 ** -0.5
    dmT = C.sb(es, [128, 4, 128], F32, "dmT")
    xiT = C.sb(es, [128, 4, 128], BF16, "xiT")
    zcol = C.sb(es, [128, 4], F32, "zcol")
    with ExitStack() as e1:
        d_i = C.sb(e1, [128, 128], mybir.dt.int32, "d_i")
        d_f = C.sb(e1, [128, 128], F32, "d_f")
        S.pool(lambda e: e.iota(d_i[:, :], pattern=[[1, 128]], base=0, channel_multiplier=-1), writes=[d_i])
        S.dve(lambda e: e.tensor_copy(out=d_f[:, :], in_=d_i[:, :]), reads=[d_i], writes=[d_f])
        S.dve(lambda e: e.tensor_scalar(out=d_f[:, :], in0=d_f[:, :], scalar1=0.0, scalar2=None, op0=ALU.max), reads=[d_f], writes=[d_f])
        i_i = C.sb(e1, [128, 128], mybir.dt.int32, "i_i")
        i_f = C.sb(e1, [128, 128], F32, "i_f")
        S.pool(lambda e: e.iota(i_i[:, :], pattern=[[1, 128]], base=1, channel_multiplier=0), writes=[i_i])
        S.dve(lambda e: e.tensor_copy(out=i_f[:, :], in_=i_i[:, :]), reads=[i_i], writes=[i_f])
        p_i = C.sb(e1, [128, 1], mybir.dt.int32, "p_i")
        p_f = C.sb(e1, [128, 1], F32, "p_f")
        S.pool(lambda e: e.iota(p_i[:, :], pattern=[[0, 1]], base=127, channel_multiplier=-1), writes=[p_i])
        S.dve(lambda e: e.tensor_copy(out=p_f[:, :], in_=p_i[:, :]), reads=[p_i], writes=[p_f])
        tmp = C.sb(e1, [128, 128], F32, "tmpd")
        for h in range(4):
            S.act(lambda e: e.activation(out=tmp[:, :], in_=d_f[:, :], func=AF.Exp, scale=lg[h]), reads=[d_f], writes=[tmp])
            S.dve(lambda e: e.scalar_tensor_tensor(out=dmT[:, h, :], in0=tmp[:, :], scalar=scale, in1=E["m_ui"][:, 0, :],
                                                   op0=ALU.mult, op1=ALU.mult), reads=[tmp, E["m_ui"]], writes=[dmT])
            S.act(lambda e: e.activation(out=xiT[:, h, :], in_=i_f[:, :], func=AF.Exp, scale=lg[h]), reads=[i_f], writes=[xiT])
            S.act(lambda e: e.activation(out=zcol[:, h:h + 1], in_=p_f[:, :], func=AF.Exp, scale=lg[h]), reads=[p_f], writes=[zcol])
        S.dve(lambda e: e.tensor_scalar(out=zcol[:, :], in0=zcol[:, :], scalar1=scale, scalar2=None, op0=ALU.mult), reads=[zcol], writes=[zcol])
        S.barrier()
    E.update(dmT=dmT, xiT=xiT, zcol=zcol)
    return E


def group_norm_T(C, es, y, nch, onesmat, eps, gcol_fn, bcol_fn, post_fn, pm, pq):
    S = C.S
    sq = C.sb(es, [128, 512], BF16, "gn_sq")
    yb16 = C.sb(es, [128, 512], BF16, "gn_yb")
    m2 = C.sb(es, [128, 512], F32, "gn_m2")
    rs = C.sb(es, [128, 512], F32, "gn_rs")
    dd = C.sb(es, [128, 512], F32, "gn_dd")
    for c in range(nch):
        S.dve(lambda e: e.tensor_copy(out=yb16[:, :], in_=y[:, c, :]), reads=[y], writes=[yb16])
        S.pe(lambda e: e.matmul(out=pm[:, :], lhsT=onesmat[:, :], rhs=yb16[:, :], start=True, stop=True), reads=[onesmat, yb16], writes=[pm])
        S.act(lambda e: e.activation(out=sq[:, :], in_=y[:, c, :], func=AF.Square), reads=[y], writes=[sq])
        S.pe(lambda e: e.matmul(out=pq[:, :], lhsT=onesmat[:, :], rhs=sq[:, :], start=True, stop=True), reads=[onesmat, sq], writes=[pq])
        S.act(lambda e: e.activation(out=m2[:, :], in_=pm[:, :], func=AF.Square), reads=[pm], writes=[m2])
        S.dve(lambda e: e.tensor_tensor(out=rs[:, :], in0=pq[:, :], in1=m2[:, :], op=ALU.subtract), reads=[pq, m2], writes=[rs])
        S.dve(lambda e: e.tensor_scalar(out=rs[:, :], in0=rs[:, :], scalar1=0.0, scalar2=float(eps), op0=ALU.max, op1=ALU.add),
              reads=[rs], writes=[rs])
        S.act(lambda e: e.activation(out=rs[:, :], in_=rs[:, :], func=AF.Sqrt), reads=[rs], writes=[rs])
        S.dve(lambda e: e.reciprocal(out=rs[:, :], in_=rs[:, :]), reads=[rs], writes=[rs])
        S.dve(lambda e: e.tensor_tensor(out=dd[:, :], in0=y[:, c, :], in1=pm[:, :], op=ALU.subtract), reads=[y, pm], writes=[dd])
        S.dve(lambda e: e.tensor_tensor(out=dd[:, :], in0=dd[:, :], in1=rs[:, :], op=ALU.mult), reads=[dd, rs], writes=[dd])
        S.pool(lambda e: e.tensor_scalar(out=dd[:, :], in0=dd[:, :], scalar1=gcol_fn(c), scalar2=bcol_fn(c), op0=ALU.mult, op1=ALU.add),
               reads=[dd], writes=[dd])
        post_fn(c, dd)


def even_mixer(C, x, l, W, P, K):
    S = C.S
    i = l // 2
    ident = K["ident"]
    w_in = W.H("ev_w_in", i)
    w_v = w_in.rearrange("(kc p) n -> p kc n", p=128)
    if DBG.get("even_stop") == 0:
        return
    with ExitStack() as es:
        E = make_even_consts(C, es, K)
        lg = E["lg"]
        gamC = [float(np.exp(128.0 * lg[h])) for h in range(4)]
        yaT = C.sb(es, [128, 4, SEQ], BF16, "yaT")
        with ExitStack() as er:
            wa2 = C.sb(er, [128, 512], BF16, "wa2")
            g2 = C.sb(er, [128, 512], BF16, "g2")
            S.dma("pool", wa2[0:64, :], W.H("rw_w2", i), writes=[wa2])
            S.dma("pool", wa2[64:128, :], W.H("rw_a2", i), writes=[wa2])
            S.dma("pool", g2[:, :], W.H("rw_g2", i), writes=[g2])
            omk = C.sb(er, [128, 4], F32, "omk")
            S.dve(lambda e: e.tensor_scalar(out=omk[:, :], in0=P.cols(("rw_k_a", i), 0, 4), scalar1=-1.0, scalar2=1.0, op0=ALU.mult, op1=ALU.add),
                  reads=[P.t], writes=[omk])
            St = C.sb(er, [128, 4, 64], F32, "St")
            Sb = C.sb(er, [128, 4, 2, 64], BF16, "Sbd")
            S.pool(lambda e: e.memset(St[:, :, :], 0.0), writes=[St])
            S.pool(lambda e: e.memset(Sb[:, :, :, :], 0.0), writes=[Sb])
            pcar = C.sb(er, [128, 14], F32, "pcar")
            S.pool(lambda e: e.memset(pcar[:, :], 0.0), writes=[pcar])
            wch = [C.sb(er, [128, 8, 128], BF16, f"wch{k}") for k in range(2)]
            nw = [0]

            def mu_col(c):
                return P.col(("rw_mu", i, 0), c) if c < 8 else P.col(("rw_mu", i, 1), c - 8)

            for blk in range(4):
                t0 = blk * 512
                with ExitStack() as e1:
                    xnT = C.sb(e1, [128, 8, 512], BF16, "xnT")
                    rms_to_T(C, e1, x, P.cols(("norm_g", l, 2)), xnT, 4, ident, tok0=blk * 4)
                    with ExitStack() as e2:
                        At = C.sb(e2, [128, 4, 512], BF16, "At")
                        Bt = C.sb(e2, [128, 4, 512], BF16, "Bt")
                        Kt = C.sb(e2, [128, 4, 512], BF16, "Kt")
                        Rq = C.sb(e2, [128, 4, 512], BF16, "Rq")
                        vT = C.sb(e2, [128, 4, 512], BF16, "vT")
                        bon = C.sb(e2, [128, 4, 512], BF16, "bon")
                        gT = C.sb(e2, [128, 4, 512], BF16, "gT")
                        gC = C.sb(e2, [128, 4, 4], F32, "gC")
                        yraw = C.sb(e2, [128, 4, 512], F32, "yraw")
                        with ExitStack() as e3:
                            pps = [C.ps(e3, [128, 512], F32, f"pp{k}") for k in range(3)]
                            pa = C.ps(e3, [128, 512], F32, "pa")
                            pb = C.ps(e3, [128, 512], F32, "pb")
                            pT = C.sb(e3, [128, 513], F32, "pT")
                            mx = [C.sb(e3, [128, 512], F32, f"mx{k}") for k in range(3)]
                            dtmp = C.sb(e3, [128, 512], F32, "dtmp")
                            xwa = C.sb(e3, [128, 512], BF16, "xwa")
                            sxg = C.sb(e3, [128, 512], BF16, "sxg")
                            kkb = C.sb(e3, [128, 512], BF16, "kkb")
                            bA = C.sb(e3, [128, 512], F32, "bA")
                            bB = C.sb(e3, [128, 512], F32, "bB")
                            eL = C.sb(e3, [128, 512], F32, "eL")
                            eLm = C.sb(e3, [128, 512], F32, "eLm")
                            asig = C.sb(e3, [128, 512], F32, "asig")
                            kk = C.sb(e3, [128, 512], F32, "kk")
                            t2 = C.sb(e3, [128, 512], F32, "t2")

                            def proj_mix(c, k_):
                                pp, m_ = pps[k_], mx[k_]
                                wt = wch[nw[0] % 2]
                                nw[0] += 1
                                S.dma("pool", wt[:, :, :], w_v[:, :, c * 128:(c + 1) * 128], writes=[wt])
                                for kc in range(8):
                                    S.pe(lambda e: e.matmul(out=pp[:, :], lhsT=wt[:, kc, :], rhs=xnT[:, kc, :],
                                                            start=(kc == 0), stop=(kc == 7)), reads=[wt, xnT], writes=[pp])
                                S.act(lambda e: e.copy(out=pT[:, 1:513], in_=pp[:, :]), reads=[pp], writes=[pT])
                                S.pool(lambda e: e.tensor_copy(out=pT[:, 0:1], in_=pcar[:, c:c + 1]), reads=[pcar.part(c)], writes=[pT])
                                S.pool(lambda e: e.tensor_copy(out=pcar[:, c:c + 1], in_=pT[:, 512:513]), reads=[pT], writes=[pcar.part(c)])
                                S.dve(lambda e: e.tensor_tensor(out=dtmp[:, :], in0=pT[:, 0:512], in1=pT[:, 1:513], op=ALU.subtract),
                                      reads=[pT], writes=[dtmp])
                                S.dve(lambda e: e.scalar_tensor_tensor(out=m_[:, :], in0=dtmp[:, :], scalar=mu_col(c), in1=pT[:, 1:513],
                                                                       op0=ALU.mult, op1=ALU.add), reads=[dtmp, pT, P.t], writes=[m_])
                                return m_

                            m_ = proj_mix(12, 0)
                            S.act(lambda e: e.activation(out=xwa[0:64, :], in_=m_[0:64, :], func=AF.Tanh), reads=[m_], writes=[xwa])
                            S.act(lambda e: e.copy(out=xwa[64:128, :], in_=m_[64:128, :]), reads=[m_], writes=[xwa])
                            m_ = proj_mix(13, 1)
                            S.act(lambda e: e.activation(out=sxg[:, :], in_=m_[:, :], func=AF.Sigmoid), reads=[m_], writes=[sxg])
                            for hp in range(4):
                                rr = proj_mix(hp, 0)
                                kx = proj_mix(4 + hp, 1)
                                vv = proj_mix(8 + hp, 2)
                                S.pe(lambda e: e.matmul(out=pa[:, :], lhsT=wa2[0:64, hp * 128:(hp + 1) * 128], rhs=xwa[0:64, :], start=True, stop=True),
                                     reads=[wa2, xwa], writes=[pa])
                                S.act(lambda e: e.activation(out=bA[:, :], in_=pa[:, :], func=AF.Sigmoid, bias=P.col(("rw_w0", i), hp)),
                                      reads=[pa, P.t], writes=[bA])
                                S.dve(lambda e: e.tensor_scalar(out=bA[:, :], in0=bA[:, :], scalar1=-float(np.exp(-0.5)), scalar2=None, op0=ALU.mult),
                                      reads=[bA], writes=[bA])
                                S.dve(lambda e: e.tensor_tensor_scan(out=bB[:, :], data0=E["seg"][:, :, :].rearrange("p a b -> p (a b)"),
                                                                     data1=bA[:, :], initial=0.0, op0=ALU.mult, op1=ALU.add),
                                      reads=[bA, E["seg"]], writes=[bB])
                                S.act(lambda e: e.activation(out=eL[:, :], in_=bB[:, :], func=AF.Exp), reads=[bB], writes=[eL])
                                S.act(lambda e: e.activation(out=eLm[:, :], in_=bB[:, :], func=AF.Exp, scale=-1.0), reads=[bB], writes=[eLm])
                                S.dve(lambda e: e.tensor_tensor(out=bA[:, :], in0=bB[:, :], in1=bA[:, :], op=ALU.subtract), reads=[bB, bA], writes=[bA])
                                S.act(lambda e: e.activation(out=bA[:, :], in_=bA[:, :], func=AF.Exp), reads=[bA], writes=[bA])
                                S.pool(lambda e: e.tensor_copy(out=gC[:, :, hp], in_=eL[:, :].rearrange("p (a b) -> p a b", a=4)[:, :, 127]),
                                       reads=[eL], writes=[gC])
                                S.pe(lambda e: e.matmul(out=pb[:, :], lhsT=wa2[64:128, hp * 128:(hp + 1) * 128], rhs=xwa[64:128, :], start=True, stop=True),
                                     reads=[wa2, xwa], writes=[pb])
                                S.act(lambda e: e.activation(out=asig[:, :], in_=pb[:, :], func=AF.Sigmoid, bias=P.col(("rw_a0", i), hp)),
                                      reads=[pb, P.t], writes=[asig])
                                S.pool(lambda e: e.tensor_scalar(out=kk[:, :], in0=kx[:, :], scalar1=P.col(("rw_k_k", i), hp), scalar2=None, op0=ALU.mult),
                                       reads=[kx, P.t], writes=[kk])
                                S.act(lambda e: e.activation(out=kkb[:, :], in_=kk[:, :], func=AF.Square), reads=[kk], writes=[kkb])
                                S.pe(lambda e: e.matmul(out=pa[:, :], lhsT=E["bo"][:, :], rhs=kkb[:, :], start=True, stop=True),
                                     reads=[E["bo"], kkb], writes=[pa])
                                S.dve(lambda e: e.tensor_scalar(out=bB[:, :], in0=pa[:, :], scalar1=1e-24, scalar2=None, op0=ALU.max), reads=[pa], writes=[bB])
                                S.act(lambda e: e.activation(out=bB[:, :], in_=bB[:, :], func=AF.Sqrt), reads=[bB], writes=[bB])
                                S.dve(lambda e: e.reciprocal(out=bB[:, :], in_=bB[:, :]), reads=[bB], writes=[bB])
                                S.dve(lambda e: e.tensor_tensor(out=kk[:, :], in0=kk[:, :], in1=bB[:, :], op=ALU.mult), reads=[kk, bB], writes=[kk])
                                S.dve(lambda e: e.scalar_tensor_tensor(out=At[:, hp, :], in0=kk[:, :], scalar=-1.0, in1=bA[:, :], op0=ALU.mult, op1=ALU.mult),
                                      reads=[kk, bA], writes=[At.part(hp)])
                                S.pool(lambda e: e.tensor_tensor(out=bB[:, :], in0=kk[:, :], in1=asig[:, :], op=ALU.mult), reads=[kk, asig], writes=[bB])
                                S.pool(lambda e: e.tensor_tensor(out=Bt[:, hp, :], in0=bB[:, :], in1=eLm[:, :], op=ALU.mult), reads=[bB, eLm], writes=[Bt.part(hp)])
                                S.dve(lambda e: e.tensor_scalar(out=t2[:, :], in0=asig[:, :], scalar1=P.col(("rw_k_a", i), hp), scalar2=omk[:, hp:hp + 1],
                                                                op0=ALU.mult, op1=ALU.add), reads=[asig, P.t, omk], writes=[t2])
                                S.dve(lambda e: e.tensor_tensor(out=t2[:, :], in0=t2[:, :], in1=kx[:, :], op=ALU.mult), reads=[t2, kx], writes=[t2])
                                S.pool(lambda e: e.tensor_tensor(out=Kt[:, hp, :], in0=t2[:, :], in1=eLm[:, :], op=ALU.mult), reads=[t2, eLm], writes=[Kt.part(hp)])
                                S.dve(lambda e: e.tensor_tensor(out=Rq[:, hp, :], in0=rr[:, :], in1=eL[:, :], op=ALU.mult), reads=[rr, eL], writes=[Rq.part(hp)])
                                S.act(lambda e: e.copy(out=vT[:, hp, :], in_=vv[:, :]), reads=[vv], writes=[vT.part(hp)])
                                S.dve(lambda e: e.scalar_tensor_tensor(out=kkb[:, :], in0=rr[:, :], scalar=P.col(("rw_r_k", i), hp), in1=t2[:, :],
                                                                       op0=ALU.mult, op1=ALU.mult), reads=[rr, t2, P.t], writes=[kkb])
                                S.pe(lambda e: e.matmul(out=pb[:, :], lhsT=E["bo"][:, :], rhs=kkb[:, :], start=True, stop=True),
                                     reads=[E["bo"], kkb], writes=[pb])
                                S.dve(lambda e: e.tensor_tensor(out=bon[:, hp, :], in0=pb[:, :], in1=vv[:, :], op=ALU.mult), reads=[pb, vv], writes=[bon.part(hp)])
                                S.pe(lambda e: e.matmul(out=pa[:, :], lhsT=g2[:, hp * 128:(hp + 1) * 128], rhs=sxg[:, :], start=True, stop=True),
                                     reads=[g2, sxg], writes=[pa])
                                S.act(lambda e: e.copy(out=gT[:, hp, :], in_=pa[:, :]), reads=[pa], writes=[gT.part(hp)])
                            S.barrier()
                        if DBG.get("even_stop") == 1:
                            continue
                        with ExitStack() as e3:
                            def bank(nm, dt=F32):
                                return C.ps(e3, [128, 512] if dt == F32 else [128, 8, 128], dt, nm)
                            pA = [bank(f"pA{k}") for k in range(3)]
                            pX = [bank(f"pX{k}") for k in range(2)]
                            ptr = bank("ptr", BF16)
                            pS = bank("pS")
                            BtT = C.sb(e3, [128, 512], BF16, "BtT")
                            KtT = C.sb(e3, [128, 512], BF16, "KtT")
                            Vtk = C.sb(e3, [128, 512], BF16, "Vtk")
                            Mak = C.sb(e3, [128, 8, 128], BF16, "Mak")
                            Nbr = C.sb(e3, [128, 8, 128], BF16, "Nbr")
                            Nkr = C.sb(e3, [128, 8, 128], BF16, "Nkr")
                            Pm = [C.sb(e3, [128, 8, 128], BF16, "Mm")]
                            PTm = [C.sb(e3, [128, 8, 128], BF16, "MTm")]
                            Xm = [C.sb(e3, [128, 8, 128], BF16, f"Xm{k}") for k in range(2)]
                            XTm = [C.sb(e3, [128, 8, 128], BF16, f"XTm{k}") for k in range(2)]
                            Ts = C.sb(e3, [128, 8, 128], BF16, "Ts")
                            TsT = C.sb(e3, [128, 8, 128], BF16, "TsT")
                            Y1s = C.sb(e3, [128, 8, 128], BF16, "Y1s")
                            Z1s = C.sb(e3, [128, 8, 128], BF16, "Z1s")
                            Gt = C.sb(e3, [128, 512], BF16, "Gt")
                            Ut = C.sb(e3, [128, 512], BF16, "Ut")
                            stmp = C.sb(e3, [128, 4, 64], F32, "stmp")

                            def hv(t_, h, csl):
                                hp_, e_ = h // 2, h % 2
                                return t_[e_ * 64:(e_ + 1) * 64, hp_, csl]

                            for c in range(4):
                                csl = slice(c * 128, (c + 1) * 128)
                                for (src, dst) in ((Bt, BtT), (Kt, KtT), (vT, Vtk)):
                                    for hp in range(4):
                                        S.pe(lambda e: e.transpose(out=ptr[:, hp, :], in_=src[:, hp, csl], identity=ident[:, :]),
                                             reads=[src, ident], writes=[ptr])
                                    S.act(lambda e: e.copy(out=dst[:, :], in_=ptr[:, 0:4, :].rearrange("p a b -> p (a b)")), reads=[ptr], writes=[dst])
                                if DBG.get('even_stop') == 1.2:
                                    continue
                                prods = ((Bt, At, Pm[0], "m_su"), (At, Bt, PTm[0], "m_sl"), (Kt, At, Mak, "m_su"),
                                         (Bt, Rq, Nbr, "m_ui"), (Kt, Rq, Nkr, "m_ui"))
                                nb = 0
                                for (lt, rt_, dst, mk) in prods:
                                    for e_ in range(2):
                                        pbk = pA[nb % 3]
                                        nb += 1
                                        for hh in range(4):
                                            h = 2 * hh + e_
                                            S.pe(lambda e: e.matmul(out=pbk[:, hh * 128:(hh + 1) * 128], lhsT=hv(lt, h, csl), rhs=hv(rt_, h, csl),
                                                                    start=True, stop=True), reads=[lt, rt_], writes=[pbk])
                                        S.dve(lambda e: e.tensor_tensor(out=dst[:, :, :].rearrange("p (a two) b -> p a two b", two=2)[:, :, e_, :],
                                                                        in0=pbk[:, :].rearrange("p (a b) -> p a b", a=4), in1=E[mk][:, :, :], op=ALU.mult),
                                              reads=[pbk, E[mk]], writes=[dst.part(("e", e_))])
                                if DBG.get('even_stop') == 1.4:
                                    continue
                                Mm, MTm = Pm[0], PTm[0]

                                def lvl(src, msk, s, dst, eng):
                                    S.op(eng, lambda e: e.tensor_tensor(out=dst[:, :, :], in0=src[:, :, :],
                                                                        in1=E[msk][:, s, :].unsqueeze(1).to_broadcast([128, 8, 128]), op=ALU.mult),
                                         reads=[src, E[msk]], writes=[dst])

                                lvl(Mm, "lvm", 0, Ts, "pool")
                                lvl(MTm, "lvmT", 0, TsT, "pool")
                                S.pool(lambda e: e.tensor_tensor(out=Xm[0][:, :, :], in0=Ts[:, :, :], in1=E["id8"][:, :, :], op=ALU.add),
                                       reads=[Ts, E["id8"]], writes=[Xm[0]])
                                S.pool(lambda e: e.tensor_tensor(out=XTm[0][:, :, :], in0=TsT[:, :, :], in1=E["id8"][:, :, :], op=ALU.add),
                                       reads=[TsT, E["id8"]], writes=[XTm[0]])
                                cur = 0
                                for s in range(1, 7):
                                    nxt = 1 - cur
                                    last = (s == 6)
                                    lvl(Mm, "lvm", s, Ts, "pool")
                                    lvl(MTm, "lvmT", s, TsT, "dve")
                                    for half in range(2):
                                        hs_ = range(half * 4, half * 4 + 4)
                                        pbk = pA[nb % 3]
                                        nb += 1
                                        for hh, h in enumerate(hs_):
                                            S.pe(lambda e: e.matmul(out=pbk[:, hh * 128:(hh + 1) * 128], lhsT=TsT[:, h, :], rhs=Xm[cur][:, h, :],
                                                                    start=True, stop=True), reads=[TsT, Xm[cur]], writes=[pbk])
                                        S.act(lambda e: e.copy(out=Y1s[:, half * 4:half * 4 + 4, :], in_=pbk[:, :].rearrange("p (a b) -> p a b", a=4)),
                                              reads=[pbk], writes=[Y1s.part(half)])
                                        if not last:
                                            pbk = pA[nb % 3]
                                            nb += 1
                                            for hh, h in enumerate(hs_):
                                                S.pe(lambda e: e.matmul(out=pbk[:, hh * 128:(hh + 1) * 128], lhsT=Ts[:, h, :], rhs=XTm[cur][:, h, :],
                                                                        start=True, stop=True), reads=[Ts, XTm[cur]], writes=[pbk])
                                            S.dve(lambda e: e.tensor_copy(out=Z1s[:, half * 4:half * 4 + 4, :], in_=pbk[:, :].rearrange("p (a b) -> p a b", a=4)),
                                                  reads=[pbk], writes=[Z1s.part(half)])
                                    for half in range(2):
                                        hs_ = range(half * 4, half * 4 + 4)
                                        px = pX[half]
                                        for hh, h in enumerate(hs_):
                                            S.pe(lambda e: e.matmul(out=px[:, hh * 128:(hh + 1) * 128], lhsT=ident[:, :], rhs=Xm[cur][:, h, :],
                                                                    start=True, stop=False), reads=[ident, Xm[cur]], writes=[px])
                                            S.pe(lambda e: e.matmul(out=px[:, hh * 128:(hh + 1) * 128], lhsT=XTm[cur][:, h, :], rhs=Y1s[:, h, :],
                                                                    start=False, stop=True), reads=[XTm[cur], Y1s], writes=[px])
                                        S.act(lambda e: e.copy(out=Xm[nxt][:, half * 4:half * 4 + 4, :], in_=px[:, :].rearrange("p (a b) -> p a b", a=4)),
                                              reads=[px], writes=[Xm[nxt].part(half)])
                                        if not last:
                                            pbk = pA[nb % 3]
                                            nb += 1
                                            for hh, h in enumerate(hs_):
                                                S.pe(lambda e: e.matmul(out=pbk[:, hh * 128:(hh + 1) * 128], lhsT=ident[:, :], rhs=XTm[cur][:, h, :],
                                                                        start=True, stop=False), reads=[ident, XTm[cur]], writes=[pbk])
                                                S.pe(lambda e: e.matmul(out=pbk[:, hh * 128:(hh + 1) * 128], lhsT=Xm[cur][:, h, :], rhs=Z1s[:, h, :],
                                                                        start=False, stop=True), reads=[Xm[cur], Z1s], writes=[pbk])
                                            S.dve(lambda e: e.tensor_copy(out=XTm[nxt][:, half * 4:half * 4 + 4, :], in_=pbk[:, :].rearrange("p (a b) -> p a b", a=4)),
                                                  reads=[pbk], writes=[XTm[nxt].part(half)])
                                    cur = nxt
                                if DBG.get('even_stop') == 1.6:
                                    continue
                                Xf = Xm[cur]
                                pg = pA[nb % 3]
                                nb += 1
                                for h in range(8):
                                    hp, e_ = h // 2, h % 2
                                    S.pe(lambda e: e.matmul(out=pg[:, h * 64:(h + 1) * 64], lhsT=At[:, hp, csl], rhs=Sb[:, hp, e_, :],
                                                            start=True, stop=False), reads=[At, Sb], writes=[pg])
                                    S.pe(lambda e: e.matmul(out=pg[:, h * 64:(h + 1) * 64], lhsT=Mak[:, h, :], rhs=Vtk[:, h * 64:(h + 1) * 64],
                                                            start=False, stop=True), reads=[Mak, Vtk], writes=[pg])
                                S.act(lambda e: e.copy(out=Gt[:, :], in_=pg[:, :]), reads=[pg], writes=[Gt])
                                if DBG.get('even_stop') == 1.65:
                                    continue
                                pu = pA[nb % 3]
                                nb += 1
                                for h in range(8):
                                    S.pe(lambda e: e.matmul(out=pu[:, h * 64:(h + 1) * 64], lhsT=Xf[:, h, :], rhs=Gt[:, h * 64:(h + 1) * 64],
                                                            start=True, stop=True), reads=[Xf, Gt], writes=[pu])
                                S.dve(lambda e: e.tensor_copy(out=Ut[:, :], in_=pu[:, :]), reads=[pu], writes=[Ut])
                                if DBG.get('even_stop') == 1.7:
                                    continue
                                py = pA[nb % 3]
                                nb += 1
                                for h in range(8):
                                    hp, e_ = h // 2, h % 2
                                    o_ = py[e_ * 64:(e_ + 1) * 64, hp * 128:(hp + 1) * 128]
                                    S.pe(lambda e: e.matmul(out=o_, lhsT=Sb[:, hp, e_, :], rhs=Rq[:, hp, csl], start=True, stop=False),
                                         reads=[Sb, Rq], writes=[py])
                                    S.pe(lambda e: e.matmul(out=o_, lhsT=Ut[:, h * 64:(h + 1) * 64], rhs=Nbr[:, h, :], start=False, stop=False),
                                         reads=[Ut, Nbr], writes=[py])
                                    S.pe(lambda e: e.matmul(out=o_, lhsT=Vtk[:, h * 64:(h + 1) * 64], rhs=Nkr[:, h, :], start=False, stop=True),
                                         reads=[Vtk, Nkr], writes=[py])
                                S.act(lambda e: e.copy(out=yraw[:, :, csl], in_=py[:, :].rearrange("p (a b) -> p a b", a=4)), reads=[py], writes=[yraw.part(c)])
                                if DBG.get('even_stop') == 1.75:
                                    continue
                                for hp in range(4):
                                    o_ = pS[:, hp * 128:(hp + 1) * 128]
                                    S.pe(lambda e: e.matmul(out=o_, lhsT=BtT[:, hp * 128:(hp + 1) * 128], rhs=Ut[:, hp * 128:(hp + 1) * 128], start=True, stop=False),
                                         reads=[BtT, Ut], writes=[pS])
                                    S.pe(lambda e: e.matmul(out=o_, lhsT=KtT[:, hp * 128:(hp + 1) * 128], rhs=Vtk[:, hp * 128:(hp + 1) * 128], start=False, stop=True),
                                         reads=[KtT, Vtk], writes=[pS])
                                for e_ in range(2):
                                    S.dve(lambda e: e.tensor_tensor(out=stmp[e_ * 64:(e_ + 1) * 64, :, :],
                                                                    in0=pS[e_ * 64:(e_ + 1) * 64, :].rearrange("p (a b c) -> p a b c", a=4, b=2)[:, :, e_, :],
                                                                    in1=St[e_ * 64:(e_ + 1) * 64, :, :], op=ALU.add),
                                          reads=[pS, St], writes=[stmp])
                                S.dve(lambda e: e.tensor_tensor(out=St[:, :, :], in0=stmp[:, :, :], in1=gC[:, c, :].unsqueeze(2).to_broadcast([128, 4, 64]),
                                                                op=ALU.mult), reads=[stmp, gC], writes=[St])
                                for e_ in range(2):
                                    S.act(lambda e: e.copy(out=Sb[e_ * 64:(e_ + 1) * 64, :, e_, :], in_=St[e_ * 64:(e_ + 1) * 64, :, :]), reads=[St], writes=[Sb])
                            S.barrier()
                        if DBG.get('even_stop') == 1.8:
                            continue
                        with ExitStack() as e3:
                            pm = C.ps(e3, [128, 512], F32, "gn_pm")
                            pq = C.ps(e3, [128, 512], F32, "gn_pq")

                            def post_a(c, dd):
                                S.pool(lambda e: e.tensor_tensor(out=dd[:, :], in0=dd[:, :], in1=bon[:, c, :], op=ALU.add), reads=[dd, bon], writes=[dd])
                                S.dve(lambda e: e.tensor_tensor(out=yaT[:, c, t0:t0 + 512], in0=dd[:, :], in1=gT[:, c, :], op=ALU.mult), reads=[dd, gT], writes=[yaT.part((c, blk))])

                            group_norm_T(C, e3, yraw, 4, E["bof"], RW_LN_EPS, lambda c: P.col(("rw_ln_g", i), c), lambda c: P.col(("rw_ln_b", i), c),
                                         post_a, pm, pq)
                            S.barrier()
        if DBG.get("even_stop") in (1, 1.2, 1.4, 1.6, 1.65, 1.7, 1.75, 1.8, 2):
            return
        ybT = C.sb(es, [128, 4, SEQ], BF16, "ybT")
        with ExitStack() as er:
            Rt = C.sb(er, [128, 4, 128], F32, "Rt")
            Rb = C.sb(er, [128, 4, 128], BF16, "Rb")
            for t_ in (Rt, Rb):
                S.pool(lambda e: e.memset(t_[:, :, :], 0.0), writes=[t_])
            wvr = C.sb(er, [128, 8, 512], BF16, "wvr")
            load_w(C, wvr, w_in, 1792 + 1024, 1792 + 1536)
            wch = [C.sb(er, [128, 8, 128], BF16, f"wchr{k}") for k in range(2)]
            nw = [0]

            def wchunk(c0):
                wt = wch[nw[0] % 2]
                nw[0] += 1
                S.dma("pool", wt[:, :, :], w_v[:, :, 1792 + c0:1792 + c0 + 128], writes=[wt])
                return wt

            for blk in range(4):
                t0 = blk * 512
                with ExitStack() as e1:
                    xnT = C.sb(e1, [128, 8, 512], BF16, "xnT")
                    rms_to_T(C, e1, x, P.cols(("norm_g", l, 2)), xnT, 4, ident, tok0=blk * 4)
                    with ExitStack() as e2:
                        qr = C.sb(e2, [128, 4, 512], BF16, "qr")
                        qx = C.sb(e2, [128, 4, 512], BF16, "qx")
                        kr = C.sb(e2, [128, 4, 512], BF16, "kr")
                        sg = C.sb(e2, [128, 4, 512], BF16, "sg")
                        vtk = C.sb(e2, [128, 4, 512], BF16, "vtk")
                        oraw = C.sb(e2, [128, 4, 512], F32, "oraw")
                        cs = E["cosT"][:, t0:t0 + 512]
                        sn = E["sinT"][:, t0:t0 + 512]
                        with ExitStack() as e3:
                            pq_ = [C.ps(e3, [128, 512], F32, f"rq{k}") for k in range(2)]
                            ps_ = [C.ps(e3, [128, 512], F32, f"rs{k}") for k in range(2)]
                            t1 = C.sb(e3, [128, 512], F32, "rt1")
                            t2 = C.sb(e3, [128, 512], F32, "rt2")
                            n_ = 0
                            for (c0, dst) in ((0, qr), (512, kr)):
                                for h in range(4):
                                    pa_, pb_ = pq_[n_ % 2], ps_[n_ % 2]
                                    n_ += 1
                                    cb = c0 + h * 128
                                    wrt = wchunk(cb)
                                    for kc in range(8):
                                        S.pe(lambda e: e.matmul(out=pa_[:, :], lhsT=wrt[:, kc, :], rhs=xnT[:, kc, :], start=(kc == 0), stop=(kc == 7)),
                                             reads=[wrt, xnT], writes=[pa_])
                                    for half in range(2):
                                        for kc in range(8):
                                            S.pe(lambda e: e.matmul(out=pb_[half * 64:(half + 1) * 64, :], lhsT=wrt[:, kc, (1 - half) * 64:(1 - half) * 64 + 64],
                                                                    rhs=xnT[:, kc, :], start=(kc == 0), stop=(kc == 7)), reads=[wrt, xnT], writes=[pb_])
                                    S.dve(lambda e: e.tensor_tensor(out=t1[:, :], in0=pa_[:, :], in1=cs, op=ALU.mult), reads=[pa_, E["cosT"]], writes=[t1])
                                    S.dve(lambda e: e.tensor_tensor(out=t2[:, :], in0=pb_[:, :], in1=sn, op=ALU.mult), reads=[pb_, E["sinT"]], writes=[t2])
                                    S.pool(lambda e: e.tensor_tensor(out=dst[:, h, :], in0=t1[:, :], in1=t2[:, :], op=ALU.add), reads=[t1, t2], writes=[dst.part(h)])
                                    if c0 == 0:
                                        S.pool(lambda e: e.tensor_tensor(out=qx[:, h, :].rearrange("p (a b) -> p a b", a=4),
                                                                         in0=qr[:, h, :].rearrange("p (a b) -> p a b", a=4),
                                                                         in1=E["xiT"][:, h, :].unsqueeze(1).to_broadcast([128, 4, 128]), op=ALU.mult),
                                               reads=[qr.part(h), E["xiT"]], writes=[qx.part(h)])
                            for h in range(4):
                                pa_ = pq_[h % 2]
                                wrt = wchunk(1536 + h * 128)
                                for kc in range(8):
                                    S.pe(lambda e: e.matmul(out=pa_[:, :], lhsT=wrt[:, kc, :], rhs=xnT[:, kc, :],
                                                            start=(kc == 0), stop=(kc == 7)), reads=[wrt, xnT], writes=[pa_])
                                S.act(lambda e: e.activation(out=sg[:, h, :], in_=pa_[:, :], func=AF.Silu), reads=[pa_], writes=[sg.part(h)])
                            for c in range(4):
                                pa_ = ps_[c % 2]
                                for kc in range(8):
                                    S.pe(lambda e: e.matmul(out=pa_[:, :], lhsT=xnT[:, kc, c * 128:(c + 1) * 128], rhs=wvr[:, kc, :],
                                                            start=(kc == 0), stop=(kc == 7)), reads=[wvr, xnT], writes=[pa_])
                                S.act(lambda e: e.copy(out=vtk[:, c, :], in_=pa_[:, :]), reads=[pa_], writes=[vtk.part(c)])
                            S.barrier()
                        with ExitStack() as e3:
                            psc = C.ps(e3, [128, 512], F32, "psc")
                            po_ = C.ps(e3, [128, 512], F32, "po_r")
                            pkv = C.ps(e3, [128, 512], F32, "pkv")
                            ptr = C.ps(e3, [128, 8, 128], BF16, "ptr_r")
                            scT = C.sb(e3, [128, 4, 128], BF16, "scT")
                            ktk = C.sb(e3, [128, 4, 128], BF16, "ktk")
                            for c in range(4):
                                csl = slice(c * 128, (c + 1) * 128)
                                for h in range(4):
                                    S.pe(lambda e: e.matmul(out=psc[:, h * 128:(h + 1) * 128], lhsT=kr[:, h, csl], rhs=qr[:, h, csl], start=True, stop=True),
                                         reads=[kr, qr], writes=[psc])
                                    S.pe(lambda e: e.transpose(out=ptr[:, h, :], in_=kr[:, h, csl], identity=ident[:, :]), reads=[kr, ident], writes=[ptr])
                                S.dve(lambda e: e.tensor_tensor(out=scT[:, :, :], in0=psc[:, :].rearrange("p (a b) -> p a b", a=4), in1=E["dmT"][:, :, :], op=ALU.mult),
                                      reads=[psc, E["dmT"]], writes=[scT])
                                S.dve(lambda e: e.tensor_tensor(out=ktk[:, :, :], in0=ptr[:, 0:4, :], in1=E["zcol"][:, :].unsqueeze(2).to_broadcast([128, 4, 128]),
                                                                op=ALU.mult), reads=[ptr, E["zcol"]], writes=[ktk])
                                for h in range(4):
                                    o_ = po_[:, h * 128:(h + 1) * 128]
                                    S.pe(lambda e: e.matmul(out=o_, lhsT=vtk[:, c, h * 128:(h + 1) * 128], rhs=scT[:, h, :], start=True, stop=False),
                                         reads=[vtk, scT], writes=[po_])
                                    S.pe(lambda e: e.matmul(out=o_, lhsT=Rb[:, h, :], rhs=qx[:, h, csl], start=False, stop=True), reads=[Rb, qx], writes=[po_])
                                S.act(lambda e: e.copy(out=oraw[:, :, csl], in_=po_[:, :].rearrange("p (a b) -> p a b", a=4)), reads=[po_], writes=[oraw.part(c)])
                                for h in range(4):
                                    S.pe(lambda e: e.matmul(out=pkv[:, h * 128:(h + 1) * 128], lhsT=ktk[:, h, :], rhs=vtk[:, c, h * 128:(h + 1) * 128],
                                                            start=True, stop=True), reads=[ktk, vtk], writes=[pkv])
                                for h in range(4):
                                    S.dve(lambda e: e.scalar_tensor_tensor(out=Rt[:, h, :], in0=Rt[:, h, :], scalar=gamC[h], in1=pkv[:, h * 128:(h + 1) * 128],
                                                                           op0=ALU.mult, op1=ALU.add), reads=[Rt, pkv], writes=[Rt])
                                S.act(lambda e: e.copy(out=Rb[:, :, :], in_=Rt[:, :, :]), reads=[Rt], writes=[Rb])
                            S.barrier()
                        with ExitStack() as e3:
                            pm = C.ps(e3, [128, 512], F32, "gn_pm")
                            pq = C.ps(e3, [128, 512], F32, "gn_pq")

                            def post_b(c, dd):
                                S.dve(lambda e: e.tensor_tensor(out=ybT[:, c, t0:t0 + 512], in0=dd[:, :], in1=sg[:, c, :], op=ALU.mult), reads=[dd, sg], writes=[ybT.part((c, blk))])

                            group_norm_T(C, e3, oraw, 4, E["on128"], EPS, lambda c: P.col(("rt_gn_g", i), c), lambda c: P.col(("rt_gn_b", i), c),
                                         post_b, pm, pq)
                            S.barrier()
        with ExitStack() as eo:
            g_bc = C.sb(eo, [128, D], F32, "g_bc")
            load_bcast_row(C, "sp", g_bc, W.L("norm_g", l)[3], D)
            wo = C.sb(eo, [128, 8, D], BF16, "wo_ev")
            load_w(C, wo, W.H("ev_w_out", i), 0, D)
            for g4 in range(4):
                chunks = [(yaT, (lambda ti, c=c, g4=g4: yaT[:, c, (g4 * 4 + ti) * 128:(g4 * 4 + ti + 1) * 128])) for c in range(4)]
                chunks += [(ybT, (lambda ti, c=c, g4=g4: ybT[:, c, (g4 * 4 + ti) * 128:(g4 * 4 + ti + 1) * 128])) for c in range(4)]
                out_proj_residual(C, x, chunks, wo, g_bc, 1.0, [g4 * 4 + t_ for t_ in range(4)])


def build_program(shapes, nseq=2, plan=None, loff=0, hoff=0):
    nc = bass.Bass("TRN2", target_bir_lowering=False)
    W = Wts(nc, shapes, loff, hoff)
    out = nc.dram_tensor("out", [nseq, SEQ, D], F32, kind="ExternalOutput").ap()
    C = Ctx(nc)
    S = C.S
    if plan is None:
        plan = [(l, ph) for l in range(DEPTH) for ph in ("ffn1", "mix", "xa", "ffn2")]
    need_mem = any(ph == "xa" for _, ph in plan)
    with ExitStack() as es:
        K = make_consts(C, es)
        P = build_params(C, es, W, K["identf"])
        x = C.sb(es, [128, NT, D], F32, "xres")
        memT = C.sb(es, [128, 8, MEM], BF16, "memT") if need_mem else None
        for s in range(nseq):
            for t4 in range(NT // 4):
                S.dma("sp", x[:, t4 * 4:(t4 + 1) * 4, :],
                      W["x"][s, t4 * 512:(t4 + 1) * 512, :].rearrange("(t p) d -> p t d", p=128),
                      writes=[x.part(t4 * 4 + i) for i in range(4)])
            if need_mem:
                prep_mem(C, es, W, P, K, s, memT)
            for (l, ph) in plan:
                if ph == "ffn1":
                    ffn_block(C, x, l, 0, W, P, K["ident"])
                elif ph == "ffn2":
                    ffn_block(C, x, l, 1, W, P, K["ident"])
                elif ph == "xa":
                    xattn_block(C, x, l, W, P, K, memT)
                elif ph == "mix":
                    if l % 2 == 0:
                        even_mixer(C, x, l, W, P, K)
                    else:
                        odd_mixer(C, x, l, W, P, K)
            for t4 in range(NT // 4):
                S.dma("sp", out[s, t4 * 512:(t4 + 1) * 512, :].rearrange("(t p) d -> p t d", p=128),
                      x[:, t4 * 4:(t4 + 1) * 4, :], reads=[x.part(t4 * 4 + i) for i in range(4)])
            S.barrier()
        S.finish()
    return nc, W


def kernel(**inputs):
    n = 8
    arrs = {k: np.ascontiguousarray(np.asarray(v), dtype=np.float32) for k, v in inputs.items()}
    per = arrs["x"].shape[0] // n
    shapes = {}
    for k, a in arrs.items():
        shapes[k] = ((per,) + a.shape[1:]) if k in ("x", "mem") else a.shape
    nc, W = build_program(shapes, nseq=per)
    in_maps = []
    for c in range(n):
        m = {}
        for k in W.aps:
            a = arrs[k]
            m[k] = a[c * per:(c + 1) * per] if k in ("x", "mem") else a
        in_maps.append(m)
    res = run_bass_kernel_spmd(nc, in_maps, core_ids=list(range(n)))
    return np.concatenate([r["out"] for r in res.results], axis=0).astype(np.float32)
```

```python
import numpy as np
import concourse.bass as bass
import concourse.mybir as mybir
from concourse.bass_utils import run_bass_kernel_spmd

F32 = mybir.dt.float32
BF16 = mybir.dt.bfloat16
ALU = mybir.AluOpType
AF = mybir.ActivationFunctionType
AX = mybir.AxisListType

D = 1024
SEQ = 2048
NT = SEQ // 128
DEPTH = 4
DFF = 2816
NFC = DFF // 128
MEM = 256
EPS = 1e-6


class Res:
    __slots__ = ("name", "writer", "readers", "parent", "parts", "psum")

    def __init__(self, name, parent=None):
        self.name = name
        self.writer = None
        self.readers = {}
        self.parent = parent
        self.parts = {}
        self.psum = parent.psum if parent is not None else False

    def part(self, key):
        r = self.parts.get(key)
        if r is None:
            r = Res(f"{self.name}/{key}", parent=self)
            self.parts[key] = r
        return r


class T:
    def __init__(self, h, name):
        self.h = h
        self.res = Res(name)

    def __getitem__(self, k):
        return self.h[k]

    def part(self, key):
        return self.res.part(key)


NDSEM = 16
COMPUTE = ("pe", "act", "dve", "pool")


class Sched:
    def __init__(self, nc):
        self.nc = nc
        self.eng = {"pe": nc.tensor, "act": nc.scalar, "dve": nc.vector, "pool": nc.gpsimd, "sp": nc.sync}
        self.sem = {}
        self.cnt = {}
        for e in self.eng:
            self.sem[e] = nc.alloc_semaphore("s_" + e)
            self.cnt[e] = 0
        self.dkeys = []
        for q in ("sp", "pool"):
            for i in range(NDSEM):
                k = ("d", q, i)
                self.sem[k] = nc.alloc_semaphore(f"s_d{q}{i}")
                self.cnt[k] = 0
                self.dkeys.append(k)
        self.known = {e: {} for e in self.eng}
        self.dnext = {"sp": 0, "pool": 0}
        self.pe_pending = None
        self.ninstr = 0

    @staticmethod
    def _rlist(r):
        if isinstance(r, T):
            return r.res
        return r

    def _deps(self, reads, writes, eng=None):
        ev = []
        for r in reads:
            r = self._rlist(r)
            if r.writer:
                ev.append(r.writer)
            if r.parent is not None and r.parent.writer:
                ev.append(r.parent.writer)
            for p in r.parts.values():
                if p.writer:
                    ev.append(p.writer)
            if r.psum:
                chain = [r] + list(r.parts.values()) + ([r.parent] if r.parent is not None else [])
                for c in chain:
                    ev.extend((k, v) for k, v in c.readers.items() if k != eng)
        for w in writes:
            w = self._rlist(w)
            chain = [w] + list(w.parts.values())
            if w.parent is not None:
                chain.append(w.parent)
            for c in chain:
                if c.writer:
                    ev.append(c.writer)
                ev.extend(c.readers.items())
        return ev

    def _wait(self, e, evs):
        kn = self.known[e]
        best = {}
        for k, v in evs:
            if k == e and e in ("pe", "sp"):
                continue
            if kn.get(k, 0) >= v:
                continue
            if best.get(k, 0) < v:
                best[k] = v
        for k, v in best.items():
            self.eng[e].wait_ge(self.sem[k], v)
            kn[k] = v

    def _record(self, ev, reads, writes):
        k, v = ev
        for r in reads:
            r = self._rlist(r)
            if r.readers.get(k, 0) < v:
                r.readers[k] = v
        for w in writes:
            w = self._rlist(w)
            w.writer = ev
            w.readers = {}

    def _flush_pe(self):
        if self.pe_pending is not None:
            self.cnt["pe"] += 1
            self.pe_pending.then_inc(self.sem["pe"], 1)
            self.pe_pending = None

    def op(self, e, fn, reads=(), writes=()):
        if e == "pe":
            self._wait(e, self._deps(reads, writes, e))
            ins = fn(self.eng[e])
            self.pe_pending = ins
            self._record((e, self.cnt[e] + 1), reads, writes)
            self.ninstr += 1
            return ins
        self._flush_pe()
        self._wait(e, self._deps(reads, writes, e))
        ins = fn(self.eng[e])
        self.cnt[e] += 1
        ins.then_inc(self.sem[e], 1)
        self._record((e, self.cnt[e]), reads, writes)
        self.ninstr += 1
        return ins

    def pe(self, fn, reads=(), writes=()):
        return self.op("pe", fn, reads, writes)

    def act(self, fn, reads=(), writes=()):
        return self.op("act", fn, reads, writes)

    def dve(self, fn, reads=(), writes=()):
        return self.op("dve", fn, reads, writes)

    def pool(self, fn, reads=(), writes=()):
        return self.op("pool", fn, reads, writes)

    def dma(self, q, out, in_, reads=(), writes=(), **kw):
        self._flush_pe()
        i = self.dnext[q]
        self.dnext[q] = (i + 1) % NDSEM
        k = ("d", q, i)
        evs = self._deps(reads, writes)
        if self.cnt[k]:
            evs.append((k, self.cnt[k]))
        self._wait(q, evs)
        ins = self.eng[q].dma_start(out=out, in_=in_, **kw)
        self.cnt[k] += 16
        ins.then_inc(self.sem[k], 16)
        self._record((k, self.cnt[k]), reads, writes)
        self.ninstr += 1
        return ins

    def barrier(self):
        self._flush_pe()
        evs = [(e, self.cnt[e]) for e in COMPUTE if self.cnt[e]]
        evs += [(k, self.cnt[k]) for k in self.dkeys if self.cnt[k]]
        for e in list(COMPUTE) + ["sp"]:
            self._wait(e, evs)

    def finish(self):
        self._flush_pe()
        evs = [(e, self.cnt[e]) for e in COMPUTE if self.cnt[e]]
        evs += [(k, self.cnt[k]) for k in self.dkeys if self.cnt[k]]
        self._wait("sp", evs)


class Ctx:
    def __init__(self, nc):
        self.nc = nc
        self.S = Sched(nc)
        self._n = 0

    def sb(self, stack, shape, dt, name=None):
        self._n += 1
        name = f"{name or 'sb'}_{self._n}"
        h = stack.enter_context(self.nc.sbuf_tensor(name, list(shape), dt))
        return T(h, name)

    def ps(self, stack, shape, dt=F32, name=None):
        self._n += 1
        name = f"{name or 'ps'}_{self._n}"
        h = stack.enter_context(self.nc.psum_tensor(name, list(shape), dt))
        t = T(h, name)
        t.res.psum = True
        return t


from contextlib import ExitStack


def load_bcast_row(C, q, dst, src_row, n):
    C.S.dma(q, dst[:, 0:n], src_row.partition_broadcast(128), writes=[dst])


def rms_to_T(C, st, x, g_col, xnT, ntiles, ident, tok0=0):
    S = C.S
    with ExitStack() as es:
        ss = C.sb(es, [128, ntiles], F32, "ss")
        rstd = C.sb(es, [128, ntiles], F32, "rstd")
        junk = C.sb(es, [128, D], BF16, "junk")
        xs = [C.sb(es, [128, D], BF16, f"xs{i}") for i in range(2)]
        tps = [C.ps(es, [128, 8, 128], BF16, f"tp{i}") for i in range(2)]
        S.dve(lambda e: e.memset(ss[:, :], 0.0), writes=[ss])
        for t in range(ntiles):
            S.act(lambda e: e.activation(out=junk[:, :], in_=x[:, tok0 + t, :], func=AF.Square,
                                         accum_out=ss[:, t:t + 1]),
                  reads=[x.part(tok0 + t)], writes=[junk, ss])
        rstd_from_ss(C, ss, rstd, ntiles, 1.0 / D, EPS)
        for t in range(ntiles):
            xb = xs[t % 2]
            tp = tps[t % 2]
            S.act(lambda e: e.activation(out=xb[:, :], in_=x[:, tok0 + t, :], func=AF.Copy,
                                         scale=rstd[:, t:t + 1]),
                  reads=[x.part(tok0 + t), rstd], writes=[xb])
            for kc in range(8):
                S.pe(lambda e: e.transpose(out=tp[:, kc, :], in_=xb[:, kc * 128:(kc + 1) * 128], identity=ident[:, :]),
                     reads=[xb, ident], writes=[tp])
            S.dve(lambda e: e.tensor_tensor(out=xnT[:, :, t * 128:(t + 1) * 128], in0=tp[:, :, :],
                                            in1=g_col.unsqueeze(2).to_broadcast([128, 8, 128]), op=ALU.mult),
                  reads=[tp], writes=[xnT.part(t)])
        S.barrier()


def rstd_from_ss(C, ss, rstd, n, scale, eps):
    S = C.S
    S.dve(lambda e: e.tensor_scalar(out=rstd[:, 0:n], in0=ss[:, 0:n], scalar1=scale, scalar2=eps,
                                    op0=ALU.mult, op1=ALU.add), reads=[ss], writes=[rstd])
    S.act(lambda e: e.activation(out=rstd[:, 0:n], in_=rstd[:, 0:n], func=AF.Sqrt), reads=[rstd], writes=[rstd])
    S.dve(lambda e: e.reciprocal(out=rstd[:, 0:n], in_=rstd[:, 0:n]), reads=[rstd], writes=[rstd])


def post_norm_residual(C, st, x, tile_idx, y_ps, g_bc, coef, scr):
    S = C.S
    ss, rstd, junk, tmp = scr
    S.dve(lambda e: e.memset(ss[:, 0:2], 0.0), writes=[ss])
    for h in range(2):
        S.act(lambda e: e.activation(out=junk[:, 0:512], in_=y_ps[h][:, :], func=AF.Square,
                                     accum_out=ss[:, h:h + 1]), reads=[y_ps[h]], writes=[junk, ss])
    S.dve(lambda e: e.tensor_tensor(out=ss[:, 2:3], in0=ss[:, 0:1], in1=ss[:, 1:2], op=ALU.add), reads=[ss], writes=[ss])
    S.dve(lambda e: e.tensor_scalar(out=rstd[:, 0:1], in0=ss[:, 2:3], scalar1=1.0 / D, scalar2=EPS,
                                    op0=ALU.mult, op1=ALU.add), reads=[ss], writes=[rstd])
    S.act(lambda e: e.activation(out=rstd[:, 0:1], in_=rstd[:, 0:1], func=AF.Sqrt), reads=[rstd], writes=[rstd])
    S.dve(lambda e: e.reciprocal(out=rstd[:, 0:1], in_=rstd[:, 0:1]), reads=[rstd], writes=[rstd])
    if coef != 1.0:
        S.dve(lambda e: e.tensor_scalar(out=rstd[:, 0:1], in0=rstd[:, 0:1], scalar1=float(coef), scalar2=None,
                                        op0=ALU.mult), reads=[rstd], writes=[rstd])
    for h in range(2):
        sl = slice(h * 512, (h + 1) * 512)
        S.dve(lambda e: e.tensor_tensor(out=tmp[:, sl], in0=y_ps[h][:, :], in1=g_bc[:, sl], op=ALU.mult),
              reads=[y_ps[h], g_bc], writes=[tmp])
        S.dve(lambda e: e.scalar_tensor_tensor(out=x[:, tile_idx, sl], in0=tmp[:, sl], scalar=rstd[:, 0:1],
                                               in1=x[:, tile_idx, sl], op0=ALU.mult, op1=ALU.add),
              reads=[tmp, rstd, x.part(tile_idx)], writes=[x.part(tile_idx)])


def ffn_block(C, x, l, j, W, P, ident):
    S = C.S
    nc = C.nc
    n_in = 0 if j == 0 else 6
    n_out = 1 if j == 0 else 7
    wg = W.L("ffn_w_gate", l)[j].rearrange("(kc p) f -> p kc f", p=128)
    wu = W.L("ffn_w_up", l)[j].rearrange("(kc p) f -> p kc f", p=128)
    wd = W.L("ffn_w_down", l)[j].rearrange("(fc p) d -> p fc d", p=128)
    TG = 1024
    with ExitStack() as es:
        g_bc = C.sb(es, [128, D], F32, "g_bc")
        load_bcast_row(C, "sp", g_bc, W.L("norm_g", l)[n_out], D)
        hT = C.sb(es, [128, NFC, TG], BF16, "hT")
        for tg in range(SEQ // TG):
            with ExitStack() as es2:
                xnT = C.sb(es2, [128, 8, TG], BF16, "xnT")
                rms_to_T(C, es2, x, P.cols(("norm_g", l, n_in)), xnT, TG // 128, ident,
                         tok0=tg * (TG // 128))
                wbuf = [(C.sb(es2, [128, 8, 256], BF16, f"wg{i}"), C.sb(es2, [128, 8, 256], BF16, f"wu{i}")) for i in range(2)]
                sg = [C.sb(es2, [128, 512], BF16, f"sg{i}") for i in range(2)]
                gps = [C.ps(es2, [128, 512], F32, f"gps{i}") for i in range(2)]
                ups = [C.ps(es2, [128, 512], F32, f"ups{i}") for i in range(2)]
                it = 0
                for f2 in range(NFC // 2):
                    wgt, wut = wbuf[f2 % 2]
                    S.dma("pool", wgt[:, :, :], wg[:, :, f2 * 256:(f2 + 1) * 256], writes=[wgt])
                    S.dma("pool", wut[:, :, :], wu[:, :, f2 * 256:(f2 + 1) * 256], writes=[wut])
                    for fi in range(2):
                        fc = f2 * 2 + fi
                        for th in range(TG // 512):
                            gp, up, sgt = gps[it % 2], ups[it % 2], sg[it % 2]
                            it += 1
                            for kc in range(8):
                                S.pe(lambda e: e.matmul(out=gp[:, :], lhsT=wgt[:, kc, fi * 128:(fi + 1) * 128],
                                                        rhs=xnT[:, kc, th * 512:(th + 1) * 512],
                                                        start=(kc == 0), stop=(kc == 7)),
                                     reads=[wgt, xnT], writes=[gp])
                            for kc in range(8):
                                S.pe(lambda e: e.matmul(out=up[:, :], lhsT=wut[:, kc, fi * 128:(fi + 1) * 128],
                                                        rhs=xnT[:, kc, th * 512:(th + 1) * 512],
                                                        start=(kc == 0), stop=(kc == 7)),
                                     reads=[wut, xnT], writes=[up])
                            S.act(lambda e: e.activation(out=sgt[:, :], in_=gp[:, :], func=AF.Silu),
                                  reads=[gp], writes=[sgt])
                            S.dve(lambda e: e.tensor_tensor(out=hT[:, fc, th * 512:(th + 1) * 512], in0=up[:, :],
                                                            in1=sgt[:, :], op=ALU.mult),
                                  reads=[up, sgt], writes=[hT.part((fc, th))])
                S.barrier()
            with ExitStack() as es3:
                wdt = C.sb(es3, [128, NFC, D], BF16, "wd")
                for f2 in range(NFC // 2):
                    S.dma("pool", wdt[:, f2 * 2:f2 * 2 + 2, :], wd[:, f2 * 2:f2 * 2 + 2, :], writes=[wdt.part(f2)])
                yps = [[C.ps(es3, [128, 512], F32, f"y{i}{h}") for h in range(2)] for i in range(2)]
                scr = (C.sb(es3, [128, 4], F32, "pss"), C.sb(es3, [128, 2], F32, "prs"),
                       C.sb(es3, [128, 512], BF16, "pjunk"), C.sb(es3, [128, D], F32, "ptmp"))
                for tt in range(TG // 128):
                    yp = yps[tt % 2]
                    for h in range(2):
                        for fc in range(NFC):
                            S.pe(lambda e: e.matmul(out=yp[h][:, :], lhsT=hT[:, fc, tt * 128:(tt + 1) * 128],
                                                    rhs=wdt[:, fc, h * 512:(h + 1) * 512],
                                                    start=(fc == 0), stop=(fc == NFC - 1)),
                                 reads=[hT, wdt.part(fc // 2)], writes=[yp[h]])
                    post_norm_residual(C, es3, x, tg * (TG // 128) + tt, yp, g_bc, 0.5, scr)
                S.barrier()


DBG = {}


class Wts:
    def __init__(self, nc, shapes, loff=0, hoff=0):
        self.nc = nc
        self.shapes = shapes
        self.aps = {}
        self.loff = loff
        self.hoff = hoff

    def __getitem__(self, name):
        if name not in self.aps:
            self.aps[name] = self.nc.dram_tensor(name, list(self.shapes[name]), F32, kind="ExternalInput").ap()
        return self.aps[name]

    def L(self, name, l):
        return self[name][l - self.loff]

    def H(self, name, i):
        return self[name][i - self.hoff]

    def hasL(self, name, l):
        return 0 <= l - self.loff < self.shapes[name][0]

    def hasH(self, name, i):
        return 0 <= i - self.hoff < self.shapes[name][0]


class Params:
    def __init__(self):
        self.rows = {}
        self.t = None

    def cols(self, key, kc0=0, n=8):
        r = self.rows[key]
        return self.t[:, kc0:kc0 + n, r]

    def col(self, key, kc):
        r = self.rows[key]
        return self.t[:, kc, r:r + 1]


def build_params(C, es, W, identf):
    S = C.S
    P = Params()
    rows = []
    for l in range(DEPTH):
        if W.hasL("norm_g", l):
            for n in range(8):
                rows.append((("norm_g", l, n), W.L("norm_g", l)[n], D))
    rows.append((("mem_g",), W["mem_norm_g"], D))
    for i in range(2):
        if "sc_conv_w" in W.shapes and W.hasH("sc_conv_w", i):
            for k in range(3):
                rows.append((("sc_w", i, k), W.H("sc_conv_w", i)[k], 512))
            rows.append((("sc_b", i), W.H("sc_conv_b", i), 512))
            rows.append((("dsa_qg", i), W.H("dsa_q_norm_g", i), 256))
        if "rw_w0" in W.shapes and W.hasH("rw_w0", i):
            for nm in ("rw_w0", "rw_a0", "rw_k_k", "rw_k_a", "rw_ln_g", "rw_ln_b", "rt_gn_g", "rt_gn_b"):
                rows.append(((nm, i), W.H(nm, i), 512))
            rows.append((("rw_r_k", i), W.H("rw_r_k", i).rearrange("h d -> (h d)"), 512))
            rows.append((("rw_mu", i, 0), W.H("rw_mu", i)[0:1024], 1024))
            rows.append((("rw_mu", i, 1), W.H("rw_mu", i)[1024:1792], 768))
    nr = len(rows)
    assert nr <= 128
    PC = C.sb(es, [128, 8, nr], F32, "PC")
    P.t = PC
    with ExitStack() as e1:
        raw = C.sb(e1, [128, D], F32, "praw")
        S.pool(lambda e: e.memset(raw[:, :], 0.0), writes=[raw])
        for r, (key, ap, n) in enumerate(rows):
            P.rows[key] = r
            S.dma("sp", raw[r:r + 1, 0:n], ap.rearrange("(o n) -> o n", o=1), writes=[raw])
        tp = C.ps(e1, [128, 8, 128], F32, "ptp")
        for kc in range(8):
            S.pe(lambda e: e.transpose(out=tp[:, kc, :], in_=raw[:, kc * 128:(kc + 1) * 128], identity=identf[:, :]),
                 reads=[raw, identf], writes=[tp])
        S.dve(lambda e: e.tensor_copy(out=PC[:, :, :], in_=tp[:, :, 0:nr]), reads=[tp], writes=[PC])
        S.barrier()
    return P


def load_w(C, dst, src2d, c0, c1, q="pool"):
    v = src2d.rearrange("(kc p) n -> p kc n", p=128)
    nk = v.shape[1]
    for kc in range(nk):
        C.S.dma(q, dst[:, kc, 0:c1 - c0], v[:, kc, c0:c1], writes=[dst.part(kc)])


def make_consts(C, es):
    S = C.S
    K = {}
    ones_f = C.sb(es, [128, 512], F32, "ones_f")
    S.pool(lambda e: e.memset(ones_f[:, :], 1.0), writes=[ones_f])
    ident = C.sb(es, [128, 128], BF16, "ident")
    identf = C.sb(es, [128, 128], F32, "identf")
    for t in (ident, identf):
        S.pool(lambda e: e.affine_select(out=t[:, :], in_=ones_f[:, 0:128], pattern=[[-1, 128]],
                                         compare_op=ALU.is_equal, fill=0.0, base=0, channel_multiplier=1),
               reads=[ones_f], writes=[t])
    ones_bf = C.sb(es, [128, 128], BF16, "ones_bf")
    S.pool(lambda e: e.memset(ones_bf[:, :], 1.0), writes=[ones_bf])
    selq = C.sb(es, [128, 8, 8], BF16, "selq")
    S.pool(lambda e: e.affine_select(out=selq[:, :, :], in_=ones_f[:, 0:64].rearrange("p (a b) -> p a b", a=8),
                                     pattern=[[1, 8], [-1, 8]], compare_op=ALU.is_equal, fill=0.0, base=0,
                                     channel_multiplier=0), reads=[ones_f], writes=[selq])
    sel8 = C.sb(es, [8, 8, 128], BF16, "sel8")
    with ExitStack() as e0:
        sel8a = C.sb(e0, [8, 8, 128], F32, "sel8a")
        S.pool(lambda e: e.memset(sel8a[:, :, :], 1.0), writes=[sel8a])
        S.pool(lambda e: e.affine_select(out=sel8[:, :, :], in_=sel8a[:, :, :], pattern=[[-1, 8], [0, 128]],
                                         compare_op=ALU.is_equal, fill=0.0, base=0, channel_multiplier=1),
               reads=[sel8a], writes=[sel8])
        S.barrier()
    zer = C.sb(es, [128, 128], F32, "zer")
    S.pool(lambda e: e.memset(zer[:, :], 0.0), writes=[zer])
    cbias = C.sb(es, [128, 128], F32, "cbias")
    S.pool(lambda e: e.affine_select(out=cbias[:, :], in_=zer[:, :], pattern=[[-1, 128]],
                                     compare_op=ALU.is_ge, fill=-1e30, base=0, channel_multiplier=1),
           reads=[zer], writes=[cbias])
    K.update(ones_f=ones_f, ident=ident, identf=identf, ones_bf=ones_bf, selq=selq, sel8=sel8, cbias=cbias, zer=zer)
    return K


def make_negm(C, es, qT, nh, k2m, K, p8):
    S = C.S
    qsq = C.sb(es, [128, nh, 512], BF16, "qsq")
    S.act(lambda e: e.activation(out=qsq[:, :, :], in_=qT[:, 0:nh, :], func=AF.Square), reads=[qT], writes=[qsq])
    for h in range(nh):
        S.pe(lambda e: e.matmul(out=p8[0:8, :], lhsT=K["selq"][:, h, :], rhs=qsq[:, h, :], start=(h == 0), stop=(h == nh - 1)),
             reads=[K["selq"], qsq], writes=[p8])
    nm = C.sb(es, [8, 512], F32, "nm")
    negm8 = C.sb(es, [8, 512], BF16, "negm8")
    S.dve(lambda e: e.tensor_scalar(out=nm[:, :], in0=p8[0:8, :], scalar1=k2m[:, 0:1], scalar2=None, op0=ALU.mult),
          reads=[p8, k2m], writes=[nm])
    S.act(lambda e: e.activation(out=nm[:, :], in_=nm[:, :], func=AF.Sqrt), reads=[nm], writes=[nm])
    S.dve(lambda e: e.tensor_scalar(out=negm8[:, :], in0=nm[:, :], scalar1=-1.0, scalar2=None, op0=ALU.mult),
          reads=[nm], writes=[negm8])
    return negm8


def attn_core(C, K, qT_ap, q_res, ktiles, negm8, h, scale, out_ap, out_res, dv, ps_s, ps_o, ps_r, pts, rinv, mask_eng="pool"):
    S = C.S
    n = len(ktiles)
    for i, (k_ap, v_ap, m_ap, rd) in enumerate(ktiles):
        sp = ps_s[i % 2]
        pt = pts[i % 2]
        S.pe(lambda e: e.matmul(out=sp[:, :], lhsT=k_ap, rhs=qT_ap, start=True, stop=False), reads=rd + [q_res], writes=[sp])
        S.pe(lambda e: e.matmul(out=sp[:, :], lhsT=K["sel8"][:, h, :], rhs=negm8[:, :], start=False, stop=(m_ap is None)),
             reads=[K["sel8"], negm8], writes=[sp])
        if m_ap is not None:
            S.pe(lambda e: e.matmul(out=sp[:, :], lhsT=K["ident"][:, :], rhs=m_ap, start=False, stop=True),
                 reads=rd + [K["ident"]], writes=[sp])
        S.act(lambda e: e.activation(out=pt[:, :], in_=sp[:, :], func=AF.Exp, scale=float(scale)), reads=[sp], writes=[pt])
        S.pe(lambda e: e.matmul(out=ps_o[0:dv, :], lhsT=v_ap, rhs=pt[:, :], start=(i == 0), stop=(i == n - 1)),
             reads=rd + [pt], writes=[ps_o])
        S.pe(lambda e: e.matmul(out=ps_r[0:dv, :], lhsT=K["ones_bf"][:, 0:dv], rhs=pt[:, :], start=(i == 0), stop=(i == n - 1)),
             reads=[K["ones_bf"], pt], writes=[ps_r])
    S.dve(lambda e: e.reciprocal(out=rinv[0:dv, :], in_=ps_r[0:dv, :]), reads=[ps_r], writes=[rinv])
    S.dve(lambda e: e.tensor_tensor(out=out_ap, in0=ps_o[0:dv, :], in1=rinv[0:dv, :], op=ALU.mult),
          reads=[ps_o, rinv], writes=[out_res])


def out_proj_residual(C, x, chunks, w_T, g_bc, coef, tiles):
    S = C.S
    n = len(chunks)
    with ExitStack() as es:
        yps = [[C.ps(es, [128, 512], F32, f"y{i}{h}") for h in range(2)] for i in range(2)]
        scr = (C.sb(es, [128, 4], F32, "pss"), C.sb(es, [128, 2], F32, "prs"),
               C.sb(es, [128, 512], BF16, "pjunk"), C.sb(es, [128, D], F32, "ptmp"))
        for ti, tt in enumerate(tiles):
            yp = yps[ti % 2]
            for h in range(2):
                for c, (yt, f) in enumerate(chunks):
                    S.pe(lambda e: e.matmul(out=yp[h][:, :], lhsT=f(ti), rhs=w_T[:, c, h * 512:(h + 1) * 512],
                                            start=(c == 0), stop=(c == n - 1)), reads=[yt, w_T], writes=[yp[h]])
            post_norm_residual(C, None, x, tt, yp, g_bc, coef, scr)
        S.barrier()


def xattn_block(C, x, l, W, P, K, memT):
    S = C.S
    ident = K["ident"]
    scale = 128 ** -0.5
    with ExitStack() as es:
        g_bc = C.sb(es, [128, D], F32, "g_bc")
        load_bcast_row(C, "sp", g_bc, W.L("norm_g", l)[5], D)
        wq = C.sb(es, [128, 8, 512], BF16, "wq")
        wk = C.sb(es, [128, 8, 512], BF16, "wk")
        wv = C.sb(es, [128, 8, 512], BF16, "wv")
        wo = C.sb(es, [128, 4, D], BF16, "wo")
        load_w(C, wk, W.L("xa_wk", l), 0, 512)
        load_w(C, wv, W.L("xa_wv", l), 0, 512)
        load_w(C, wq, W.L("xa_wq", l), 0, 512)
        wov = W.L("xa_wo", l).rearrange("(c p) d -> p c d", p=128)
        for c in range(4):
            S.dma("pool", wo[:, c, :], wov[:, c, :], writes=[wo.part(c)])
        kT = C.sb(es, [128, 4, MEM], BF16, "kT")
        vtok = C.sb(es, [128, 2, 512], BF16, "vtok")
        k2m = C.sb(es, [8, 1], F32, "k2m")
        with ExitStack() as e1:
            pk = C.ps(e1, [128, 512], F32, "pk")
            for h in range(4):
                for kc in range(8):
                    S.pe(lambda e: e.matmul(out=pk[:, 0:MEM], lhsT=wk[:, kc, h * 128:(h + 1) * 128], rhs=memT[:, kc, :],
                                            start=(kc == 0), stop=(kc == 7)), reads=[wk, memT], writes=[pk])
                S.act(lambda e: e.copy(out=kT[:, h, :], in_=pk[:, 0:MEM]), reads=[pk], writes=[kT])
            for mt in range(2):
                for kc in range(8):
                    S.pe(lambda e: e.matmul(out=pk[:, :], lhsT=memT[:, kc, mt * 128:(mt + 1) * 128], rhs=wv[:, kc, :],
                                            start=(kc == 0), stop=(kc == 7)), reads=[wv, memT], writes=[pk])
                S.dve(lambda e: e.tensor_copy(out=vtok[:, mt, :], in_=pk[:, :]), reads=[pk], writes=[vtok])
            ksq = C.sb(e1, [128, 4, MEM], BF16, "ksq")
            S.act(lambda e: e.activation(out=ksq[:, :, :], in_=kT[:, :, :], func=AF.Square), reads=[kT], writes=[ksq])
            for h in range(4):
                S.pe(lambda e: e.matmul(out=pk[0:8, 0:MEM], lhsT=K["selq"][:, h, :], rhs=ksq[:, h, :], start=(h == 0), stop=(h == 3)),
                     reads=[K["selq"], ksq], writes=[pk])
            S.dve(lambda e: e.reduce_max(out=k2m[:, 0:1], in_=pk[0:8, 0:MEM], axis=AX.X), reads=[pk], writes=[k2m])
            S.barrier()
        for qg in range(4):
            with ExitStack() as e2:
                oT = C.sb(e2, [128, 4, 512], BF16, "oT")
                with ExitStack() as e3:
                    xnT = C.sb(e3, [128, 8, 512], BF16, "xnT")
                    rms_to_T(C, e3, x, P.cols(("norm_g", l, 4)), xnT, 4, ident, tok0=qg * 4)
                    qT = C.sb(e3, [128, 4, 512], BF16, "qT")
                    pq = [C.ps(e3, [128, 512], F32, f"pq{i}") for i in range(2)]
                    for h in range(4):
                        for kc in range(8):
                            S.pe(lambda e: e.matmul(out=pq[h % 2][:, :], lhsT=wq[:, kc, h * 128:(h + 1) * 128], rhs=xnT[:, kc, :],
                                                    start=(kc == 0), stop=(kc == 7)), reads=[wq, xnT], writes=[pq[h % 2]])
                        S.act(lambda e: e.copy(out=qT[:, h, :], in_=pq[h % 2][:, :]), reads=[pq[h % 2]], writes=[qT.part(h)])
                    negm8 = make_negm(C, e3, qT, 4, k2m, K, pq[0])
                    ps_s = [C.ps(e3, [128, 512], F32, f"ss{i}") for i in range(2)]
                    ps_o = C.ps(e3, [128, 512], F32, "pso")
                    ps_r = C.ps(e3, [128, 512], F32, "psr")
                    pts = [C.sb(e3, [128, 512], BF16, f"pt{i}") for i in range(2)]
                    rinv = C.sb(e3, [128, 512], F32, "rinv")
                    for h in range(4):
                        kt_list = [(kT[:, h, kt * 128:(kt + 1) * 128], vtok[:, kt, h * 128:(h + 1) * 128], None, [kT, vtok])
                                   for kt in range(2)]
                        attn_core(C, K, qT[:, h, :], qT, kt_list, negm8, h, scale, oT[:, h, :], oT.part(h), 128,
                                  ps_s, ps_o, ps_r, pts, rinv)
                    S.barrier()
                out_proj_residual(C, x, [(oT, (lambda ti, c=c: oT[:, c, ti * 128:(ti + 1) * 128])) for c in range(4)],
                                  wo, g_bc, 1.0, [qg * 4 + i for i in range(4)])


def prep_mem(C, es, W, P, K, s, memT):
    S = C.S
    with ExitStack() as e1:
        mt = C.sb(e1, [128, 2, D], F32, "memraw")
        S.dma("sp", mt[:, :, :], W["mem"][s].rearrange("(t p) d -> p t d", p=128), writes=[mt.part(0), mt.part(1)])
        rms_to_T(C, e1, mt, P.cols(("mem_g",)), memT, 2, K["ident"], tok0=0)


def odd_mixer(C, x, l, W, P, K):
    S = C.S
    i = l // 2
    ident = K["ident"]
    w_in = W.H("od_w_in", i)
    NEG_SEL = -3.0e38
    with ExitStack() as es:
        g_bc = C.sb(es, [128, D], F32, "g_bc")
        load_bcast_row(C, "sp", g_bc, W.L("norm_g", l)[3], D)
        ycT = C.sb(es, [128, 4, SEQ], BF16, "ycT")
        ydT = C.sb(es, [128, 4, SEQ], BF16, "ydT")
        cqnT = C.sb(es, [128, 2, SEQ], BF16, "cqnT")
        ckvT = C.sb(es, [128, SEQ], BF16, "ckvT")
        ckvtok = C.sb(es, [128, NT, 128], BF16, "ckvtok")
        kidxT2 = C.sb(es, [128, SEQ], BF16, "kidxT2")
        widx = C.sb(es, [128, NT, 8], F32, "widx")
        absw = C.sb(es, [128, NT, 8], F32, "absw")
        sgnw = C.sb(es, [128, NT, 8], F32, "sgnw")
        carry = C.sb(es, [128, 4, 2], F32, "carry")
        S.pool(lambda e: e.memset(carry[:, :, :], 0.0), writes=[carry])
        gkv_bc = C.sb(es, [128, 128], F32, "gkv_bc")
        load_bcast_row(C, "sp", gkv_bc, W.H("dsa_kv_norm_g", i), 128)
        TG = 1024
        with ExitStack() as e1:
            wsm = C.sb(e1, [128, 8, 456], BF16, "wsm")
            load_w(C, wsm, w_in, 0, 456)
            wscs = [C.sb(e1, [128, 8, 3, 128], BF16, f"wsc{k}") for k in range(2)]
            ss = C.sb(e1, [128, 2], F32, "ss2")
            rs = C.sb(e1, [128, 2], F32, "rs2")
            junk = C.sb(e1, [128, 256], BF16, "junk2")
            cqs = C.sb(e1, [128, 256], BF16, "cqs")
            kid2 = C.sb(e1, [128, 2, 64], BF16, "kid2")
            hs = C.sb(e1, [128, 512], F32, "hs")
            ub = C.sb(e1, [128, 514], F32, "ub")
            yb = C.sb(e1, [128, 512], F32, "yb")
            w_v = w_in.rearrange("(kc p) n -> p kc n", p=128)
            for tg in range(SEQ // TG):
                with ExitStack() as e2:
                    xnT = C.sb(e2, [128, 8, TG], BF16, "xnT")
                    rms_to_T(C, e2, x, P.cols(("norm_g", l, 2)), xnT, TG // 128, ident, tok0=tg * (TG // 128))
                    pp = C.ps(e2, [128, 512], F32, "pp")
                    tp = C.ps(e2, [128, 8, 128], BF16, "tp4")
                    for tt in range(TG // 128 if DBG.get("odd_stop") != 0.25 else 0):
                        Tt = tg * (TG // 128) + tt
                        for kc in range(8):
                            S.pe(lambda e: e.matmul(out=pp[:, 0:456], lhsT=xnT[:, kc, tt * 128:(tt + 1) * 128], rhs=wsm[:, kc, 0:456],
                                                    start=(kc == 0), stop=(kc == 7)), reads=[xnT, wsm], writes=[pp])
                        S.dve(lambda e: e.memset(ss[:, :], 0.0), writes=[ss])
                        S.act(lambda e: e.activation(out=junk[:, 0:256], in_=pp[:, 0:256], func=AF.Square, accum_out=ss[:, 0:1]),
                              reads=[pp], writes=[junk, ss])
                        S.act(lambda e: e.activation(out=junk[:, 0:128], in_=pp[:, 256:384], func=AF.Square, accum_out=ss[:, 1:2]),
                              reads=[pp], writes=[junk, ss])
                        S.dve(lambda e: e.tensor_scalar(out=rs[:, 0:1], in0=ss[:, 0:1], scalar1=1.0 / 256, scalar2=EPS,
                                                        op0=ALU.mult, op1=ALU.add), reads=[ss], writes=[rs])
                        S.dve(lambda e: e.tensor_scalar(out=rs[:, 1:2], in0=ss[:, 1:2], scalar1=1.0 / 128, scalar2=EPS,
                                                        op0=ALU.mult, op1=ALU.add), reads=[ss], writes=[rs])
                        S.act(lambda e: e.activation(out=rs[:, :], in_=rs[:, :], func=AF.Sqrt), reads=[rs], writes=[rs])
                        S.dve(lambda e: e.reciprocal(out=rs[:, :], in_=rs[:, :]), reads=[rs], writes=[rs])
                        S.act(lambda e: e.activation(out=cqs[:, :], in_=pp[:, 0:256], func=AF.Copy, scale=rs[:, 0:1]),
                              reads=[pp, rs], writes=[cqs])
                        S.dve(lambda e: e.scalar_tensor_tensor(out=ckvtok[:, Tt, :], in0=pp[:, 256:384], scalar=rs[:, 1:2],
                                                               in1=gkv_bc[:, :], op0=ALU.mult, op1=ALU.mult),
                              reads=[pp, rs, gkv_bc], writes=[ckvtok.part(Tt)])
                        S.dve(lambda e: e.tensor_copy(out=kid2[:, :, :], in_=pp[:, 384:448].unsqueeze(1).to_broadcast([128, 2, 64])),
                              reads=[pp], writes=[kid2])
                        S.dve(lambda e: e.tensor_copy(out=widx[:, Tt, :], in_=pp[:, 448:456]), reads=[pp], writes=[widx.part(Tt)])
                        for c in range(2):
                            S.pe(lambda e: e.transpose(out=tp[:, c, :], in_=cqs[:, c * 128:(c + 1) * 128], identity=ident[:, :]),
                                 reads=[cqs, ident], writes=[tp])
                        S.pe(lambda e: e.transpose(out=tp[:, 2, :], in_=ckvtok[:, Tt, :], identity=ident[:, :]),
                             reads=[ckvtok.part(Tt), ident], writes=[tp])
                        S.pe(lambda e: e.transpose(out=tp[:, 3, :], in_=kid2[:, :, :].rearrange("p a b -> p (a b)"), identity=ident[:, :]),
                             reads=[kid2, ident], writes=[tp])
                        tsl = slice(Tt * 128, (Tt + 1) * 128)
                        S.dve(lambda e: e.tensor_tensor(out=cqnT[:, :, tsl], in0=tp[:, 0:2, :],
                                                        in1=P.cols(("dsa_qg", i), 0, 2).unsqueeze(2).to_broadcast([128, 2, 128]),
                                                        op=ALU.mult), reads=[tp, P.t], writes=[cqnT.part(Tt)])
                        S.act(lambda e: e.copy(out=ckvT[:, tsl], in_=tp[:, 2, :]), reads=[tp], writes=[ckvT.part(Tt)])
                        S.act(lambda e: e.copy(out=kidxT2[:, tsl], in_=tp[:, 3, :]), reads=[tp], writes=[kidxT2.part(Tt)])
                    pcs = [C.ps(e2, [128, 512], F32, f"pc{k}") for k in range(3)]
                    for fc in range(4 if DBG.get("odd_stop") != 0.5 else 0):
                        wsc = wscs[fc % 2]
                        for kc in range(8):
                            S.dma("pool", wsc[:, kc, :, :],
                                  w_v[:, kc, 456:1992].rearrange("p (j c) -> p j c", j=3)[:, :, fc * 128:(fc + 1) * 128],
                                  writes=[wsc.part(kc)])
                        for th in range(TG // 512):
                            tok0 = tg * TG + th * 512
                            for j3 in range(3):
                                for kc in range(8):
                                    S.pe(lambda e: e.matmul(out=pcs[j3][:, :], lhsT=wsc[:, kc, j3, :], rhs=xnT[:, kc, th * 512:(th + 1) * 512],
                                                            start=(kc == 0), stop=(kc == 7)), reads=[wsc, xnT], writes=[pcs[j3]])
                            S.act(lambda e: e.copy(out=hs[:, :], in_=pcs[0][:, :]), reads=[pcs[0]], writes=[hs])
                            S.pool(lambda e: e.tensor_copy(out=ub[:, 0:2], in_=carry[:, fc, :]), reads=[carry.part(fc)], writes=[ub])
                            S.dve(lambda e: e.tensor_tensor(out=ub[:, 2:514], in0=pcs[2][:, :], in1=hs[:, :], op=ALU.mult),
                                  reads=[pcs[2], hs], writes=[ub])
                            S.pool(lambda e: e.tensor_copy(out=carry[:, fc, :], in_=ub[:, 512:514]), reads=[ub], writes=[carry.part(fc)])
                            S.pool(lambda e: e.tensor_scalar(out=yb[:, :], in0=ub[:, 2:514], scalar1=P.col(("sc_w", i, 2), fc),
                                                             scalar2=P.col(("sc_b", i), fc), op0=ALU.mult, op1=ALU.add),
                                   reads=[ub, P.t], writes=[yb])
                            S.dve(lambda e: e.scalar_tensor_tensor(out=yb[:, :], in0=ub[:, 1:513], scalar=P.col(("sc_w", i, 1), fc),
                                                                   in1=yb[:, :], op0=ALU.mult, op1=ALU.add), reads=[ub, yb, P.t], writes=[yb])
                            S.dve(lambda e: e.scalar_tensor_tensor(out=yb[:, :], in0=ub[:, 0:512], scalar=P.col(("sc_w", i, 0), fc),
                                                                   in1=yb[:, :], op0=ALU.mult, op1=ALU.add), reads=[ub, yb, P.t], writes=[yb])
                            S.dve(lambda e: e.tensor_tensor(out=ydT[:, fc, tok0:tok0 + 512], in0=pcs[1][:, :], in1=yb[:, :], op=ALU.mult),
                                  reads=[pcs[1], yb], writes=[ydT.part((fc, tok0))])
                    S.barrier()
            S.act(lambda e: e.activation(out=absw[:, :, :], in_=widx[:, :, :], func=AF.Abs), reads=[widx], writes=[absw])
            S.act(lambda e: e.activation(out=sgnw[:, :, :], in_=widx[:, :, :], func=AF.Sign), reads=[widx], writes=[sgnw])
            S.barrier()
        if DBG.get("odd_stop") in (1, 0.5, 0.25):
            return
        with ExitStack() as e1:
            wqi = C.sb(e1, [128, 2, 512], BF16, "wqi")
            wuq = C.sb(e1, [128, 2, 512], BF16, "wuq")
            wuk = C.sb(e1, [128, 4, 128], BF16, "wuk")
            wuv = C.sb(e1, [128, 8, 64], BF16, "wuv")
            load_w(C, wqi, W.H("dsa_w_qi", i).rearrange("r h d -> r (h d)"), 0, 512)
            load_w(C, wuq, W.H("dsa_w_uq", i).rearrange("r h d -> r (h d)"), 0, 512)
            S.dma("pool", wuk[:, :, :], W.H("dsa_w_uk", i).rearrange("(hp e) d c -> (e d) hp c", e=2), writes=[wuk])
            S.dma("pool", wuv[:, :, :], W.H("dsa_w_uv", i).rearrange("h c d -> c h d"), writes=[wuv])
            k2m = C.sb(e1, [8, 1], F32, "k2m")
            with ExitStack() as e2:
                ksq = C.sb(e2, [128, SEQ], BF16, "ksq")
                k2b = C.sb(e2, [8, 4], F32, "k2b")
                pk = C.ps(e2, [128, 512], F32, "pk")
                S.act(lambda e: e.activation(out=ksq[:, :], in_=ckvT[:, :], func=AF.Square), reads=[ckvT], writes=[ksq])
                for b in range(4):
                    S.pe(lambda e: e.matmul(out=pk[0:8, :], lhsT=K["ones_bf"][:, 0:8], rhs=ksq[:, b * 512:(b + 1) * 512], start=True, stop=True),
                         reads=[K["ones_bf"], ksq], writes=[pk])
                    S.dve(lambda e: e.reduce_max(out=k2b[:, b:b + 1], in_=pk[0:8, :], axis=AX.X), reads=[pk], writes=[k2b])
                S.dve(lambda e: e.reduce_max(out=k2m[:, 0:1], in_=k2b[:, :], axis=AX.X), reads=[k2b], writes=[k2m])
                S.barrier()
            for qg in range(4):
                tsl = slice(qg * 512, (qg + 1) * 512)
                nkt = 4 * qg + 4
                with ExitStack() as e2:
                    qidxT = C.sb(e2, [128, 4, 512], BF16, "qidxT")
                    qhT = C.sb(e2, [128, 4, 512], BF16, "qhT")
                    qlatT = C.sb(e2, [128, 8, 512], BF16, "qlatT")
                    maskT = C.sb(e2, [128, nkt, 512], BF16, "maskT")
                    S.pool(lambda e: e.memset(maskT[:, :, :], -30000.0), writes=[maskT])
                    with ExitStack() as e3:
                        pq = [C.ps(e3, [128, 512], F32, f"pq{k}") for k in range(2)]
                        n_ = 0
                        for (wt, dst) in ((wqi, qidxT), (wuq, qhT)):
                            for hp in range(4):
                                p_ = pq[n_ % 2]
                                n_ += 1
                                for rc in range(2):
                                    S.pe(lambda e: e.matmul(out=p_[:, :], lhsT=wt[:, rc, hp * 128:(hp + 1) * 128], rhs=cqnT[:, rc, tsl],
                                                            start=(rc == 0), stop=(rc == 1)), reads=[wt, cqnT], writes=[p_])
                                S.act(lambda e: e.copy(out=dst[:, hp, :], in_=p_[:, :]), reads=[p_], writes=[dst.part(hp)])
                        for h in range(8):
                            hp, e_ = h // 2, h % 2
                            p_ = pq[h % 2]
                            S.pe(lambda e: e.matmul(out=p_[:, :], lhsT=wuk[e_ * 64:(e_ + 1) * 64, hp, :], rhs=qhT[e_ * 64:(e_ + 1) * 64, hp, :],
                                                    start=True, stop=True), reads=[wuk, qhT], writes=[p_])
                            S.dve(lambda e: e.tensor_copy(out=qlatT[:, h, :], in_=p_[:, :]), reads=[p_], writes=[qlatT.part(h)])
                        S.barrier()
                    if DBG.get("odd_stop") == 2:
                        continue
                    with ExitStack() as e3:
                        lps = [C.ps(e3, [128, 512], F32, f"lp{k}") for k in range(2)]
                        tpm = C.ps(e3, [128, 8, 128], BF16, "tpm")
                        sc = C.sb(e3, [128, SEQ], F32, "sc")
                        mk = C.sb(e3, [128, SEQ], BF16, "mk")
                        rb = [C.sb(e3, [128, 512], F32, f"rb{k}") for k in range(2)]
                        st4 = C.sb(e3, [128, 4], F32, "st4")
                        uu = C.sb(e3, [128, 1], F32, "uu")
                        cntt = C.sb(e3, [128, 1], F32, "cntt")
                        dd_ = C.sb(e3, [128, 1], F32, "dd_")
                        n_ = 0
                        for ql in range(4):
                            qt = 4 * qg + ql
                            nk = (qt + 1) * 128
                            for kb in range((nk + 511) // 512):
                                n = min(512, nk - kb * 512)
                                ksl = slice(kb * 512, kb * 512 + n)
                                for h in range(8):
                                    hp, e_ = h // 2, h % 2
                                    lp = lps[n_ % 2]
                                    r_ = rb[n_ % 2]
                                    n_ += 1
                                    S.pe(lambda e: e.matmul(out=lp[:, 0:n], lhsT=qidxT[e_ * 64:(e_ + 1) * 64, hp, ql * 128:(ql + 1) * 128],
                                                            rhs=kidxT2[e_ * 64:(e_ + 1) * 64, ksl], start=True, stop=True),
                                         reads=[qidxT, kidxT2], writes=[lp])
                                    S.act(lambda e: e.activation(out=r_[:, 0:n], in_=lp[:, 0:n], func=AF.Relu, scale=absw[:, qt, h:h + 1]),
                                          reads=[lp, absw], writes=[r_])
                                    eng = "dve"
                                    if h == 0:
                                        S.op(eng, lambda e: e.tensor_scalar(out=sc[:, ksl], in0=r_[:, 0:n], scalar1=sgnw[:, qt, 0:1],
                                                                            scalar2=None, op0=ALU.mult), reads=[r_, sgnw], writes=[sc])
                                    elif eng == "dve":
                                        S.dve(lambda e: e.scalar_tensor_tensor(out=sc[:, ksl], in0=r_[:, 0:n], scalar=sgnw[:, qt, h:h + 1],
                                                                               in1=sc[:, ksl], op0=ALU.mult, op1=ALU.add),
                                              reads=[r_, sgnw, sc], writes=[sc])
                                    else:
                                        S.pool(lambda e: e.tensor_scalar(out=r_[:, 0:n], in0=r_[:, 0:n], scalar1=sgnw[:, qt, h:h + 1],
                                                                         scalar2=None, op0=ALU.mult), reads=[r_, sgnw], writes=[r_])
                                        S.pool(lambda e: e.tensor_tensor(out=sc[:, ksl], in0=sc[:, ksl], in1=r_[:, 0:n], op=ALU.add),
                                               reads=[r_, sc], writes=[sc])
                            dsl = slice(qt * 128, (qt + 1) * 128)
                            if qt >= 2:
                                S.dve(lambda e: e.tensor_reduce(out=st4[:, 0:1], in_=sc[:, 0:nk], axis=AX.X, op=ALU.max), reads=[sc], writes=[st4])
                                S.dve(lambda e: e.tensor_reduce(out=st4[:, 1:2], in_=sc[:, 0:nk], axis=AX.X, op=ALU.min), reads=[sc], writes=[st4])
                                S.dve(lambda e: e.tensor_tensor(out=st4[:, 2:3], in0=st4[:, 0:1], in1=st4[:, 1:2], op=ALU.subtract), reads=[st4], writes=[st4])
                                S.dve(lambda e: e.tensor_scalar(out=st4[:, 2:3], in0=st4[:, 2:3], scalar1=1e-30, scalar2=None, op0=ALU.max), reads=[st4], writes=[st4])
                                S.dve(lambda e: e.reciprocal(out=st4[:, 3:4], in_=st4[:, 2:3]), reads=[st4], writes=[st4])
                                S.dve(lambda e: e.tensor_scalar(out=sc[:, 0:nk], in0=sc[:, 0:nk], scalar1=st4[:, 1:2], scalar2=st4[:, 3:4],
                                                                op0=ALU.subtract, op1=ALU.mult), reads=[sc, st4], writes=[sc])
                                S.pool(lambda e: e.tensor_tensor(out=sc[:, dsl], in0=sc[:, dsl], in1=K["cbias"][:, :], op=ALU.add),
                                       reads=[sc, K["cbias"]], writes=[sc])
                                S.dve(lambda e: e.memset(uu[:, :], 0.5), writes=[uu])
                                for it in range(16):
                                    S.dve(lambda e: e.tensor_scalar(out=mk[:, 0:nk], in0=sc[:, 0:nk], scalar1=uu[:, 0:1], scalar2=0.0,
                                                                    op0=ALU.is_ge, op1=ALU.add, accum_out=cntt[:, 0:1]),
                                          reads=[sc, uu], writes=[mk, cntt])
                                    S.dve(lambda e: e.tensor_scalar(out=dd_[:, :], in0=cntt[:, :], scalar1=255.5, scalar2=float(2.0 ** -(it + 1)),
                                                                    op0=ALU.is_ge, op1=ALU.mult), reads=[cntt], writes=[dd_])
                                    S.dve(lambda e: e.scalar_tensor_tensor(out=uu[:, :], in0=dd_[:, :], scalar=-float(2.0 ** -(it + 2)), in1=uu[:, :],
                                                                           op0=ALU.add, op1=ALU.add), reads=[dd_, uu], writes=[uu])
                                S.dve(lambda e: e.tensor_scalar(out=mk[:, 0:nk], in0=sc[:, 0:nk], scalar1=uu[:, 0:1], scalar2=None, op0=ALU.is_ge),
                                      reads=[sc, uu], writes=[mk])
                            else:
                                S.pool(lambda e: e.tensor_tensor(out=sc[:, dsl], in0=sc[:, dsl], in1=K["cbias"][:, :], op=ALU.add),
                                       reads=[sc, K["cbias"]], writes=[sc])
                                S.dve(lambda e: e.tensor_single_scalar(out=mk[:, 0:nk], in_=sc[:, 0:nk], scalar=-1e29, op=ALU.is_gt),
                                      reads=[sc], writes=[mk])
                            for k0 in range(0, qt + 1, 8):
                                cnt = min(8, qt + 1 - k0)
                                for kk in range(cnt):
                                    kt = k0 + kk
                                    S.pe(lambda e: e.transpose(out=tpm[:, kk, :], in_=mk[:, kt * 128:(kt + 1) * 128], identity=ident[:, :]),
                                         reads=[mk, ident], writes=[tpm])
                                S.dve(lambda e: e.tensor_scalar(out=maskT[:, k0:k0 + cnt, ql * 128:(ql + 1) * 128], in0=tpm[:, 0:cnt, :],
                                                                scalar1=30000.0, scalar2=-30000.0, op0=ALU.mult, op1=ALU.add),
                                      reads=[tpm], writes=[maskT])
                        S.barrier()
                    if DBG.get("odd_stop") == 3:
                        continue
                    with ExitStack() as e3:
                        p8 = C.ps(e3, [128, 512], F32, "p8")
                        negm8 = make_negm(C, e3, qlatT, 8, k2m, K, p8)
                        ps_s = [C.ps(e3, [128, 512], F32, f"ss{k}") for k in range(2)]
                        ps_o = C.ps(e3, [128, 512], F32, "pso")
                        ps_r = C.ps(e3, [128, 512], F32, "psr")
                        po = C.ps(e3, [128, 512], F32, "po")
                        pts = [C.sb(e3, [128, 512], BF16, f"pt{k}") for k in range(2)]
                        rinv = C.sb(e3, [128, 512], F32, "rinv")
                        olat = [C.sb(e3, [128, 512], BF16, f"olat{k}") for k in range(2)]
                        for h in range(8):
                            hp, e_ = h // 2, h % 2
                            kt_list = [(ckvT[:, kt * 128:(kt + 1) * 128], ckvtok[:, kt, :], maskT[:, kt, :], [ckvT, ckvtok, maskT])
                                       for kt in range(nkt)]
                            ol = olat[h % 2]
                            attn_core(C, K, qlatT[:, h, :], qlatT, kt_list, negm8, h, 64 ** -0.5, ol[:, :], ol, 128,
                                      ps_s, ps_o, ps_r, pts, rinv, mask_eng=("pool" if h % 2 else "dve"))
                            S.pe(lambda e: e.matmul(out=po[e_ * 64:(e_ + 1) * 64, :], lhsT=wuv[:, h, :], rhs=ol[:, :], start=True, stop=True),
                                 reads=[wuv, ol], writes=[po])
                            if e_ == 1:
                                S.act(lambda e: e.copy(out=ycT[:, hp, tsl], in_=po[:, :]), reads=[po], writes=[ycT.part((hp, qg))])
                        S.barrier()
        with ExitStack() as e1:
            wo = C.sb(e1, [128, 8, D], BF16, "wo")
            load_w(C, wo, W.H("od_w_out", i), 0, D)
            for g4 in range(4):
                chunks = [(ycT, (lambda ti, c=c, g4=g4: ycT[:, c, (g4 * 4 + ti) * 128:(g4 * 4 + ti + 1) * 128])) for c in range(4)]
                chunks += [(ydT, (lambda ti, c=c, g4=g4: ydT[:, c, (g4 * 4 + ti) * 128:(g4 * 4 + ti + 1) * 128])) for c in range(4)]
                out_proj_residual(C, x, chunks, wo, g_bc, 1.0, [g4 * 4 + t_ for t_ in range(4)])


RW_LN_EPS = 64e-5
TWO_PI = 6.283185307179586


def make_even_consts(C, es, K):
    S = C.S
    ones_f = K["ones_f"]
    E = {}
    o4 = ones_f[:, 0:512].rearrange("p (a b) -> p a b", a=4)
    for nm, pat, cm, op in (("m_su", [[0, 4], [1, 128]], -1, ALU.is_gt), ("m_ui", [[0, 4], [1, 128]], -1, ALU.is_ge),
                            ("m_sl", [[0, 4], [-1, 128]], 1, ALU.is_gt)):
        t = C.sb(es, [128, 4, 128], F32, nm)
        S.pool(lambda e: e.affine_select(out=t[:, :, :], in_=o4, pattern=pat, compare_op=op, fill=0.0, base=0,
                                         channel_multiplier=cm), reads=[ones_f], writes=[t])
        E[nm] = t
    lvm = C.sb(es, [128, 7, 128], BF16, "lvm")
    lvmT = C.sb(es, [128, 7, 128], BF16, "lvmT")
    with ExitStack() as e0:
        I32 = mybir.dt.int32
        pi = C.sb(e0, [128, 128], I32, "lv_pi")
        fi = C.sb(e0, [128, 128], I32, "lv_fi")
        S.pool(lambda e: e.iota(pi[:, :], pattern=[[0, 128]], base=0, channel_multiplier=1), writes=[pi])
        S.pool(lambda e: e.iota(fi[:, :], pattern=[[1, 128]], base=0, channel_multiplier=0), writes=[fi])
        ta = C.sb(e0, [128, 128], I32, "lv_ta")
        tb = C.sb(e0, [128, 128], I32, "lv_tb")
        eq = C.sb(e0, [128, 128], F32, "lv_eq")
        bp = C.sb(e0, [128, 128], F32, "lv_bp")
        bq = C.sb(e0, [128, 128], F32, "lv_bq")
        nbp = C.sb(e0, [128, 128], F32, "lv_nbp")
        nbq = C.sb(e0, [128, 128], F32, "lv_nbq")
        for s in range(7):
            S.dve(lambda e: e.tensor_scalar(out=ta[:, :], in0=pi[:, :], scalar1=s + 1, scalar2=None, op0=ALU.arith_shift_right), reads=[pi], writes=[ta])
            S.dve(lambda e: e.tensor_scalar(out=tb[:, :], in0=fi[:, :], scalar1=s + 1, scalar2=None, op0=ALU.arith_shift_right), reads=[fi], writes=[tb])
            S.dve(lambda e: e.tensor_tensor(out=eq[:, :], in0=ta[:, :], in1=tb[:, :], op=ALU.is_equal), reads=[ta, tb], writes=[eq])
            S.dve(lambda e: e.tensor_scalar(out=ta[:, :], in0=pi[:, :], scalar1=s, scalar2=1, op0=ALU.arith_shift_right, op1=ALU.bitwise_and), reads=[pi], writes=[ta])
            S.dve(lambda e: e.tensor_scalar(out=tb[:, :], in0=fi[:, :], scalar1=s, scalar2=1, op0=ALU.arith_shift_right, op1=ALU.bitwise_and), reads=[fi], writes=[tb])
            S.dve(lambda e: e.tensor_copy(out=bp[:, :], in_=ta[:, :]), reads=[ta], writes=[bp])
            S.dve(lambda e: e.tensor_copy(out=bq[:, :], in_=tb[:, :]), reads=[tb], writes=[bq])
            S.dve(lambda e: e.tensor_scalar(out=nbp[:, :], in0=bp[:, :], scalar1=-1.0, scalar2=1.0, op0=ALU.mult, op1=ALU.add), reads=[bp], writes=[nbp])
            S.dve(lambda e: e.tensor_scalar(out=nbq[:, :], in0=bq[:, :], scalar1=-1.0, scalar2=1.0, op0=ALU.mult, op1=ALU.add), reads=[bq], writes=[nbq])
            S.dve(lambda e: e.tensor_tensor(out=nbp[:, :], in0=nbp[:, :], in1=bq[:, :], op=ALU.mult), reads=[nbp, bq], writes=[nbp])
            S.dve(lambda e: e.tensor_tensor(out=lvm[:, s, :], in0=nbp[:, :], in1=eq[:, :], op=ALU.mult), reads=[nbp, eq], writes=[lvm])
            S.dve(lambda e: e.tensor_tensor(out=nbq[:, :], in0=nbq[:, :], in1=bp[:, :], op=ALU.mult), reads=[nbq, bp], writes=[nbq])
            S.dve(lambda e: e.tensor_tensor(out=lvmT[:, s, :], in0=nbq[:, :], in1=eq[:, :], op=ALU.mult), reads=[nbq, eq], writes=[lvmT])
        S.barrier()
    E.update(lvm=lvm, lvmT=lvmT)
    id8 = C.sb(es, [128, 8, 128], BF16, "id8")
    with ExitStack() as e0:
        ones8 = C.sb(e0, [128, 8, 128], F32, "ones8")
        S.pool(lambda e: e.memset(ones8[:, :, :], 1.0), writes=[ones8])
        S.pool(lambda e: e.affine_select(out=id8[:, :, :], in_=ones8[:, :, :], pattern=[[0, 8], [-1, 128]], compare_op=ALU.is_equal, fill=0.0,
                                         base=0, channel_multiplier=1), reads=[ones8], writes=[id8])
        S.barrier()
    E["id8"] = id8
    bo = C.sb(es, [128, 128], BF16, "blockones")
    bof = C.sb(es, [128, 128], BF16, "blockones_f")
    S.pool(lambda e: e.memset(bo[:, :], 0.0), writes=[bo])
    S.pool(lambda e: e.memset(bof[:, :], 0.0), writes=[bof])
    for b in range(2):
        S.pool(lambda e: e.memset(bo[b * 64:(b + 1) * 64, b * 64:(b + 1) * 64], 1.0), writes=[bo])
        S.pool(lambda e: e.memset(bof[b * 64:(b + 1) * 64, b * 64:(b + 1) * 64], 1.0 / 64), writes=[bof])
    on128 = C.sb(es, [128, 128], BF16, "on128")
    S.pool(lambda e: e.memset(on128[:, :], 1.0 / 128), writes=[on128])
    E.update(bo=bo, bof=bof, on128=on128)
    seg = C.sb(es, [128, 4, 128], F32, "seg")
    S.pool(lambda e: e.memset(seg[:, :, :], 1.0), writes=[seg])
    S.pool(lambda e: e.memset(seg[:, :, 0:1], 0.0), writes=[seg])
    E["seg"] = seg
    cosT = C.sb(es, [128, SEQ], BF16, "cosT")
    sinT = C.sb(es, [128, SEQ], BF16, "sinT")
    with ExitStack() as e1:
        jc_i = C.sb(e1, [128, 1], mybir.dt.int32, "jc_i")
        for b in range(2):
            S.pool(lambda e: e.iota(jc_i[b * 64:(b + 1) * 64, :], pattern=[[0, 1]], base=0, channel_multiplier=1), writes=[jc_i])
        jc = C.sb(e1, [128, 1], F32, "jc")
        S.dve(lambda e: e.tensor_copy(out=jc[:, :], in_=jc_i[:, :]), reads=[jc_i], writes=[jc])
        invf = C.sb(e1, [128, 1], F32, "invf")
        S.act(lambda e: e.activation(out=invf[:, :], in_=jc[:, :], func=AF.Exp, scale=-float(np.log(10000.0)) / 64.0),
              reads=[jc], writes=[invf])
        tp_i = C.sb(e1, [128, SEQ], mybir.dt.int32, "tp_i")
        S.pool(lambda e: e.iota(tp_i[:, :], pattern=[[1, SEQ]], base=0, channel_multiplier=0), writes=[tp_i])
        ang = C.sb(e1, [128, SEQ], F32, "ang")
        S.dve(lambda e: e.tensor_copy(out=ang[:, :], in_=tp_i[:, :]), reads=[tp_i], writes=[ang])
        S.dve(lambda e: e.tensor_scalar(out=ang[:, :], in0=ang[:, :], scalar1=invf[:, 0:1], scalar2=None, op0=ALU.mult),
              reads=[ang, invf], writes=[ang])
        sgn = C.sb(e1, [128, 1], F32, "sgn")
        S.pool(lambda e: e.memset(sgn[0:64, :], -1.0), writes=[sgn])
        S.pool(lambda e: e.memset(sgn[64:128, :], 1.0), writes=[sgn])
        red = C.sb(e1, [128, SEQ], F32, "red")
        qi = C.sb(e1, [128, SEQ], mybir.dt.int32, "qi")
        qf = C.sb(e1, [128, SEQ], F32, "qf")
        for (dst, shift) in ((sinT, 0.0), (cosT, float(np.pi / 2))):
            S.dve(lambda e: e.tensor_scalar(out=qf[:, :], in0=ang[:, :], scalar1=shift, scalar2=1.0 / TWO_PI, op0=ALU.add, op1=ALU.mult),
                  reads=[ang], writes=[qf])
            S.dve(lambda e: e.tensor_copy(out=qi[:, :], in_=qf[:, :]), reads=[qf], writes=[qi])
            S.dve(lambda e: e.tensor_copy(out=qf[:, :], in_=qi[:, :]), reads=[qi], writes=[qf])
            S.dve(lambda e: e.scalar_tensor_tensor(out=red[:, :], in0=qf[:, :], scalar=-TWO_PI, in1=ang[:, :], op0=ALU.mult, op1=ALU.add),
                  reads=[qf, ang], writes=[red])
            if shift:
                S.dve(lambda e: e.tensor_scalar(out=red[:, :], in0=red[:, :], scalar1=shift, scalar2=None, op0=ALU.add), reads=[red], writes=[red])
            S.dve(lambda e: e.tensor_scalar(out=qf[:, :], in0=red[:, :], scalar1=float(np.pi), scalar2=-TWO_PI, op0=ALU.is_gt, op1=ALU.mult),
                  reads=[red], writes=[qf])
            S.dve(lambda e: e.tensor_tensor(out=red[:, :], in0=red[:, :], in1=qf[:, :], op=ALU.add), reads=[red, qf], writes=[red])
            S.dve(lambda e: e.tensor_scalar(out=qf[:, :], in0=red[:, :], scalar1=-float(np.pi), scalar2=TWO_PI, op0=ALU.is_lt, op1=ALU.mult),
                  reads=[red], writes=[qf])
            S.dve(lambda e: e.tensor_tensor(out=red[:, :], in0=red[:, :], in1=qf[:, :], op=ALU.add), reads=[red, qf], writes=[red])
            S.dve(lambda e: e.tensor_scalar(out=red[:, :], in0=red[:, :], scalar1=3.14159, scalar2=-3.14159, op0=ALU.min, op1=ALU.max),
                  reads=[red], writes=[red])
            if shift:
                S.act(lambda e: e.activation(out=dst[:, :], in_=red[:, :], func=AF.Sin), reads=[red], writes=[dst])
            else:
                S.act(lambda e: e.activation(out=red[:, :], in_=red[:, :], func=AF.Sin), reads=[red], writes=[red])
                S.dve(lambda e: e.tensor_scalar(out=dst[:, :], in0=red[:, :], scalar1=sgn[:, 0:1], scalar2=None, op0=ALU.mult),
                      reads=[red, sgn], writes=[dst])
        S.barrier()
    E.update(cosT=cosT, sinT=sinT)
    lg = [float(np.log1p(-2.0 ** (-5.0 - h))) for h in range(4)]
    E["lg"] = lg
    scale = 128 ** -0.5
    dmT = C.sb(es, [128, 4, 128], F32, "dmT")
    xiT = C.sb(es, [128, 4, 128], BF16, "xiT")
    zcol = C.sb(es, [128, 4], F32, "zcol")
    with ExitStack() as e1:
        d_i = C.sb(e1, [128, 128], mybir.dt.int32, "d_i")
        d_f = C.sb(e1, [128, 128], F32, "d_f")
        S.pool(lambda e: e.iota(d_i[:, :], pattern=[[1, 128]], base=0, channel_multiplier=-1), writes=[d_i])
        S.dve(lambda e: e.tensor_copy(out=d_f[:, :], in_=d_i[:, :]), reads=[d_i], writes=[d_f])
        S.dve(lambda e: e.tensor_scalar(out=d_f[:, :], in0=d_f[:, :], scalar1=0.0, scalar2=None, op0=ALU.max), reads=[d_f], writes=[d_f])
        i_i = C.sb(e1, [128, 128], mybir.dt.int32, "i_i")
        i_f = C.sb(e1, [128, 128], F32, "i_f")
        S.pool(lambda e: e.iota(i_i[:, :], pattern=[[1, 128]], base=1, channel_multiplier=0), writes=[i_i])
        S.dve(lambda e: e.tensor_copy(out=i_f[:, :], in_=i_i[:, :]), reads=[i_i], writes=[i_f])
        p_i = C.sb(e1, [128, 1], mybir.dt.int32, "p_i")
        p_f = C.sb(e1, [128, 1], F32, "p_f")
        S.pool(lambda e: e.iota(p_i[:, :], pattern=[[0, 1]], base=127, channel_multiplier=-1), writes=[p_i])
        S.dve(lambda e: e.tensor_copy(out=p_f[:, :], in_=p_i[:, :]), reads=[p_i], writes=[p_f])
        tmp = C.sb(e1, [128, 128], F32, "tmpd")
        for h in range(4):
            S.act(lambda e: e.activation(out=tmp[:, :], in_=d_f[:, :], func=AF.Exp, scale=lg[h]), reads=[d_f], writes=[tmp])
            S.dve(lambda e: e.scalar_tensor_tensor(out=dmT[:, h, :], in0=tmp[:, :], scalar=scale, in1=E["m_ui"][:, 0, :],
                                                   op0=ALU.mult, op1=ALU.mult), reads=[tmp, E["m_ui"]], writes=[dmT])
            S.act(lambda e: e.activation(out=xiT[:, h, :], in_=i_f[:, :], func=AF.Exp, scale=lg[h]), reads=[i_f], writes=[xiT])
            S.act(lambda e: e.activation(out=zcol[:, h:h + 1], in_=p_f[:, :], func=AF.Exp, scale=lg[h]), reads=[p_f], writes=[zcol])
        S.dve(lambda e: e.tensor_scalar(out=zcol[:, :], in0=zcol[:, :], scalar1=scale, scalar2=None, op0=ALU.mult), reads=[zcol], writes=[zcol])
        S.barrier()
    E.update(dmT=dmT, xiT=xiT, zcol=zcol)
    return E


def group_norm_T(C, es, y, nch, onesmat, eps, gcol_fn, bcol_fn, post_fn, pm, pq):
    S = C.S
    sq = C.sb(es, [128, 512], BF16, "gn_sq")
    yb16 = C.sb(es, [128, 512], BF16, "gn_yb")
    m2 = C.sb(es, [128, 512], F32, "gn_m2")
    rs = C.sb(es, [128, 512], F32, "gn_rs")
    dd = C.sb(es, [128, 512], F32, "gn_dd")
    for c in range(nch):
        S.dve(lambda e: e.tensor_copy(out=yb16[:, :], in_=y[:, c, :]), reads=[y], writes=[yb16])
        S.pe(lambda e: e.matmul(out=pm[:, :], lhsT=onesmat[:, :], rhs=yb16[:, :], start=True, stop=True), reads=[onesmat, yb16], writes=[pm])
        S.act(lambda e: e.activation(out=sq[:, :], in_=y[:, c, :], func=AF.Square), reads=[y], writes=[sq])
        S.pe(lambda e: e.matmul(out=pq[:, :], lhsT=onesmat[:, :], rhs=sq[:, :], start=True, stop=True), reads=[onesmat, sq], writes=[pq])
        S.act(lambda e: e.activation(out=m2[:, :], in_=pm[:, :], func=AF.Square), reads=[pm], writes=[m2])
        S.dve(lambda e: e.tensor_tensor(out=rs[:, :], in0=pq[:, :], in1=m2[:, :], op=ALU.subtract), reads=[pq, m2], writes=[rs])
        S.dve(lambda e: e.tensor_scalar(out=rs[:, :], in0=rs[:, :], scalar1=0.0, scalar2=float(eps), op0=ALU.max, op1=ALU.add),
              reads=[rs], writes=[rs])
        S.act(lambda e: e.activation(out=rs[:, :], in_=rs[:, :], func=AF.Sqrt), reads=[rs], writes=[rs])
        S.dve(lambda e: e.reciprocal(out=rs[:, :], in_=rs[:, :]), reads=[rs], writes=[rs])
        S.dve(lambda e: e.tensor_tensor(out=dd[:, :], in0=y[:, c, :], in1=pm[:, :], op=ALU.subtract), reads=[y, pm], writes=[dd])
        S.dve(lambda e: e.tensor_tensor(out=dd[:, :], in0=dd[:, :], in1=rs[:, :], op=ALU.mult), reads=[dd, rs], writes=[dd])
        S.pool(lambda e: e.tensor_scalar(out=dd[:, :], in0=dd[:, :], scalar1=gcol_fn(c), scalar2=bcol_fn(c), op0=ALU.mult, op1=ALU.add),
               reads=[dd], writes=[dd])
        post_fn(c, dd)


def even_mixer(C, x, l, W, P, K):
    S = C.S
    i = l // 2
    ident = K["ident"]
    w_in = W.H("ev_w_in", i)
    w_v = w_in.rearrange("(kc p) n -> p kc n", p=128)
    if DBG.get("even_stop") == 0:
        return
    with ExitStack() as es:
        E = make_even_consts(C, es, K)
        lg = E["lg"]
        gamC = [float(np.exp(128.0 * lg[h])) for h in range(4)]
        yaT = C.sb(es, [128, 4, SEQ], BF16, "yaT")
        with ExitStack() as er:
            wa2 = C.sb(er, [128, 512], BF16, "wa2")
            g2 = C.sb(er, [128, 512], BF16, "g2")
            S.dma("pool", wa2[0:64, :], W.H("rw_w2", i), writes=[wa2])
            S.dma("pool", wa2[64:128, :], W.H("rw_a2", i), writes=[wa2])
            S.dma("pool", g2[:, :], W.H("rw_g2", i), writes=[g2])
            omk = C.sb(er, [128, 4], F32, "omk")
            S.dve(lambda e: e.tensor_scalar(out=omk[:, :], in0=P.cols(("rw_k_a", i), 0, 4), scalar1=-1.0, scalar2=1.0, op0=ALU.mult, op1=ALU.add),
                  reads=[P.t], writes=[omk])
            St = C.sb(er, [128, 4, 64], F32, "St")
            Sb = C.sb(er, [128, 4, 2, 64], BF16, "Sbd")
            S.pool(lambda e: e.memset(St[:, :, :], 0.0), writes=[St])
            S.pool(lambda e: e.memset(Sb[:, :, :, :], 0.0), writes=[Sb])
            pcar = C.sb(er, [128, 14], F32, "pcar")
            S.pool(lambda e: e.memset(pcar[:, :], 0.0), writes=[pcar])
            wch = [C.sb(er, [128, 8, 128], BF16, f"wch{k}") for k in range(2)]
            nw = [0]

            def mu_col(c):
                return P.col(("rw_mu", i, 0), c) if c < 8 else P.col(("rw_mu", i, 1), c - 8)

            for blk in range(4):
                t0 = blk * 512
                with ExitStack() as e1:
                    xnT = C.sb(e1, [128, 8, 512], BF16, "xnT")
                    rms_to_T(C, e1, x, P.cols(("norm_g", l, 2)), xnT, 4, ident, tok0=blk * 4)
                    with ExitStack() as e2:
                        At = C.sb(e2, [128, 4, 512], BF16, "At")
                        Bt = C.sb(e2, [128, 4, 512], BF16, "Bt")
                        Kt = C.sb(e2, [128, 4, 512], BF16, "Kt")
                        Rq = C.sb(e2, [128, 4, 512], BF16, "Rq")
                        vT = C.sb(e2, [128, 4, 512], BF16, "vT")
                        bon = C.sb(e2, [128, 4, 512], BF16, "bon")
                        gT = C.sb(e2, [128, 4, 512], BF16, "gT")
                        gC = C.sb(e2, [128, 4, 4], F32, "gC")
                        yraw = C.sb(e2, [128, 4, 512], F32, "yraw")
                        with ExitStack() as e3:
                            pps = [C.ps(e3, [128, 512], F32, f"pp{k}") for k in range(3)]
                            pa = C.ps(e3, [128, 512], F32, "pa")
                            pb = C.ps(e3, [128, 512], F32, "pb")
                            pT = C.sb(e3, [128, 513], F32, "pT")
                            mx = [C.sb(e3, [128, 512], F32, f"mx{k}") for k in range(3)]
                            dtmp = C.sb(e3, [128, 512], F32, "dtmp")
                            xwa = C.sb(e3, [128, 512], BF16, "xwa")
                            sxg = C.sb(e3, [128, 512], BF16, "sxg")
                            kkb = C.sb(e3, [128, 512], BF16, "kkb")
                            bA = C.sb(e3, [128, 512], F32, "bA")
                            bB = C.sb(e3, [128, 512], F32, "bB")
                            eL = C.sb(e3, [128, 512], F32, "eL")
                            eLm = C.sb(e3, [128, 512], F32, "eLm")
                            asig = C.sb(e3, [128, 512], F32, "asig")
                            kk = C.sb(e3, [128, 512], F32, "kk")
                            t2 = C.sb(e3, [128, 512], F32, "t2")

                            def proj_mix(c, k_):
                                pp, m_ = pps[k_], mx[k_]
                                wt = wch[nw[0] % 2]
                                nw[0] += 1
                                S.dma("pool", wt[:, :, :], w_v[:, :, c * 128:(c + 1) * 128], writes=[wt])
                                for kc in range(8):
                                    S.pe(lambda e: e.matmul(out=pp[:, :], lhsT=wt[:, kc, :], rhs=xnT[:, kc, :],
                                                            start=(kc == 0), stop=(kc == 7)), reads=[wt, xnT], writes=[pp])
                                S.act(lambda e: e.copy(out=pT[:, 1:513], in_=pp[:, :]), reads=[pp], writes=[pT])
                                S.pool(lambda e: e.tensor_copy(out=pT[:, 0:1], in_=pcar[:, c:c + 1]), reads=[pcar.part(c)], writes=[pT])
                                S.pool(lambda e: e.tensor_copy(out=pcar[:, c:c + 1], in_=pT[:, 512:513]), reads=[pT], writes=[pcar.part(c)])
                                S.dve(lambda e: e.tensor_tensor(out=dtmp[:, :], in0=pT[:, 0:512], in1=pT[:, 1:513], op=ALU.subtract),
                                      reads=[pT], writes=[dtmp])
                                S.dve(lambda e: e.scalar_tensor_tensor(out=m_[:, :], in0=dtmp[:, :], scalar=mu_col(c), in1=pT[:, 1:513],
                                                                       op0=ALU.mult, op1=ALU.add), reads=[dtmp, pT, P.t], writes=[m_])
                                return m_

                            m_ = proj_mix(12, 0)
                            S.act(lambda e: e.activation(out=xwa[0:64, :], in_=m_[0:64, :], func=AF.Tanh), reads=[m_], writes=[xwa])
                            S.act(lambda e: e.copy(out=xwa[64:128, :], in_=m_[64:128, :]), reads=[m_], writes=[xwa])
                            m_ = proj_mix(13, 1)
                            S.act(lambda e: e.activation(out=sxg[:, :], in_=m_[:, :], func=AF.Sigmoid), reads=[m_], writes=[sxg])
                            for hp in range(4):
                                rr = proj_mix(hp, 0)
                                kx = proj_mix(4 + hp, 1)
                                vv = proj_mix(8 + hp, 2)
                                S.pe(lambda e: e.matmul(out=pa[:, :], lhsT=wa2[0:64, hp * 128:(hp + 1) * 128], rhs=xwa[0:64, :], start=True, stop=True),
                                     reads=[wa2, xwa], writes=[pa])
                                S.act(lambda e: e.activation(out=bA[:, :], in_=pa[:, :], func=AF.Sigmoid, bias=P.col(("rw_w0", i), hp)),
                                      reads=[pa, P.t], writes=[bA])
                                S.dve(lambda e: e.tensor_scalar(out=bA[:, :], in0=bA[:, :], scalar1=-float(np.exp(-0.5)), scalar2=None, op0=ALU.mult),
                                      reads=[bA], writes=[bA])
                                S.dve(lambda e: e.tensor_tensor_scan(out=bB[:, :], data0=E["seg"][:, :, :].rearrange("p a b -> p (a b)"),
                                                                     data1=bA[:, :], initial=0.0, op0=ALU.mult, op1=ALU.add),
                                      reads=[bA, E["seg"]], writes=[bB])
                                S.act(lambda e: e.activation(out=eL[:, :], in_=bB[:, :], func=AF.Exp), reads=[bB], writes=[eL])
                                S.act(lambda e: e.activation(out=eLm[:, :], in_=bB[:, :], func=AF.Exp, scale=-1.0), reads=[bB], writes=[eLm])
                                S.dve(lambda e: e.tensor_tensor(out=bA[:, :], in0=bB[:, :], in1=bA[:, :], op=ALU.subtract), reads=[bB, bA], writes=[bA])
                                S.act(lambda e: e.activation(out=bA[:, :], in_=bA[:, :], func=AF.Exp), reads=[bA], writes=[bA])
                                S.pool(lambda e: e.tensor_copy(out=gC[:, :, hp], in_=eL[:, :].rearrange("p (a b) -> p a b", a=4)[:, :, 127]),
                                       reads=[eL], writes=[gC])
                                S.pe(lambda e: e.matmul(out=pb[:, :], lhsT=wa2[64:128, hp * 128:(hp + 1) * 128], rhs=xwa[64:128, :], start=True, stop=True),
                                     reads=[wa2, xwa], writes=[pb])
                                S.act(lambda e: e.activation(out=asig[:, :], in_=pb[:, :], func=AF.Sigmoid, bias=P.col(("rw_a0", i), hp)),
                                      reads=[pb, P.t], writes=[asig])
                                S.pool(lambda e: e.tensor_scalar(out=kk[:, :], in0=kx[:, :], scalar1=P.col(("rw_k_k", i), hp), scalar2=None, op0=ALU.mult),
                                       reads=[kx, P.t], writes=[kk])
                                S.act(lambda e: e.activation(out=kkb[:, :], in_=kk[:, :], func=AF.Square), reads=[kk], writes=[kkb])
                                S.pe(lambda e: e.matmul(out=pa[:, :], lhsT=E["bo"][:, :], rhs=kkb[:, :], start=True, stop=True),
                                     reads=[E["bo"], kkb], writes=[pa])
                                S.dve(lambda e: e.tensor_scalar(out=bB[:, :], in0=pa[:, :], scalar1=1e-24, scalar2=None, op0=ALU.max), reads=[pa], writes=[bB])
                                S.act(lambda e: e.activation(out=bB[:, :], in_=bB[:, :], func=AF.Sqrt), reads=[bB], writes=[bB])
                                S.dve(lambda e: e.reciprocal(out=bB[:, :], in_=bB[:, :]), reads=[bB], writes=[bB])
                                S.dve(lambda e: e.tensor_tensor(out=kk[:, :], in0=kk[:, :], in1=bB[:, :], op=ALU.mult), reads=[kk, bB], writes=[kk])
                                S.dve(lambda e: e.scalar_tensor_tensor(out=At[:, hp, :], in0=kk[:, :], scalar=-1.0, in1=bA[:, :], op0=ALU.mult, op1=ALU.mult),
                                      reads=[kk, bA], writes=[At.part(hp)])
                                S.pool(lambda e: e.tensor_tensor(out=bB[:, :], in0=kk[:, :], in1=asig[:, :], op=ALU.mult), reads=[kk, asig], writes=[bB])
                                S.pool(lambda e: e.tensor_tensor(out=Bt[:, hp, :], in0=bB[:, :], in1=eLm[:, :], op=ALU.mult), reads=[bB, eLm], writes=[Bt.part(hp)])
                                S.dve(lambda e: e.tensor_scalar(out=t2[:, :], in0=asig[:, :], scalar1=P.col(("rw_k_a", i), hp), scalar2=omk[:, hp:hp + 1],
                                                                op0=ALU.mult, op1=ALU.add), reads=[asig, P.t, omk], writes=[t2])
                                S.dve(lambda e: e.tensor_tensor(out=t2[:, :], in0=t2[:, :], in1=kx[:, :], op=ALU.mult), reads=[t2, kx], writes=[t2])
                                S.pool(lambda e: e.tensor_tensor(out=Kt[:, hp, :], in0=t2[:, :], in1=eLm[:, :], op=ALU.mult), reads=[t2, eLm], writes=[Kt.part(hp)])
                                S.dve(lambda e: e.tensor_tensor(out=Rq[:, hp, :], in0=rr[:, :], in1=eL[:, :], op=ALU.mult), reads=[rr, eL], writes=[Rq.part(hp)])
                                S.act(lambda e: e.copy(out=vT[:, hp, :], in_=vv[:, :]), reads=[vv], writes=[vT.part(hp)])
                                S.dve(lambda e: e.scalar_tensor_tensor(out=kkb[:, :], in0=rr[:, :], scalar=P.col(("rw_r_k", i), hp), in1=t2[:, :],
                                                                       op0=ALU.mult, op1=ALU.mult), reads=[rr, t2, P.t], writes=[kkb])
                                S.pe(lambda e: e.matmul(out=pb[:, :], lhsT=E["bo"][:, :], rhs=kkb[:, :], start=True, stop=True),
                                     reads=[E["bo"], kkb], writes=[pb])
                                S.dve(lambda e: e.tensor_tensor(out=bon[:, hp, :], in0=pb[:, :], in1=vv[:, :], op=ALU.mult), reads=[pb, vv], writes=[bon.part(hp)])
                                S.pe(lambda e: e.matmul(out=pa[:, :], lhsT=g2[:, hp * 128:(hp + 1) * 128], rhs=sxg[:, :], start=True, stop=True),
                                     reads=[g2, sxg], writes=[pa])
                                S.act(lambda e: e.copy(out=gT[:, hp, :], in_=pa[:, :]), reads=[pa], writes=[gT.part(hp)])
                            S.barrier()
                        if DBG.get("even_stop") == 1:
                            continue
                        with ExitStack() as e3:
                            def bank(nm, dt=F32):
                                return C.ps(e3, [128, 512] if dt == F32 else [128, 8, 128], dt, nm)
                            pA = [bank(f"pA{k}") for k in range(3)]
                            pX = [bank(f"pX{k}") for k in range(2)]
                            ptr = bank("ptr", BF16)
                            pS = bank("pS")
                            BtT = C.sb(e3, [128, 512], BF16, "BtT")
                            KtT = C.sb(e3, [128, 512], BF16, "KtT")
                            Vtk = C.sb(e3, [128, 512], BF16, "Vtk")
                            Mak = C.sb(e3, [128, 8, 128], BF16, "Mak")
                            Nbr = C.sb(e3, [128, 8, 128], BF16, "Nbr")
                            Nkr = C.sb(e3, [128, 8, 128], BF16, "Nkr")
                            Pm = [C.sb(e3, [128, 8, 128], BF16, "Mm")]
                            PTm = [C.sb(e3, [128, 8, 128], BF16, "MTm")]
                            Xm = [C.sb(e3, [128, 8, 128], BF16, f"Xm{k}") for k in range(2)]
                            XTm = [C.sb(e3, [128, 8, 128], BF16, f"XTm{k}") for k in range(2)]
                            Ts2 = [C.sb(e3, [128, 8, 128], BF16, f"Ts{k}") for k in range(2)]
                            TsT2 = [C.sb(e3, [128, 8, 128], BF16, f"TsT{k}") for k in range(2)]
                            Y1s = C.sb(e3, [128, 8, 128], BF16, "Y1s")
                            Z1s = C.sb(e3, [128, 8, 128], BF16, "Z1s")
                            Gt = C.sb(e3, [128, 512], BF16, "Gt")
                            Ut = C.sb(e3, [128, 512], BF16, "Ut")
                            stmp = C.sb(e3, [128, 4, 64], F32, "stmp")

                            def hv(t_, h, csl):
                                hp_, e_ = h // 2, h % 2
                                return t_[e_ * 64:(e_ + 1) * 64, hp_, csl]

                            for c in range(4):
                                csl = slice(c * 128, (c + 1) * 128)
                                for (src, dst) in ((Bt, BtT), (Kt, KtT), (vT, Vtk)):
                                    for hp in range(4):
                                        S.pe(lambda e: e.transpose(out=ptr[:, hp, :], in_=src[:, hp, csl], identity=ident[:, :]),
                                             reads=[src, ident], writes=[ptr])
                                    S.act(lambda e: e.copy(out=dst[:, :], in_=ptr[:, 0:4, :].rearrange("p a b -> p (a b)")), reads=[ptr], writes=[dst])
                                if DBG.get('even_stop') == 1.2:
                                    continue
                                prods = ((Bt, At, Pm[0], "m_su"), (At, Bt, PTm[0], "m_sl"), (Kt, At, Mak, "m_su"),
                                         (Bt, Rq, Nbr, "m_ui"), (Kt, Rq, Nkr, "m_ui"))
                                nb = 0
                                for (lt, rt_, dst, mk) in prods:
                                    for e_ in range(2):
                                        pbk = pA[nb % 3]
                                        nb += 1
                                        for hh in range(4):
                                            h = 2 * hh + e_
                                            S.pe(lambda e: e.matmul(out=pbk[:, hh * 128:(hh + 1) * 128], lhsT=hv(lt, h, csl), rhs=hv(rt_, h, csl),
                                                                    start=True, stop=True), reads=[lt, rt_], writes=[pbk])
                                        S.dve(lambda e: e.tensor_tensor(out=dst[:, :, :].rearrange("p (a two) b -> p a two b", two=2)[:, :, e_, :],
                                                                        in0=pbk[:, :].rearrange("p (a b) -> p a b", a=4), in1=E[mk][:, :, :], op=ALU.mult),
                                              reads=[pbk, E[mk]], writes=[dst.part(("e", e_))])
                                if DBG.get('even_stop') == 1.4:
                                    continue
                                Mm, MTm = Pm[0], PTm[0]

                                def lvl(src, msk, s, dst, eng):
                                    S.op(eng, lambda e: e.tensor_tensor(out=dst[:, :, :], in0=src[:, :, :],
                                                                        in1=E[msk][:, s, :].unsqueeze(1).to_broadcast([128, 8, 128]), op=ALU.mult),
                                         reads=[src, E[msk]], writes=[dst])

                                Ts, TsT = Ts2[0], TsT2[0]
                                lvl(Mm, "lvm", 0, Ts, "pool")
                                lvl(MTm, "lvmT", 0, TsT, "pool")
                                lvl(Mm, "lvm", 1, Ts2[1], "pool")
                                lvl(MTm, "lvmT", 1, TsT2[1], "pool")
                                S.pool(lambda e: e.tensor_tensor(out=Xm[0][:, :, :], in0=Ts[:, :, :], in1=E["id8"][:, :, :], op=ALU.add),
                                       reads=[Ts, E["id8"]], writes=[Xm[0]])
                                S.pool(lambda e: e.tensor_tensor(out=XTm[0][:, :, :], in0=TsT[:, :, :], in1=E["id8"][:, :, :], op=ALU.add),
                                       reads=[TsT, E["id8"]], writes=[XTm[0]])
                                cur = 0
                                for s in range(1, 7):
                                    nxt = 1 - cur
                                    last = (s == 6)
                                    Ts, TsT = Ts2[s % 2], TsT2[s % 2]
                                    for half in range(2):
                                        hs_ = range(half * 4, half * 4 + 4)
                                        pbk = pA[nb % 3]
                                        nb += 1
                                        for hh, h in enumerate(hs_):
                                            S.pe(lambda e: e.matmul(out=pbk[:, hh * 128:(hh + 1) * 128], lhsT=TsT[:, h, :], rhs=Xm[cur][:, h, :],
                                                                    start=True, stop=True), reads=[TsT, Xm[cur]], writes=[pbk])
                                        S.act(lambda e: e.copy(out=Y1s[:, half * 4:half * 4 + 4, :], in_=pbk[:, :].rearrange("p (a b) -> p a b", a=4)),
                                              reads=[pbk], writes=[Y1s.part(half)])
                                        if not last:
                                            pbk = pA[nb % 3]
                                            nb += 1
                                            for hh, h in enumerate(hs_):
                                                S.pe(lambda e: e.matmul(out=pbk[:, hh * 128:(hh + 1) * 128], lhsT=Ts[:, h, :], rhs=XTm[cur][:, h, :],
                                                                        start=True, stop=True), reads=[Ts, XTm[cur]], writes=[pbk])
                                            S.dve(lambda e: e.tensor_copy(out=Z1s[:, half * 4:half * 4 + 4, :], in_=pbk[:, :].rearrange("p (a b) -> p a b", a=4)),
                                                  reads=[pbk], writes=[Z1s.part(half)])
                                    if not last:
                                        lvl(Mm, "lvm", s + 1, Ts2[(s + 1) % 2], "pool")
                                        lvl(MTm, "lvmT", s + 1, TsT2[(s + 1) % 2], "pool")
                                    for half in range(2):
                                        hs_ = range(half * 4, half * 4 + 4)
                                        px = pX[half]
                                        for hh, h in enumerate(hs_):
                                            S.pe(lambda e: e.matmul(out=px[:, hh * 128:(hh + 1) * 128], lhsT=ident[:, :], rhs=Xm[cur][:, h, :],
                                                                    start=True, stop=False), reads=[ident, Xm[cur]], writes=[px])
                                            S.pe(lambda e: e.matmul(out=px[:, hh * 128:(hh + 1) * 128], lhsT=XTm[cur][:, h, :], rhs=Y1s[:, h, :],
                                                                    start=False, stop=True), reads=[XTm[cur], Y1s], writes=[px])
                                        S.act(lambda e: e.copy(out=Xm[nxt][:, half * 4:half * 4 + 4, :], in_=px[:, :].rearrange("p (a b) -> p a b", a=4)),
                                              reads=[px], writes=[Xm[nxt].part(half)])
                                        if not last:
                                            pbk = pA[nb % 3]
                                            nb += 1
                                            for hh, h in enumerate(hs_):
                                                S.pe(lambda e: e.matmul(out=pbk[:, hh * 128:(hh + 1) * 128], lhsT=ident[:, :], rhs=XTm[cur][:, h, :],
                                                                        start=True, stop=False), reads=[ident, XTm[cur]], writes=[pbk])
                                                S.pe(lambda e: e.matmul(out=pbk[:, hh * 128:(hh + 1) * 128], lhsT=Xm[cur][:, h, :], rhs=Z1s[:, h, :],
                                                                        start=False, stop=True), reads=[Xm[cur], Z1s], writes=[pbk])
                                            S.dve(lambda e: e.tensor_copy(out=XTm[nxt][:, half * 4:half * 4 + 4, :], in_=pbk[:, :].rearrange("p (a b) -> p a b", a=4)),
                                                  reads=[pbk], writes=[XTm[nxt].part(half)])
                                    cur = nxt
                                if DBG.get('even_stop') == 1.6:
                                    continue
                                Xf = Xm[cur]
                                pg = pA[nb % 3]
                                nb += 1
                                for h in range(8):
                                    hp, e_ = h // 2, h % 2
                                    S.pe(lambda e: e.matmul(out=pg[:, h * 64:(h + 1) * 64], lhsT=At[:, hp, csl], rhs=Sb[:, hp, e_, :],
                                                            start=True, stop=False), reads=[At, Sb], writes=[pg])
                                    S.pe(lambda e: e.matmul(out=pg[:, h * 64:(h + 1) * 64], lhsT=Mak[:, h, :], rhs=Vtk[:, h * 64:(h + 1) * 64],
                                                            start=False, stop=True), reads=[Mak, Vtk], writes=[pg])
                                S.act(lambda e: e.copy(out=Gt[:, :], in_=pg[:, :]), reads=[pg], writes=[Gt])
                                if DBG.get('even_stop') == 1.65:
                                    continue
                                pu = pA[nb % 3]
                                nb += 1
                                for h in range(8):
                                    S.pe(lambda e: e.matmul(out=pu[:, h * 64:(h + 1) * 64], lhsT=Xf[:, h, :], rhs=Gt[:, h * 64:(h + 1) * 64],
                                                            start=True, stop=True), reads=[Xf, Gt], writes=[pu])
                                S.dve(lambda e: e.tensor_copy(out=Ut[:, :], in_=pu[:, :]), reads=[pu], writes=[Ut])
                                if DBG.get('even_stop') == 1.7:
                                    continue
                                py = pA[nb % 3]
                                nb += 1
                                for h in range(8):
                                    hp, e_ = h // 2, h % 2
                                    o_ = py[e_ * 64:(e_ + 1) * 64, hp * 128:(hp + 1) * 128]
                                    S.pe(lambda e: e.matmul(out=o_, lhsT=Sb[:, hp, e_, :], rhs=Rq[:, hp, csl], start=True, stop=False),
                                         reads=[Sb, Rq], writes=[py])
                                    S.pe(lambda e: e.matmul(out=o_, lhsT=Ut[:, h * 64:(h + 1) * 64], rhs=Nbr[:, h, :], start=False, stop=False),
                                         reads=[Ut, Nbr], writes=[py])
                                    S.pe(lambda e: e.matmul(out=o_, lhsT=Vtk[:, h * 64:(h + 1) * 64], rhs=Nkr[:, h, :], start=False, stop=True),
                                         reads=[Vtk, Nkr], writes=[py])
                                S.act(lambda e: e.copy(out=yraw[:, :, csl], in_=py[:, :].rearrange("p (a b) -> p a b", a=4)), reads=[py], writes=[yraw.part(c)])
                                if DBG.get('even_stop') == 1.75:
                                    continue
                                for hp in range(4):
                                    o_ = pS[:, hp * 128:(hp + 1) * 128]
                                    S.pe(lambda e: e.matmul(out=o_, lhsT=BtT[:, hp * 128:(hp + 1) * 128], rhs=Ut[:, hp * 128:(hp + 1) * 128], start=True, stop=False),
                                         reads=[BtT, Ut], writes=[pS])
                                    S.pe(lambda e: e.matmul(out=o_, lhsT=KtT[:, hp * 128:(hp + 1) * 128], rhs=Vtk[:, hp * 128:(hp + 1) * 128], start=False, stop=True),
                                         reads=[KtT, Vtk], writes=[pS])
                                for e_ in range(2):
                                    S.dve(lambda e: e.tensor_tensor(out=stmp[e_ * 64:(e_ + 1) * 64, :, :],
                                                                    in0=pS[e_ * 64:(e_ + 1) * 64, :].rearrange("p (a b c) -> p a b c", a=4, b=2)[:, :, e_, :],
                                                                    in1=St[e_ * 64:(e_ + 1) * 64, :, :], op=ALU.add),
                                          reads=[pS, St], writes=[stmp])
                                S.dve(lambda e: e.tensor_tensor(out=St[:, :, :], in0=stmp[:, :, :], in1=gC[:, c, :].unsqueeze(2).to_broadcast([128, 4, 64]),
                                                                op=ALU.mult), reads=[stmp, gC], writes=[St])
                                for e_ in range(2):
                                    S.act(lambda e: e.copy(out=Sb[e_ * 64:(e_ + 1) * 64, :, e_, :], in_=St[e_ * 64:(e_ + 1) * 64, :, :]), reads=[St], writes=[Sb])
                            S.barrier()
                        if DBG.get('even_stop') == 1.8:
                            continue
                        with ExitStack() as e3:
                            pm = C.ps(e3, [128, 512], F32, "gn_pm")
                            pq = C.ps(e3, [128, 512], F32, "gn_pq")

                            def post_a(c, dd):
                                S.pool(lambda e: e.tensor_tensor(out=dd[:, :], in0=dd[:, :], in1=bon[:, c, :], op=ALU.add), reads=[dd, bon], writes=[dd])
                                S.dve(lambda e: e.tensor_tensor(out=yaT[:, c, t0:t0 + 512], in0=dd[:, :], in1=gT[:, c, :], op=ALU.mult), reads=[dd, gT], writes=[yaT.part((c, blk))])

                            group_norm_T(C, e3, yraw, 4, E["bof"], RW_LN_EPS, lambda c: P.col(("rw_ln_g", i), c), lambda c: P.col(("rw_ln_b", i), c),
                                         post_a, pm, pq)
                            S.barrier()
        if DBG.get("even_stop") in (1, 1.2, 1.4, 1.6, 1.65, 1.7, 1.75, 1.8, 2):
            return
        ybT = C.sb(es, [128, 4, SEQ], BF16, "ybT")
        with ExitStack() as er:
            Rt = C.sb(er, [128, 4, 128], F32, "Rt")
            Rb = C.sb(er, [128, 4, 128], BF16, "Rb")
            for t_ in (Rt, Rb):
                S.pool(lambda e: e.memset(t_[:, :, :], 0.0), writes=[t_])
            wvr = C.sb(er, [128, 8, 512], BF16, "wvr")
            load_w(C, wvr, w_in, 1792 + 1024, 1792 + 1536)
            wch = [C.sb(er, [128, 8, 128], BF16, f"wchr{k}") for k in range(2)]
            nw = [0]

            def wchunk(c0):
                wt = wch[nw[0] % 2]
                nw[0] += 1
                S.dma("pool", wt[:, :, :], w_v[:, :, 1792 + c0:1792 + c0 + 128], writes=[wt])
                return wt

            for blk in range(4):
                t0 = blk * 512
                with ExitStack() as e1:
                    xnT = C.sb(e1, [128, 8, 512], BF16, "xnT")
                    rms_to_T(C, e1, x, P.cols(("norm_g", l, 2)), xnT, 4, ident, tok0=blk * 4)
                    with ExitStack() as e2:
                        qr = C.sb(e2, [128, 4, 512], BF16, "qr")
                        qx = C.sb(e2, [128, 4, 512], BF16, "qx")
                        kr = C.sb(e2, [128, 4, 512], BF16, "kr")
                        sg = C.sb(e2, [128, 4, 512], BF16, "sg")
                        vtk = C.sb(e2, [128, 4, 512], BF16, "vtk")
                        oraw = C.sb(e2, [128, 4, 512], F32, "oraw")
                        cs = E["cosT"][:, t0:t0 + 512]
                        sn = E["sinT"][:, t0:t0 + 512]
                        with ExitStack() as e3:
                            pq_ = [C.ps(e3, [128, 512], F32, f"rq{k}") for k in range(2)]
                            ps_ = [C.ps(e3, [128, 512], F32, f"rs{k}") for k in range(2)]
                            t1 = C.sb(e3, [128, 512], F32, "rt1")
                            t2 = C.sb(e3, [128, 512], F32, "rt2")
                            n_ = 0
                            for (c0, dst) in ((0, qr), (512, kr)):
                                for h in range(4):
                                    pa_, pb_ = pq_[n_ % 2], ps_[n_ % 2]
                                    n_ += 1
                                    cb = c0 + h * 128
                                    wrt = wchunk(cb)
                                    for kc in range(8):
                                        S.pe(lambda e: e.matmul(out=pa_[:, :], lhsT=wrt[:, kc, :], rhs=xnT[:, kc, :], start=(kc == 0), stop=(kc == 7)),
                                             reads=[wrt, xnT], writes=[pa_])
                                    for half in range(2):
                                        for kc in range(8):
                                            S.pe(lambda e: e.matmul(out=pb_[half * 64:(half + 1) * 64, :], lhsT=wrt[:, kc, (1 - half) * 64:(1 - half) * 64 + 64],
                                                                    rhs=xnT[:, kc, :], start=(kc == 0), stop=(kc == 7)), reads=[wrt, xnT], writes=[pb_])
                                    S.dve(lambda e: e.tensor_tensor(out=t1[:, :], in0=pa_[:, :], in1=cs, op=ALU.mult), reads=[pa_, E["cosT"]], writes=[t1])
                                    S.dve(lambda e: e.tensor_tensor(out=t2[:, :], in0=pb_[:, :], in1=sn, op=ALU.mult), reads=[pb_, E["sinT"]], writes=[t2])
                                    S.pool(lambda e: e.tensor_tensor(out=dst[:, h, :], in0=t1[:, :], in1=t2[:, :], op=ALU.add), reads=[t1, t2], writes=[dst.part(h)])
                                    if c0 == 0:
                                        S.pool(lambda e: e.tensor_tensor(out=qx[:, h, :].rearrange("p (a b) -> p a b", a=4),
                                                                         in0=qr[:, h, :].rearrange("p (a b) -> p a b", a=4),
                                                                         in1=E["xiT"][:, h, :].unsqueeze(1).to_broadcast([128, 4, 128]), op=ALU.mult),
                                               reads=[qr.part(h), E["xiT"]], writes=[qx.part(h)])
                            for h in range(4):
                                pa_ = pq_[h % 2]
                                wrt = wchunk(1536 + h * 128)
                                for kc in range(8):
                                    S.pe(lambda e: e.matmul(out=pa_[:, :], lhsT=wrt[:, kc, :], rhs=xnT[:, kc, :],
                                                            start=(kc == 0), stop=(kc == 7)), reads=[wrt, xnT], writes=[pa_])
                                S.act(lambda e: e.activation(out=sg[:, h, :], in_=pa_[:, :], func=AF.Silu), reads=[pa_], writes=[sg.part(h)])
                            for c in range(4):
                                pa_ = ps_[c % 2]
                                for kc in range(8):
                                    S.pe(lambda e: e.matmul(out=pa_[:, :], lhsT=xnT[:, kc, c * 128:(c + 1) * 128], rhs=wvr[:, kc, :],
                                                            start=(kc == 0), stop=(kc == 7)), reads=[wvr, xnT], writes=[pa_])
                                S.act(lambda e: e.copy(out=vtk[:, c, :], in_=pa_[:, :]), reads=[pa_], writes=[vtk.part(c)])
                            S.barrier()
                        with ExitStack() as e3:
                            psc = C.ps(e3, [128, 512], F32, "psc")
                            po_ = C.ps(e3, [128, 512], F32, "po_r")
                            pkv = C.ps(e3, [128, 512], F32, "pkv")
                            ptr = C.ps(e3, [128, 8, 128], BF16, "ptr_r")
                            scT = C.sb(e3, [128, 4, 128], BF16, "scT")
                            ktk = C.sb(e3, [128, 4, 128], BF16, "ktk")
                            for c in range(4):
                                csl = slice(c * 128, (c + 1) * 128)
                                for h in range(4):
                                    S.pe(lambda e: e.matmul(out=psc[:, h * 128:(h + 1) * 128], lhsT=kr[:, h, csl], rhs=qr[:, h, csl], start=True, stop=True),
                                         reads=[kr, qr], writes=[psc])
                                    S.pe(lambda e: e.transpose(out=ptr[:, h, :], in_=kr[:, h, csl], identity=ident[:, :]), reads=[kr, ident], writes=[ptr])
                                S.dve(lambda e: e.tensor_tensor(out=scT[:, :, :], in0=psc[:, :].rearrange("p (a b) -> p a b", a=4), in1=E["dmT"][:, :, :], op=ALU.mult),
                                      reads=[psc, E["dmT"]], writes=[scT])
                                S.dve(lambda e: e.tensor_tensor(out=ktk[:, :, :], in0=ptr[:, 0:4, :], in1=E["zcol"][:, :].unsqueeze(2).to_broadcast([128, 4, 128]),
                                                                op=ALU.mult), reads=[ptr, E["zcol"]], writes=[ktk])
                                for h in range(4):
                                    o_ = po_[:, h * 128:(h + 1) * 128]
                                    S.pe(lambda e: e.matmul(out=o_, lhsT=vtk[:, c, h * 128:(h + 1) * 128], rhs=scT[:, h, :], start=True, stop=False),
                                         reads=[vtk, scT], writes=[po_])
                                    S.pe(lambda e: e.matmul(out=o_, lhsT=Rb[:, h, :], rhs=qx[:, h, csl], start=False, stop=True), reads=[Rb, qx], writes=[po_])
                                S.act(lambda e: e.copy(out=oraw[:, :, csl], in_=po_[:, :].rearrange("p (a b) -> p a b", a=4)), reads=[po_], writes=[oraw.part(c)])
                                for h in range(4):
                                    S.pe(lambda e: e.matmul(out=pkv[:, h * 128:(h + 1) * 128], lhsT=ktk[:, h, :], rhs=vtk[:, c, h * 128:(h + 1) * 128],
                                                            start=True, stop=True), reads=[ktk, vtk], writes=[pkv])
                                for h in range(4):
                                    S.dve(lambda e: e.scalar_tensor_tensor(out=Rt[:, h, :], in0=Rt[:, h, :], scalar=gamC[h], in1=pkv[:, h * 128:(h + 1) * 128],
                                                                           op0=ALU.mult, op1=ALU.add), reads=[Rt, pkv], writes=[Rt])
                                S.act(lambda e: e.copy(out=Rb[:, :, :], in_=Rt[:, :, :]), reads=[Rt], writes=[Rb])
                            S.barrier()
                        with ExitStack() as e3:
                            pm = C.ps(e3, [128, 512], F32, "gn_pm")
                            pq = C.ps(e3, [128, 512], F32, "gn_pq")

                            def post_b(c, dd):
                                S.dve(lambda e: e.tensor_tensor(out=ybT[:, c, t0:t0 + 512], in0=dd[:, :], in1=sg[:, c, :], op=ALU.mult), reads=[dd, sg], writes=[ybT.part((c, blk))])

                            group_norm_T(C, e3, oraw, 4, E["on128"], EPS, lambda c: P.col(("rt_gn_g", i), c), lambda c: P.col(("rt_gn_b", i), c),
                                         post_b, pm, pq)
                            S.barrier()
        with ExitStack() as eo:
            g_bc = C.sb(eo, [128, D], F32, "g_bc")
            load_bcast_row(C, "sp", g_bc, W.L("norm_g", l)[3], D)
            wo = C.sb(eo, [128, 8, D], BF16, "wo_ev")
            load_w(C, wo, W.H("ev_w_out", i), 0, D)
            for g4 in range(4):
                chunks = [(yaT, (lambda ti, c=c, g4=g4: yaT[:, c, (g4 * 4 + ti) * 128:(g4 * 4 + ti + 1) * 128])) for c in range(4)]
                chunks += [(ybT, (lambda ti, c=c, g4=g4: ybT[:, c, (g4 * 4 + ti) * 128:(g4 * 4 + ti + 1) * 128])) for c in range(4)]
                out_proj_residual(C, x, chunks, wo, g_bc, 1.0, [g4 * 4 + t_ for t_ in range(4)])


def build_program(shapes, nseq=2, plan=None, loff=0, hoff=0):
    nc = bass.Bass("TRN2", target_bir_lowering=False)
    W = Wts(nc, shapes, loff, hoff)
    out = nc.dram_tensor("out", [nseq, SEQ, D], F32, kind="ExternalOutput").ap()
    C = Ctx(nc)
    S = C.S
    if plan is None:
        plan = [(l, ph) for l in range(DEPTH) for ph in ("ffn1", "mix", "xa", "ffn2")]
    need_mem = any(ph == "xa" for _, ph in plan)
    with ExitStack() as es:
        K = make_consts(C, es)
        P = build_params(C, es, W, K["identf"])
        x = C.sb(es, [128, NT, D], F32, "xres")
        memT = C.sb(es, [128, 8, MEM], BF16, "memT") if need_mem else None
        for s in range(nseq):
            for t4 in range(NT // 4):
                S.dma("sp", x[:, t4 * 4:(t4 + 1) * 4, :],
                      W["x"][s, t4 * 512:(t4 + 1) * 512, :].rearrange("(t p) d -> p t d", p=128),
                      writes=[x.part(t4 * 4 + i) for i in range(4)])
            if need_mem:
                prep_mem(C, es, W, P, K, s, memT)
            for (l, ph) in plan:
                if ph == "ffn1":
                    ffn_block(C, x, l, 0, W, P, K["ident"])
                elif ph == "ffn2":
                    ffn_block(C, x, l, 1, W, P, K["ident"])
                elif ph == "xa":
                    xattn_block(C, x, l, W, P, K, memT)
                elif ph == "mix":
                    if l % 2 == 0:
                        even_mixer(C, x, l, W, P, K)
                    else:
                        odd_mixer(C, x, l, W, P, K)
            for t4 in range(NT // 4):
                S.dma("sp", out[s, t4 * 512:(t4 + 1) * 512, :].rearrange("(t p) d -> p t d", p=128),
                      x[:, t4 * 4:(t4 + 1) * 4, :], reads=[x.part(t4 * 4 + i) for i in range(4)])
            S.barrier()
        S.finish()
    return nc, W


def kernel(**inputs):
    n = 8
    arrs = {k: np.ascontiguousarray(np.asarray(v), dtype=np.float32) for k, v in inputs.items()}
    per = arrs["x"].shape[0] // n
    shapes = {}
    for k, a in arrs.items():
        shapes[k] = ((per,) + a.shape[1:]) if k in ("x", "mem") else a.shape
    nc, W = build_program(shapes, nseq=per)
    in_maps = []
    for c in range(n):
        m = {}
        for k in W.aps:
            a = arrs[k]
            m[k] = a[c * per:(c + 1) * per] if k in ("x", "mem") else a
        in_maps.append(m)
    res = run_bass_kernel_spmd(nc, in_maps, core_ids=list(range(n)))
    return np.concatenate([r["out"] for r in res.results], axis=0).astype(np.float32)
```

```python
import numpy as np
import concourse.bass as bass
import concourse.mybir as mybir
from concourse.bass_utils import run_bass_kernel_spmd

F32 = mybir.dt.float32
BF16 = mybir.dt.bfloat16
ALU = mybir.AluOpType
AF = mybir.ActivationFunctionType
AX = mybir.AxisListType

D = 1024
SEQ = 2048
NT = SEQ // 128
DEPTH = 4
DFF = 2816
NFC = DFF // 128
MEM = 256
EPS = 1e-6


class Res:
    __slots__ = ("name", "writer", "readers", "parent", "parts", "psum")

    def __init__(self, name, parent=None):
        self.name = name
        self.writer = None
        self.readers = {}
        self.parent = parent
        self.parts = {}
        self.psum = parent.psum if parent is not None else False

    def part(self, key):
        r = self.parts.get(key)
        if r is None:
            r = Res(f"{self.name}/{key}", parent=self)
            self.parts[key] = r
        return r


class T:
    def __init__(self, h, name):
        self.h = h
        self.res = Res(name)

    def __getitem__(self, k):
        return self.h[k]

    def part(self, key):
        return self.res.part(key)


NDSEM = 16
COMPUTE = ("pe", "act", "dve", "pool")


class Sched:
    def __init__(self, nc):
        self.nc = nc
        self.eng = {"pe": nc.tensor, "act": nc.scalar, "dve": nc.vector, "pool": nc.gpsimd, "sp": nc.sync}
        self.sem = {}
        self.cnt = {}
        for e in self.eng:
            self.sem[e] = nc.alloc_semaphore("s_" + e)
            self.cnt[e] = 0
        self.dkeys = []
        for q in ("sp", "pool"):
            for i in range(NDSEM):
                k = ("d", q, i)
                self.sem[k] = nc.alloc_semaphore(f"s_d{q}{i}")
                self.cnt[k] = 0
                self.dkeys.append(k)
        self.known = {e: {} for e in self.eng}
        self.dnext = {"sp": 0, "pool": 0}
        self.pe_pending = None
        self.ninstr = 0

    @staticmethod
    def _rlist(r):
        if isinstance(r, T):
            return r.res
        return r

    def _deps(self, reads, writes, eng=None):
        ev = []
        for r in reads:
            r = self._rlist(r)
            if r.writer:
                ev.append(r.writer)
            if r.parent is not None and r.parent.writer:
                ev.append(r.parent.writer)
            for p in r.parts.values():
                if p.writer:
                    ev.append(p.writer)
            if r.psum:
                chain = [r] + list(r.parts.values()) + ([r.parent] if r.parent is not None else [])
                for c in chain:
                    ev.extend((k, v) for k, v in c.readers.items() if k != eng)
        for w in writes:
            w = self._rlist(w)
            chain = [w] + list(w.parts.values())
            if w.parent is not None:
                chain.append(w.parent)
            for c in chain:
                if c.writer:
                    ev.append(c.writer)
                ev.extend(c.readers.items())
        return ev

    def _wait(self, e, evs):
        kn = self.known[e]
        best = {}
        for k, v in evs:
            if k == e and e in ("pe", "sp"):
                continue
            if kn.get(k, 0) >= v:
                continue
            if best.get(k, 0) < v:
                best[k] = v
        for k, v in best.items():
            self.eng[e].wait_ge(self.sem[k], v)
            kn[k] = v

    def _record(self, ev, reads, writes):
        k, v = ev
        for r in reads:
            r = self._rlist(r)
            if r.readers.get(k, 0) < v:
                r.readers[k] = v
        for w in writes:
            w = self._rlist(w)
            w.writer = ev
            w.readers = {}

    def _flush_pe(self):
        if self.pe_pending is not None:
            self.cnt["pe"] += 1
            self.pe_pending.then_inc(self.sem["pe"], 1)
            self.pe_pending = None

    def op(self, e, fn, reads=(), writes=()):
        if e == "pe":
            self._wait(e, self._deps(reads, writes, e))
            ins = fn(self.eng[e])
            self.pe_pending = ins
            self._record((e, self.cnt[e] + 1), reads, writes)
            self.ninstr += 1
            return ins
        self._flush_pe()
        self._wait(e, self._deps(reads, writes, e))
        ins = fn(self.eng[e])
        self.cnt[e] += 1
        ins.then_inc(self.sem[e], 1)
        self._record((e, self.cnt[e]), reads, writes)
        self.ninstr += 1
        return ins

    def pe(self, fn, reads=(), writes=()):
        return self.op("pe", fn, reads, writes)

    def act(self, fn, reads=(), writes=()):
        return self.op("act", fn, reads, writes)

    def dve(self, fn, reads=(), writes=()):
        return self.op("dve", fn, reads, writes)

    def pool(self, fn, reads=(), writes=()):
        return self.op("pool", fn, reads, writes)

    def dma(self, q, out, in_, reads=(), writes=(), **kw):
        self._flush_pe()
        i = self.dnext[q]
        self.dnext[q] = (i + 1) % NDSEM
        k = ("d", q, i)
        evs = self._deps(reads, writes)
        if self.cnt[k]:
            evs.append((k, self.cnt[k]))
        self._wait(q, evs)
        ins = self.eng[q].dma_start(out=out, in_=in_, **kw)
        self.cnt[k] += 16
        ins.then_inc(self.sem[k], 16)
        self._record((k, self.cnt[k]), reads, writes)
        self.ninstr += 1
        return ins

    def barrier(self):
        self._flush_pe()
        evs = [(e, self.cnt[e]) for e in COMPUTE if self.cnt[e]]
        evs += [(k, self.cnt[k]) for k in self.dkeys if self.cnt[k]]
        for e in list(COMPUTE) + ["sp"]:
            self._wait(e, evs)

    def finish(self):
        self._flush_pe()
        evs = [(e, self.cnt[e]) for e in COMPUTE if self.cnt[e]]
        evs += [(k, self.cnt[k]) for k in self.dkeys if self.cnt[k]]
        self._wait("sp", evs)


class Ctx:
    def __init__(self, nc):
        self.nc = nc
        self.S = Sched(nc)
        self._n = 0

    def sb(self, stack, shape, dt, name=None):
        self._n += 1
        name = f"{name or 'sb'}_{self._n}"
        h = stack.enter_context(self.nc.sbuf_tensor(name, list(shape), dt))
        return T(h, name)

    def ps(self, stack, shape, dt=F32, name=None):
        self._n += 1
        name = f"{name or 'ps'}_{self._n}"
        h = stack.enter_context(self.nc.psum_tensor(name, list(shape), dt))
        t = T(h, name)
        t.res.psum = True
        return t


from contextlib import ExitStack


def load_bcast_row(C, q, dst, src_row, n):
    C.S.dma(q, dst[:, 0:n], src_row.partition_broadcast(128), writes=[dst])


def rms_to_T(C, st, x, g_col, xnT, ntiles, ident, tok0=0):
    S = C.S
    with ExitStack() as es:
        ss = C.sb(es, [128, ntiles], F32, "ss")
        rstd = C.sb(es, [128, ntiles], F32, "rstd")
        junk = C.sb(es, [128, D], BF16, "junk")
        xs = [C.sb(es, [128, D], BF16, f"xs{i}") for i in range(2)]
        tps = [C.ps(es, [128, 8, 128], BF16, f"tp{i}") for i in range(2)]
        S.dve(lambda e: e.memset(ss[:, :], 0.0), writes=[ss])
        for t in range(ntiles):
            S.act(lambda e: e.activation(out=junk[:, :], in_=x[:, tok0 + t, :], func=AF.Square,
                                         accum_out=ss[:, t:t + 1]),
                  reads=[x.part(tok0 + t)], writes=[junk, ss])
        rstd_from_ss(C, ss, rstd, ntiles, 1.0 / D, EPS)
        for t in range(ntiles):
            xb = xs[t % 2]
            tp = tps[t % 2]
            S.act(lambda e: e.activation(out=xb[:, :], in_=x[:, tok0 + t, :], func=AF.Copy,
                                         scale=rstd[:, t:t + 1]),
                  reads=[x.part(tok0 + t), rstd], writes=[xb])
            for kc in range(8):
                S.pe(lambda e: e.transpose(out=tp[:, kc, :], in_=xb[:, kc * 128:(kc + 1) * 128], identity=ident[:, :]),
                     reads=[xb, ident], writes=[tp])
            S.dve(lambda e: e.tensor_tensor(out=xnT[:, :, t * 128:(t + 1) * 128], in0=tp[:, :, :],
                                            in1=g_col.unsqueeze(2).to_broadcast([128, 8, 128]), op=ALU.mult),
                  reads=[tp], writes=[xnT.part(t)])
        S.barrier()


def rstd_from_ss(C, ss, rstd, n, scale, eps):
    S = C.S
    S.dve(lambda e: e.tensor_scalar(out=rstd[:, 0:n], in0=ss[:, 0:n], scalar1=scale, scalar2=eps,
                                    op0=ALU.mult, op1=ALU.add), reads=[ss], writes=[rstd])
    S.act(lambda e: e.activation(out=rstd[:, 0:n], in_=rstd[:, 0:n], func=AF.Sqrt), reads=[rstd], writes=[rstd])
    S.dve(lambda e: e.reciprocal(out=rstd[:, 0:n], in_=rstd[:, 0:n]), reads=[rstd], writes=[rstd])


def post_norm_residual(C, st, x, tile_idx, y_ps, g_bc, coef, scr):
    S = C.S
    ss, rstd, junk, tmp = scr
    S.dve(lambda e: e.memset(ss[:, 0:2], 0.0), writes=[ss])
    for h in range(2):
        S.act(lambda e: e.activation(out=junk[:, 0:512], in_=y_ps[h][:, :], func=AF.Square,
                                     accum_out=ss[:, h:h + 1]), reads=[y_ps[h]], writes=[junk, ss])
    S.dve(lambda e: e.tensor_tensor(out=ss[:, 2:3], in0=ss[:, 0:1], in1=ss[:, 1:2], op=ALU.add), reads=[ss], writes=[ss])
    S.dve(lambda e: e.tensor_scalar(out=rstd[:, 0:1], in0=ss[:, 2:3], scalar1=1.0 / D, scalar2=EPS,
                                    op0=ALU.mult, op1=ALU.add), reads=[ss], writes=[rstd])
    S.act(lambda e: e.activation(out=rstd[:, 0:1], in_=rstd[:, 0:1], func=AF.Sqrt), reads=[rstd], writes=[rstd])
    S.dve(lambda e: e.reciprocal(out=rstd[:, 0:1], in_=rstd[:, 0:1]), reads=[rstd], writes=[rstd])
    if coef != 1.0:
        S.dve(lambda e: e.tensor_scalar(out=rstd[:, 0:1], in0=rstd[:, 0:1], scalar1=float(coef), scalar2=None,
                                        op0=ALU.mult), reads=[rstd], writes=[rstd])
    for h in range(2):
        sl = slice(h * 512, (h + 1) * 512)
        S.dve(lambda e: e.tensor_tensor(out=tmp[:, sl], in0=y_ps[h][:, :], in1=g_bc[:, sl], op=ALU.mult),
              reads=[y_ps[h], g_bc], writes=[tmp])
        S.dve(lambda e: e.scalar_tensor_tensor(out=x[:, tile_idx, sl], in0=tmp[:, sl], scalar=rstd[:, 0:1],
                                               in1=x[:, tile_idx, sl], op0=ALU.mult, op1=ALU.add),
              reads=[tmp, rstd, x.part(tile_idx)], writes=[x.part(tile_idx)])


def ffn_block(C, x, l, j, W, P, ident):
    S = C.S
    nc = C.nc
    n_in = 0 if j == 0 else 6
    n_out = 1 if j == 0 else 7
    wg = W.L("ffn_w_gate", l)[j].rearrange("(kc p) f -> p kc f", p=128)
    wu = W.L("ffn_w_up", l)[j].rearrange("(kc p) f -> p kc f", p=128)
    wd = W.L("ffn_w_down", l)[j].rearrange("(fc p) d -> p fc d", p=128)
    TG = 1024
    with ExitStack() as es:
        g_bc = C.sb(es, [128, D], F32, "g_bc")
        load_bcast_row(C, "sp", g_bc, W.L("norm_g", l)[n_out], D)
        hT = C.sb(es, [128, NFC, TG], BF16, "hT")
        for tg in range(SEQ // TG):
            with ExitStack() as es2:
                xnT = C.sb(es2, [128, 8, TG], BF16, "xnT")
                rms_to_T(C, es2, x, P.cols(("norm_g", l, n_in)), xnT, TG // 128, ident,
                         tok0=tg * (TG // 128))
                wbuf = [(C.sb(es2, [128, 8, 256], BF16, f"wg{i}"), C.sb(es2, [128, 8, 256], BF16, f"wu{i}")) for i in range(2)]
                sg = [C.sb(es2, [128, 512], BF16, f"sg{i}") for i in range(2)]
                gps = [C.ps(es2, [128, 512], F32, f"gps{i}") for i in range(2)]
                ups = [C.ps(es2, [128, 512], F32, f"ups{i}") for i in range(2)]
                it = 0
                for f2 in range(NFC // 2):
                    wgt, wut = wbuf[f2 % 2]
                    S.dma("pool", wgt[:, :, :], wg[:, :, f2 * 256:(f2 + 1) * 256], writes=[wgt])
                    S.dma("pool", wut[:, :, :], wu[:, :, f2 * 256:(f2 + 1) * 256], writes=[wut])
                    for fi in range(2):
                        fc = f2 * 2 + fi
                        for th in range(TG // 512):
                            gp, up, sgt = gps[it % 2], ups[it % 2], sg[it % 2]
                            it += 1
                            for kc in range(8):
                                S.pe(lambda e: e.matmul(out=gp[:, :], lhsT=wgt[:, kc, fi * 128:(fi + 1) * 128],
                                                        rhs=xnT[:, kc, th * 512:(th + 1) * 512],
                                                        start=(kc == 0), stop=(kc == 7)),
                                     reads=[wgt, xnT], writes=[gp])
                            for kc in range(8):
                                S.pe(lambda e: e.matmul(out=up[:, :], lhsT=wut[:, kc, fi * 128:(fi + 1) * 128],
                                                        rhs=xnT[:, kc, th * 512:(th + 1) * 512],
                                                        start=(kc == 0), stop=(kc == 7)),
                                     reads=[wut, xnT], writes=[up])
                            S.act(lambda e: e.activation(out=sgt[:, :], in_=gp[:, :], func=AF.Silu),
                                  reads=[gp], writes=[sgt])
                            S.dve(lambda e: e.tensor_tensor(out=hT[:, fc, th * 512:(th + 1) * 512], in0=up[:, :],
                                                            in1=sgt[:, :], op=ALU.mult),
                                  reads=[up, sgt], writes=[hT.part((fc, th))])
                S.barrier()
            with ExitStack() as es3:
                wdt = C.sb(es3, [128, NFC, D], BF16, "wd")
                for f2 in range(NFC // 2):
                    S.dma("pool", wdt[:, f2 * 2:f2 * 2 + 2, :], wd[:, f2 * 2:f2 * 2 + 2, :], writes=[wdt.part(f2)])
                yps = [[C.ps(es3, [128, 512], F32, f"y{i}{h}") for h in range(2)] for i in range(2)]
                scr = (C.sb(es3, [128, 4], F32, "pss"), C.sb(es3, [128, 2], F32, "prs"),
                       C.sb(es3, [128, 512], BF16, "pjunk"), C.sb(es3, [128, D], F32, "ptmp"))
                for tt in range(TG // 128):
                    yp = yps[tt % 2]
                    for h in range(2):
                        for fc in range(NFC):
                            S.pe(lambda e: e.matmul(out=yp[h][:, :], lhsT=hT[:, fc, tt * 128:(tt + 1) * 128],
                                                    rhs=wdt[:, fc, h * 512:(h + 1) * 512],
                                                    start=(fc == 0), stop=(fc == NFC - 1)),
                                 reads=[hT, wdt.part(fc // 2)], writes=[yp[h]])
                    post_norm_residual(C, es3, x, tg * (TG // 128) + tt, yp, g_bc, 0.5, scr)
                S.barrier()


DBG = {}


class Wts:
    def __init__(self, nc, shapes, loff=0, hoff=0):
        self.nc = nc
        self.shapes = shapes
        self.aps = {}
        self.loff = loff
        self.hoff = hoff

    def __getitem__(self, name):
        if name not in self.aps:
            self.aps[name] = self.nc.dram_tensor(name, list(self.shapes[name]), F32, kind="ExternalInput").ap()
        return self.aps[name]

    def L(self, name, l):
        return self[name][l - self.loff]

    def H(self, name, i):
        return self[name][i - self.hoff]

    def hasL(self, name, l):
        return 0 <= l - self.loff < self.shapes[name][0]

    def hasH(self, name, i):
        return 0 <= i - self.hoff < self.shapes[name][0]


class Params:
    def __init__(self):
        self.rows = {}
        self.t = None

    def cols(self, key, kc0=0, n=8):
        r = self.rows[key]
        return self.t[:, kc0:kc0 + n, r]

    def col(self, key, kc):
        r = self.rows[key]
        return self.t[:, kc, r:r + 1]


def build_params(C, es, W, identf):
    S = C.S
    P = Params()
    rows = []
    for l in range(DEPTH):
        if W.hasL("norm_g", l):
            for n in range(8):
                rows.append((("norm_g", l, n), W.L("norm_g", l)[n], D))
    rows.append((("mem_g",), W["mem_norm_g"], D))
    for i in range(2):
        if "sc_conv_w" in W.shapes and W.hasH("sc_conv_w", i):
            for k in range(3):
                rows.append((("sc_w", i, k), W.H("sc_conv_w", i)[k], 512))
            rows.append((("sc_b", i), W.H("sc_conv_b", i), 512))
            rows.append((("dsa_qg", i), W.H("dsa_q_norm_g", i), 256))
        if "rw_w0" in W.shapes and W.hasH("rw_w0", i):
            for nm in ("rw_w0", "rw_a0", "rw_k_k", "rw_k_a", "rw_ln_g", "rw_ln_b", "rt_gn_g", "rt_gn_b"):
                rows.append(((nm, i), W.H(nm, i), 512))
            rows.append((("rw_r_k", i), W.H("rw_r_k", i).rearrange("h d -> (h d)"), 512))
            rows.append((("rw_mu", i, 0), W.H("rw_mu", i)[0:1024], 1024))
            rows.append((("rw_mu", i, 1), W.H("rw_mu", i)[1024:1792], 768))
    nr = len(rows)
    assert nr <= 128
    PC = C.sb(es, [128, 8, nr], F32, "PC")
    P.t = PC
    with ExitStack() as e1:
        raw = C.sb(e1, [128, D], F32, "praw")
        S.pool(lambda e: e.memset(raw[:, :], 0.0), writes=[raw])
        for r, (key, ap, n) in enumerate(rows):
            P.rows[key] = r
            S.dma("sp", raw[r:r + 1, 0:n], ap.rearrange("(o n) -> o n", o=1), writes=[raw])
        tp = C.ps(e1, [128, 8, 128], F32, "ptp")
        for kc in range(8):
            S.pe(lambda e: e.transpose(out=tp[:, kc, :], in_=raw[:, kc * 128:(kc + 1) * 128], identity=identf[:, :]),
                 reads=[raw, identf], writes=[tp])
        S.dve(lambda e: e.tensor_copy(out=PC[:, :, :], in_=tp[:, :, 0:nr]), reads=[tp], writes=[PC])
        S.barrier()
    return P


def load_w(C, dst, src2d, c0, c1, q="pool"):
    v = src2d.rearrange("(kc p) n -> p kc n", p=128)
    nk = v.shape[1]
    for kc in range(nk):
        C.S.dma(q, dst[:, kc, 0:c1 - c0], v[:, kc, c0:c1], writes=[dst.part(kc)])


def make_consts(C, es):
    S = C.S
    K = {}
    ones_f = C.sb(es, [128, 512], F32, "ones_f")
    S.pool(lambda e: e.memset(ones_f[:, :], 1.0), writes=[ones_f])
    ident = C.sb(es, [128, 128], BF16, "ident")
    identf = C.sb(es, [128, 128], F32, "identf")
    for t in (ident, identf):
        S.pool(lambda e: e.affine_select(out=t[:, :], in_=ones_f[:, 0:128], pattern=[[-1, 128]],
                                         compare_op=ALU.is_equal, fill=0.0, base=0, channel_multiplier=1),
               reads=[ones_f], writes=[t])
    ones_bf = C.sb(es, [128, 128], BF16, "ones_bf")
    S.pool(lambda e: e.memset(ones_bf[:, :], 1.0), writes=[ones_bf])
    selq = C.sb(es, [128, 8, 8], BF16, "selq")
    S.pool(lambda e: e.affine_select(out=selq[:, :, :], in_=ones_f[:, 0:64].rearrange("p (a b) -> p a b", a=8),
                                     pattern=[[1, 8], [-1, 8]], compare_op=ALU.is_equal, fill=0.0, base=0,
                                     channel_multiplier=0), reads=[ones_f], writes=[selq])
    sel8 = C.sb(es, [8, 8, 128], BF16, "sel8")
    with ExitStack() as e0:
        sel8a = C.sb(e0, [8, 8, 128], F32, "sel8a")
        S.pool(lambda e: e.memset(sel8a[:, :, :], 1.0), writes=[sel8a])
        S.pool(lambda e: e.affine_select(out=sel8[:, :, :], in_=sel8a[:, :, :], pattern=[[-1, 8], [0, 128]],
                                         compare_op=ALU.is_equal, fill=0.0, base=0, channel_multiplier=1),
               reads=[sel8a], writes=[sel8])
        S.barrier()
    zer = C.sb(es, [128, 128], F32, "zer")
    S.pool(lambda e: e.memset(zer[:, :], 0.0), writes=[zer])
    cbias = C.sb(es, [128, 128], F32, "cbias")
    S.pool(lambda e: e.affine_select(out=cbias[:, :], in_=zer[:, :], pattern=[[-1, 128]],
                                     compare_op=ALU.is_ge, fill=-1e30, base=0, channel_multiplier=1),
           reads=[zer], writes=[cbias])
    K.update(ones_f=ones_f, ident=ident, identf=identf, ones_bf=ones_bf, selq=selq, sel8=sel8, cbias=cbias, zer=zer)
    return K


def make_negm(C, es, qT, nh, k2m, K, p8):
    S = C.S
    qsq = C.sb(es, [128, nh, 512], BF16, "qsq")
    S.act(lambda e: e.activation(out=qsq[:, :, :], in_=qT[:, 0:nh, :], func=AF.Square), reads=[qT], writes=[qsq])
    for h in range(nh):
        S.pe(lambda e: e.matmul(out=p8[0:8, :], lhsT=K["selq"][:, h, :], rhs=qsq[:, h, :], start=(h == 0), stop=(h == nh - 1)),
             reads=[K["selq"], qsq], writes=[p8])
    nm = C.sb(es, [8, 512], F32, "nm")
    negm8 = C.sb(es, [8, 512], BF16, "negm8")
    S.dve(lambda e: e.tensor_scalar(out=nm[:, :], in0=p8[0:8, :], scalar1=k2m[:, 0:1], scalar2=None, op0=ALU.mult),
          reads=[p8, k2m], writes=[nm])
    S.act(lambda e: e.activation(out=nm[:, :], in_=nm[:, :], func=AF.Sqrt), reads=[nm], writes=[nm])
    S.dve(lambda e: e.tensor_scalar(out=negm8[:, :], in0=nm[:, :], scalar1=-1.0, scalar2=None, op0=ALU.mult),
          reads=[nm], writes=[negm8])
    return negm8


def attn_core(C, K, qT_ap, q_res, ktiles, negm8, h, scale, out_ap, out_res, dv, ps_s, ps_o, ps_r, pts, rinv, mask_eng="pool"):
    S = C.S
    n = len(ktiles)
    for i, (k_ap, v_ap, m_ap, rd) in enumerate(ktiles):
        sp = ps_s[i % len(ps_s)]
        pt = pts[i % len(pts)]
        S.pe(lambda e: e.matmul(out=sp[:, :], lhsT=k_ap, rhs=qT_ap, start=True, stop=False), reads=rd + [q_res], writes=[sp])
        S.pe(lambda e: e.matmul(out=sp[:, :], lhsT=K["sel8"][:, h, :], rhs=negm8[:, :], start=False, stop=(m_ap is None)),
             reads=[K["sel8"], negm8], writes=[sp])
        if m_ap is not None:
            S.pe(lambda e: e.matmul(out=sp[:, :], lhsT=K["ident"][:, :], rhs=m_ap, start=False, stop=True),
                 reads=rd + [K["ident"]], writes=[sp])
        S.act(lambda e: e.activation(out=pt[:, :], in_=sp[:, :], func=AF.Exp, scale=float(scale)), reads=[sp], writes=[pt])
        S.pe(lambda e: e.matmul(out=ps_o[0:dv, :], lhsT=v_ap, rhs=pt[:, :], start=(i == 0), stop=(i == n - 1)),
             reads=rd + [pt], writes=[ps_o])
        S.pe(lambda e: e.matmul(out=ps_r[0:dv, :], lhsT=K["ones_bf"][:, 0:dv], rhs=pt[:, :], start=(i == 0), stop=(i == n - 1)),
             reads=[K["ones_bf"], pt], writes=[ps_r])
    S.dve(lambda e: e.reciprocal(out=rinv[0:dv, :], in_=ps_r[0:dv, :]), reads=[ps_r], writes=[rinv])
    S.dve(lambda e: e.tensor_tensor(out=out_ap, in0=ps_o[0:dv, :], in1=rinv[0:dv, :], op=ALU.mult),
          reads=[ps_o, rinv], writes=[out_res])


def out_proj_residual(C, x, chunks, w_T, g_bc, coef, tiles):
    S = C.S
    n = len(chunks)
    with ExitStack() as es:
        yps = [[C.ps(es, [128, 512], F32, f"y{i}{h}") for h in range(2)] for i in range(2)]
        scr = (C.sb(es, [128, 4], F32, "pss"), C.sb(es, [128, 2], F32, "prs"),
               C.sb(es, [128, 512], BF16, "pjunk"), C.sb(es, [128, D], F32, "ptmp"))
        for ti, tt in enumerate(tiles):
            yp = yps[ti % 2]
            for h in range(2):
                for c, (yt, f) in enumerate(chunks):
                    S.pe(lambda e: e.matmul(out=yp[h][:, :], lhsT=f(ti), rhs=w_T[:, c, h * 512:(h + 1) * 512],
                                            start=(c == 0), stop=(c == n - 1)), reads=[yt, w_T], writes=[yp[h]])
            post_norm_residual(C, None, x, tt, yp, g_bc, coef, scr)
        S.barrier()


def xattn_block(C, x, l, W, P, K, memT):
    S = C.S
    ident = K["ident"]
    scale = 128 ** -0.5
    with ExitStack() as es:
        g_bc = C.sb(es, [128, D], F32, "g_bc")
        load_bcast_row(C, "sp", g_bc, W.L("norm_g", l)[5], D)
        wq = C.sb(es, [128, 8, 512], BF16, "wq")
        wk = C.sb(es, [128, 8, 512], BF16, "wk")
        wv = C.sb(es, [128, 8, 512], BF16, "wv")
        wo = C.sb(es, [128, 4, D], BF16, "wo")
        load_w(C, wk, W.L("xa_wk", l), 0, 512)
        load_w(C, wv, W.L("xa_wv", l), 0, 512)
        load_w(C, wq, W.L("xa_wq", l), 0, 512)
        wov = W.L("xa_wo", l).rearrange("(c p) d -> p c d", p=128)
        for c in range(4):
            S.dma("pool", wo[:, c, :], wov[:, c, :], writes=[wo.part(c)])
        kT = C.sb(es, [128, 4, MEM], BF16, "kT")
        vtok = C.sb(es, [128, 2, 512], BF16, "vtok")
        k2m = C.sb(es, [8, 1], F32, "k2m")
        with ExitStack() as e1:
            pk = C.ps(e1, [128, 512], F32, "pk")
            for h in range(4):
                for kc in range(8):
                    S.pe(lambda e: e.matmul(out=pk[:, 0:MEM], lhsT=wk[:, kc, h * 128:(h + 1) * 128], rhs=memT[:, kc, :],
                                            start=(kc == 0), stop=(kc == 7)), reads=[wk, memT], writes=[pk])
                S.act(lambda e: e.copy(out=kT[:, h, :], in_=pk[:, 0:MEM]), reads=[pk], writes=[kT])
            for mt in range(2):
                for kc in range(8):
                    S.pe(lambda e: e.matmul(out=pk[:, :], lhsT=memT[:, kc, mt * 128:(mt + 1) * 128], rhs=wv[:, kc, :],
                                            start=(kc == 0), stop=(kc == 7)), reads=[wv, memT], writes=[pk])
                S.dve(lambda e: e.tensor_copy(out=vtok[:, mt, :], in_=pk[:, :]), reads=[pk], writes=[vtok])
            ksq = C.sb(e1, [128, 4, MEM], BF16, "ksq")
            S.act(lambda e: e.activation(out=ksq[:, :, :], in_=kT[:, :, :], func=AF.Square), reads=[kT], writes=[ksq])
            for h in range(4):
                S.pe(lambda e: e.matmul(out=pk[0:8, 0:MEM], lhsT=K["selq"][:, h, :], rhs=ksq[:, h, :], start=(h == 0), stop=(h == 3)),
                     reads=[K["selq"], ksq], writes=[pk])
            S.dve(lambda e: e.reduce_max(out=k2m[:, 0:1], in_=pk[0:8, 0:MEM], axis=AX.X), reads=[pk], writes=[k2m])
            S.barrier()
        for qg in range(4):
            with ExitStack() as e2:
                oT = C.sb(e2, [128, 4, 512], BF16, "oT")
                with ExitStack() as e3:
                    xnT = C.sb(e3, [128, 8, 512], BF16, "xnT")
                    rms_to_T(C, e3, x, P.cols(("norm_g", l, 4)), xnT, 4, ident, tok0=qg * 4)
                    qT = C.sb(e3, [128, 4, 512], BF16, "qT")
                    pq = [C.ps(e3, [128, 512], F32, f"pq{i}") for i in range(2)]
                    for h in range(4):
                        for kc in range(8):
                            S.pe(lambda e: e.matmul(out=pq[h % 2][:, :], lhsT=wq[:, kc, h * 128:(h + 1) * 128], rhs=xnT[:, kc, :],
                                                    start=(kc == 0), stop=(kc == 7)), reads=[wq, xnT], writes=[pq[h % 2]])
                        S.act(lambda e: e.copy(out=qT[:, h, :], in_=pq[h % 2][:, :]), reads=[pq[h % 2]], writes=[qT.part(h)])
                    negm8 = make_negm(C, e3, qT, 4, k2m, K, pq[0])
                    ps_s = [C.ps(e3, [128, 512], F32, f"ss{i}") for i in range(2)]
                    ps_o = C.ps(e3, [128, 512], F32, "pso")
                    ps_r = C.ps(e3, [128, 512], F32, "psr")
                    pts = [C.sb(e3, [128, 512], BF16, f"pt{i}") for i in range(2)]
                    rinv = C.sb(e3, [128, 512], F32, "rinv")
                    for h in range(4):
                        kt_list = [(kT[:, h, kt * 128:(kt + 1) * 128], vtok[:, kt, h * 128:(h + 1) * 128], None, [kT, vtok])
                                   for kt in range(2)]
                        attn_core(C, K, qT[:, h, :], qT, kt_list, negm8, h, scale, oT[:, h, :], oT.part(h), 128,
                                  ps_s, ps_o, ps_r, pts, rinv)
                    S.barrier()
                out_proj_residual(C, x, [(oT, (lambda ti, c=c: oT[:, c, ti * 128:(ti + 1) * 128])) for c in range(4)],
                                  wo, g_bc, 1.0, [qg * 4 + i for i in range(4)])


def prep_mem(C, es, W, P, K, s, memT):
    S = C.S
    with ExitStack() as e1:
        mt = C.sb(e1, [128, 2, D], F32, "memraw")
        S.dma("sp", mt[:, :, :], W["mem"][s].rearrange("(t p) d -> p t d", p=128), writes=[mt.part(0), mt.part(1)])
        rms_to_T(C, e1, mt, P.cols(("mem_g",)), memT, 2, K["ident"], tok0=0)


def odd_mixer(C, x, l, W, P, K):
    S = C.S
    i = l // 2
    ident = K["ident"]
    w_in = W.H("od_w_in", i)
    NEG_SEL = -3.0e38
    with ExitStack() as es:
        g_bc = C.sb(es, [128, D], F32, "g_bc")
        load_bcast_row(C, "sp", g_bc, W.L("norm_g", l)[3], D)
        ycT = C.sb(es, [128, 4, SEQ], BF16, "ycT")
        ydT = C.sb(es, [128, 4, SEQ], BF16, "ydT")
        cqnT = C.sb(es, [128, 2, SEQ], BF16, "cqnT")
        ckvT = C.sb(es, [128, SEQ], BF16, "ckvT")
        ckvtok = C.sb(es, [128, NT, 128], BF16, "ckvtok")
        kidxT2 = C.sb(es, [128, SEQ], BF16, "kidxT2")
        widx = C.sb(es, [128, NT, 8], F32, "widx")
        absw = C.sb(es, [128, NT, 8], F32, "absw")
        sgnw = C.sb(es, [128, NT, 8], F32, "sgnw")
        carry = C.sb(es, [128, 4, 2], F32, "carry")
        S.pool(lambda e: e.memset(carry[:, :, :], 0.0), writes=[carry])
        gkv_bc = C.sb(es, [128, 128], F32, "gkv_bc")
        load_bcast_row(C, "sp", gkv_bc, W.H("dsa_kv_norm_g", i), 128)
        TG = 1024
        with ExitStack() as e1:
            wsm = C.sb(e1, [128, 8, 456], BF16, "wsm")
            load_w(C, wsm, w_in, 0, 456)
            wscs = [C.sb(e1, [128, 8, 3, 128], BF16, f"wsc{k}") for k in range(2)]
            ss = C.sb(e1, [128, 2], F32, "ss2")
            rs = C.sb(e1, [128, 2], F32, "rs2")
            junk = C.sb(e1, [128, 256], BF16, "junk2")
            cqs = C.sb(e1, [128, 256], BF16, "cqs")
            kid2 = C.sb(e1, [128, 2, 64], BF16, "kid2")
            hs = C.sb(e1, [128, 512], F32, "hs")
            ub = C.sb(e1, [128, 514], F32, "ub")
            yb = C.sb(e1, [128, 512], F32, "yb")
            w_v = w_in.rearrange("(kc p) n -> p kc n", p=128)
            for tg in range(SEQ // TG):
                with ExitStack() as e2:
                    xnT = C.sb(e2, [128, 8, TG], BF16, "xnT")
                    rms_to_T(C, e2, x, P.cols(("norm_g", l, 2)), xnT, TG // 128, ident, tok0=tg * (TG // 128))
                    pp = C.ps(e2, [128, 512], F32, "pp")
                    tp = C.ps(e2, [128, 8, 128], BF16, "tp4")
                    for tt in range(TG // 128 if DBG.get("odd_stop") != 0.25 else 0):
                        Tt = tg * (TG // 128) + tt
                        for kc in range(8):
                            S.pe(lambda e: e.matmul(out=pp[:, 0:456], lhsT=xnT[:, kc, tt * 128:(tt + 1) * 128], rhs=wsm[:, kc, 0:456],
                                                    start=(kc == 0), stop=(kc == 7)), reads=[xnT, wsm], writes=[pp])
                        S.dve(lambda e: e.memset(ss[:, :], 0.0), writes=[ss])
                        S.act(lambda e: e.activation(out=junk[:, 0:256], in_=pp[:, 0:256], func=AF.Square, accum_out=ss[:, 0:1]),
                              reads=[pp], writes=[junk, ss])
                        S.act(lambda e: e.activation(out=junk[:, 0:128], in_=pp[:, 256:384], func=AF.Square, accum_out=ss[:, 1:2]),
                              reads=[pp], writes=[junk, ss])
                        S.dve(lambda e: e.tensor_scalar(out=rs[:, 0:1], in0=ss[:, 0:1], scalar1=1.0 / 256, scalar2=EPS,
                                                        op0=ALU.mult, op1=ALU.add), reads=[ss], writes=[rs])
                        S.dve(lambda e: e.tensor_scalar(out=rs[:, 1:2], in0=ss[:, 1:2], scalar1=1.0 / 128, scalar2=EPS,
                                                        op0=ALU.mult, op1=ALU.add), reads=[ss], writes=[rs])
                        S.act(lambda e: e.activation(out=rs[:, :], in_=rs[:, :], func=AF.Sqrt), reads=[rs], writes=[rs])
                        S.dve(lambda e: e.reciprocal(out=rs[:, :], in_=rs[:, :]), reads=[rs], writes=[rs])
                        S.act(lambda e: e.activation(out=cqs[:, :], in_=pp[:, 0:256], func=AF.Copy, scale=rs[:, 0:1]),
                              reads=[pp, rs], writes=[cqs])
                        S.dve(lambda e: e.scalar_tensor_tensor(out=ckvtok[:, Tt, :], in0=pp[:, 256:384], scalar=rs[:, 1:2],
                                                               in1=gkv_bc[:, :], op0=ALU.mult, op1=ALU.mult),
                              reads=[pp, rs, gkv_bc], writes=[ckvtok.part(Tt)])
                        S.dve(lambda e: e.tensor_copy(out=kid2[:, :, :], in_=pp[:, 384:448].unsqueeze(1).to_broadcast([128, 2, 64])),
                              reads=[pp], writes=[kid2])
                        S.dve(lambda e: e.tensor_copy(out=widx[:, Tt, :], in_=pp[:, 448:456]), reads=[pp], writes=[widx.part(Tt)])
                        for c in range(2):
                            S.pe(lambda e: e.transpose(out=tp[:, c, :], in_=cqs[:, c * 128:(c + 1) * 128], identity=ident[:, :]),
                                 reads=[cqs, ident], writes=[tp])
                        S.pe(lambda e: e.transpose(out=tp[:, 2, :], in_=ckvtok[:, Tt, :], identity=ident[:, :]),
                             reads=[ckvtok.part(Tt), ident], writes=[tp])
                        S.pe(lambda e: e.transpose(out=tp[:, 3, :], in_=kid2[:, :, :].rearrange("p a b -> p (a b)"), identity=ident[:, :]),
                             reads=[kid2, ident], writes=[tp])
                        tsl = slice(Tt * 128, (Tt + 1) * 128)
                        S.dve(lambda e: e.tensor_tensor(out=cqnT[:, :, tsl], in0=tp[:, 0:2, :],
                                                        in1=P.cols(("dsa_qg", i), 0, 2).unsqueeze(2).to_broadcast([128, 2, 128]),
                                                        op=ALU.mult), reads=[tp, P.t], writes=[cqnT.part(Tt)])
                        S.act(lambda e: e.copy(out=ckvT[:, tsl], in_=tp[:, 2, :]), reads=[tp], writes=[ckvT.part(Tt)])
                        S.act(lambda e: e.copy(out=kidxT2[:, tsl], in_=tp[:, 3, :]), reads=[tp], writes=[kidxT2.part(Tt)])
                    pcs = [C.ps(e2, [128, 512], F32, f"pc{k}") for k in range(3)]
                    for fc in range(4 if DBG.get("odd_stop") != 0.5 else 0):
                        wsc = wscs[fc % 2]
                        for kc in range(8):
                            S.dma("pool", wsc[:, kc, :, :],
                                  w_v[:, kc, 456:1992].rearrange("p (j c) -> p j c", j=3)[:, :, fc * 128:(fc + 1) * 128],
                                  writes=[wsc.part(kc)])
                        for th in range(TG // 512):
                            tok0 = tg * TG + th * 512
                            for j3 in range(3):
                                for kc in range(8):
                                    S.pe(lambda e: e.matmul(out=pcs[j3][:, :], lhsT=wsc[:, kc, j3, :], rhs=xnT[:, kc, th * 512:(th + 1) * 512],
                                                            start=(kc == 0), stop=(kc == 7)), reads=[wsc, xnT], writes=[pcs[j3]])
                            S.act(lambda e: e.copy(out=hs[:, :], in_=pcs[0][:, :]), reads=[pcs[0]], writes=[hs])
                            S.pool(lambda e: e.tensor_copy(out=ub[:, 0:2], in_=carry[:, fc, :]), reads=[carry.part(fc)], writes=[ub])
                            S.dve(lambda e: e.tensor_tensor(out=ub[:, 2:514], in0=pcs[2][:, :], in1=hs[:, :], op=ALU.mult),
                                  reads=[pcs[2], hs], writes=[ub])
                            S.pool(lambda e: e.tensor_copy(out=carry[:, fc, :], in_=ub[:, 512:514]), reads=[ub], writes=[carry.part(fc)])
                            S.pool(lambda e: e.tensor_scalar(out=yb[:, :], in0=ub[:, 2:514], scalar1=P.col(("sc_w", i, 2), fc),
                                                             scalar2=P.col(("sc_b", i), fc), op0=ALU.mult, op1=ALU.add),
                                   reads=[ub, P.t], writes=[yb])
                            S.dve(lambda e: e.scalar_tensor_tensor(out=yb[:, :], in0=ub[:, 1:513], scalar=P.col(("sc_w", i, 1), fc),
                                                                   in1=yb[:, :], op0=ALU.mult, op1=ALU.add), reads=[ub, yb, P.t], writes=[yb])
                            S.dve(lambda e: e.scalar_tensor_tensor(out=yb[:, :], in0=ub[:, 0:512], scalar=P.col(("sc_w", i, 0), fc),
                                                                   in1=yb[:, :], op0=ALU.mult, op1=ALU.add), reads=[ub, yb, P.t], writes=[yb])
                            S.dve(lambda e: e.tensor_tensor(out=ydT[:, fc, tok0:tok0 + 512], in0=pcs[1][:, :], in1=yb[:, :], op=ALU.mult),
                                  reads=[pcs[1], yb], writes=[ydT.part((fc, tok0))])
                    S.barrier()
            S.act(lambda e: e.activation(out=absw[:, :, :], in_=widx[:, :, :], func=AF.Abs), reads=[widx], writes=[absw])
            S.act(lambda e: e.activation(out=sgnw[:, :, :], in_=widx[:, :, :], func=AF.Sign), reads=[widx], writes=[sgnw])
            S.barrier()
        if DBG.get("odd_stop") in (1, 0.5, 0.25):
            return
        with ExitStack() as e1:
            wqi = C.sb(e1, [128, 2, 512], BF16, "wqi")
            wuq = C.sb(e1, [128, 2, 512], BF16, "wuq")
            wuk = C.sb(e1, [128, 4, 128], BF16, "wuk")
            wuv = C.sb(e1, [128, 8, 64], BF16, "wuv")
            load_w(C, wqi, W.H("dsa_w_qi", i).rearrange("r h d -> r (h d)"), 0, 512)
            load_w(C, wuq, W.H("dsa_w_uq", i).rearrange("r h d -> r (h d)"), 0, 512)
            S.dma("pool", wuk[:, :, :], W.H("dsa_w_uk", i).rearrange("(hp e) d c -> (e d) hp c", e=2), writes=[wuk])
            S.dma("pool", wuv[:, :, :], W.H("dsa_w_uv", i).rearrange("h c d -> c h d"), writes=[wuv])
            k2m = C.sb(e1, [8, 1], F32, "k2m")
            with ExitStack() as e2:
                ksq = C.sb(e2, [128, SEQ], BF16, "ksq")
                k2b = C.sb(e2, [8, 4], F32, "k2b")
                pk = C.ps(e2, [128, 512], F32, "pk")
                S.act(lambda e: e.activation(out=ksq[:, :], in_=ckvT[:, :], func=AF.Square), reads=[ckvT], writes=[ksq])
                for b in range(4):
                    S.pe(lambda e: e.matmul(out=pk[0:8, :], lhsT=K["ones_bf"][:, 0:8], rhs=ksq[:, b * 512:(b + 1) * 512], start=True, stop=True),
                         reads=[K["ones_bf"], ksq], writes=[pk])
                    S.dve(lambda e: e.reduce_max(out=k2b[:, b:b + 1], in_=pk[0:8, :], axis=AX.X), reads=[pk], writes=[k2b])
                S.dve(lambda e: e.reduce_max(out=k2m[:, 0:1], in_=k2b[:, :], axis=AX.X), reads=[k2b], writes=[k2m])
                S.barrier()
            for qg in range(4):
                tsl = slice(qg * 512, (qg + 1) * 512)
                nkt = 4 * qg + 4
                with ExitStack() as e2:
                    qidxT = C.sb(e2, [128, 4, 512], BF16, "qidxT")
                    qhT = C.sb(e2, [128, 4, 512], BF16, "qhT")
                    qlatT = C.sb(e2, [128, 8, 512], BF16, "qlatT")
                    maskT = C.sb(e2, [128, nkt, 512], BF16, "maskT")
                    S.pool(lambda e: e.memset(maskT[:, :, :], -30000.0), writes=[maskT])
                    with ExitStack() as e3:
                        pq = [C.ps(e3, [128, 512], F32, f"pq{k}") for k in range(2)]
                        n_ = 0
                        for (wt, dst) in ((wqi, qidxT), (wuq, qhT)):
                            for hp in range(4):
                                p_ = pq[n_ % 2]
                                n_ += 1
                                for rc in range(2):
                                    S.pe(lambda e: e.matmul(out=p_[:, :], lhsT=wt[:, rc, hp * 128:(hp + 1) * 128], rhs=cqnT[:, rc, tsl],
                                                            start=(rc == 0), stop=(rc == 1)), reads=[wt, cqnT], writes=[p_])
                                S.act(lambda e: e.copy(out=dst[:, hp, :], in_=p_[:, :]), reads=[p_], writes=[dst.part(hp)])
                        for h in range(8):
                            hp, e_ = h // 2, h % 2
                            p_ = pq[h % 2]
                            S.pe(lambda e: e.matmul(out=p_[:, :], lhsT=wuk[e_ * 64:(e_ + 1) * 64, hp, :], rhs=qhT[e_ * 64:(e_ + 1) * 64, hp, :],
                                                    start=True, stop=True), reads=[wuk, qhT], writes=[p_])
                            S.dve(lambda e: e.tensor_copy(out=qlatT[:, h, :], in_=p_[:, :]), reads=[p_], writes=[qlatT.part(h)])
                        S.barrier()
                    if DBG.get("odd_stop") == 2:
                        continue
                    with ExitStack() as e3:
                        lps = [C.ps(e3, [128, 512], F32, f"lp{k}") for k in range(2)]
                        tpm = C.ps(e3, [128, 8, 128], BF16, "tpm")
                        sc = C.sb(e3, [128, SEQ], F32, "sc")
                        mk = C.sb(e3, [128, SEQ], BF16, "mk")
                        rb = [C.sb(e3, [128, 512], F32, f"rb{k}") for k in range(2)]
                        st4 = C.sb(e3, [128, 4], F32, "st4")
                        uu = C.sb(e3, [128, 1], F32, "uu")
                        cntt = C.sb(e3, [128, 1], F32, "cntt")
                        dd_ = C.sb(e3, [128, 1], F32, "dd_")
                        n_ = 0
                        for ql in range(4):
                            qt = 4 * qg + ql
                            nk = (qt + 1) * 128
                            for kb in range((nk + 511) // 512):
                                n = min(512, nk - kb * 512)
                                ksl = slice(kb * 512, kb * 512 + n)
                                for h in range(8):
                                    hp, e_ = h // 2, h % 2
                                    lp = lps[n_ % 2]
                                    r_ = rb[n_ % 2]
                                    n_ += 1
                                    S.pe(lambda e: e.matmul(out=lp[:, 0:n], lhsT=qidxT[e_ * 64:(e_ + 1) * 64, hp, ql * 128:(ql + 1) * 128],
                                                            rhs=kidxT2[e_ * 64:(e_ + 1) * 64, ksl], start=True, stop=True),
                                         reads=[qidxT, kidxT2], writes=[lp])
                                    S.act(lambda e: e.activation(out=r_[:, 0:n], in_=lp[:, 0:n], func=AF.Relu, scale=absw[:, qt, h:h + 1]),
                                          reads=[lp, absw], writes=[r_])
                                    eng = "dve"
                                    if h == 0:
                                        S.op(eng, lambda e: e.tensor_scalar(out=sc[:, ksl], in0=r_[:, 0:n], scalar1=sgnw[:, qt, 0:1],
                                                                            scalar2=None, op0=ALU.mult), reads=[r_, sgnw], writes=[sc])
                                    elif eng == "dve":
                                        S.dve(lambda e: e.scalar_tensor_tensor(out=sc[:, ksl], in0=r_[:, 0:n], scalar=sgnw[:, qt, h:h + 1],
                                                                               in1=sc[:, ksl], op0=ALU.mult, op1=ALU.add),
                                              reads=[r_, sgnw, sc], writes=[sc])
                                    else:
                                        S.pool(lambda e: e.tensor_scalar(out=r_[:, 0:n], in0=r_[:, 0:n], scalar1=sgnw[:, qt, h:h + 1],
                                                                         scalar2=None, op0=ALU.mult), reads=[r_, sgnw], writes=[r_])
                                        S.pool(lambda e: e.tensor_tensor(out=sc[:, ksl], in0=sc[:, ksl], in1=r_[:, 0:n], op=ALU.add),
                                               reads=[r_, sc], writes=[sc])
                            dsl = slice(qt * 128, (qt + 1) * 128)
                            if qt >= 2:
                                S.dve(lambda e: e.tensor_reduce(out=st4[:, 0:1], in_=sc[:, 0:nk], axis=AX.X, op=ALU.max), reads=[sc], writes=[st4])
                                S.dve(lambda e: e.tensor_reduce(out=st4[:, 1:2], in_=sc[:, 0:nk], axis=AX.X, op=ALU.min), reads=[sc], writes=[st4])
                                S.dve(lambda e: e.tensor_tensor(out=st4[:, 2:3], in0=st4[:, 0:1], in1=st4[:, 1:2], op=ALU.subtract), reads=[st4], writes=[st4])
                                S.dve(lambda e: e.tensor_scalar(out=st4[:, 2:3], in0=st4[:, 2:3], scalar1=1e-30, scalar2=None, op0=ALU.max), reads=[st4], writes=[st4])
                                S.dve(lambda e: e.reciprocal(out=st4[:, 3:4], in_=st4[:, 2:3]), reads=[st4], writes=[st4])
                                S.dve(lambda e: e.tensor_scalar(out=sc[:, 0:nk], in0=sc[:, 0:nk], scalar1=st4[:, 1:2], scalar2=st4[:, 3:4],
                                                                op0=ALU.subtract, op1=ALU.mult), reads=[sc, st4], writes=[sc])
                                S.pool(lambda e: e.tensor_tensor(out=sc[:, dsl], in0=sc[:, dsl], in1=K["cbias"][:, :], op=ALU.add),
                                       reads=[sc, K["cbias"]], writes=[sc])
                                S.dve(lambda e: e.memset(uu[:, :], 0.5), writes=[uu])
                                for it in range(16):
                                    S.dve(lambda e: e.tensor_scalar(out=mk[:, 0:nk], in0=sc[:, 0:nk], scalar1=uu[:, 0:1], scalar2=0.0,
                                                                    op0=ALU.is_ge, op1=ALU.add, accum_out=cntt[:, 0:1]),
                                          reads=[sc, uu], writes=[mk, cntt])
                                    S.dve(lambda e: e.tensor_scalar(out=dd_[:, :], in0=cntt[:, :], scalar1=255.5, scalar2=float(2.0 ** -(it + 1)),
                                                                    op0=ALU.is_ge, op1=ALU.mult), reads=[cntt], writes=[dd_])
                                    S.dve(lambda e: e.scalar_tensor_tensor(out=uu[:, :], in0=dd_[:, :], scalar=-float(2.0 ** -(it + 2)), in1=uu[:, :],
                                                                           op0=ALU.add, op1=ALU.add), reads=[dd_, uu], writes=[uu])
                                S.dve(lambda e: e.tensor_scalar(out=mk[:, 0:nk], in0=sc[:, 0:nk], scalar1=uu[:, 0:1], scalar2=None, op0=ALU.is_ge),
                                      reads=[sc, uu], writes=[mk])
                            else:
                                S.pool(lambda e: e.tensor_tensor(out=sc[:, dsl], in0=sc[:, dsl], in1=K["cbias"][:, :], op=ALU.add),
                                       reads=[sc, K["cbias"]], writes=[sc])
                                S.dve(lambda e: e.tensor_single_scalar(out=mk[:, 0:nk], in_=sc[:, 0:nk], scalar=-1e29, op=ALU.is_gt),
                                      reads=[sc], writes=[mk])
                            for k0 in range(0, qt + 1, 8):
                                cnt = min(8, qt + 1 - k0)
                                for kk in range(cnt):
                                    kt = k0 + kk
                                    S.pe(lambda e: e.transpose(out=tpm[:, kk, :], in_=mk[:, kt * 128:(kt + 1) * 128], identity=ident[:, :]),
                                         reads=[mk, ident], writes=[tpm])
                                S.dve(lambda e: e.tensor_scalar(out=maskT[:, k0:k0 + cnt, ql * 128:(ql + 1) * 128], in0=tpm[:, 0:cnt, :],
                                                                scalar1=30000.0, scalar2=-30000.0, op0=ALU.mult, op1=ALU.add),
                                      reads=[tpm], writes=[maskT])
                        S.barrier()
                    if DBG.get("odd_stop") == 3:
                        continue
                    with ExitStack() as e3:
                        p8 = C.ps(e3, [128, 512], F32, "p8")
                        negm8 = make_negm(C, e3, qlatT, 8, k2m, K, p8)
                        ps_s = [C.ps(e3, [128, 512], F32, f"ss{k}") for k in range(3)]
                        ps_o = C.ps(e3, [128, 512], F32, "pso")
                        ps_r = C.ps(e3, [128, 512], F32, "psr")
                        po = C.ps(e3, [128, 512], F32, "po")
                        pts = [C.sb(e3, [128, 512], BF16, f"pt{k}") for k in range(3)]
                        rinv = C.sb(e3, [128, 512], F32, "rinv")
                        olat = [C.sb(e3, [128, 512], BF16, f"olat{k}") for k in range(2)]
                        for h in range(8):
                            hp, e_ = h // 2, h % 2
                            kt_list = [(ckvT[:, kt * 128:(kt + 1) * 128], ckvtok[:, kt, :], maskT[:, kt, :], [ckvT, ckvtok, maskT])
                                       for kt in range(nkt)]
                            ol = olat[h % 2]
                            attn_core(C, K, qlatT[:, h, :], qlatT, kt_list, negm8, h, 64 ** -0.5, ol[:, :], ol, 128,
                                      ps_s, ps_o, ps_r, pts, rinv, mask_eng=("pool" if h % 2 else "dve"))
                            S.pe(lambda e: e.matmul(out=po[e_ * 64:(e_ + 1) * 64, :], lhsT=wuv[:, h, :], rhs=ol[:, :], start=True, stop=True),
                                 reads=[wuv, ol], writes=[po])
                            if e_ == 1:
                                S.act(lambda e: e.copy(out=ycT[:, hp, tsl], in_=po[:, :]), reads=[po], writes=[ycT.part((hp, qg))])
                        S.barrier()
        with ExitStack() as e1:
            wo = C.sb(e1, [128, 8, D], BF16, "wo")
            load_w(C, wo, W.H("od_w_out", i), 0, D)
            for g4 in range(4):
                chunks = [(ycT, (lambda ti, c=c, g4=g4: ycT[:, c, (g4 * 4 + ti) * 128:(g4 * 4 + ti + 1) * 128])) for c in range(4)]
                chunks += [(ydT, (lambda ti, c=c, g4=g4: ydT[:, c, (g4 * 4 + ti) * 128:(g4 * 4 + ti + 1) * 128])) for c in range(4)]
                out_proj_residual(C, x, chunks, wo, g_bc, 1.0, [g4 * 4 + t_ for t_ in range(4)])


RW_LN_EPS = 64e-5
TWO_PI = 6.283185307179586


def make_even_consts(C, es, K):
    S = C.S
    ones_f = K["ones_f"]
    E = {}
    o4 = ones_f[:, 0:512].rearrange("p (a b) -> p a b", a=4)
    for nm, pat, cm, op in (("m_su", [[0, 4], [1, 128]], -1, ALU.is_gt), ("m_ui", [[0, 4], [1, 128]], -1, ALU.is_ge),
                            ("m_sl", [[0, 4], [-1, 128]], 1, ALU.is_gt)):
        t = C.sb(es, [128, 4, 128], F32, nm)
        S.pool(lambda e: e.affine_select(out=t[:, :, :], in_=o4, pattern=pat, compare_op=op, fill=0.0, base=0,
                                         channel_multiplier=cm), reads=[ones_f], writes=[t])
        E[nm] = t
    lvm = C.sb(es, [128, 7, 128], BF16, "lvm")
    lvmT = C.sb(es, [128, 7, 128], BF16, "lvmT")
    with ExitStack() as e0:
        I32 = mybir.dt.int32
        pi = C.sb(e0, [128, 128], I32, "lv_pi")
        fi = C.sb(e0, [128, 128], I32, "lv_fi")
        S.pool(lambda e: e.iota(pi[:, :], pattern=[[0, 128]], base=0, channel_multiplier=1), writes=[pi])
        S.pool(lambda e: e.iota(fi[:, :], pattern=[[1, 128]], base=0, channel_multiplier=0), writes=[fi])
        ta = C.sb(e0, [128, 128], I32, "lv_ta")
        tb = C.sb(e0, [128, 128], I32, "lv_tb")
        eq = C.sb(e0, [128, 128], F32, "lv_eq")
        bp = C.sb(e0, [128, 128], F32, "lv_bp")
        bq = C.sb(e0, [128, 128], F32, "lv_bq")
        nbp = C.sb(e0, [128, 128], F32, "lv_nbp")
        nbq = C.sb(e0, [128, 128], F32, "lv_nbq")
        for s in range(7):
            S.dve(lambda e: e.tensor_scalar(out=ta[:, :], in0=pi[:, :], scalar1=s + 1, scalar2=None, op0=ALU.arith_shift_right), reads=[pi], writes=[ta])
            S.dve(lambda e: e.tensor_scalar(out=tb[:, :], in0=fi[:, :], scalar1=s + 1, scalar2=None, op0=ALU.arith_shift_right), reads=[fi], writes=[tb])
            S.dve(lambda e: e.tensor_tensor(out=eq[:, :], in0=ta[:, :], in1=tb[:, :], op=ALU.is_equal), reads=[ta, tb], writes=[eq])
            S.dve(lambda e: e.tensor_scalar(out=ta[:, :], in0=pi[:, :], scalar1=s, scalar2=1, op0=ALU.arith_shift_right, op1=ALU.bitwise_and), reads=[pi], writes=[ta])
            S.dve(lambda e: e.tensor_scalar(out=tb[:, :], in0=fi[:, :], scalar1=s, scalar2=1, op0=ALU.arith_shift_right, op1=ALU.bitwise_and), reads=[fi], writes=[tb])
            S.dve(lambda e: e.tensor_copy(out=bp[:, :], in_=ta[:, :]), reads=[ta], writes=[bp])
            S.dve(lambda e: e.tensor_copy(out=bq[:, :], in_=tb[:, :]), reads=[tb], writes=[bq])
            S.dve(lambda e: e.tensor_scalar(out=nbp[:, :], in0=bp[:, :], scalar1=-1.0, scalar2=1.0, op0=ALU.mult, op1=ALU.add), reads=[bp], writes=[nbp])
            S.dve(lambda e: e.tensor_scalar(out=nbq[:, :], in0=bq[:, :], scalar1=-1.0, scalar2=1.0, op0=ALU.mult, op1=ALU.add), reads=[bq], writes=[nbq])
            S.dve(lambda e: e.tensor_tensor(out=nbp[:, :], in0=nbp[:, :], in1=bq[:, :], op=ALU.mult), reads=[nbp, bq], writes=[nbp])
            S.dve(lambda e: e.tensor_tensor(out=lvm[:, s, :], in0=nbp[:, :], in1=eq[:, :], op=ALU.mult), reads=[nbp, eq], writes=[lvm])
            S.dve(lambda e: e.tensor_tensor(out=nbq[:, :], in0=nbq[:, :], in1=bp[:, :], op=ALU.mult), reads=[nbq, bp], writes=[nbq])
            S.dve(lambda e: e.tensor_tensor(out=lvmT[:, s, :], in0=nbq[:, :], in1=eq[:, :], op=ALU.mult), reads=[nbq, eq], writes=[lvmT])
        S.barrier()
    E.update(lvm=lvm, lvmT=lvmT)
    id8 = C.sb(es, [128, 8, 128], BF16, "id8")
    with ExitStack() as e0:
        ones8 = C.sb(e0, [128, 8, 128], F32, "ones8")
        S.pool(lambda e: e.memset(ones8[:, :, :], 1.0), writes=[ones8])
        S.pool(lambda e: e.affine_select(out=id8[:, :, :], in_=ones8[:, :, :], pattern=[[0, 8], [-1, 128]], compare_op=ALU.is_equal, fill=0.0,
                                         base=0, channel_multiplier=1), reads=[ones8], writes=[id8])
        S.barrier()
    E["id8"] = id8
    bo = C.sb(es, [128, 128], BF16, "blockones")
    bof = C.sb(es, [128, 128], BF16, "blockones_f")
    S.pool(lambda e: e.memset(bo[:, :], 0.0), writes=[bo])
    S.pool(lambda e: e.memset(bof[:, :], 0.0), writes=[bof])
    for b in range(2):
        S.pool(lambda e: e.memset(bo[b * 64:(b + 1) * 64, b * 64:(b + 1) * 64], 1.0), writes=[bo])
        S.pool(lambda e: e.memset(bof[b * 64:(b + 1) * 64, b * 64:(b + 1) * 64], 1.0 / 64), writes=[bof])
    on128 = C.sb(es, [128, 128], BF16, "on128")
    S.pool(lambda e: e.memset(on128[:, :], 1.0 / 128), writes=[on128])
    E.update(bo=bo, bof=bof, on128=on128)
    seg = C.sb(es, [128, 4, 128], F32, "seg")
    S.pool(lambda e: e.memset(seg[:, :, :], 1.0), writes=[seg])
    S.pool(lambda e: e.memset(seg[:, :, 0:1], 0.0), writes=[seg])
    E["seg"] = seg
    cosT = C.sb(es, [128, SEQ], BF16, "cosT")
    sinT = C.sb(es, [128, SEQ], BF16, "sinT")
    with ExitStack() as e1:
        jc_i = C.sb(e1, [128, 1], mybir.dt.int32, "jc_i")
        for b in range(2):
            S.pool(lambda e: e.iota(jc_i[b * 64:(b + 1) * 64, :], pattern=[[0, 1]], base=0, channel_multiplier=1), writes=[jc_i])
        jc = C.sb(e1, [128, 1], F32, "jc")
        S.dve(lambda e: e.tensor_copy(out=jc[:, :], in_=jc_i[:, :]), reads=[jc_i], writes=[jc])
        invf = C.sb(e1, [128, 1], F32, "invf")
        S.act(lambda e: e.activation(out=invf[:, :], in_=jc[:, :], func=AF.Exp, scale=-float(np.log(10000.0)) / 64.0),
              reads=[jc], writes=[invf])
        tp_i = C.sb(e1, [128, SEQ], mybir.dt.int32, "tp_i")
        S.pool(lambda e: e.iota(tp_i[:, :], pattern=[[1, SEQ]], base=0, channel_multiplier=0), writes=[tp_i])
        ang = C.sb(e1, [128, SEQ], F32, "ang")
        S.dve(lambda e: e.tensor_copy(out=ang[:, :], in_=tp_i[:, :]), reads=[tp_i], writes=[ang])
        S.dve(lambda e: e.tensor_scalar(out=ang[:, :], in0=ang[:, :], scalar1=invf[:, 0:1], scalar2=None, op0=ALU.mult),
              reads=[ang, invf], writes=[ang])
        sgn = C.sb(e1, [128, 1], F32, "sgn")
        S.pool(lambda e: e.memset(sgn[0:64, :], -1.0), writes=[sgn])
        S.pool(lambda e: e.memset(sgn[64:128, :], 1.0), writes=[sgn])
        red = C.sb(e1, [128, SEQ], F32, "red")
        qi = C.sb(e1, [128, SEQ], mybir.dt.int32, "qi")
        qf = C.sb(e1, [128, SEQ], F32, "qf")
        for (dst, shift) in ((sinT, 0.0), (cosT, float(np.pi / 2))):
            S.dve(lambda e: e.tensor_scalar(out=qf[:, :], in0=ang[:, :], scalar1=shift, scalar2=1.0 / TWO_PI, op0=ALU.add, op1=ALU.mult),
                  reads=[ang], writes=[qf])
            S.dve(lambda e: e.tensor_copy(out=qi[:, :], in_=qf[:, :]), reads=[qf], writes=[qi])
            S.dve(lambda e: e.tensor_copy(out=qf[:, :], in_=qi[:, :]), reads=[qi], writes=[qf])
            S.dve(lambda e: e.scalar_tensor_tensor(out=red[:, :], in0=qf[:, :], scalar=-TWO_PI, in1=ang[:, :], op0=ALU.mult, op1=ALU.add),
                  reads=[qf, ang], writes=[red])
            if shift:
                S.dve(lambda e: e.tensor_scalar(out=red[:, :], in0=red[:, :], scalar1=shift, scalar2=None, op0=ALU.add), reads=[red], writes=[red])
            S.dve(lambda e: e.tensor_scalar(out=qf[:, :], in0=red[:, :], scalar1=float(np.pi), scalar2=-TWO_PI, op0=ALU.is_gt, op1=ALU.mult),
                  reads=[red], writes=[qf])
            S.dve(lambda e: e.tensor_tensor(out=red[:, :], in0=red[:, :], in1=qf[:, :], op=ALU.add), reads=[red, qf], writes=[red])
            S.dve(lambda e: e.tensor_scalar(out=qf[:, :], in0=red[:, :], scalar1=-float(np.pi), scalar2=TWO_PI, op0=ALU.is_lt, op1=ALU.mult),
                  reads=[red], writes=[qf])
            S.dve(lambda e: e.tensor_tensor(out=red[:, :], in0=red[:, :], in1=qf[:, :], op=ALU.add), reads=[red, qf], writes=[red])
            S.dve(lambda e: e.tensor_scalar(out=red[:, :], in0=red[:, :], scalar1=3.14159, scalar2=-3.14159, op0=ALU.min, op1=ALU.max),
                  reads=[red], writes=[red])
            if shift:
                S.act(lambda e: e.activation(out=dst[:, :], in_=red[:, :], func=AF.Sin), reads=[red], writes=[dst])
            else:
                S.act(lambda e: e.activation(out=red[:, :], in_=red[:, :], func=AF.Sin), reads=[red], writes=[red])
                S.dve(lambda e: e.tensor_scalar(out=dst[:, :], in0=red[:, :], scalar1=sgn[:, 0:1], scalar2=None, op0=ALU.mult),
                      reads=[red, sgn], writes=[dst])
        S.barrier()
    E.update(cosT=cosT, sinT=sinT)
    lg = [float(np.log1p(-2.0 ** (-5.0 - h))) for h in range(4)]
    E["lg"] = lg
    scale = 128 ** -0.5
    dmT = C.sb(es, [128, 4, 128], F32, "dmT")
    xiT = C.sb(es, [128, 4, 128], BF16, "xiT")
    zcol = C.sb(es, [128, 4], F32, "zcol")
    with ExitStack() as e1:
        d_i = C.sb(e1, [128, 128], mybir.dt.int32, "d_i")
        d_f = C.sb(e1, [128, 128], F32, "d_f")
        S.pool(lambda e: e.iota(d_i[:, :], pattern=[[1, 128]], base=0, channel_multiplier=-1), writes=[d_i])
        S.dve(lambda e: e.tensor_copy(out=d_f[:, :], in_=d_i[:, :]), reads=[d_i], writes=[d_f])
        S.dve(lambda e: e.tensor_scalar(out=d_f[:, :], in0=d_f[:, :], scalar1=0.0, scalar2=None, op0=ALU.max), reads=[d_f], writes=[d_f])
        i_i = C.sb(e1, [128, 128], mybir.dt.int32, "i_i")
        i_f = C.sb(e1, [128, 128], F32, "i_f")
        S.pool(lambda e: e.iota(i_i[:, :], pattern=[[1, 128]], base=1, channel_multiplier=0), writes=[i_i])
        S.dve(lambda e: e.tensor_copy(out=i_f[:, :], in_=i_i[:, :]), reads=[i_i], writes=[i_f])
        p_i = C.sb(e1, [128, 1], mybir.dt.int32, "p_i")
        p_f = C.sb(e1, [128, 1], F32, "p_f")
        S.pool(lambda e: e.iota(p_i[:, :], pattern=[[0, 1]], base=127, channel_multiplier=-1), writes=[p_i])
        S.dve(lambda e: e.tensor_copy(out=p_f[:, :], in_=p_i[:, :]), reads=[p_i], writes=[p_f])
        tmp = C.sb(e1, [128, 128], F32, "tmpd")
        for h in range(4):
            S.act(lambda e: e.activation(out=tmp[:, :], in_=d_f[:, :], func=AF.Exp, scale=lg[h]), reads=[d_f], writes=[tmp])
            S.dve(lambda e: e.scalar_tensor_tensor(out=dmT[:, h, :], in0=tmp[:, :], scalar=scale, in1=E["m_ui"][:, 0, :],
                                                   op0=ALU.mult, op1=ALU.mult), reads=[tmp, E["m_ui"]], writes=[dmT])
            S.act(lambda e: e.activation(out=xiT[:, h, :], in_=i_f[:, :], func=AF.Exp, scale=lg[h]), reads=[i_f], writes=[xiT])
            S.act(lambda e: e.activation(out=zcol[:, h:h + 1], in_=p_f[:, :], func=AF.Exp, scale=lg[h]), reads=[p_f], writes=[zcol])
        S.dve(lambda e: e.tensor_scalar(out=zcol[:, :], in0=zcol[:, :], scalar1=scale, scalar2=None, op0=ALU.mult), reads=[zcol], writes=[zcol])
        S.barrier()
    E.update(dmT=dmT, xiT=xiT, zcol=zcol)
    return E


def group_norm_T(C, es, y, nch, onesmat, eps, gcol_fn, bcol_fn, post_fn, pm, pq):
    S = C.S
    sq = C.sb(es, [128, 512], BF16, "gn_sq")
    yb16 = C.sb(es, [128, 512], BF16, "gn_yb")
    m2 = C.sb(es, [128, 512], F32, "gn_m2")
    rs = C.sb(es, [128, 512], F32, "gn_rs")
    dd = C.sb(es, [128, 512], F32, "gn_dd")
    for c in range(nch):
        S.dve(lambda e: e.tensor_copy(out=yb16[:, :], in_=y[:, c, :]), reads=[y], writes=[yb16])
        S.pe(lambda e: e.matmul(out=pm[:, :], lhsT=onesmat[:, :], rhs=yb16[:, :], start=True, stop=True), reads=[onesmat, yb16], writes=[pm])
        S.act(lambda e: e.activation(out=sq[:, :], in_=y[:, c, :], func=AF.Square), reads=[y], writes=[sq])
        S.pe(lambda e: e.matmul(out=pq[:, :], lhsT=onesmat[:, :], rhs=sq[:, :], start=True, stop=True), reads=[onesmat, sq], writes=[pq])
        S.act(lambda e: e.activation(out=m2[:, :], in_=pm[:, :], func=AF.Square), reads=[pm], writes=[m2])
        S.dve(lambda e: e.tensor_tensor(out=rs[:, :], in0=pq[:, :], in1=m2[:, :], op=ALU.subtract), reads=[pq, m2], writes=[rs])
        S.dve(lambda e: e.tensor_scalar(out=rs[:, :], in0=rs[:, :], scalar1=0.0, scalar2=float(eps), op0=ALU.max, op1=ALU.add),
              reads=[rs], writes=[rs])
        S.act(lambda e: e.activation(out=rs[:, :], in_=rs[:, :], func=AF.Sqrt), reads=[rs], writes=[rs])
        S.dve(lambda e: e.reciprocal(out=rs[:, :], in_=rs[:, :]), reads=[rs], writes=[rs])
        S.dve(lambda e: e.tensor_tensor(out=dd[:, :], in0=y[:, c, :], in1=pm[:, :], op=ALU.subtract), reads=[y, pm], writes=[dd])
        S.dve(lambda e: e.tensor_tensor(out=dd[:, :], in0=dd[:, :], in1=rs[:, :], op=ALU.mult), reads=[dd, rs], writes=[dd])
        S.pool(lambda e: e.tensor_scalar(out=dd[:, :], in0=dd[:, :], scalar1=gcol_fn(c), scalar2=bcol_fn(c), op0=ALU.mult, op1=ALU.add),
               reads=[dd], writes=[dd])
        post_fn(c, dd)


def even_mixer(C, x, l, W, P, K):
    S = C.S
    i = l // 2
    ident = K["ident"]
    w_in = W.H("ev_w_in", i)
    w_v = w_in.rearrange("(kc p) n -> p kc n", p=128)
    if DBG.get("even_stop") == 0:
        return
    with ExitStack() as es:
        E = make_even_consts(C, es, K)
        lg = E["lg"]
        gamC = [float(np.exp(128.0 * lg[h])) for h in range(4)]
        yaT = C.sb(es, [128, 4, SEQ], BF16, "yaT")
        with ExitStack() as er:
            wa2 = C.sb(er, [128, 512], BF16, "wa2")
            g2 = C.sb(er, [128, 512], BF16, "g2")
            S.dma("pool", wa2[0:64, :], W.H("rw_w2", i), writes=[wa2])
            S.dma("pool", wa2[64:128, :], W.H("rw_a2", i), writes=[wa2])
            S.dma("pool", g2[:, :], W.H("rw_g2", i), writes=[g2])
            omk = C.sb(er, [128, 4], F32, "omk")
            S.dve(lambda e: e.tensor_scalar(out=omk[:, :], in0=P.cols(("rw_k_a", i), 0, 4), scalar1=-1.0, scalar2=1.0, op0=ALU.mult, op1=ALU.add),
                  reads=[P.t], writes=[omk])
            St = C.sb(er, [128, 4, 64], F32, "St")
            Sb = C.sb(er, [128, 4, 2, 64], BF16, "Sbd")
            S.pool(lambda e: e.memset(St[:, :, :], 0.0), writes=[St])
            S.pool(lambda e: e.memset(Sb[:, :, :, :], 0.0), writes=[Sb])
            pcar = C.sb(er, [128, 14], F32, "pcar")
            S.pool(lambda e: e.memset(pcar[:, :], 0.0), writes=[pcar])
            wch = [C.sb(er, [128, 8, 128], BF16, f"wch{k}") for k in range(2)]
            nw = [0]

            def mu_col(c):
                return P.col(("rw_mu", i, 0), c) if c < 8 else P.col(("rw_mu", i, 1), c - 8)

            for blk in range(4):
                t0 = blk * 512
                with ExitStack() as e1:
                    xnT = C.sb(e1, [128, 8, 512], BF16, "xnT")
                    rms_to_T(C, e1, x, P.cols(("norm_g", l, 2)), xnT, 4, ident, tok0=blk * 4)
                    with ExitStack() as e2:
                        At = C.sb(e2, [128, 4, 512], BF16, "At")
                        Bt = C.sb(e2, [128, 4, 512], BF16, "Bt")
                        Kt = C.sb(e2, [128, 4, 512], BF16, "Kt")
                        Rq = C.sb(e2, [128, 4, 512], BF16, "Rq")
                        vT = C.sb(e2, [128, 4, 512], BF16, "vT")
                        bon = C.sb(e2, [128, 4, 512], BF16, "bon")
                        gT = C.sb(e2, [128, 4, 512], BF16, "gT")
                        gC = C.sb(e2, [128, 4, 4], F32, "gC")
                        yraw = C.sb(e2, [128, 4, 512], F32, "yraw")
                        with ExitStack() as e3:
                            pps = [C.ps(e3, [128, 512], F32, f"pp{k}") for k in range(3)]
                            pa = C.ps(e3, [128, 512], F32, "pa")
                            pb = C.ps(e3, [128, 512], F32, "pb")
                            pT = C.sb(e3, [128, 513], F32, "pT")
                            mx = [C.sb(e3, [128, 512], F32, f"mx{k}") for k in range(3)]
                            dtmp = C.sb(e3, [128, 512], F32, "dtmp")
                            xwa = C.sb(e3, [128, 512], BF16, "xwa")
                            sxg = C.sb(e3, [128, 512], BF16, "sxg")
                            kkb = C.sb(e3, [128, 512], BF16, "kkb")
                            bA = C.sb(e3, [128, 512], F32, "bA")
                            bB = C.sb(e3, [128, 512], F32, "bB")
                            eL = C.sb(e3, [128, 512], F32, "eL")
                            eLm = C.sb(e3, [128, 512], F32, "eLm")
                            asig = C.sb(e3, [128, 512], F32, "asig")
                            kk = C.sb(e3, [128, 512], F32, "kk")
                            t2 = C.sb(e3, [128, 512], F32, "t2")

                            def proj_mix(c, k_):
                                pp, m_ = pps[k_], mx[k_]
                                wt = wch[nw[0] % 2]
                                nw[0] += 1
                                S.dma("pool", wt[:, :, :], w_v[:, :, c * 128:(c + 1) * 128], writes=[wt])
                                for kc in range(8):
                                    S.pe(lambda e: e.matmul(out=pp[:, :], lhsT=wt[:, kc, :], rhs=xnT[:, kc, :],
                                                            start=(kc == 0), stop=(kc == 7)), reads=[wt, xnT], writes=[pp])
                                S.act(lambda e: e.copy(out=pT[:, 1:513], in_=pp[:, :]), reads=[pp], writes=[pT])
                                S.pool(lambda e: e.tensor_copy(out=pT[:, 0:1], in_=pcar[:, c:c + 1]), reads=[pcar.part(c)], writes=[pT])
                                S.pool(lambda e: e.tensor_copy(out=pcar[:, c:c + 1], in_=pT[:, 512:513]), reads=[pT], writes=[pcar.part(c)])
                                S.dve(lambda e: e.tensor_tensor(out=dtmp[:, :], in0=pT[:, 0:512], in1=pT[:, 1:513], op=ALU.subtract),
                                      reads=[pT], writes=[dtmp])
                                S.dve(lambda e: e.scalar_tensor_tensor(out=m_[:, :], in0=dtmp[:, :], scalar=mu_col(c), in1=pT[:, 1:513],
                                                                       op0=ALU.mult, op1=ALU.add), reads=[dtmp, pT, P.t], writes=[m_])
                                return m_

                            m_ = proj_mix(12, 0)
                            S.act(lambda e: e.activation(out=xwa[0:64, :], in_=m_[0:64, :], func=AF.Tanh), reads=[m_], writes=[xwa])
                            S.act(lambda e: e.copy(out=xwa[64:128, :], in_=m_[64:128, :]), reads=[m_], writes=[xwa])
                            m_ = proj_mix(13, 1)
                            S.act(lambda e: e.activation(out=sxg[:, :], in_=m_[:, :], func=AF.Sigmoid), reads=[m_], writes=[sxg])
                            for hp in range(4):
                                rr = proj_mix(hp, 0)
                                kx = proj_mix(4 + hp, 1)
                                vv = proj_mix(8 + hp, 2)
                                S.pe(lambda e: e.matmul(out=pa[:, :], lhsT=wa2[0:64, hp * 128:(hp + 1) * 128], rhs=xwa[0:64, :], start=True, stop=True),
                                     reads=[wa2, xwa], writes=[pa])
                                S.act(lambda e: e.activation(out=bA[:, :], in_=pa[:, :], func=AF.Sigmoid, bias=P.col(("rw_w0", i), hp)),
                                      reads=[pa, P.t], writes=[bA])
                                S.dve(lambda e: e.tensor_scalar(out=bA[:, :], in0=bA[:, :], scalar1=-float(np.exp(-0.5)), scalar2=None, op0=ALU.mult),
                                      reads=[bA], writes=[bA])
                                S.dve(lambda e: e.tensor_tensor_scan(out=bB[:, :], data0=E["seg"][:, :, :].rearrange("p a b -> p (a b)"),
                                                                     data1=bA[:, :], initial=0.0, op0=ALU.mult, op1=ALU.add),
                                      reads=[bA, E["seg"]], writes=[bB])
                                S.act(lambda e: e.activation(out=eL[:, :], in_=bB[:, :], func=AF.Exp), reads=[bB], writes=[eL])
                                S.act(lambda e: e.activation(out=eLm[:, :], in_=bB[:, :], func=AF.Exp, scale=-1.0), reads=[bB], writes=[eLm])
                                S.dve(lambda e: e.tensor_tensor(out=bA[:, :], in0=bB[:, :], in1=bA[:, :], op=ALU.subtract), reads=[bB, bA], writes=[bA])
                                S.act(lambda e: e.activation(out=bA[:, :], in_=bA[:, :], func=AF.Exp), reads=[bA], writes=[bA])
                                S.pool(lambda e: e.tensor_copy(out=gC[:, :, hp], in_=eL[:, :].rearrange("p (a b) -> p a b", a=4)[:, :, 127]),
                                       reads=[eL], writes=[gC])
                                S.pe(lambda e: e.matmul(out=pb[:, :], lhsT=wa2[64:128, hp * 128:(hp + 1) * 128], rhs=xwa[64:128, :], start=True, stop=True),
                                     reads=[wa2, xwa], writes=[pb])
                                S.act(lambda e: e.activation(out=asig[:, :], in_=pb[:, :], func=AF.Sigmoid, bias=P.col(("rw_a0", i), hp)),
                                      reads=[pb, P.t], writes=[asig])
                                S.pool(lambda e: e.tensor_scalar(out=kk[:, :], in0=kx[:, :], scalar1=P.col(("rw_k_k", i), hp), scalar2=None, op0=ALU.mult),
                                       reads=[kx, P.t], writes=[kk])
                                S.act(lambda e: e.activation(out=kkb[:, :], in_=kk[:, :], func=AF.Square), reads=[kk], writes=[kkb])
                                S.pe(lambda e: e.matmul(out=pa[:, :], lhsT=E["bo"][:, :], rhs=kkb[:, :], start=True, stop=True),
                                     reads=[E["bo"], kkb], writes=[pa])
                                S.dve(lambda e: e.tensor_scalar(out=bB[:, :], in0=pa[:, :], scalar1=1e-24, scalar2=None, op0=ALU.max), reads=[pa], writes=[bB])
                                S.act(lambda e: e.activation(out=bB[:, :], in_=bB[:, :], func=AF.Sqrt), reads=[bB], writes=[bB])
                                S.dve(lambda e: e.reciprocal(out=bB[:, :], in_=bB[:, :]), reads=[bB], writes=[bB])
                                S.dve(lambda e: e.tensor_tensor(out=kk[:, :], in0=kk[:, :], in1=bB[:, :], op=ALU.mult), reads=[kk, bB], writes=[kk])
                                S.dve(lambda e: e.scalar_tensor_tensor(out=At[:, hp, :], in0=kk[:, :], scalar=-1.0, in1=bA[:, :], op0=ALU.mult, op1=ALU.mult),
                                      reads=[kk, bA], writes=[At.part(hp)])
                                S.pool(lambda e: e.tensor_tensor(out=bB[:, :], in0=kk[:, :], in1=asig[:, :], op=ALU.mult), reads=[kk, asig], writes=[bB])
                                S.pool(lambda e: e.tensor_tensor(out=Bt[:, hp, :], in0=bB[:, :], in1=eLm[:, :], op=ALU.mult), reads=[bB, eLm], writes=[Bt.part(hp)])
                                S.dve(lambda e: e.tensor_scalar(out=t2[:, :], in0=asig[:, :], scalar1=P.col(("rw_k_a", i), hp), scalar2=omk[:, hp:hp + 1],
                                                                op0=ALU.mult, op1=ALU.add), reads=[asig, P.t, omk], writes=[t2])
                                S.dve(lambda e: e.tensor_tensor(out=t2[:, :], in0=t2[:, :], in1=kx[:, :], op=ALU.mult), reads=[t2, kx], writes=[t2])
                                S.pool(lambda e: e.tensor_tensor(out=Kt[:, hp, :], in0=t2[:, :], in1=eLm[:, :], op=ALU.mult), reads=[t2, eLm], writes=[Kt.part(hp)])
                                S.dve(lambda e: e.tensor_tensor(out=Rq[:, hp, :], in0=rr[:, :], in1=eL[:, :], op=ALU.mult), reads=[rr, eL], writes=[Rq.part(hp)])
                                S.act(lambda e: e.copy(out=vT[:, hp, :], in_=vv[:, :]), reads=[vv], writes=[vT.part(hp)])
                                S.dve(lambda e: e.scalar_tensor_tensor(out=kkb[:, :], in0=rr[:, :], scalar=P.col(("rw_r_k", i), hp), in1=t2[:, :],
                                                                       op0=ALU.mult, op1=ALU.mult), reads=[rr, t2, P.t], writes=[kkb])
                                S.pe(lambda e: e.matmul(out=pb[:, :], lhsT=E["bo"][:, :], rhs=kkb[:, :], start=True, stop=True),
                                     reads=[E["bo"], kkb], writes=[pb])
                                S.dve(lambda e: e.tensor_tensor(out=bon[:, hp, :], in0=pb[:, :], in1=vv[:, :], op=ALU.mult), reads=[pb, vv], writes=[bon.part(hp)])
                                S.pe(lambda e: e.matmul(out=pa[:, :], lhsT=g2[:, hp * 128:(hp + 1) * 128], rhs=sxg[:, :], start=True, stop=True),
                                     reads=[g2, sxg], writes=[pa])
                                S.act(lambda e: e.copy(out=gT[:, hp, :], in_=pa[:, :]), reads=[pa], writes=[gT.part(hp)])
                            S.barrier()
                        if DBG.get("even_stop") == 1:
                            continue
                        with ExitStack() as e3:
                            def bank(nm, dt=F32):
                                return C.ps(e3, [128, 512] if dt == F32 else [128, 8, 128], dt, nm)
                            pA = [bank(f"pA{k}") for k in range(3)]
                            pX = [bank(f"pX{k}") for k in range(2)]
                            ptr = bank("ptr", BF16)
                            pS = bank("pS")
                            BtT = C.sb(e3, [128, 512], BF16, "BtT")
                            KtT = C.sb(e3, [128, 512], BF16, "KtT")
                            Vtk = C.sb(e3, [128, 512], BF16, "Vtk")
                            Mak = C.sb(e3, [128, 8, 128], BF16, "Mak")
                            Nbr = C.sb(e3, [128, 8, 128], BF16, "Nbr")
                            Nkr = C.sb(e3, [128, 8, 128], BF16, "Nkr")
                            Pm = [C.sb(e3, [128, 8, 128], BF16, "Mm")]
                            PTm = [C.sb(e3, [128, 8, 128], BF16, "MTm")]
                            Xm = [C.sb(e3, [128, 8, 128], BF16, f"Xm{k}") for k in range(2)]
                            XTm = [C.sb(e3, [128, 8, 128], BF16, f"XTm{k}") for k in range(2)]
                            Ts2 = [C.sb(e3, [128, 8, 128], BF16, f"Ts{k}") for k in range(2)]
                            TsT2 = [C.sb(e3, [128, 8, 128], BF16, f"TsT{k}") for k in range(2)]
                            Y1s = C.sb(e3, [128, 8, 128], BF16, "Y1s")
                            Z1s = C.sb(e3, [128, 8, 128], BF16, "Z1s")
                            Gt = C.sb(e3, [128, 512], BF16, "Gt")
                            Ut = C.sb(e3, [128, 512], BF16, "Ut")
                            stmp = C.sb(e3, [128, 4, 64], F32, "stmp")

                            def hv(t_, h, csl):
                                hp_, e_ = h // 2, h % 2
                                return t_[e_ * 64:(e_ + 1) * 64, hp_, csl]

                            for c in range(4):
                                csl = slice(c * 128, (c + 1) * 128)
                                for (src, dst) in ((Bt, BtT), (Kt, KtT), (vT, Vtk)):
                                    for hp in range(4):
                                        S.pe(lambda e: e.transpose(out=ptr[:, hp, :], in_=src[:, hp, csl], identity=ident[:, :]),
                                             reads=[src, ident], writes=[ptr])
                                    S.act(lambda e: e.copy(out=dst[:, :], in_=ptr[:, 0:4, :].rearrange("p a b -> p (a b)")), reads=[ptr], writes=[dst])
                                if DBG.get('even_stop') == 1.2:
                                    continue
                                prods = ((Bt, At, Pm[0], "m_su"), (At, Bt, PTm[0], "m_sl"), (Kt, At, Mak, "m_su"),
                                         (Bt, Rq, Nbr, "m_ui"), (Kt, Rq, Nkr, "m_ui"))
                                nb = 0
                                for (lt, rt_, dst, mk) in prods:
                                    for e_ in range(2):
                                        pbk = pA[nb % 3]
                                        nb += 1
                                        for hh in range(4):
                                            h = 2 * hh + e_
                                            S.pe(lambda e: e.matmul(out=pbk[:, hh * 128:(hh + 1) * 128], lhsT=hv(lt, h, csl), rhs=hv(rt_, h, csl),
                                                                    start=True, stop=True), reads=[lt, rt_], writes=[pbk])
                                        S.dve(lambda e: e.tensor_tensor(out=dst[:, :, :].rearrange("p (a two) b -> p a two b", two=2)[:, :, e_, :],
                                                                        in0=pbk[:, :].rearrange("p (a b) -> p a b", a=4), in1=E[mk][:, :, :], op=ALU.mult),
                                              reads=[pbk, E[mk]], writes=[dst.part(("e", e_))])
                                if DBG.get('even_stop') == 1.4:
                                    continue
                                Mm, MTm = Pm[0], PTm[0]

                                def lvl(src, msk, s, dst, eng):
                                    S.op(eng, lambda e: e.tensor_tensor(out=dst[:, :, :], in0=src[:, :, :],
                                                                        in1=E[msk][:, s, :].unsqueeze(1).to_broadcast([128, 8, 128]), op=ALU.mult),
                                         reads=[src, E[msk]], writes=[dst])

                                Ts, TsT = Ts2[0], TsT2[0]
                                lvl(Mm, "lvm", 0, Ts, "pool")
                                lvl(MTm, "lvmT", 0, TsT, "pool")
                                lvl(Mm, "lvm", 1, Ts2[1], "pool")
                                lvl(MTm, "lvmT", 1, TsT2[1], "pool")
                                S.pool(lambda e: e.tensor_tensor(out=Xm[0][:, :, :], in0=Ts[:, :, :], in1=E["id8"][:, :, :], op=ALU.add),
                                       reads=[Ts, E["id8"]], writes=[Xm[0]])
                                S.pool(lambda e: e.tensor_tensor(out=XTm[0][:, :, :], in0=TsT[:, :, :], in1=E["id8"][:, :, :], op=ALU.add),
                                       reads=[TsT, E["id8"]], writes=[XTm[0]])
                                cur = 0
                                for s in range(1, 7):
                                    nxt = 1 - cur
                                    last = (s == 6)
                                    Ts, TsT = Ts2[s % 2], TsT2[s % 2]
                                    for half in range(2):
                                        hs_ = range(half * 4, half * 4 + 4)
                                        pbk = pA[nb % 3]
                                        nb += 1
                                        for hh, h in enumerate(hs_):
                                            S.pe(lambda e: e.matmul(out=pbk[:, hh * 128:(hh + 1) * 128], lhsT=TsT[:, h, :], rhs=Xm[cur][:, h, :],
                                                                    start=True, stop=True), reads=[TsT, Xm[cur]], writes=[pbk])
                                        S.act(lambda e: e.copy(out=Y1s[:, half * 4:half * 4 + 4, :], in_=pbk[:, :].rearrange("p (a b) -> p a b", a=4)),
                                              reads=[pbk], writes=[Y1s.part(half)])
                                        if not last:
                                            pbk = pA[nb % 3]
                                            nb += 1
                                            for hh, h in enumerate(hs_):
                                                S.pe(lambda e: e.matmul(out=pbk[:, hh * 128:(hh + 1) * 128], lhsT=Ts[:, h, :], rhs=XTm[cur][:, h, :],
                                                                        start=True, stop=True), reads=[Ts, XTm[cur]], writes=[pbk])
                                            S.dve(lambda e: e.tensor_copy(out=Z1s[:, half * 4:half * 4 + 4, :], in_=pbk[:, :].rearrange("p (a b) -> p a b", a=4)),
                                                  reads=[pbk], writes=[Z1s.part(half)])
                                    if not last:
                                        lvl(Mm, "lvm", s + 1, Ts2[(s + 1) % 2], "pool")
                                        lvl(MTm, "lvmT", s + 1, TsT2[(s + 1) % 2], "pool")
                                    for half in range(2):
                                        hs_ = range(half * 4, half * 4 + 4)
                                        px = pX[half]
                                        for hh, h in enumerate(hs_):
                                            S.pe(lambda e: e.matmul(out=px[:, hh * 128:(hh + 1) * 128], lhsT=ident[:, :], rhs=Xm[cur][:, h, :],
                                                                    start=True, stop=False), reads=[ident, Xm[cur]], writes=[px])
                                            S.pe(lambda e: e.matmul(out=px[:, hh * 128:(hh + 1) * 128], lhsT=XTm[cur][:, h, :], rhs=Y1s[:, h, :],
                                                                    start=False, stop=True), reads=[XTm[cur], Y1s], writes=[px])
                                        S.act(lambda e: e.copy(out=Xm[nxt][:, half * 4:half * 4 + 4, :], in_=px[:, :].rearrange("p (a b) -> p a b", a=4)),
                                              reads=[px], writes=[Xm[nxt].part(half)])
                                        if not last:
                                            pbk = pA[nb % 3]
                                            nb += 1
                                            for hh, h in enumerate(hs_):
                                                S.pe(lambda e: e.matmul(out=pbk[:, hh * 128:(hh + 1) * 128], lhsT=ident[:, :], rhs=XTm[cur][:, h, :],
                                                                        start=True, stop=False), reads=[ident, XTm[cur]], writes=[pbk])
                                                S.pe(lambda e: e.matmul(out=pbk[:, hh * 128:(hh + 1) * 128], lhsT=Xm[cur][:, h, :], rhs=Z1s[:, h, :],
                                                                        start=False, stop=True), reads=[Xm[cur], Z1s], writes=[pbk])
                                            S.dve(lambda e: e.tensor_copy(out=XTm[nxt][:, half * 4:half * 4 + 4, :], in_=pbk[:, :].rearrange("p (a b) -> p a b", a=4)),
                                                  reads=[pbk], writes=[XTm[nxt].part(half)])
                                    cur = nxt
                                if DBG.get('even_stop') == 1.6:
                                    continue
                                Xf = Xm[cur]
                                pg = pA[nb % 3]
                                nb += 1
                                for h in range(8):
                                    hp, e_ = h // 2, h % 2
                                    S.pe(lambda e: e.matmul(out=pg[:, h * 64:(h + 1) * 64], lhsT=At[:, hp, csl], rhs=Sb[:, hp, e_, :],
                                                            start=True, stop=False), reads=[At, Sb], writes=[pg])
                                    S.pe(lambda e: e.matmul(out=pg[:, h * 64:(h + 1) * 64], lhsT=Mak[:, h, :], rhs=Vtk[:, h * 64:(h + 1) * 64],
                                                            start=False, stop=True), reads=[Mak, Vtk], writes=[pg])
                                S.act(lambda e: e.copy(out=Gt[:, :], in_=pg[:, :]), reads=[pg], writes=[Gt])
                                if DBG.get('even_stop') == 1.65:
                                    continue
                                pu = pA[nb % 3]
                                nb += 1
                                for h in range(8):
                                    S.pe(lambda e: e.matmul(out=pu[:, h * 64:(h + 1) * 64], lhsT=Xf[:, h, :], rhs=Gt[:, h * 64:(h + 1) * 64],
                                                            start=True, stop=True), reads=[Xf, Gt], writes=[pu])
                                S.dve(lambda e: e.tensor_copy(out=Ut[:, :], in_=pu[:, :]), reads=[pu], writes=[Ut])
                                if DBG.get('even_stop') == 1.7:
                                    continue
                                py = pA[nb % 3]
                                nb += 1
                                for h in range(8):
                                    hp, e_ = h // 2, h % 2
                                    o_ = py[e_ * 64:(e_ + 1) * 64, hp * 128:(hp + 1) * 128]
                                    S.pe(lambda e: e.matmul(out=o_, lhsT=Sb[:, hp, e_, :], rhs=Rq[:, hp, csl], start=True, stop=False),
                                         reads=[Sb, Rq], writes=[py])
                                    S.pe(lambda e: e.matmul(out=o_, lhsT=Ut[:, h * 64:(h + 1) * 64], rhs=Nbr[:, h, :], start=False, stop=False),
                                         reads=[Ut, Nbr], writes=[py])
                                    S.pe(lambda e: e.matmul(out=o_, lhsT=Vtk[:, h * 64:(h + 1) * 64], rhs=Nkr[:, h, :], start=False, stop=True),
                                         reads=[Vtk, Nkr], writes=[py])
                                S.act(lambda e: e.copy(out=yraw[:, :, csl], in_=py[:, :].rearrange("p (a b) -> p a b", a=4)), reads=[py], writes=[yraw.part(c)])
                                if DBG.get('even_stop') == 1.75:
                                    continue
                                for hp in range(4):
                                    o_ = pS[:, hp * 128:(hp + 1) * 128]
                                    S.pe(lambda e: e.matmul(out=o_, lhsT=BtT[:, hp * 128:(hp + 1) * 128], rhs=Ut[:, hp * 128:(hp + 1) * 128], start=True, stop=False),
                                         reads=[BtT, Ut], writes=[pS])
                                    S.pe(lambda e: e.matmul(out=o_, lhsT=KtT[:, hp * 128:(hp + 1) * 128], rhs=Vtk[:, hp * 128:(hp + 1) * 128], start=False, stop=True),
                                         reads=[KtT, Vtk], writes=[pS])
                                for e_ in range(2):
                                    S.dve(lambda e: e.tensor_tensor(out=stmp[e_ * 64:(e_ + 1) * 64, :, :],
                                                                    in0=pS[e_ * 64:(e_ + 1) * 64, :].rearrange("p (a b c) -> p a b c", a=4, b=2)[:, :, e_, :],
                                                                    in1=St[e_ * 64:(e_ + 1) * 64, :, :], op=ALU.add),
                                          reads=[pS, St], writes=[stmp])
                                S.dve(lambda e: e.tensor_tensor(out=St[:, :, :], in0=stmp[:, :, :], in1=gC[:, c, :].unsqueeze(2).to_broadcast([128, 4, 64]),
                                                                op=ALU.mult), reads=[stmp, gC], writes=[St])
                                for e_ in range(2):
                                    S.act(lambda e: e.copy(out=Sb[e_ * 64:(e_ + 1) * 64, :, e_, :], in_=St[e_ * 64:(e_ + 1) * 64, :, :]), reads=[St], writes=[Sb])
                            S.barrier()
                        if DBG.get('even_stop') == 1.8:
                            continue
                        with ExitStack() as e3:
                            pm = C.ps(e3, [128, 512], F32, "gn_pm")
                            pq = C.ps(e3, [128, 512], F32, "gn_pq")

                            def post_a(c, dd):
                                S.pool(lambda e: e.tensor_tensor(out=dd[:, :], in0=dd[:, :], in1=bon[:, c, :], op=ALU.add), reads=[dd, bon], writes=[dd])
                                S.dve(lambda e: e.tensor_tensor(out=yaT[:, c, t0:t0 + 512], in0=dd[:, :], in1=gT[:, c, :], op=ALU.mult), reads=[dd, gT], writes=[yaT.part((c, blk))])

                            group_norm_T(C, e3, yraw, 4, E["bof"], RW_LN_EPS, lambda c: P.col(("rw_ln_g", i), c), lambda c: P.col(("rw_ln_b", i), c),
                                         post_a, pm, pq)
                            S.barrier()
        if DBG.get("even_stop") in (1, 1.2, 1.4, 1.6, 1.65, 1.7, 1.75, 1.8, 2):
            return
        ybT = C.sb(es, [128, 4, SEQ], BF16, "ybT")
        with ExitStack() as er:
            Rt = C.sb(er, [128, 4, 128], F32, "Rt")
            Rb = C.sb(er, [128, 4, 128], BF16, "Rb")
            for t_ in (Rt, Rb):
                S.pool(lambda e: e.memset(t_[:, :, :], 0.0), writes=[t_])
            wvr = C.sb(er, [128, 8, 512], BF16, "wvr")
            load_w(C, wvr, w_in, 1792 + 1024, 1792 + 1536)
            wch = [C.sb(er, [128, 8, 128], BF16, f"wchr{k}") for k in range(2)]
            nw = [0]

            def wchunk(c0):
                wt = wch[nw[0] % 2]
                nw[0] += 1
                S.dma("pool", wt[:, :, :], w_v[:, :, 1792 + c0:1792 + c0 + 128], writes=[wt])
                return wt

            for blk in range(4):
                t0 = blk * 512
                with ExitStack() as e1:
                    xnT = C.sb(e1, [128, 8, 512], BF16, "xnT")
                    rms_to_T(C, e1, x, P.cols(("norm_g", l, 2)), xnT, 4, ident, tok0=blk * 4)
                    with ExitStack() as e2:
                        qr = C.sb(e2, [128, 4, 512], BF16, "qr")
                        qx = C.sb(e2, [128, 4, 512], BF16, "qx")
                        kr = C.sb(e2, [128, 4, 512], BF16, "kr")
                        sg = C.sb(e2, [128, 4, 512], BF16, "sg")
                        vtk = C.sb(e2, [128, 4, 512], BF16, "vtk")
                        oraw = C.sb(e2, [128, 4, 512], F32, "oraw")
                        cs = E["cosT"][:, t0:t0 + 512]
                        sn = E["sinT"][:, t0:t0 + 512]
                        with ExitStack() as e3:
                            pq_ = [C.ps(e3, [128, 512], F32, f"rq{k}") for k in range(2)]
                            ps_ = [C.ps(e3, [128, 512], F32, f"rs{k}") for k in range(2)]
                            t1 = C.sb(e3, [128, 512], F32, "rt1")
                            t2 = C.sb(e3, [128, 512], F32, "rt2")
                            n_ = 0
                            for (c0, dst) in ((0, qr), (512, kr)):
                                for h in range(4):
                                    pa_, pb_ = pq_[n_ % 2], ps_[n_ % 2]
                                    n_ += 1
                                    cb = c0 + h * 128
                                    wrt = wchunk(cb)
                                    for kc in range(8):
                                        S.pe(lambda e: e.matmul(out=pa_[:, :], lhsT=wrt[:, kc, :], rhs=xnT[:, kc, :], start=(kc == 0), stop=(kc == 7)),
                                             reads=[wrt, xnT], writes=[pa_])
                                    for half in range(2):
                                        for kc in range(8):
                                            S.pe(lambda e: e.matmul(out=pb_[half * 64:(half + 1) * 64, :], lhsT=wrt[:, kc, (1 - half) * 64:(1 - half) * 64 + 64],
                                                                    rhs=xnT[:, kc, :], start=(kc == 0), stop=(kc == 7)), reads=[wrt, xnT], writes=[pb_])
                                    S.dve(lambda e: e.tensor_tensor(out=t1[:, :], in0=pa_[:, :], in1=cs, op=ALU.mult), reads=[pa_, E["cosT"]], writes=[t1])
                                    S.dve(lambda e: e.tensor_tensor(out=t2[:, :], in0=pb_[:, :], in1=sn, op=ALU.mult), reads=[pb_, E["sinT"]], writes=[t2])
                                    S.pool(lambda e: e.tensor_tensor(out=dst[:, h, :], in0=t1[:, :], in1=t2[:, :], op=ALU.add), reads=[t1, t2], writes=[dst.part(h)])
                                    if c0 == 0:
                                        S.pool(lambda e: e.tensor_tensor(out=qx[:, h, :].rearrange("p (a b) -> p a b", a=4),
                                                                         in0=qr[:, h, :].rearrange("p (a b) -> p a b", a=4),
                                                                         in1=E["xiT"][:, h, :].unsqueeze(1).to_broadcast([128, 4, 128]), op=ALU.mult),
                                               reads=[qr.part(h), E["xiT"]], writes=[qx.part(h)])
                            for h in range(4):
                                pa_ = pq_[h % 2]
                                wrt = wchunk(1536 + h * 128)
                                for kc in range(8):
                                    S.pe(lambda e: e.matmul(out=pa_[:, :], lhsT=wrt[:, kc, :], rhs=xnT[:, kc, :],
                                                            start=(kc == 0), stop=(kc == 7)), reads=[wrt, xnT], writes=[pa_])
                                S.act(lambda e: e.activation(out=sg[:, h, :], in_=pa_[:, :], func=AF.Silu), reads=[pa_], writes=[sg.part(h)])
                            for c in range(4):
                                pa_ = ps_[c % 2]
                                for kc in range(8):
                                    S.pe(lambda e: e.matmul(out=pa_[:, :], lhsT=xnT[:, kc, c * 128:(c + 1) * 128], rhs=wvr[:, kc, :],
                                                            start=(kc == 0), stop=(kc == 7)), reads=[wvr, xnT], writes=[pa_])
                                S.act(lambda e: e.copy(out=vtk[:, c, :], in_=pa_[:, :]), reads=[pa_], writes=[vtk.part(c)])
                            S.barrier()
                        with ExitStack() as e3:
                            psc = C.ps(e3, [128, 512], F32, "psc")
                            po_ = C.ps(e3, [128, 512], F32, "po_r")
                            pkv = C.ps(e3, [128, 512], F32, "pkv")
                            ptr = C.ps(e3, [128, 8, 128], BF16, "ptr_r")
                            scT = C.sb(e3, [128, 4, 128], BF16, "scT")
                            ktk = C.sb(e3, [128, 4, 128], BF16, "ktk")
                            for c in range(4):
                                csl = slice(c * 128, (c + 1) * 128)
                                for h in range(4):
                                    S.pe(lambda e: e.matmul(out=psc[:, h * 128:(h + 1) * 128], lhsT=kr[:, h, csl], rhs=qr[:, h, csl], start=True, stop=True),
                                         reads=[kr, qr], writes=[psc])
                                    S.pe(lambda e: e.transpose(out=ptr[:, h, :], in_=kr[:, h, csl], identity=ident[:, :]), reads=[kr, ident], writes=[ptr])
                                S.dve(lambda e: e.tensor_tensor(out=scT[:, :, :], in0=psc[:, :].rearrange("p (a b) -> p a b", a=4), in1=E["dmT"][:, :, :], op=ALU.mult),
                                      reads=[psc, E["dmT"]], writes=[scT])
                                S.dve(lambda e: e.tensor_tensor(out=ktk[:, :, :], in0=ptr[:, 0:4, :], in1=E["zcol"][:, :].unsqueeze(2).to_broadcast([128, 4, 128]),
                                                                op=ALU.mult), reads=[ptr, E["zcol"]], writes=[ktk])
                                for h in range(4):
                                    o_ = po_[:, h * 128:(h + 1) * 128]
                                    S.pe(lambda e: e.matmul(out=o_, lhsT=vtk[:, c, h * 128:(h + 1) * 128], rhs=scT[:, h, :], start=True, stop=False),
                                         reads=[vtk, scT], writes=[po_])
                                    S.pe(lambda e: e.matmul(out=o_, lhsT=Rb[:, h, :], rhs=qx[:, h, csl], start=False, stop=True), reads=[Rb, qx], writes=[po_])
                                S.act(lambda e: e.copy(out=oraw[:, :, csl], in_=po_[:, :].rearrange("p (a b) -> p a b", a=4)), reads=[po_], writes=[oraw.part(c)])
                                for h in range(4):
                                    S.pe(lambda e: e.matmul(out=pkv[:, h * 128:(h + 1) * 128], lhsT=ktk[:, h, :], rhs=vtk[:, c, h * 128:(h + 1) * 128],
                                                            start=True, stop=True), reads=[ktk, vtk], writes=[pkv])
                                for h in range(4):
                                    S.dve(lambda e: e.scalar_tensor_tensor(out=Rt[:, h, :], in0=Rt[:, h, :], scalar=gamC[h], in1=pkv[:, h * 128:(h + 1) * 128],
                                                                           op0=ALU.mult, op1=ALU.add), reads=[Rt, pkv], writes=[Rt])
                                S.act(lambda e: e.copy(out=Rb[:, :, :], in_=Rt[:, :, :]), reads=[Rt], writes=[Rb])
                            S.barrier()
                        with ExitStack() as e3:
                            pm = C.ps(e3, [128, 512], F32, "gn_pm")
                            pq = C.ps(e3, [128, 512], F32, "gn_pq")

                            def post_b(c, dd):
                                S.dve(lambda e: e.tensor_tensor(out=ybT[:, c, t0:t0 + 512], in0=dd[:, :], in1=sg[:, c, :], op=ALU.mult), reads=[dd, sg], writes=[ybT.part((c, blk))])

                            group_norm_T(C, e3, oraw, 4, E["on128"], EPS, lambda c: P.col(("rt_gn_g", i), c), lambda c: P.col(("rt_gn_b", i), c),
                                         post_b, pm, pq)
                            S.barrier()
        with ExitStack() as eo:
            g_bc = C.sb(eo, [128, D], F32, "g_bc")
            load_bcast_row(C, "sp", g_bc, W.L("norm_g", l)[3], D)
            wo = C.sb(eo, [128, 8, D], BF16, "wo_ev")
            load_w(C, wo, W.H("ev_w_out", i), 0, D)
            for g4 in range(4):
                chunks = [(yaT, (lambda ti, c=c, g4=g4: yaT[:, c, (g4 * 4 + ti) * 128:(g4 * 4 + ti + 1) * 128])) for c in range(4)]
                chunks += [(ybT, (lambda ti, c=c, g4=g4: ybT[:, c, (g4 * 4 + ti) * 128:(g4 * 4 + ti + 1) * 128])) for c in range(4)]
                out_proj_residual(C, x, chunks, wo, g_bc, 1.0, [g4 * 4 + t_ for t_ in range(4)])


def build_program(shapes, nseq=2, plan=None, loff=0, hoff=0):
    nc = bass.Bass("TRN2", target_bir_lowering=False)
    W = Wts(nc, shapes, loff, hoff)
    out = nc.dram_tensor("out", [nseq, SEQ, D], F32, kind="ExternalOutput").ap()
    C = Ctx(nc)
    S = C.S
    if plan is None:
        plan = [(l, ph) for l in range(DEPTH) for ph in ("ffn1", "mix", "xa", "ffn2")]
    need_mem = any(ph == "xa" for _, ph in plan)
    with ExitStack() as es:
        K = make_consts(C, es)
        P = build_params(C, es, W, K["identf"])
        x = C.sb(es, [128, NT, D], F32, "xres")
        memT = C.sb(es, [128, 8, MEM], BF16, "memT") if need_mem else None
        for s in range(nseq):
            for t4 in range(NT // 4):
                S.dma("sp", x[:, t4 * 4:(t4 + 1) * 4, :],
                      W["x"][s, t4 * 512:(t4 + 1) * 512, :].rearrange("(t p) d -> p t d", p=128),
                      writes=[x.part(t4 * 4 + i) for i in range(4)])
            if need_mem:
                prep_mem(C, es, W, P, K, s, memT)
            for (l, ph) in plan:
                if ph == "ffn1":
                    ffn_block(C, x, l, 0, W, P, K["ident"])
                elif ph == "ffn2":
                    ffn_block(C, x, l, 1, W, P, K["ident"])
                elif ph == "xa":
                    xattn_block(C, x, l, W, P, K, memT)
                elif ph == "mix":
                    if l % 2 == 0:
                        even_mixer(C, x, l, W, P, K)
                    else:
                        odd_mixer(C, x, l, W, P, K)
            for t4 in range(NT // 4):
                S.dma("sp", out[s, t4 * 512:(t4 + 1) * 512, :].rearrange("(t p) d -> p t d", p=128),
                      x[:, t4 * 4:(t4 + 1) * 4, :], reads=[x.part(t4 * 4 + i) for i in range(4)])
            S.barrier()
        S.finish()
    return nc, W


def kernel(**inputs):
    n = 8
    arrs = {k: np.ascontiguousarray(np.asarray(v), dtype=np.float32) for k, v in inputs.items()}
    per = arrs["x"].shape[0] // n
    shapes = {}
    for k, a in arrs.items():
        shapes[k] = ((per,) + a.shape[1:]) if k in ("x", "mem") else a.shape
    nc, W = build_program(shapes, nseq=per)
    in_maps = []
    for c in range(n):
        m = {}
        for k in W.aps:
            a = arrs[k]
            m[k] = a[c * per:(c + 1) * per] if k in ("x", "mem") else a
        in_maps.append(m)
    res = run_bass_kernel_spmd(nc, in_maps, core_ids=list(range(n)))
    return np.concatenate([r["out"] for r in res.results], axis=0).astype(np.float32)
```
